# Optimizing a Trainium2 kernel written in Bass

```python
import math
import jax
import jax.numpy as jnp
from jax import lax
import numpy as np

D_MODEL = 1024
BATCH = 8
SEQ = 2048
DEPTH = 4

CTX_LEN = 256
GRID_W = 64
S5_WIDTH = 512
S5_GROUP = 16
S5_GROUPS = S5_WIDTH // S5_GROUP
S5_STATE = 64
DT_MIN = 1e-3
DT_MAX = 1e-1
MLA_HEADS = 8
MLA_NOPE = 64
MLA_ROPE = 32
MLA_V = 64
MLA_Q_RANK = 384
MLA_KV_RANK = 256
MLA_SCALE = (MLA_NOPE + MLA_ROPE) ** -0.5
WIN_Q_HEADS = 8
WIN_KV_HEADS = 2
WIN_GROUP = WIN_Q_HEADS // WIN_KV_HEADS
WIN_HEAD_DIM = 64
WINDOW = 128
BLOCK = 128
WIN_SCALE = WIN_HEAD_DIM ** -0.5
N_BRANCH = 3
BRANCH_WIDTH = 512
N_EXPERTS = 16
EXPERT_FF = 1024
CAPACITY_FACTOR = 2
ROPE_BASE = 10000.0
LN_EPS = 1e-6
NEG_INF = -1e30
ALPHA = (2 * DEPTH) ** 0.25
BETA = (8 * DEPTH) ** -0.25
IN_SIZES = (S5_WIDTH, MLA_Q_RANK, MLA_KV_RANK, MLA_ROPE,
            WIN_Q_HEADS * WIN_HEAD_DIM, WIN_KV_HEADS * WIN_HEAD_DIM, WIN_KV_HEADS * WIN_HEAD_DIM,
            N_BRANCH * D_MODEL)
D_IN = sum(IN_SIZES)
IN_POINTS = tuple(int(p) for p in np.cumsum(IN_SIZES)[:-1])

kernel_name = 'hybrid_s5_mla_swa_ecmoe_dit'


def layer_norm(x, gain=None, bias=None):
    xf = x.astype(jnp.float32)
    mu = jnp.mean(xf, axis=-1, keepdims=True)
    var = jnp.mean(jnp.square(xf - mu), axis=-1, keepdims=True)
    y = (xf - mu) * lax.rsqrt(var + LN_EPS)
    if gain is not None:
        y = y * gain.astype(jnp.float32) + bias.astype(jnp.float32)
    return y.astype(x.dtype)


def rms_norm(x, gain):
    xf = x.astype(jnp.float32)
    y = xf * lax.rsqrt(jnp.mean(xf * xf, axis=-1, keepdims=True) + LN_EPS) * gain.astype(jnp.float32)
    return y.astype(x.dtype)


def modulate(x, shift, scale):
    return layer_norm(x) * (1 + scale) + shift


def adaln_params(cond, lp):
    return jnp.split(jax.nn.silu(cond) @ lp['w_ada'] + lp['b_ada'], 6, axis=-1)


def axial_rope(x, row, col):
    d = x.shape[-1]
    nf = d // 4
    freqs = ROPE_BASE ** (-jnp.arange(nf, dtype=jnp.float32) / nf)
    ang = jnp.concatenate([row[:, None] * freqs, col[:, None] * freqs], axis=-1)[:, None, :]
    cos = jnp.cos(ang).astype(x.dtype)
    sin = jnp.sin(ang).astype(x.dtype)
    x1, x2 = x[..., : d // 2], x[..., d // 2:]
    return jnp.concatenate([x1 * cos - x2 * sin, x1 * sin + x2 * cos], axis=-1)


def dense_attention(q, k, v, scale, sink=None):
    s = jnp.einsum('bqkgd,bskd->bkgqs', q, k).astype(jnp.float32) * scale
    if sink is not None:
        sk = jnp.broadcast_to(sink.astype(jnp.float32)[None, :, :, None, None], s.shape[:-1] + (1,))
        s = jnp.concatenate([s, sk], axis=-1)
    p = jax.nn.softmax(s, axis=-1)[..., : k.shape[1]].astype(v.dtype)
    return jnp.einsum('bkgqs,bskd->bqkgd', p, v)


def cmul(ar, ai, br, bi):
    return ar * br - ai * bi, ar * bi + ai * br


def s5_discretise(lam_re, lam_im, log_dt, b_re, b_im):
    lam_re = lam_re.astype(jnp.float32)
    lam_im = lam_im.astype(jnp.float32)
    dt = jnp.exp(log_dt.astype(jnp.float32))[:, None]
    mag = jnp.exp(lam_re * dt)
    ar, ai = mag * jnp.cos(lam_im * dt), mag * jnp.sin(lam_im * dt)
    den = lam_re * lam_re + lam_im * lam_im
    qr = ((ar - 1) * lam_re + ai * lam_im) / den
    qi = (ai * lam_re - (ar - 1) * lam_im) / den
    bbr, bbi = cmul(qr[..., None], qi[..., None], b_re.astype(jnp.float32), b_im.astype(jnp.float32))
    return ar, ai, bbr, bbi


def _s5_combine(e1, e2):
    a1r, a1i, b1r, b1i = e1
    a2r, a2i, b2r, b2i = e2
    ar, ai = cmul(a2r, a2i, a1r, a1i)
    br, bi = cmul(a2r, a2i, b1r, b1i)
    return ar, ai, br + b2r, bi + b2i


def s5_scan(u, ar, ai, bbr, bbi, s0):
    bu_r = jnp.einsum('blgh,gph->blgp', u, bbr)
    bu_i = jnp.einsum('blgh,gph->blgp', u, bbi)
    if s0 is not None:
        sr, si = cmul(ar, ai, s0[0], s0[1])
        bu_r = bu_r.at[:, 0].add(sr)
        bu_i = bu_i.at[:, 0].add(si)
    L = u.shape[1]
    a_r = jnp.broadcast_to(ar, (1, L) + ar.shape)
    a_i = jnp.broadcast_to(ai, (1, L) + ai.shape)
    _, _, xr, xi = lax.associative_scan(_s5_combine, (a_r, a_i, bu_r, bu_i), axis=1)
    return xr, xi


def s5_readout(xr, xi, c_re, c_im):
    return (jnp.einsum('blgp,ghp->blgh', xr, c_re.astype(jnp.float32))
            - jnp.einsum('blgp,ghp->blgh', xi, c_im.astype(jnp.float32)))


def s5_branch(u_c, u_l, lp, update_ctx):
    B, Lc, _ = u_c.shape
    L = u_l.shape[1]
    uc = u_c.reshape(B, Lc, S5_GROUPS, S5_GROUP).astype(jnp.float32)
    ul = u_l.reshape(B, L, S5_GROUPS, S5_GROUP).astype(jnp.float32)
    d_skip = lp['s5_d'].astype(jnp.float32).reshape(S5_GROUPS, S5_GROUP)
    y_l = d_skip * ul
    y_c = d_skip * uc if update_ctx else None
    for d in range(2):
        ar, ai, bbr, bbi = s5_discretise(lp['s5_lam_re'][d], lp['s5_lam_im'][d], lp['s5_log_dt'][d],
                                         lp['s5_b_re'][d], lp['s5_b_im'][d])
        order = (lambda t: t[:, ::-1]) if d == 1 else (lambda t: t)
        xr_c, xi_c = s5_scan(order(uc), ar, ai, bbr, bbi, None)
        xr_l, xi_l = s5_scan(order(ul), ar, ai, bbr, bbi, (xr_c[:, -1], xi_c[:, -1]))
        y_l = y_l + order(s5_readout(xr_l, xi_l, lp['s5_c_re'][d], lp['s5_c_im'][d]))
        if update_ctx:
            y_c = y_c + order(s5_readout(xr_c, xi_c, lp['s5_c_re'][d], lp['s5_c_im'][d]))

    def glu(y, n):
        g = jax.nn.gelu(y.reshape(B, n, S5_WIDTH))
        return (g * jax.nn.sigmoid(g @ lp['s5_w_glu'].astype(jnp.float32) + lp['s5_b_glu'].astype(jnp.float32))).astype(u_l.dtype)

    out_l = glu(y_l, L)
    out_c = glu(y_c, Lc) if update_ctx else None
    return out_c, out_l


def mla_queries(qa, lp):
    B, N, _ = qa.shape
    q = (rms_norm(qa, lp['mla_q_norm']) @ lp['mla_w_uq']).reshape(B, N, MLA_HEADS, MLA_NOPE + MLA_ROPE)
    return q[..., :MLA_NOPE], q[..., MLA_NOPE:]


def mla_keys_values(kva, lp):
    B, N, _ = kva.shape
    kv = (rms_norm(kva, lp['mla_kv_norm']) @ lp['mla_w_ukv']).reshape(B, N, MLA_HEADS, MLA_NOPE + MLA_V)
    return kv[..., :MLA_NOPE], kv[..., MLA_NOPE:]


def mla_branch(qa_c, kva_c, kr_c, qa_l, kva_l, kr_l, lp, row, col, update_ctx):
    B, Lc, _ = kva_c.shape
    L = kva_l.shape[1]
    kn_c, v_c = mla_keys_values(kva_c, lp)
    k_ctx = jnp.concatenate([kn_c, jnp.broadcast_to(kr_c[:, :, None, :], (B, Lc, MLA_HEADS, MLA_ROPE))], axis=-1)
    kn_l, v_l = mla_keys_values(kva_l, lp)
    kr_rot = axial_rope(kr_l[:, :, None, :], row, col)
    k_lat = jnp.concatenate([kn_l, jnp.broadcast_to(kr_rot, (B, L, MLA_HEADS, MLA_ROPE))], axis=-1)
    qn_l, qr_l = mla_queries(qa_l, lp)
    q_rot = jnp.concatenate([qn_l, axial_rope(qr_l, row, col)], axis=-1)
    q_plain = jnp.concatenate([qn_l, qr_l], axis=-1)
    nb = L // BLOCK
    dk = MLA_NOPE + MLA_ROPE
    qr_b = q_rot.reshape(B, nb, BLOCK, MLA_HEADS, dk).swapaxes(0, 1)
    qp_b = q_plain.reshape(B, nb, BLOCK, MLA_HEADS, dk).swapaxes(0, 1)

    def block_attention(qs):
        qrb, qpb = qs
        s_lat = jnp.einsum('bqhd,bkhd->bhqk', qrb, k_lat).astype(jnp.float32)
        s_ctx = jnp.einsum('bqhd,bkhd->bhqk', qpb, k_ctx).astype(jnp.float32)
        p = jax.nn.softmax(jnp.concatenate([s_lat, s_ctx], axis=-1) * MLA_SCALE, axis=-1).astype(v_l.dtype)
        return (jnp.einsum('bhqk,bkhd->bqhd', p[..., :L], v_l)
                + jnp.einsum('bhqk,bkhd->bqhd', p[..., L:], v_c))

    o = lax.map(block_attention, (qr_b, qp_b))
    out_l = o.swapaxes(0, 1).reshape(B, L, MLA_HEADS * MLA_V)
    out_c = None
    if update_ctx:
        qn_c, qr_c = mla_queries(qa_c, lp)
        q_c = jnp.concatenate([qn_c, qr_c], axis=-1)[:, :, :, None, :]
        out_c = dense_attention(q_c, k_ctx, v_c, MLA_SCALE).reshape(B, Lc, MLA_HEADS * MLA_V)
    return out_c, out_l


def window_branch(qc, kc, vc, ql, kl, vl, sink, row, col, update_ctx):
    B, Lc, _ = kc.shape
    L = kl.shape[1]
    nb = L // BLOCK
    sink = sink.reshape(WIN_KV_HEADS, WIN_GROUP)
    kc = kc.reshape(B, Lc, WIN_KV_HEADS, WIN_HEAD_DIM)
    vc = vc.reshape(B, Lc, WIN_KV_HEADS, WIN_HEAD_DIM)
    q = ql.reshape(B, L, WIN_Q_HEADS, WIN_HEAD_DIM)
    qb = axial_rope(q, row, col).reshape(B, nb, BLOCK, WIN_KV_HEADS, WIN_GROUP, WIN_HEAD_DIM)
    qpb = q.reshape(B, nb, BLOCK, WIN_KV_HEADS, WIN_GROUP, WIN_HEAD_DIM)
    k_rot = axial_rope(kl.reshape(B, L, WIN_KV_HEADS, WIN_HEAD_DIM), row, col)
    v = vl.reshape(B, L, WIN_KV_HEADS, WIN_HEAD_DIM)

    def band(t):
        tp = jnp.pad(t, ((0, 0), (BLOCK, BLOCK), (0, 0), (0, 0))).reshape(B, nb + 2, BLOCK, WIN_KV_HEADS, WIN_HEAD_DIM)
        return jnp.concatenate([tp[:, :-2], tp[:, 1:-1], tp[:, 2:]], axis=2)

    kb, vb = band(k_rot), band(v)
    blk = jnp.arange(nb)[:, None, None]
    r = jnp.arange(BLOCK)[None, :, None]
    j = jnp.arange(3 * BLOCK)[None, None, :]
    key_pos = blk * BLOCK - BLOCK + j
    valid = (jnp.abs(j - BLOCK - r) <= WINDOW) & (key_pos >= 0) & (key_pos < L)
    s_band = jnp.einsum('bnqkgd,bnskd->bnkgqs', qb, kb).astype(jnp.float32) * WIN_SCALE
    s_band = jnp.where(valid[None, :, None, None], s_band, NEG_INF)
    s_ctx = jnp.einsum('bnqkgd,bskd->bnkgqs', qpb, kc).astype(jnp.float32) * WIN_SCALE
    sk = jnp.broadcast_to(sink.astype(jnp.float32)[None, None, :, :, None, None], s_ctx.shape[:-1] + (1,))
    p = jax.nn.softmax(jnp.concatenate([s_band, s_ctx, sk], axis=-1), axis=-1)
    nk = 3 * BLOCK
    o = (jnp.einsum('bnkgqs,bnskd->bnqkgd', p[..., :nk].astype(v.dtype), vb)
         + jnp.einsum('bnkgqs,bskd->bnqkgd', p[..., nk:nk + Lc].astype(v.dtype), vc))
    out_l = o.reshape(B, L, WIN_Q_HEADS * WIN_HEAD_DIM)
    out_c = None
    if update_ctx:
        q_c = qc.reshape(B, Lc, WIN_KV_HEADS, WIN_GROUP, WIN_HEAD_DIM)
        out_c = dense_attention(q_c, kc, vc, WIN_SCALE, sink).reshape(B, Lc, WIN_Q_HEADS * WIN_HEAD_DIM)
    return out_c, out_l


def merge_branches(outs, gate_logits, lp):
    B, N, _ = gate_logits.shape
    o = jnp.stack(outs, axis=2)
    proj = jnp.einsum('bnkw,kwd->bnkd', o, lp['w_branch'])
    g = jax.nn.sigmoid(gate_logits.reshape(B, N, N_BRANCH, D_MODEL))
    return jnp.sum(g * proj, axis=2) @ lp['w_out']


def token_mixer(hc, hl, lp, row, col, update_ctx):
    zc = jnp.split(hc @ lp['w_in'], IN_POINTS, axis=-1)
    zl = jnp.split(hl @ lp['w_in'], IN_POINTS, axis=-1)
    s5_c, s5_l = s5_branch(zc[0], zl[0], lp, update_ctx)
    mla_c, mla_l = mla_branch(zc[1], zc[2], zc[3], zl[1], zl[2], zl[3], lp, row, col, update_ctx)
    win_c, win_l = window_branch(zc[4], zc[5], zc[6], zl[4], zl[5], zl[6], lp['win_sink'], row, col, update_ctx)
    yl = merge_branches((s5_l, mla_l, win_l), zl[7], lp)
    yc = merge_branches((s5_c, mla_c, win_c), zc[7], lp) if update_ctx else None
    return yc, yl


def expert_choice_ffn(h, lp):
    B, N, D = h.shape
    cap = CAPACITY_FACTOR * N // N_EXPERTS
    aff = jax.nn.softmax((h @ lp['w_router']).astype(jnp.float32), axis=-1)
    g, idx = lax.top_k(aff.swapaxes(1, 2), cap)
    xs = jax.vmap(lambda hb, ib: hb[ib])(h, idx)
    a = jnp.einsum('becd,edf->becf', xs, lp['w_gate'])
    u = jnp.einsum('becd,edf->becf', xs, lp['w_up'])
    y = jnp.einsum('becf,efd->becd', jax.nn.silu(a) * u, lp['w_down']) * g[..., None].astype(h.dtype)
    return jax.vmap(lambda ib, yb: jnp.zeros((N, D), yb.dtype).at[ib.reshape(-1)].add(yb.reshape(-1, D)))(idx, y)


def trunk_layer(xc, xl, c, c_ctx, lp, row, col, update_ctx):
    mod_l = [m[:, None, :] for m in adaln_params(c, lp)]
    mod_c = adaln_params(c_ctx, lp)
    hc = modulate(xc, mod_c[0], mod_c[1])
    hl = modulate(xl, mod_l[0], mod_l[1])
    yc, yl = token_mixer(hc, hl, lp, row, col, update_ctx)
    xl = layer_norm(ALPHA * xl + mod_l[2] * yl, lp['ln1_g'], lp['ln1_b'])
    fl = expert_choice_ffn(modulate(xl, mod_l[3], mod_l[4]), lp)
    xl = layer_norm(ALPHA * xl + mod_l[5] * fl, lp['ln2_g'], lp['ln2_b'])
    if update_ctx:
        xc = layer_norm(ALPHA * xc + mod_c[2] * yc, lp['ln1_g'], lp['ln1_b'])
        fc = expert_choice_ffn(modulate(xc, mod_c[3], mod_c[4]), lp)
        xc = layer_norm(ALPHA * xc + mod_c[5] * fc, lp['ln2_g'], lp['ln2_b'])
    return xc, xl


def setup_inputs(seed: int = 0) -> dict:
    key = jax.random.key(seed)
    k = jax.random.split(key, 32)
    f = jnp.float32
    G, P, HG = S5_GROUPS, S5_STATE, S5_GROUP

    def nrm(i, shape, scale):
        return scale * jax.random.normal(k[i], shape, f)

    lam_im = jnp.broadcast_to(jnp.pi * jnp.arange(P, dtype=f), (DEPTH, 2, G, P))
    return {
        'x': nrm(0, (BATCH, SEQ, D_MODEL), 1.0),
        'c': nrm(1, (BATCH, D_MODEL), 1.0),
        'ctx': nrm(2, (BATCH, CTX_LEN, D_MODEL), 1.0),
        'c_ctx': nrm(3, (D_MODEL,), 1.0),
        'w_ada': nrm(4, (DEPTH, D_MODEL, 6 * D_MODEL), D_MODEL ** -0.5),
        'b_ada': nrm(5, (DEPTH, 6 * D_MODEL), 0.01),
        'w_in': nrm(6, (DEPTH, D_MODEL, D_IN), D_MODEL ** -0.5),
        's5_lam_re': -0.5 + nrm(7, (DEPTH, 2, G, P), 0.01),
        's5_lam_im': lam_im,
        's5_log_dt': jax.random.uniform(k[8], (DEPTH, 2, G), f, math.log(DT_MIN), math.log(DT_MAX)),
        's5_b_re': nrm(9, (DEPTH, 2, G, P, HG), (2 * HG) ** -0.5),
        's5_b_im': nrm(10, (DEPTH, 2, G, P, HG), (2 * HG) ** -0.5),
        's5_c_re': nrm(11, (DEPTH, 2, G, HG, P), (2 * P) ** -0.5),
        's5_c_im': nrm(12, (DEPTH, 2, G, HG, P), (2 * P) ** -0.5),
        's5_d': nrm(13, (DEPTH, S5_WIDTH), 1.0),
        's5_w_glu': nrm(14, (DEPTH, S5_WIDTH, S5_WIDTH), S5_WIDTH ** -0.5),
        's5_b_glu': nrm(15, (DEPTH, S5_WIDTH), 0.01),
        'mla_q_norm': 1.0 + nrm(16, (DEPTH, MLA_Q_RANK), 0.02),
        'mla_w_uq': nrm(17, (DEPTH, MLA_Q_RANK, MLA_HEADS * (MLA_NOPE + MLA_ROPE)), MLA_Q_RANK ** -0.5),
        'mla_kv_norm': 1.0 + nrm(18, (DEPTH, MLA_KV_RANK), 0.02),
        'mla_w_ukv': nrm(19, (DEPTH, MLA_KV_RANK, MLA_HEADS * (MLA_NOPE + MLA_V)), MLA_KV_RANK ** -0.5),
        'win_sink': nrm(20, (DEPTH, WIN_Q_HEADS), 0.5),
        'w_branch': nrm(21, (DEPTH, N_BRANCH, BRANCH_WIDTH, D_MODEL), BRANCH_WIDTH ** -0.5),
        'w_out': nrm(22, (DEPTH, D_MODEL, D_MODEL), BETA * D_MODEL ** -0.5),
        'ln1_g': 1.0 + nrm(23, (DEPTH, D_MODEL), 0.02),
        'ln1_b': nrm(24, (DEPTH, D_MODEL), 0.01),
        'ln2_g': 1.0 + nrm(25, (DEPTH, D_MODEL), 0.02),
        'ln2_b': nrm(26, (DEPTH, D_MODEL), 0.01),
        'w_router': nrm(27, (DEPTH, D_MODEL, N_EXPERTS), D_MODEL ** -0.5),
        'w_gate': nrm(28, (DEPTH, N_EXPERTS, D_MODEL, EXPERT_FF), D_MODEL ** -0.5),
        'w_up': nrm(29, (DEPTH, N_EXPERTS, D_MODEL, EXPERT_FF), D_MODEL ** -0.5),
        'w_down': nrm(30, (DEPTH, N_EXPERTS, EXPERT_FF, D_MODEL), BETA * EXPERT_FF ** -0.5),
    }


def reference(x, c, ctx, c_ctx, w_ada, b_ada, w_in, s5_lam_re, s5_lam_im, s5_log_dt, s5_b_re, s5_b_im,
              s5_c_re, s5_c_im, s5_d, s5_w_glu, s5_b_glu, mla_q_norm, mla_w_uq, mla_kv_norm, mla_w_ukv,
              win_sink, w_branch, w_out, ln1_g, ln1_b, ln2_g, ln2_b, w_router, w_gate, w_up, w_down):
    L = x.shape[1]
    rows = L // GRID_W
    row = jnp.repeat(jnp.arange(rows, dtype=jnp.float32), GRID_W)
    col = jnp.tile(jnp.arange(GRID_W, dtype=jnp.float32), rows)
    xc, xl = ctx, x
    for i in range(DEPTH):
        lp = {
            'w_ada': w_ada[i], 'b_ada': b_ada[i], 'w_in': w_in[i],
            's5_lam_re': s5_lam_re[i], 's5_lam_im': s5_lam_im[i], 's5_log_dt': s5_log_dt[i],
            's5_b_re': s5_b_re[i], 's5_b_im': s5_b_im[i], 's5_c_re': s5_c_re[i], 's5_c_im': s5_c_im[i],
            's5_d': s5_d[i], 's5_w_glu': s5_w_glu[i], 's5_b_glu': s5_b_glu[i],
            'mla_q_norm': mla_q_norm[i], 'mla_w_uq': mla_w_uq[i], 'mla_kv_norm': mla_kv_norm[i],
            'mla_w_ukv': mla_w_ukv[i], 'win_sink': win_sink[i], 'w_branch': w_branch[i], 'w_out': w_out[i],
            'ln1_g': ln1_g[i], 'ln1_b': ln1_b[i], 'ln2_g': ln2_g[i], 'ln2_b': ln2_b[i],
            'w_router': w_router[i], 'w_gate': w_gate[i], 'w_up': w_up[i], 'w_down': w_down[i],
        }
        xc, xl = trunk_layer(xc, xl, c, c_ctx, lp, row, col, update_ctx=(i < DEPTH - 1))
    return xl
```

```python
import numpy as np
from contextlib import ExitStack
import concourse.bass as bass
import concourse.mybir as mybir
from concourse.bass_utils import run_bass_kernel_spmd

F32 = mybir.dt.float32
BF16 = mybir.dt.bfloat16
I32 = mybir.dt.int32
AF = mybir.ActivationFunctionType
ALU = mybir.AluOpType

ENGS = ("pe", "dve", "act", "pool", "sp")
D = 1024
NT = 2304
LC = 256
L = 2048
DEPTH = 4
ALPHA = (2 * DEPTH) ** 0.25
EPS = 1e-6
NZT = 45
TBS = [(0, 256, 1), (256, 512, 0), (768, 512, 0), (1280, 512, 0), (1792, 512, 0)]


class Prog:
    def __init__(self, nc, es, n_dma_sems=24):
        self.nc = nc
        self.q = {e: [] for e in ENGS}
        self.sem = {}
        self.cnt = {}
        for e in ENGS:
            self.sem[e] = es.enter_context(nc.semaphore("s_" + e))
            self.cnt[e] = 0
        self.dma_sems = []
        for i in range(n_dma_sems):
            nm = "d%d" % i
            self.sem[nm] = es.enter_context(nc.semaphore("s_" + nm))
            self.cnt[nm] = 0
            self.dma_sems.append(nm)
        self.dma_rr = 0
        self.seen = {e: {} for e in ENGS}
        self.lastw = {}
        self.readers = {}
        self.nops = 0

    def _deps(self, eng, reads, writes):
        deps = {}

        def need(st, v):
            if st == eng and eng == "pe":
                return
            if deps.get(st, 0) < v:
                deps[st] = v

        for k in reads:
            lw = self.lastw.get(k)
            if lw is not None:
                need(*lw)
        for k in writes:
            lw = self.lastw.get(k)
            if lw is not None:
                need(*lw)
            for r in self.readers.get(k, ()):
                need(*r)
        return deps

    def _emit_waits(self, eng, deps):
        for st, v in deps.items():
            if self.seen[eng].get(st, 0) < v:
                self.seen[eng][st] = v
                sem = self.sem[st]
                self.q[eng].append(lambda e, sem=sem, v=v: e.wait_ge(sem, v))

    def _record(self, done, reads, writes):
        for k in reads:
            self.readers.setdefault(k, []).append(done)
        for k in writes:
            self.lastw[k] = done
            self.readers[k] = []

    def op(self, eng, fn, reads=(), writes=()):
        deps = self._deps(eng, reads, writes)
        self._emit_waits(eng, deps)
        self.cnt[eng] += 1
        sem = self.sem[eng]
        self.q[eng].append(lambda e, fn=fn, sem=sem: fn(e).then_inc(sem, 1))
        self._record((eng, self.cnt[eng]), reads, writes)
        self.nops += 1

    def dma(self, qeng, out, in_, reads=(), writes=(), **kw):
        st = self.dma_sems[self.dma_rr % len(self.dma_sems)]
        self.dma_rr += 1
        deps = self._deps(st, reads, writes)
        if self.cnt[st] > 0:
            deps[st] = self.cnt[st]
        self._emit_waits(qeng, deps)
        self.cnt[st] += 16
        sem = self.sem[st]
        self.q[qeng].append(
            lambda e, out=out, in_=in_, sem=sem, kw=kw: e.dma_start(out=out, in_=in_, **kw).then_inc(sem, 16))
        self._record((st, self.cnt[st]), reads, writes)
        self.nops += 1

    def barrier(self):
        for eng in ENGS:
            deps = {}
            for st in self.sem:
                if st != eng and self.cnt[st] > 0:
                    deps[st] = self.cnt[st]
            self._emit_waits(eng, deps)
        self.lastw = {}
        self.readers = {}

    def replay(self):
        nc = self.nc
        q = self.q
        with nc.Block() as block:
            @block.tensor
            def _(e):
                for f in q["pe"]:
                    f(e)

            @block.vector
            def _(e):
                for f in q["dve"]:
                    f(e)

            @block.scalar
            def _(e):
                for f in q["act"]:
                    f(e)

            @block.gpsimd
            def _(e):
                for f in q["pool"]:
                    f(e)

            @block.sync
            def _(e):
                for f in q["sp"]:
                    f(e)
        self.q = {e: [] for e in ENGS}


_UID = [0]


def U(name):
    _UID[0] += 1
    return "%s_u%d" % (name, _UID[0])


class Rot:
    def __init__(self, nc, es, name, n, shape, dtype, psum=False):
        self.name = name
        self.n = n
        self.i = 0
        if psum:
            self.t = [es.enter_context(nc.psum_tensor(U("%s%d" % (name, j)), shape, dtype)) for j in range(n)]
        else:
            self.t = [es.enter_context(nc.sbuf_tensor(U("%s%d" % (name, j)), shape, dtype)) for j in range(n)]

    def next(self):
        j = self.i % self.n
        self.i += 1
        return self.t[j], "%s%d" % (self.name, j)


class K:
    pass


def build(nlayers=DEPTH, dbg=()):
    nc = bass.Bass("TRN2", target_bir_lowering=False)
    k = K()
    k.nc = nc
    k.dbg = dbg
    din = lambda name, shape, dt=F32: nc.dram_tensor(name, list(shape), dt, kind="ExternalInput").ap()

    def dscr(name, shape, dt):
        kind = "ExternalOutput" if name in dbg else "Internal"
        return nc.dram_tensor(name, list(shape), dt, kind=kind).ap()

    k.xin = din("xin", [NT, D])
    k.condT = din("condT", [128, 8, 2])
    k.ident = din("ident", [128, 128])
    k.w_ada = din("w_ada", [DEPTH, D, 6 * D])
    k.bada2 = din("bada2", [DEPTH, 128, 48, 2])
    k.w_inx = din("w_inx", [DEPTH, D, NZT * 128])
    k.s5_bT = din("s5_bT", [DEPTH, 2, 2, 16, 128, 128])
    k.s5_cL = din("s5_cL", [DEPTH, 128, 2, 16, 2, 16])
    k.s5_lane = din("s5_lane", [DEPTH, 128, 3, 32])
    k.s5_dg = din("s5_dg", [DEPTH, 128, 2, 4])
    k.s5_wglu = din("s5_wglu", [DEPTH, 512, 512])
    k.tau = din("tau", [128, NT])
    k.mla_g = din("mla_g", [DEPTH, 128, 5])
    k.w_uqx = din("w_uqx", [DEPTH, 384, 1280])
    k.w_ukvk = din("w_ukvk", [DEPTH, 256, 512])
    k.w_ukvv = din("w_ukvv", [DEPTH, 256, 512])
    k.rope_mla = din("rope_mla", [2, 128, L])
    k.rope_win = din("rope_win", [2, 128, L])
    k.wmask = din("wmask", [2, 128, 128])
    k.win_sink = din("win_sink", [DEPTH, 8])
    k.w_branch = din("w_branch", [DEPTH, 1536, D])
    k.w_out = din("w_out", [DEPTH, D, D])
    k.lnp = din("lnp", [DEPTH, 128, 4, 8])
    k.w_router = din("w_router", [DEPTH, D, 16])
    k.w_gate = din("w_gate", [DEPTH, 16, D, D])
    k.w_up = din("w_up", [DEPTH, 16, D, D])
    k.w_down = din("w_down", [DEPTH, 16, D, D])
    k.sel16 = din("sel16", [16, 16, 128])
    k.out = nc.dram_tensor("out", [L, D], F32, kind="ExternalOutput").ap()
    k.brT = [dscr(nm, [512, NT], BF16) for nm in ("s5T", "mlaT", "winT")]
    k.xT = dscr("xT", [D, NT], F32)
    k.zT = dscr("zT", [NZT * 128, NT], BF16)
    k.vtok = dscr("vtok", [NT, 128], BF16)
    k.h2T = dscr("h2T", [D, NT], BF16)

    with ExitStack() as es:
        P = Prog(nc, es)
        k.P = P
        k.identf = es.enter_context(nc.sbuf_tensor(U("identf"), [128, 128], F32))
        k.identb = es.enter_context(nc.sbuf_tensor(U("identb"), [128, 128], BF16))
        k.onesm = es.enter_context(nc.sbuf_tensor(U("onesm"), [128, 128], F32))
        k.mod = es.enter_context(nc.sbuf_tensor(U("mod"), [128, DEPTH, 48, 2], F32))
        k.epsc = es.enter_context(nc.sbuf_tensor(U("epsc"), [128, 1], F32))
        k.ps = [es.enter_context(nc.psum_tensor("ps%d" % i, [128, 512], F32)) for i in range(8)]
        k.psi = 0

        stage_init(k)
        stage_ada(k, nlayers)
        k.onesb = es.enter_context(nc.sbuf_tensor(U("onesb"), [128, 512], BF16))
        k.onesf = es.enter_context(nc.sbuf_tensor(U("onesf"), [128, 128], F32))
        P.op("dve", lambda e: e.memset(k.onesb[:], 1.0), writes=["onesb"])
        P.op("dve", lambda e: e.memset(k.onesf[:], 1.0), writes=["onesf"])
        k.halfpi = es.enter_context(nc.sbuf_tensor(U("halfpi"), [128, 1], F32))
        P.op("dve", lambda e: e.memset(k.halfpi[:], float(np.pi / 2)), writes=["halfpi"])
        for li in range(nlayers):
            if "skip_win" not in dbg:
                stage_ln_win(k, li)
            if "skip_s5" not in dbg:
                stage_s5(k, li)
            if "skip_mla" not in dbg:
                stage_mla(k, li)
            stage_win(k, li)
            stage_merge(k, li)
            stage_moe(k, li)
        stage_out(k)
    return nc


def dump(k, name, ap, shape, dt, reads):
    if name not in k.dbg:
        return
    t = k.nc.dram_tensor(name, list(shape), dt, kind="ExternalOutput").ap()
    k.P.dma("sp", t, ap, reads=reads)


def psn(k, lo=0, hi=8):
    j = lo + (k.psi % (hi - lo))
    k.psi += 1
    return k.ps[j], "ps%d" % j


def stage_end(k):
    k.P.barrier()
    k.P.replay()


def stage_init(k):
    nc, P = k.nc, k.P
    P.dma("sp", k.identf[:], k.ident, writes=["identf"])
    P.dma("pool", k.identb[:], k.ident, writes=["identb"])
    P.op("dve", lambda e: e.memset(k.onesm[:], 1.0 / D), writes=["onesm"])
    P.op("dve", lambda e: e.memset(k.epsc[:], EPS), writes=["epsc"])
    with ExitStack() as st:
        xr = Rot(nc, st, "xr", 2, [128, D], F32)
        xo = Rot(nc, st, "xo", 2, [128, 8, 128], F32)
        xTv = k.xT.rearrange("(k p) t -> p k t", p=128)
        for tt in range(NT // 128):
            xt, xk = xr.next()
            P.dma("sp", xt[:], k.xin[tt * 128:(tt + 1) * 128, :], writes=[xk])
            ot, ok = xo.next()
            for half in range(2):
                pt, pk = psn(k)
                for kk in range(4):
                    kf = half * 4 + kk
                    P.op("pe", lambda e, pt=pt, kk=kk, kf=kf, xt=xt: e.transpose(pt[:, kk * 128:(kk + 1) * 128], xt[:, kf * 128:(kf + 1) * 128], k.identf[:]),
                         reads=[xk, "identf"], writes=[pk])
                eng = "act" if half == 0 else "dve"
                if eng == "act":
                    P.op("act", lambda e, pt=pt, ot=ot, half=half: e.copy(ot[:, half * 4:(half + 1) * 4, :], pt[:].rearrange("p (k t) -> p k t", k=4)),
                         reads=[pk], writes=[ok + "h%d" % half])
                else:
                    P.op("dve", lambda e, pt=pt, ot=ot, half=half: e.tensor_copy(ot[:, half * 4:(half + 1) * 4, :], pt[:].rearrange("p (k t) -> p k t", k=4)),
                         reads=[pk], writes=[ok + "h%d" % half])
            P.dma("sp", xTv[:, :, tt * 128:(tt + 1) * 128], ot[:], reads=[ok + "h0", ok + "h1"], writes=["xT"])
        stage_end(k)


def stage_ada(k, nlayers):
    nc, P = k.nc, k.P
    with ExitStack() as st:
        sc = st.enter_context(nc.sbuf_tensor(U("sc"), [128, 8, 2], F32))
        bt = st.enter_context(nc.sbuf_tensor(U("bt"), [128, DEPTH, 48, 2], F32))
        wa = Rot(nc, st, "wa", 2, [128, 8, 768], F32)
        P.dma("sp", sc[:], k.condT, writes=["sc"])
        P.dma("sp", bt[:], k.bada2.rearrange("l p m s -> p l m s"), writes=["bt"])
        P.op("act", lambda e: e.activation(out=sc[:], in_=sc[:], func=AF.Silu), reads=["sc"], writes=["sc"])
        for li in range(nlayers):
            wv = k.w_ada[li].rearrange("(k p) n -> p k n", p=128)
            for cb in range(8):
                wt, wk = wa.next()
                P.dma("sp", wt[:], wv[:, :, cb * 768:(cb + 1) * 768], writes=[wk])
                pt, pk = psn(k)
                for mt in range(6):
                    for kk in range(8):
                        P.op("pe", lambda e, pt=pt, wt=wt, mt=mt, kk=kk: e.matmul(pt[:, mt * 2:mt * 2 + 2], wt[:, kk, mt * 128:(mt + 1) * 128], sc[:, kk, :], start=(kk == 0), stop=(kk == 7)),
                             reads=[wk, "sc"], writes=[pk])
                P.op("dve", lambda e, pt=pt, li=li, cb=cb: e.tensor_tensor(k.mod[:, li, cb * 6:(cb + 1) * 6, :], pt[:, 0:12].rearrange("p (m s) -> p m s", s=2), bt[:, li, cb * 6:(cb + 1) * 6, :], ALU.add),
                     reads=[pk, "bt"], writes=["mod"])
            for j in (1, 4):
                P.op("dve", lambda e, li=li, j=j: e.tensor_scalar_add(k.mod[:, li, j * 8:(j + 1) * 8, :], k.mod[:, li, j * 8:(j + 1) * 8, :], 1.0),
                     reads=["mod"], writes=["mod"])
        stage_end(k)


def ln_stats(k, xb, xk, w, tmp):
    nc, P = k.nc, k.P
    sq, mean, rstd, m2 = tmp["sq"], tmp["mean"], tmp["rstd"], tmp["m2"]
    P.op("act", lambda e: e.activation(out=sq[:, :, :w], in_=xb[:, :, :w], func=AF.Square), reads=[xk], writes=["sq"])
    p1, k1 = psn(k)
    p2, k2 = psn(k)
    for kk in range(8):
        P.op("pe", lambda e, kk=kk: e.matmul(p1[:, :w], k.onesm[:], xb[:, kk, :w], start=(kk == 0), stop=(kk == 7)), reads=[xk, "onesm"], writes=[k1])
    for kk in range(8):
        P.op("pe", lambda e, kk=kk: e.matmul(p2[:, :w], k.onesm[:], sq[:, kk, :w], start=(kk == 0), stop=(kk == 7)), reads=["sq", "onesm"], writes=[k2])
    P.op("act", lambda e: e.copy(mean[:, :w], p1[:, :w]), reads=[k1], writes=["mean"])
    P.op("dve", lambda e: e.tensor_tensor(m2[:, :w], mean[:, :w], mean[:, :w], ALU.mult), reads=["mean"], writes=["m2"])
    P.op("dve", lambda e: e.tensor_tensor(m2[:, :w], p2[:, :w], m2[:, :w], ALU.subtract), reads=[k2, "m2"], writes=["m2"])
    P.op("act", lambda e: e.activation(out=m2[:, :w], in_=m2[:, :w], func=AF.Sqrt, bias=k.epsc[:], scale=1.0), reads=["m2", "epsc"], writes=["m2"])
    P.op("dve", lambda e: e.reciprocal(rstd[:, :w], m2[:, :w]), reads=["m2"], writes=["rstd"])


def ln_tmp(nc, st):
    return {
        "sq": st.enter_context(nc.sbuf_tensor(U("ln_sq"), [128, 8, 512], F32)),
        "mean": st.enter_context(nc.sbuf_tensor(U("ln_mean"), [128, 512], F32)),
        "rstd": st.enter_context(nc.sbuf_tensor(U("ln_rstd"), [128, 512], F32)),
        "m2": st.enter_context(nc.sbuf_tensor(U("ln_m2"), [128, 512], F32)),
        "t": st.enter_context(nc.sbuf_tensor(U("ln_t"), [128, 512], F32)),
    }


def ln_apply(k, xb, xk, w, tmp, kk, out, okeys, scale_ap, bias_ap, extra_reads=()):
    P = k.P
    t = tmp["t"]
    P.op("dve", lambda e: e.tensor_tensor(t[:, :w], xb[:, kk, :w], tmp["mean"][:, :w], ALU.subtract), reads=[xk, "mean"], writes=["ln_t"])
    P.op("dve", lambda e: e.tensor_tensor(t[:, :w], t[:, :w], tmp["rstd"][:, :w], ALU.mult), reads=["ln_t", "rstd"], writes=["ln_t"])
    P.op("act", lambda e: e.activation(out=out, in_=t[:, :w], func=AF.Identity, scale=scale_ap, bias=bias_ap), reads=["ln_t", "mod"] + list(extra_reads), writes=okeys)


def stage_ln_win(k, li):
    nc, P = k.nc, k.P
    with ExitStack() as st:
        hT = st.enter_context(nc.sbuf_tensor(U("hT"), [128, 8, NT], BF16))
        with ExitStack() as st2:
            tmp = ln_tmp(nc, st2)
            xbr = Rot(nc, st2, "xb", 2, [128, 8, 512], F32)
            xTv = k.xT.rearrange("(k p) t -> p k t", p=128)
            for (t0, w, isc) in TBS:
                xb, xk = xbr.next()
                P.dma("sp", xb[:, :, :w], xTv[:, :, t0:t0 + w], reads=["xT"], writes=[xk])
                ln_stats(k, xb, xk, w, tmp)
                for kk in range(8):
                    ln_apply(k, xb, xk, w, tmp, kk, hT[:, kk, t0:t0 + w], ["hT%d" % kk],
                             k.mod[:, li, 8 + kk, isc:isc + 1], k.mod[:, li, 0 + kk, isc:isc + 1])
            P.barrier()
        wr = Rot(nc, st, "wr", 3, [128, 8, 128], BF16)
        zs = Rot(nc, st, "zs", 3, [128, NT], BF16)
        vt = st.enter_context(nc.sbuf_tensor(U("vt"), [128, 18, 128], BF16))
        wv = k.w_inx[li].rearrange("(k p) n -> p k n", p=128)
        ev = 0
        for m in range(NZT):
            wt, wk = wr.next()
            P.dma("pool", wt[:], wv[:, :, m * 128:(m + 1) * 128], writes=[wk])
            zt, zk = zs.next()
            mrows = 64 if m == 44 else 128
            for (t0, w, isc) in TBS:
                pt, pk = psn(k)
                for kk in range(8):
                    P.op("pe", lambda e, pt=pt, wt=wt, kk=kk, t0=t0, w=w, mrows=mrows: e.matmul(pt[:mrows, :w], wt[:, kk, :mrows], hT[:, kk, t0:t0 + w], start=(kk == 0), stop=(kk == 7)),
                         reads=[wk, "hT%d" % kk], writes=[pk])
                gate = 15 <= m <= 38
                if gate:
                    P.op("act", lambda e, pt=pt, zt=zt, t0=t0, w=w: e.activation(out=zt[:, t0:t0 + w], in_=pt[:, :w], func=AF.Sigmoid), reads=[pk], writes=[zk + "_%d" % t0])
                elif ev % 2 == 0:
                    P.op("act", lambda e, pt=pt, zt=zt, t0=t0, w=w, mrows=mrows: e.copy(zt[:mrows, t0:t0 + w], pt[:mrows, :w]), reads=[pk], writes=[zk + "_%d" % t0])
                else:
                    P.op("dve", lambda e, pt=pt, zt=zt, t0=t0, w=w, mrows=mrows: e.tensor_copy(zt[:mrows, t0:t0 + w], pt[:mrows, :w]), reads=[pk], writes=[zk + "_%d" % t0])
                ev += 1
            P.dma("sp", k.zT[m * 128:m * 128 + mrows, :], zt[:mrows, :], reads=[zk + "_%d" % t[0] for t in TBS], writes=["zT"])
            if m == 14:
                for tt in range(18):
                    pt, pk = psn(k)
                    for kk in range(8):
                        P.op("pe", lambda e, pt=pt, wt=wt, kk=kk, tt=tt: e.matmul(pt[:, :128], hT[:, kk, tt * 128:(tt + 1) * 128], wt[:, kk, :], start=(kk == 0), stop=(kk == 7)),
                             reads=[wk, "hT%d" % kk], writes=[pk])
                    P.op("dve", lambda e, pt=pt, tt=tt: e.tensor_copy(vt[:, tt, :], pt[:, :128]), reads=[pk], writes=["vt"])
                P.dma("sp", k.vtok.rearrange("(t p) c -> p t c", p=128), vt[:], reads=["vt"], writes=["vtok"])
        stage_end(k)


def stage_s5(k, li):
    nc, P = k.nc, k.P
    TWO_PI = float(2 * np.pi)
    with ExitStack() as st:
        sb = lambda name, shape, dt=F32: st.enter_context(nc.sbuf_tensor(U(name), shape, dt))
        lane = sb("lane", [128, 3, 32])
        dg = sb("dg", [128, 2, 4])
        craw = sb("craw", [128, 2, 16, 2, 16])
        BW = sb("BW", [128, 64, 128], BF16)
        CW = sb("CW", [128, 64, 128], BF16)
        wglu = sb("wglu", [128, 4, 512], BF16)
        tau = sb("tau", [128, NT])
        names = ["dt", "rho", "thn", "fr", "sn", "cs", "ar", "ai", "rden", "qr", "qi", "nqr", "nqi", "tA", "tB"]
        lp = {n: sb("lp_" + n, [128, 32]) for n in names}
        lpi = sb("lp_it", [128, 32], I32)
        P.dma("sp", lane[:], k.s5_lane[li], writes=["lane"])
        P.dma("sp", dg[:], k.s5_dg[li], writes=["dg"])
        P.dma("sp", craw[:], k.s5_cL[li], writes=["craw"])
        P.dma("sp", tau[:], k.tau, writes=["tau"])
        bsrc = k.s5_bT[li].rearrange("d r t p c -> p (d r t) c")
        for j in range(8):
            P.dma("pool", BW[:, j * 8:(j + 1) * 8, :], bsrc[:, j * 8:(j + 1) * 8, :], writes=["BW"])
        P.dma("pool", wglu[:], k.s5_wglu[li].rearrange("(k p) n -> p k n", p=128), writes=["wglu"])
        P.op("pool", lambda e: e.memset(CW[:], 0.0), writes=["CW"])
        lre, lim, ldt = lane[:, 0, :], lane[:, 1, :], lane[:, 2, :]
        R = ["lane", "lp"]
        W = ["lp"]
        V = lambda fn: P.op("dve", fn, reads=R, writes=W)
        A = lambda fn: P.op("act", fn, reads=R + ["halfpi"], writes=W)
        A(lambda e: e.activation(out=lp["dt"][:], in_=ldt, func=AF.Exp))
        V(lambda e: e.tensor_tensor(lp["tA"][:], lre, lp["dt"][:], ALU.mult))
        A(lambda e: e.activation(out=lp["rho"][:], in_=lp["tA"][:], func=AF.Exp))
        V(lambda e: e.tensor_tensor(lp["thn"][:], lim, lp["dt"][:], ALU.mult))
        V(lambda e: e.tensor_scalar(lp["thn"][:], lp["thn"][:], float(1.0 / TWO_PI), None, ALU.mult))
        V(lambda e: e.tensor_copy(lpi[:], lp["thn"][:]))
        V(lambda e: e.tensor_copy(lp["tB"][:], lpi[:]))
        V(lambda e: e.tensor_tensor(lp["fr"][:], lp["thn"][:], lp["tB"][:], ALU.subtract))
        A(lambda e: e.activation(out=lp["sn"][:], in_=lp["fr"][:], func=AF.Sin, scale=TWO_PI))
        A(lambda e: e.activation(out=lp["fr"][:], in_=lp["fr"][:], func=AF.Abs))
        A(lambda e: e.activation(out=lp["cs"][:], in_=lp["fr"][:], func=AF.Sin, scale=-TWO_PI, bias=k.halfpi[:]))
        V(lambda e: e.tensor_tensor(lp["ar"][:], lp["rho"][:], lp["cs"][:], ALU.mult))
        V(lambda e: e.tensor_tensor(lp["ai"][:], lp["rho"][:], lp["sn"][:], ALU.mult))
        V(lambda e: e.tensor_tensor(lp["tA"][:], lre, lre, ALU.mult))
        V(lambda e: e.tensor_tensor(lp["tB"][:], lim, lim, ALU.mult))
        V(lambda e: e.tensor_tensor(lp["tA"][:], lp["tA"][:], lp["tB"][:], ALU.add))
        V(lambda e: e.reciprocal(lp["rden"][:], lp["tA"][:]))
        V(lambda e: e.tensor_scalar_add(lp["ar"][:], lp["ar"][:], -1.0))
        V(lambda e: e.tensor_tensor(lp["tA"][:], lp["ar"][:], lre, ALU.mult))
        V(lambda e: e.tensor_tensor(lp["tB"][:], lp["ai"][:], lim, ALU.mult))
        V(lambda e: e.tensor_tensor(lp["tA"][:], lp["tA"][:], lp["tB"][:], ALU.add))
        V(lambda e: e.tensor_tensor(lp["qr"][:], lp["tA"][:], lp["rden"][:], ALU.mult))
        V(lambda e: e.tensor_tensor(lp["tA"][:], lp["ai"][:], lre, ALU.mult))
        V(lambda e: e.tensor_tensor(lp["tB"][:], lp["ar"][:], lim, ALU.mult))
        V(lambda e: e.tensor_tensor(lp["tA"][:], lp["tA"][:], lp["tB"][:], ALU.subtract))
        V(lambda e: e.tensor_tensor(lp["qi"][:], lp["tA"][:], lp["rden"][:], ALU.mult))
        V(lambda e: e.tensor_scalar(lp["nqr"][:], lp["qr"][:], -1.0, None, ALU.mult))
        V(lambda e: e.tensor_scalar(lp["nqi"][:], lp["qi"][:], -1.0, None, ALU.mult))
        for n_ in ("rho", "thn", "sn", "cs", "qr", "qi", "dt"):
            dump(k, "lp_" + n_, lp[n_][:], [128, 32], F32, ["lp"])
        ctmp = sb("ctmp", [128, 16])
        for d in range(2):
            for lt in range(16):
                col = d * 16 + lt
                cr, ci = craw[:, d, lt, 0, :], craw[:, d, lt, 1, :]
                for half in range(2):
                    g = 2 * lt + half
                    gl = g % 8
                    ps_ = slice(half * 64, half * 64 + 64)
                    for ri in range(2):
                        s1 = lp["qi"] if ri == 0 else lp["nqr"]
                        s2 = lp["qr"] if ri == 0 else lp["nqi"]
                        op1 = ALU.subtract if ri == 0 else ALU.add
                        P.op("dve", lambda e, ci=ci, s1=s1, col=col, ps_=ps_: e.tensor_scalar(ctmp[ps_, :], ci[ps_, :], s1[ps_, col:col + 1], None, ALU.mult), reads=["craw", "lp"], writes=["ctmp"])
                        P.op("dve", lambda e, cr=cr, s2=s2, col=col, ps_=ps_, ri=ri, gl=gl, op1=op1: e.scalar_tensor_tensor(CW[ps_, col * 2 + ri, gl * 16:(gl + 1) * 16], cr[ps_, :], s2[ps_, col:col + 1], ctmp[ps_, :], ALU.mult, op1), reads=["craw", "lp", "ctmp"], writes=["CW"])
        ut = sb("ut", [128, NT], BF16)
        it = sb("s5it", [128, NT], I32)
        fr = sb("s5fr", [128, NT])
        Sn = sb("s5S", [128, NT])
        Cs = sb("s5C", [128, NT])
        t1 = sb("s5t1", [128, NT])
        t2 = sb("s5t2", [128, NT])
        t3 = sb("s5t3", [128, NT])
        zr = sb("s5zr", [128, NT])
        zi = sb("s5zi", [128, NT])
        xr = sb("s5xr", [128, NT], BF16)
        xi = sb("s5xi", [128, NT], BF16)
        gT = sb("s5g", [128, 4, NT], BF16)
        ysr = Rot(nc, st, "s5ys", 2, [128, 512], F32)
        segs = [(0, LC), (LC, NT)]
        for gt in range(4):
            P.dma("sp", ut[:], k.zT[gt * 128:(gt + 1) * 128, :], reads=["zT"], writes=["ut"])
            for d in range(2):
                for l4 in range(4):
                    lt = gt * 4 + l4
                    col = d * 16 + lt
                    thn = lp["thn"][:, col:col + 1]
                    first = (d == 0 and l4 == 0)
                    last = (d == 1 and l4 == 3)
                    for (a, b) in segs:
                        src = tau[:, a:b] if d == 0 else (tau[:, b - 1::-1] if a == 0 else tau[:, b - 1:a - 1:-1])
                        P.op("dve", lambda e, src=src, a=a, b=b, thn=thn: e.tensor_scalar(it[:, a:b], src, thn, None, ALU.mult), reads=["tau", "lp"], writes=["it"])
                        P.op("dve", lambda e, src=src, a=a, b=b, thn=thn: e.scalar_tensor_tensor(fr[:, a:b], src, thn, it[:, a:b], ALU.mult, ALU.subtract), reads=["tau", "lp", "it"], writes=["fr"])
                    P.op("act", lambda e: e.activation(out=Sn[:], in_=fr[:], func=AF.Sin, scale=TWO_PI), reads=["fr"], writes=["Sn"])
                    P.op("act", lambda e: e.activation(out=fr[:], in_=fr[:], func=AF.Abs), reads=["fr"], writes=["fr"])
                    P.op("act", lambda e: e.activation(out=Cs[:], in_=fr[:], func=AF.Sin, scale=-TWO_PI, bias=k.halfpi[:]), reads=["fr", "halfpi"], writes=["Cs"])
                    for (t0, w, isc) in TBS:
                        pr, kr = psn(k, 5, 8)
                        pi_, ki = psn(k, 5, 8)
                        sl = slice(t0, t0 + w)
                        P.op("pe", lambda e, pr=pr, sl=sl, w=w, d=d, lt=lt: e.matmul(pr[:, :w], BW[:, (d * 2 + 0) * 16 + lt, :], ut[:, sl], start=True, stop=True), reads=["BW", "ut"], writes=[kr])
                        P.op("pe", lambda e, pi_=pi_, sl=sl, w=w, d=d, lt=lt: e.matmul(pi_[:, :w], BW[:, (d * 2 + 1) * 16 + lt, :], ut[:, sl], start=True, stop=True), reads=["BW", "ut"], writes=[ki])
                        P.op("dve", lambda e, pr=pr, sl=sl, w=w: e.tensor_tensor(t1[:, sl], pr[:, :w], Cs[:, sl], ALU.mult), reads=[kr, "Cs"], writes=["t1"])
                        P.op("dve", lambda e, pi_=pi_, sl=sl, w=w: e.tensor_tensor(t2[:, sl], pi_[:, :w], Sn[:, sl], ALU.mult), reads=[ki, "Sn"], writes=["t2"])
                        P.op("pool", lambda e, sl=sl: e.tensor_tensor(t1[:, sl], t1[:, sl], t2[:, sl], ALU.add), reads=["t1", "t2"], writes=["t1"])
                        P.op("dve", lambda e, pi_=pi_, sl=sl, w=w: e.tensor_tensor(t2[:, sl], pi_[:, :w], Cs[:, sl], ALU.mult), reads=[ki, "Cs", "t1"], writes=["t2"])
                        P.op("dve", lambda e, pr=pr, sl=sl, w=w: e.tensor_tensor(t3[:, sl], pr[:, :w], Sn[:, sl], ALU.mult), reads=[kr, "Sn"], writes=["t3"])
                        P.op("pool", lambda e, sl=sl: e.tensor_tensor(t2[:, sl], t2[:, sl], t3[:, sl], ALU.subtract), reads=["t2", "t3"], writes=["t2"])
                    rho = lp["rho"][:, col:col + 1]
                    for (src, dst) in ((t1, zr), (t2, zi)):
                        if d == 0:
                            P.op("dve", lambda e, src=src, dst=dst, rho=rho: e.tensor_tensor_scan(dst[:, 0:LC], rho.to_broadcast([128, LC]), src[:, 0:LC], 0.0, ALU.mult, ALU.add), reads=["t1", "t2", "lp"], writes=["z"])
                            P.op("dve", lambda e, src=src, dst=dst, rho=rho: e.tensor_tensor_scan(dst[:, LC:NT], rho.to_broadcast([128, L]), src[:, LC:NT], dst[:, LC - 1:LC], ALU.mult, ALU.add), reads=["t1", "t2", "lp", "z"], writes=["z"])
                        else:
                            P.op("dve", lambda e, src=src, dst=dst, rho=rho: e.tensor_tensor_scan(dst[:, LC - 1::-1], rho.to_broadcast([128, LC]), src[:, LC - 1::-1], 0.0, ALU.mult, ALU.add), reads=["t1", "t2", "lp"], writes=["z"])
                            P.op("dve", lambda e, src=src, dst=dst, rho=rho: e.tensor_tensor_scan(dst[:, NT - 1:LC - 1:-1], rho.to_broadcast([128, L]), src[:, NT - 1:LC - 1:-1], dst[:, 0:1], ALU.mult, ALU.add), reads=["t1", "t2", "lp", "z"], writes=["z"])
                    P.op("pool", lambda e: e.tensor_tensor(t1[:], Cs[:], zr[:], ALU.mult), reads=["Cs", "z"], writes=["t1"])
                    P.op("pool", lambda e: e.tensor_tensor(t3[:], Sn[:], zi[:], ALU.mult), reads=["Sn", "z"], writes=["t3"])
                    P.op("pool", lambda e: e.tensor_tensor(xr[:], t1[:], t3[:], ALU.subtract), reads=["t1", "t3"], writes=["xr"])
                    P.op("pool", lambda e: e.tensor_tensor(t2[:], Sn[:], zr[:], ALU.mult), reads=["Sn", "z"], writes=["t2"])
                    P.op("pool", lambda e: e.tensor_tensor(t3[:], Cs[:], zi[:], ALU.mult), reads=["Cs", "z", "xr"], writes=["t3"])
                    P.op("pool", lambda e: e.tensor_tensor(xi[:], t2[:], t3[:], ALU.add), reads=["t2", "t3"], writes=["xi"])
                    if gt == 0 and d == 0 and l4 == 0:
                        dump(k, "d_Sn", Sn[:], [128, NT], F32, ["Sn"])
                        dump(k, "d_Cs", Cs[:], [128, NT], F32, ["Cs"])
                        dump(k, "d_zr", zr[:], [128, NT], F32, ["z"])
                        dump(k, "d_zi", zi[:], [128, NT], F32, ["z"])
                        dump(k, "d_xr", xr[:], [128, NT], BF16, ["xr"])
                        dump(k, "d_xi", xi[:], [128, NT], BF16, ["xi"])
                    for bi, (t0, w, isc) in enumerate(TBS):
                        sl = slice(t0, t0 + w)
                        yk = "ps%d" % bi
                        P.op("pe", lambda e, bi=bi, sl=sl, w=w, col=col, first=first: e.matmul(k.ps[bi][:, :w], CW[:, col * 2 + 0, :], xr[:, sl], start=first, stop=False), reads=["CW", "xr"], writes=[yk])
                        P.op("pe", lambda e, bi=bi, sl=sl, w=w, col=col, last=last: e.matmul(k.ps[bi][:, :w], CW[:, col * 2 + 1, :], xi[:, sl], start=False, stop=last), reads=["CW", "xi"], writes=[yk])
            for bi, (t0, w, isc) in enumerate(TBS):
                sl = slice(t0, t0 + w)
                ys, ysk = ysr.next()
                P.op("dve", lambda e, bi=bi, sl=sl, w=w, ys=ys, gt=gt: e.scalar_tensor_tensor(ys[:, :w], ut[:, sl], dg[:, 0, gt:gt + 1], k.ps[bi][:, :w], ALU.mult, ALU.add), reads=["ut", "dg", "ps%d" % bi], writes=[ysk])
                P.op("act", lambda e, sl=sl, w=w, ys=ys, gt=gt: e.activation(out=gT[:, gt, sl], in_=ys[:, :w], func=AF.Gelu_apprx_tanh), reads=[ysk], writes=["gT%d" % gt])
        so = Rot(nc, st, "s5o", 2, [128, NT], BF16)
        sgr = Rot(nc, st, "s5sg", 2, [128, 512], F32)
        for mo in range(4):
            ot, ok = so.next()
            for (t0, w, isc) in TBS:
                sl = slice(t0, t0 + w)
                pt, pk = psn(k)
                for kk in range(4):
                    P.op("pe", lambda e, pt=pt, kk=kk, sl=sl, w=w, mo=mo: e.matmul(pt[:, :w], wglu[:, kk, mo * 128:(mo + 1) * 128], gT[:, kk, sl], start=(kk == 0), stop=(kk == 3)), reads=["wglu", "gT%d" % kk], writes=[pk])
                sg, sgk = sgr.next()
                P.op("act", lambda e, pt=pt, w=w, sg=sg, mo=mo: e.activation(out=sg[:, :w], in_=pt[:, :w], func=AF.Sigmoid, bias=dg[:, 1, mo:mo + 1], scale=1.0), reads=[pk, "dg"], writes=[sgk])
                P.op("dve", lambda e, sg=sg, sl=sl, w=w, ot=ot, mo=mo: e.tensor_tensor(ot[:, sl], gT[:, mo, sl], sg[:, :w], ALU.mult), reads=[sgk, "gT%d" % mo], writes=[ok + "_%d" % t0])
            P.dma("sp", k.brT[0][mo * 128:(mo + 1) * 128, :], ot[:], reads=[ok + "_%d" % t[0] for t in TBS], writes=["s5T"])
        stage_end(k)


def rms_norm_T(k, st, src, nk, gains, dst, tag):
    nc, P = k.nc, k.P
    sq = st.enter_context(nc.sbuf_tensor(U("rms_sq"), [128, nk, 512], BF16))
    rinv = st.enter_context(nc.sbuf_tensor(U("rms_ri"), [128, 512], F32))
    for (t0, w, isc) in TBS:
        sl = slice(t0, t0 + w)
        P.op("act", lambda e, sl=sl, w=w: e.activation(out=sq[:, :, :w], in_=src[:, :, sl], func=AF.Square), reads=[tag + "src"], writes=[tag + "sq"])
        pt, pk = psn(k)
        for kk in range(nk):
            P.op("pe", lambda e, pt=pt, kk=kk, w=w: e.matmul(pt[:, :w], k.onesb[:, 0:128], sq[:, kk, :w], start=(kk == 0), stop=(kk == nk - 1)), reads=[tag + "sq", "onesb"], writes=[pk])
        P.op("act", lambda e, pt=pt, w=w: e.activation(out=rinv[:, :w], in_=pt[:, :w], func=AF.Sqrt, scale=float(1.0 / (nk * 128)), bias=k.epsc[:]), reads=[pk, "epsc"], writes=[tag + "ri"])
        P.op("dve", lambda e, w=w: e.reciprocal(rinv[:, :w], rinv[:, :w]), reads=[tag + "ri"], writes=[tag + "ri"])
        for kk in range(nk):
            P.op("dve", lambda e, kk=kk, sl=sl, w=w: e.scalar_tensor_tensor(dst[:, kk, sl], src[:, kk, sl], gains[:, kk:kk + 1], rinv[:, :w], ALU.mult, ALU.mult), reads=[tag + "src", tag + "ri", "mg"], writes=[tag + "dst"])


def softmax_pv(k, ost_rot, score_fn, nkc, va_fn, nq, scale, out_dram, tagp, PTr, extra_den=None, post=None):
    nc, P = k.nc, k.P
    po, pok = psn(k, 0, 2)
    for kc in range(nkc):
        pscr, psk = psn(k, 2, 6)
        score_fn(kc, pscr, psk)
        pt, ptk = PTr.next()
        P.op("act", lambda e, pscr=pscr, pt=pt: e.activation(out=pt[:, :nq], in_=pscr[:, :nq], func=AF.Exp, scale=scale), reads=[psk], writes=[ptk])
        if post is not None:
            post(kc, pt, ptk)
        va, vak = va_fn(kc)
        P.op("pe", lambda e, po=po, va=va, pt=pt, kc=kc: e.matmul(po[0:65, :nq], va, pt[:, :nq], start=(kc == 0), stop=(kc == nkc - 1 and extra_den is None)), reads=[vak, ptk], writes=[pok])
    if extra_den is not None:
        extra_den(po, pok)
    rv = k.att_rv
    ob = k.att_ob
    P.op("dve", lambda e, po=po: e.reciprocal(rv[64:65, :nq], po[64:65, :nq]), reads=[pok], writes=["att_rv"])
    pb, pbk = psn(k, 6, 8)
    P.op("pe", lambda e, pb=pb: e.matmul(pb[0:64, :nq], k.onesf[64:65, 0:64], rv[64:65, :nq], start=True, stop=True), reads=["att_rv", "onesf"], writes=[pbk])
    P.op("act", lambda e, po=po: e.copy(ob[0:64, :nq], po[0:64, :nq]), reads=[pok], writes=["att_ob"])
    ot, otk = ost_rot.next()
    P.op("dve", lambda e, pb=pb, ot=ot: e.tensor_tensor(ot[0:64, :nq], ob[0:64, :nq], pb[0:64, :nq], ALU.mult), reads=["att_ob", pbk], writes=[otk])
    P.dma("sp", out_dram, ot[0:64, :nq], reads=[otk], writes=[tagp])


def stage_mla(k, li):
    nc, P = k.nc, k.P
    SC = float(96 ** -0.5)
    with ExitStack() as st:
        sb = lambda name, shape, dt=F32: st.enter_context(nc.sbuf_tensor(U(name), shape, dt))
        mg = sb("mg", [128, 5])
        P.dma("sp", mg[:], k.mla_g[li], writes=["mg"])
        QN = sb("QN", [128, 4, NT], BF16)
        RP = sb("RPl", [128, 3, NT], BF16)
        QR2 = sb("QR2", [128, 3, L], BF16)
        KN = sb("KN", [128, 4, NT], BF16)
        KR4 = sb("KR4", [128, NT], BF16)
        VA = sb("VA", [128, 18, 8, 65], BF16)
        k.att_rv = sb("att_rv", [128, 512])
        k.att_ob = sb("att_ob", [128, 512])
        P.op("pool", lambda e: e.memset(VA[:], 1.0), writes=["VA"])
        with ExitStack() as st2:
            sb2 = lambda name, shape, dt=F32: st2.enter_context(nc.sbuf_tensor(U(name), shape, dt))
            qa = sb2("qa", [128, 3, NT], BF16)
            kva = sb2("kva", [128, 2, NT], BF16)
            qn = sb2("qn", [128, 3, NT], BF16)
            kvn = sb2("kvn", [128, 2, NT], BF16)
            RS = sb2("RSl", [128, 3, L], BF16)
            KP = sb2("KP", [128, NT], BF16)
            KS = sb2("KS", [128, L], BF16)
            rope = sb2("ropem", [128, 2, L])
            wuq = sb2("wuq", [128, 3, 1280], BF16)
            wkk = sb2("wkk", [128, 2, 512], BF16)
            wvv = sb2("wvv", [128, 2, 512], BF16)
            tA = sb2("mtA", [128, 512])
            tB = sb2("mtB", [128, 512])
            P.dma("sp", qa[:], k.zT[4 * 128:7 * 128, :].rearrange("(k p) t -> p k t", p=128), reads=["zT"], writes=["qsrc"])
            P.dma("sp", kva[:], k.zT[7 * 128:9 * 128, :].rearrange("(k p) t -> p k t", p=128), reads=["zT"], writes=["ksrc"])
            P.dma("sp", rope[:], k.rope_mla.rearrange("c p t -> p c t"), writes=["rope"])
            for b in range(4):
                P.dma("sp", KP[b * 32:(b + 1) * 32, :], k.zT[44 * 128:44 * 128 + 32, :], reads=["zT"], writes=["KP"])
                P.dma("sp", KS[b * 32:(b + 1) * 32, :], k.zT[44 * 128 + 32:44 * 128 + 64, LC:NT], reads=["zT"], writes=["KS"])
            for j in range(3):
                P.dma("pool", wuq[:, j, :], k.w_uqx[li][j * 128:(j + 1) * 128, :], writes=["wuq"])
            P.dma("pool", wkk[:], k.w_ukvk[li].rearrange("(k p) n -> p k n", p=128), writes=["wkk"])
            P.dma("pool", wvv[:], k.w_ukvv[li].rearrange("(k p) n -> p k n", p=128), writes=["wvv"])
            rms_norm_T(k, st2, qa, 3, mg[:, 0:3], qn, "q")
            rms_norm_T(k, st2, kva, 2, mg[:, 3:5], kvn, "k")
            P.op("dve", lambda e: e.tensor_copy(KR4[:, 0:LC], KP[:, 0:LC]), reads=["KP"], writes=["KR4"])
            for c in range(4):
                sl = slice(c * 512, (c + 1) * 512)
                sln = slice(LC + c * 512, LC + (c + 1) * 512)
                P.op("dve", lambda e, sl=sl, sln=sln: e.tensor_tensor(tA[:], KP[:, sln], rope[:, 0, sl], ALU.mult), reads=["KP", "rope"], writes=["mtA"])
                P.op("pool", lambda e, sl=sl: e.tensor_tensor(tB[:], KS[:, sl], rope[:, 1, sl], ALU.mult), reads=["KS", "rope"], writes=["mtB"])
                P.op("dve", lambda e, sln=sln: e.tensor_tensor(KR4[:, sln], tA[:], tB[:], ALU.add), reads=["mtA", "mtB"], writes=["KR4"])
            for m in range(10):
                for (t0, w, isc) in TBS:
                    sl = slice(t0, t0 + w)
                    if m >= 7 and isc:
                        continue
                    pt, pk = psn(k)
                    for kk in range(3):
                        P.op("pe", lambda e, pt=pt, kk=kk, sl=sl, w=w, m=m: e.matmul(pt[:, :w], wuq[:, kk, m * 128:(m + 1) * 128], qn[:, kk, sl], start=(kk == 0), stop=(kk == 2)), reads=["wuq", "qdst"], writes=[pk])
                    if m < 4:
                        dst = QN[:, m, sl]
                    elif m < 7:
                        dst = RP[:, m - 4, sl]
                    else:
                        dst = RS[:, m - 7, t0 - LC:t0 - LC + w]
                    P.op("act", lambda e, pt=pt, dst=dst, w=w: e.copy(dst, pt[:, :w]), reads=[pk], writes=["Q%d" % m])
            for j in range(3):
                for c in range(4):
                    sl = slice(c * 512, (c + 1) * 512)
                    sln = slice(LC + c * 512, LC + (c + 1) * 512)
                    P.op("dve", lambda e, sl=sl, sln=sln, j=j: e.tensor_tensor(tA[:], RP[:, j, sln], rope[:, 0, sl], ALU.mult), reads=["Q%d" % (4 + j), "rope"], writes=["mtA"])
                    P.op("pool", lambda e, sl=sl, j=j: e.tensor_tensor(tB[:], RS[:, j, sl], rope[:, 1, sl], ALU.mult), reads=["Q%d" % (7 + j), "rope"], writes=["mtB"])
                    P.op("dve", lambda e, sl=sl, j=j: e.tensor_tensor(QR2[:, j, sl], tA[:], tB[:], ALU.add), reads=["mtA", "mtB"], writes=["QR2"])
            for j in range(4):
                for (t0, w, isc) in TBS:
                    sl = slice(t0, t0 + w)
                    pt, pk = psn(k)
                    for kk in range(2):
                        P.op("pe", lambda e, pt=pt, kk=kk, sl=sl, w=w, j=j: e.matmul(pt[:, :w], wkk[:, kk, j * 128:(j + 1) * 128], kvn[:, kk, sl], start=(kk == 0), stop=(kk == 1)), reads=["wkk", "kdst"], writes=[pk])
                    P.op("act", lambda e, pt=pt, sl=sl, w=w, j=j: e.copy(KN[:, j, sl], pt[:, :w]), reads=[pk], writes=["KN"])
            for tt in range(18):
                pt, pk = psn(k)
                for kk in range(2):
                    P.op("pe", lambda e, pt=pt, kk=kk, tt=tt: e.matmul(pt[:, :512], kvn[:, kk, tt * 128:(tt + 1) * 128], wvv[:, kk, :], start=(kk == 0), stop=(kk == 1)), reads=["wvv", "kdst"], writes=[pk])
                P.op("dve", lambda e, pt=pt, tt=tt: e.tensor_copy(VA[:, tt, :, 0:64], pt[:, :512].rearrange("p (h d) -> p h d", h=8)), reads=[pk], writes=["VA"])
            P.barrier()
        PTr = Rot(nc, st, "mPT", 3, [128, 512], BF16)
        ostr = Rot(nc, st, "most", 2, [128, 512], BF16)
        for h in range(8):
            j, half = h // 2, h % 2
            nb = slice(half * 64, half * 64 + 64)
            b = h % 3
            rb = slice(b * 32, b * 32 + 32)
            rj = h // 3
            for qb in range(5):
                if qb < 4:
                    q0, nq = LC + qb * 512, 512
                    nkc = 18
                else:
                    q0, nq = 0, LC
                    nkc = 2
                qs = slice(q0, q0 + nq)

                def score_fn(kc, pscr, psk, qs=qs, nq=nq, qb=qb, nb=nb, rb=rb, rj=rj, j=j, q0=q0):
                    ks = slice(kc * 128, (kc + 1) * 128)
                    P.op("pe", lambda e: e.matmul(pscr[:, :nq], KN[nb, j, ks], QN[nb, j, qs], start=True, stop=False), reads=["KN", "Q%d" % j], writes=[psk])
                    if kc >= 2:
                        P.op("pe", lambda e: e.matmul(pscr[:, :nq], KR4[rb, ks], QR2[rb, rj, q0 - LC:q0 - LC + nq], start=False, stop=True), reads=["KR4", "QR2"], writes=[psk])
                    else:
                        P.op("pe", lambda e: e.matmul(pscr[:, :nq], KR4[rb, ks], RP[rb, rj, qs], start=False, stop=True), reads=["KR4", "Q%d" % (4 + rj)], writes=[psk])

                softmax_pv(k, ostr, score_fn, nkc, lambda kc, h=h: (VA[:, kc, h, :], "VA"), nq, SC,
                           k.brT[1][h * 64:(h + 1) * 64, q0:q0 + nq], "mlaT", PTr)
        stage_end(k)


def stage_win(k, li):
    nc, P = k.nc, k.P
    SC = float(64 ** -0.5)
    with ExitStack() as st:
        sb = lambda name, shape, dt=F32: st.enter_context(nc.sbuf_tensor(U(name), shape, dt))
        Qp = sb("wQp", [128, 4, NT], BF16)
        Qs = sb("wQs", [128, 4, L], BF16)
        Qr = sb("wQr", [128, 4, L], BF16)
        Kp = sb("wKp", [128, 2, NT], BF16)
        Ks = sb("wKs", [128, 2, L], BF16)
        Kr = sb("wKr", [128, 2, L], BF16)
        VW = sb("wVW", [128, 18, 2, 65], BF16)
        rope = sb("wrope", [128, 2, L])
        msk = sb("wmsk", [128, 2, 128], BF16)
        SK = sb("wSK", [1, 8, 65], BF16)
        skr = sb("wskr", [1, 8])
        tA = sb("wtA", [128, 512])
        tB = sb("wtB", [128, 512])
        k.att_rv = sb("watt_rv", [128, 512])
        k.att_ob = sb("watt_ob", [128, 512])
        P.op("pool", lambda e: e.memset(VW[:], 1.0), writes=["VW"])
        P.op("pool", lambda e: e.memset(SK[:], 0.0), writes=["SK"])
        P.dma("sp", skr[:], k.win_sink[li:li + 1, :], writes=["skr"])
        P.op("act", lambda e: e.activation(out=SK[0:1, :, 64], in_=skr[0:1, :], func=AF.Exp), reads=["skr", "SK"], writes=["SK"])
        P.dma("sp", Qp[:], k.zT[9 * 128:13 * 128, :].rearrange("(k p) t -> p k t", p=128), reads=["zT"], writes=["Qp"])
        P.dma("sp", Qs[:], k.zT[39 * 128:43 * 128, LC:NT].rearrange("(k p) t -> p k t", p=128), reads=["zT"], writes=["Qs"])
        for kk in range(2):
            for half in range(2):
                P.dma("sp", Kp[half * 64:(half + 1) * 64, kk, :], k.zT[13 * 128 + kk * 64:13 * 128 + (kk + 1) * 64, :], reads=["zT"], writes=["Kp"])
                P.dma("sp", Ks[half * 64:(half + 1) * 64, kk, :], k.zT[43 * 128 + kk * 64:43 * 128 + (kk + 1) * 64, LC:NT], reads=["zT"], writes=["Ks"])
        P.dma("sp", rope[:], k.rope_win.rearrange("c p t -> p c t"), writes=["rope"])
        P.dma("pool", msk[:], k.wmask.rearrange("c p t -> p c t"), writes=["msk"])
        for c_ in range(2):
            P.dma("sp", VW[:, :, c_, 0:64], k.vtok[:, c_ * 64:(c_ + 1) * 64].rearrange("(t p) d -> p t d", p=128), reads=["vtok", "VW"], writes=["VW"])
        for (src, ssw, dst, n, tg) in ((Qp, Qs, Qr, 4, "Q"), (Kp, Ks, Kr, 2, "K")):
            for j in range(n):
                for c in range(4):
                    sl = slice(c * 512, (c + 1) * 512)
                    sln = slice(LC + c * 512, LC + (c + 1) * 512)
                    P.op("dve", lambda e, sl=sl, sln=sln, j=j, src=src: e.tensor_tensor(tA[:], src[:, j, sln], rope[:, 0, sl], ALU.mult), reads=[tg + "p", "rope"], writes=["wtA"])
                    P.op("pool", lambda e, sl=sl, j=j, ssw=ssw: e.tensor_tensor(tB[:], ssw[:, j, sl], rope[:, 1, sl], ALU.mult), reads=[tg + "s", "rope"], writes=["wtB"])
                    P.op("dve", lambda e, sl=sl, j=j, dst=dst: e.tensor_tensor(dst[:, j, sl], tA[:], tB[:], ALU.add), reads=["wtA", "wtB"], writes=[tg + "r"])
        PTr = Rot(nc, st, "wPT", 3, [128, 512], BF16)
        ostr = Rot(nc, st, "wost", 3, [128, 512], BF16)
        for h in range(8):
            kk = h // 4
            j, half = h // 2, h % 2
            hb = slice(half * 64, half * 64 + 64)

            def den(po, pok, h=h, nq=128):
                pass
            for n in range(17):
                if n < 16:
                    q0, nq = LC + n * 128, 128
                    chunks = [("b", n + d, d) for d in (-1, 0, 1) if 0 <= n + d < 16] + [("c", 0, 0), ("c", 1, 0)]
                else:
                    q0, nq = 0, LC
                    chunks = [("c", 0, 0), ("c", 1, 0)]

                def score_fn(kc, pscr, psk, chunks=chunks, q0=q0, nq=nq, hb=hb, j=j, kk=kk):
                    typ, ci, d = chunks[kc]
                    if typ == "b":
                        P.op("pe", lambda e: e.matmul(pscr[:, :nq], Kr[hb, kk, ci * 128:(ci + 1) * 128], Qr[hb, j, q0 - LC:q0 - LC + nq], start=True, stop=True), reads=["Kr", "Qr"], writes=[psk])
                    else:
                        P.op("pe", lambda e: e.matmul(pscr[:, :nq], Kp[hb, kk, ci * 128:(ci + 1) * 128], Qp[hb, j, q0:q0 + nq], start=True, stop=True), reads=["Kp", "Qp"], writes=[psk])

                def post(kc, pt, ptk, chunks=chunks):
                    typ, ci, d = chunks[kc]
                    if typ == "b" and d != 0:
                        mi = 0 if d == -1 else 1
                        P.op("dve", lambda e: e.tensor_tensor(pt[:, :128], pt[:, :128], msk[:, mi, :], ALU.mult), reads=[ptk, "msk"], writes=[ptk])

                def va_fn(kc, chunks=chunks, kk=kk):
                    typ, ci, d = chunks[kc]
                    tt = (2 + ci) if typ == "b" else ci
                    return VW[:, tt, kk, :], "VW"

                def den(po, pok, h=h, nq=nq):
                    P.op("pe", lambda e: e.matmul(po[0:65, :nq], SK[0:1, h, :], k.onesb[0:1, :nq], start=False, stop=True), reads=["SK", "onesb"], writes=[pok])

                softmax_pv(k, ostr, score_fn, len(chunks), va_fn, nq, SC, k.brT[2][h * 64:(h + 1) * 64, q0:q0 + nq], "winT", PTr, extra_den=den, post=post)
        stage_end(k)


def stage_merge(k, li):
    nc, P = k.nc, k.P
    with ExitStack() as st:
        sb = lambda name, shape, dt=F32: st.enter_context(nc.sbuf_tensor(U(name), shape, dt))
        wbr = sb("wbr", [128, 12, D], BF16)
        wout = sb("wout", [128, 8, D], BF16)
        lnp = sb("lnp", [128, 4, 8])
        k.lnp_t = lnp
        P.dma("sp", lnp[:], k.lnp[li], writes=["mod"])
        for j in range(12):
            P.dma("pool", wbr[:, j, :], k.w_branch[li][j * 128:(j + 1) * 128, :], writes=["wbr"])
        for j in range(8):
            P.dma("pool", wout[:, j, :], k.w_out[li][j * 128:(j + 1) * 128, :], writes=["wout"])
        tmp = ln_tmp(nc, st)
        obr = [sb("ob%d" % b, [128, 4, 512], BF16) for b in range(3)]
        gbr = [sb("gb%d" % b, [128, 8, 512], BF16) for b in range(3)]
        mT = sb("mT", [128, 8, 512], BF16)
        mt1 = sb("mt1", [128, 512])
        mt2 = sb("mt2", [128, 512])
        xb = sb("mxb", [128, 8, 512])
        rb = sb("mrb", [128, 8, 512])
        x1 = sb("mx1", [128, 8, 512])
        h2 = sb("mh2", [128, 8, 512], BF16)
        xTv = k.xT.rearrange("(k p) t -> p k t", p=128)
        h2v = k.h2T.rearrange("(k p) t -> p k t", p=128)
        for (t0, w, isc) in TBS:
            sl = slice(t0, t0 + w)
            for b in range(3):
                P.dma("sp", obr[b][:, :, :w], k.brT[b][:, sl].rearrange("(k p) t -> p k t", p=128), reads=["brT"], writes=["ob%d" % b])
                P.dma("sp", gbr[b][:, :, :w], k.zT[(15 + 8 * b) * 128:(23 + 8 * b) * 128, sl].rearrange("(k p) t -> p k t", p=128), reads=["zT"], writes=["gb%d" % b])
            P.dma("sp", xb[:, :, :w], xTv[:, :, sl], reads=["xT"], writes=["mxb"])
            for mo in range(8):
                for b in range(3):
                    pt, pk = psn(k)
                    for kk in range(4):
                        P.op("pe", lambda e, pt=pt, kk=kk, b=b, mo=mo, w=w: e.matmul(pt[:, :w], wbr[:, b * 4 + kk, mo * 128:(mo + 1) * 128], obr[b][:, kk, :w], start=(kk == 0), stop=(kk == 3)), reads=["wbr", "ob%d" % b], writes=[pk])
                    if b == 0:
                        P.op("dve", lambda e, pt=pt, mo=mo, w=w: e.tensor_tensor(mt1[:, :w], pt[:, :w], gbr[0][:, mo, :w], ALU.mult), reads=[pk, "gb0"], writes=["mt1"])
                    elif b == 1:
                        P.op("dve", lambda e, pt=pt, mo=mo, w=w: e.tensor_tensor(mt2[:, :w], pt[:, :w], gbr[1][:, mo, :w], ALU.mult), reads=[pk, "gb1"], writes=["mt2"])
                        P.op("pool", lambda e, w=w: e.tensor_tensor(mt1[:, :w], mt1[:, :w], mt2[:, :w], ALU.add), reads=["mt1", "mt2"], writes=["mt1"])
                    else:
                        P.op("dve", lambda e, pt=pt, mo=mo, w=w: e.tensor_tensor(mt2[:, :w], pt[:, :w], gbr[2][:, mo, :w], ALU.mult), reads=[pk, "gb2"], writes=["mt2"])
                        P.op("pool", lambda e, mo=mo, w=w: e.tensor_tensor(mT[:, mo, :w], mt1[:, :w], mt2[:, :w], ALU.add), reads=["mt1", "mt2"], writes=["mT"])
            for mo in range(8):
                pt, pk = psn(k)
                for kk in range(8):
                    P.op("pe", lambda e, pt=pt, kk=kk, mo=mo, w=w: e.matmul(pt[:, :w], wout[:, kk, mo * 128:(mo + 1) * 128], mT[:, kk, :w], start=(kk == 0), stop=(kk == 7)), reads=["wout", "mT"], writes=[pk])
                P.op("act", lambda e, pt=pt, mo=mo, w=w, isc=isc: e.activation(out=mt1[:, :w], in_=pt[:, :w], func=AF.Identity, scale=k.mod[:, li, 16 + mo, isc:isc + 1]), reads=[pk, "mod"], writes=["mt1"])
                P.op("dve", lambda e, mo=mo, w=w: e.scalar_tensor_tensor(rb[:, mo, :w], xb[:, mo, :w], float(ALPHA), mt1[:, :w], ALU.mult, ALU.add), reads=["mxb", "mt1"], writes=["mrb"])
            ln_stats(k, rb, "mrb", w, tmp)
            for kk in range(8):
                ln_apply(k, rb, "mrb", w, tmp, kk, x1[:, kk, :w], ["mx1"], lnp[:, 0, kk:kk + 1], lnp[:, 1, kk:kk + 1])
            P.dma("sp", xTv[:, :, sl], x1[:, :, :w], reads=["mx1"], writes=["xT"])
            ln_stats(k, x1, "mx1", w, tmp)
            for kk in range(8):
                ln_apply(k, x1, "mx1", w, tmp, kk, h2[:, kk, :w], ["mh2"], k.mod[:, li, 32 + kk, isc:isc + 1], k.mod[:, li, 24 + kk, isc:isc + 1])
            P.dma("sp", h2v[:, :, sl], h2[:, :, :w], reads=["mh2"], writes=["h2T"])
        stage_end(k)


def stage_moe(k, li):
    nc, P = k.nc, k.P
    with ExitStack() as st0:
        fT = st0.enter_context(nc.sbuf_tensor(U("efT"), [128, 8, NT], BF16))
        lnp = st0.enter_context(nc.sbuf_tensor(U("elnp"), [128, 4, 8], F32))
        st = st0.enter_context(ExitStack())
        sb = lambda name, shape, dt=F32: st.enter_context(nc.sbuf_tensor(U(name), shape, dt))
        h2 = sb("eh2", [128, 8, NT], BF16)
        wr = sb("ewr", [128, 8, 16], BF16)
        aff = sb("eaff", [16, NT])
        wk_ = sb("ewk", [16, NT])
        gw = sb("egw", [16, NT])
        m8 = sb("em8", [16, 8])
        thr = sb("ethr", [16, 2])
        sel = sb("esel", [16, 16, 128])
        gwb = sb("egwb", [128, NT], BF16)
        P.dma("sp", lnp[:], k.lnp[li], writes=["mod"])
        P.dma("sp", h2[:], k.h2T.rearrange("(k p) t -> p k t", p=128), reads=["h2T"], writes=["eh2"])
        P.dma("pool", wr[:], k.w_router[li].rearrange("(k p) n -> p k n", p=128), writes=["ewr"])
        P.dma("sp", sel[:], k.sel16, writes=["esel"])
        for (t0, w, isc) in TBS:
            sl = slice(t0, t0 + w)
            pt, pk = psn(k)
            for kk in range(8):
                P.op("pe", lambda e, pt=pt, kk=kk, sl=sl, w=w: e.matmul(pt[0:16, :w], wr[:, kk, :], h2[:, kk, sl], start=(kk == 0), stop=(kk == 7)), reads=["ewr", "eh2"], writes=[pk])
            P.op("act", lambda e, pt=pt, sl=sl, w=w: e.activation(out=wk_[:, sl], in_=pt[0:16, :w], func=AF.Exp), reads=[pk], writes=["ewk"])
            p2, k2 = psn(k)
            P.op("pe", lambda e, p2=p2, sl=sl, w=w: e.matmul(p2[0:16, :w], k.onesf[0:16, 0:16], wk_[:, sl], start=True, stop=True), reads=["ewk", "onesf"], writes=[k2])
            P.op("dve", lambda e, p2=p2, sl=sl, w=w: e.reciprocal(gw[:, sl], p2[0:16, :w]), reads=[k2], writes=["egw"])
            P.op("dve", lambda e, sl=sl: e.tensor_tensor(aff[:, sl], wk_[:, sl], gw[:, sl], ALU.mult), reads=["ewk", "egw"], writes=["eaff"])
        P.op("dve", lambda e: e.tensor_copy(wk_[:], aff[:]), reads=["eaff"], writes=["ewk"])
        for si, (a, b, cap) in enumerate(((0, LC, 32), (LC, NT, 256))):
            for r in range(cap // 8):
                P.op("dve", lambda e, a=a, b=b: e.max(out=m8[:], in_=wk_[:, a:b]), reads=["ewk"], writes=["em8"])
                if r < cap // 8 - 1:
                    P.op("dve", lambda e, a=a, b=b: e.match_replace(out=wk_[:, a:b], in_to_replace=m8[:], in_values=wk_[:, a:b], imm_value=-1.0), reads=["ewk", "em8"], writes=["ewk"])
            P.op("dve", lambda e, si=si: e.tensor_copy(thr[:, si:si + 1], m8[:, 7:8]), reads=["em8"], writes=["ethr"])
            P.op("dve", lambda e, a=a, b=b, si=si: e.scalar_tensor_tensor(gw[:, a:b], aff[:, a:b], thr[:, si:si + 1], aff[:, a:b], ALU.is_ge, ALU.mult), reads=["eaff", "ethr"], writes=["egw"])
        mw = Rot(nc, st, "emw", 24, [128, D], BF16)
        act = sb("eact", [128, 8, 512], BF16)
        sa = Rot(nc, st, "esa", 2, [128, 512], F32)
        au = Rot(nc, st, "eau", 2, [128, 512], F32)
        for ex in range(16):
            ws = {}
            for nm, src in (("g", k.w_gate), ("u", k.w_up), ("d", k.w_down)):
                for kk in range(8):
                    t, tk = mw.next()
                    P.dma("pool", t[:], src[li, ex, kk * 128:(kk + 1) * 128, :], writes=[tk])
                    ws[(nm, kk)] = (t, tk)
            for (t0, w, isc) in TBS:
                sl = slice(t0, t0 + w)
                pb, pbk = psn(k, 6, 8)
                P.op("pe", lambda e, pb=pb, sl=sl, w=w, ex=ex: e.matmul(pb[:, :w], sel[:, ex, :], gw[:, sl], start=True, stop=True), reads=["esel", "egw"], writes=[pbk])
                P.op("act", lambda e, pb=pb, sl=sl, w=w: e.copy(gwb[:, sl], pb[:, :w]), reads=[pbk], writes=["egwb"])
            for (t0, w, isc) in TBS:
                sl = slice(t0, t0 + w)
                for mo in range(8):
                    pa, pak = psn(k, 0, 3)
                    pu, puk = psn(k, 3, 6)
                    for kk in range(8):
                        wt, wtk = ws[("g", kk)]
                        P.op("pe", lambda e, pa=pa, wt=wt, kk=kk, mo=mo, sl=sl, w=w: e.matmul(pa[:, :w], wt[:, mo * 128:(mo + 1) * 128], h2[:, kk, sl], start=(kk == 0), stop=(kk == 7)), reads=[wtk, "eh2"], writes=[pak])
                    for kk in range(8):
                        wt, wtk = ws[("u", kk)]
                        P.op("pe", lambda e, pu=pu, wt=wt, kk=kk, mo=mo, sl=sl, w=w: e.matmul(pu[:, :w], wt[:, mo * 128:(mo + 1) * 128], h2[:, kk, sl], start=(kk == 0), stop=(kk == 7)), reads=[wtk, "eh2"], writes=[puk])
                    s_, sk_ = sa.next()
                    a_, ak_ = au.next()
                    P.op("act", lambda e, pa=pa, s_=s_, w=w: e.activation(out=s_[:, :w], in_=pa[:, :w], func=AF.Silu), reads=[pak], writes=[sk_])
                    P.op("dve", lambda e, pu=pu, s_=s_, a_=a_, w=w: e.tensor_tensor(a_[:, :w], pu[:, :w], s_[:, :w], ALU.mult), reads=[puk, sk_], writes=[ak_])
                    P.op("pool", lambda e, a_=a_, mo=mo, sl=sl, w=w: e.tensor_tensor(act[:, mo, :w], a_[:, :w], gwb[:, sl], ALU.mult), reads=[ak_, "egwb"], writes=["eact"])
                for mo in range(8):
                    py, pyk = psn(k, 6, 8)
                    for kk in range(8):
                        wt, wtk = ws[("d", kk)]
                        P.op("pe", lambda e, py=py, wt=wt, kk=kk, mo=mo, w=w: e.matmul(py[:, :w], wt[:, mo * 128:(mo + 1) * 128], act[:, kk, :w], start=(kk == 0), stop=(kk == 7)), reads=[wtk, "eact"], writes=[pyk])
                    if ex == 0:
                        P.op("dve", lambda e, py=py, mo=mo, sl=sl, w=w: e.tensor_copy(fT[:, mo, sl], py[:, :w]), reads=[pyk], writes=["efT"])
                    else:
                        P.op("dve", lambda e, py=py, mo=mo, sl=sl, w=w: e.tensor_tensor(fT[:, mo, sl], fT[:, mo, sl], py[:, :w], ALU.add), reads=[pyk, "efT"], writes=["efT"])
        P.barrier()
        st.close()
        st = st0
        tmp = ln_tmp(nc, st)
        xb = sb("exb", [128, 8, 512])
        rb = sb("erb", [128, 8, 512])
        x2 = sb("ex2", [128, 8, 512])
        xTv = k.xT.rearrange("(k p) t -> p k t", p=128)
        for (t0, w, isc) in TBS:
            sl = slice(t0, t0 + w)
            P.dma("sp", xb[:, :, :w], xTv[:, :, sl], reads=["xT"], writes=["exb"])
            for mo in range(8):
                P.op("act", lambda e, mo=mo, sl=sl, w=w, isc=isc: e.activation(out=tmp["t"][:, :w], in_=fT[:, mo, sl], func=AF.Identity, scale=k.mod[:, li, 40 + mo, isc:isc + 1]), reads=["efT", "mod"], writes=["ln_t"])
                P.op("dve", lambda e, mo=mo, w=w: e.scalar_tensor_tensor(rb[:, mo, :w], xb[:, mo, :w], float(ALPHA), tmp["t"][:, :w], ALU.mult, ALU.add), reads=["exb", "ln_t"], writes=["erb"])
            ln_stats(k, rb, "erb", w, tmp)
            for kk in range(8):
                ln_apply(k, rb, "erb", w, tmp, kk, x2[:, kk, :w], ["ex2"], lnp[:, 2, kk:kk + 1], lnp[:, 3, kk:kk + 1])
            P.dma("sp", xTv[:, :, sl], x2[:, :, :w], reads=["ex2"], writes=["xT"])
        stage_end(k)

def stage_out(k):
    nc, P = k.nc, k.P
    with ExitStack() as st:
        xr = Rot(nc, st, "oxr", 2, [128, 8, 128], F32)
        xo = Rot(nc, st, "oxo", 2, [128, D], F32)
        xTv = k.xT.rearrange("(k p) t -> p k t", p=128)
        for tt in range(L // 128):
            xt, xk = xr.next()
            P.dma("sp", xt[:], xTv[:, :, LC + tt * 128:LC + (tt + 1) * 128], reads=["xT"], writes=[xk])
            ot, ok = xo.next()
            for half in range(2):
                pt, pk = psn(k)
                for kk in range(4):
                    kf = half * 4 + kk
                    P.op("pe", lambda e, pt=pt, kk=kk, kf=kf, xt=xt: e.transpose(pt[:, kk * 128:(kk + 1) * 128], xt[:, kf, :], k.identf[:]),
                         reads=[xk, "identf"], writes=[pk])
                if half == 0:
                    P.op("act", lambda e, pt=pt, ot=ot: e.copy(ot[:, 0:512], pt[:]), reads=[pk], writes=[ok + "h0"])
                else:
                    P.op("dve", lambda e, pt=pt, ot=ot: e.tensor_copy(ot[:, 512:1024], pt[:]), reads=[pk], writes=[ok + "h1"])
            P.dma("sp", k.out[tt * 128:(tt + 1) * 128, :], ot[:], reads=[ok + "h0", ok + "h1"], writes=["out"])
        stage_end(k)


Z_ORDER = None


def _zcols():
    u = np.arange(0, 512)
    qa = np.arange(512, 896)
    kva = np.arange(896, 1152)
    kr = np.arange(1152, 1184)
    wq = np.arange(1184, 1696)
    wk = np.arange(1696, 1824)
    wv = np.arange(1824, 1952)
    gates = np.arange(1952, 5024)
    wq_sw = wq.reshape(8, 2, 32)[:, ::-1, :].reshape(-1)
    wk_sw = wk.reshape(2, 2, 32)[:, ::-1, :].reshape(-1)
    kr_sw = kr.reshape(2, 16)[::-1].reshape(-1)
    cols = np.concatenate([u, qa, kva, wq, wk, wv, gates, wq_sw, wk_sw, kr, kr_sw])
    assert cols.size == 5696
    return cols


def prep_shared(inp):
    f = lambda a: np.ascontiguousarray(np.asarray(a, np.float32))
    sh = {}
    sh["ident"] = np.eye(128, dtype=np.float32)
    sh["w_ada"] = f(inp["w_ada"])
    b = f(inp["b_ada"]).reshape(DEPTH, 48, 128).transpose(0, 2, 1)
    sh["bada2"] = f(np.repeat(b[:, :, :, None], 2, axis=3))
    cols = _zcols()
    wx = np.zeros((DEPTH, D, NZT * 128), np.float32)
    wx[:, :, :cols.size] = f(inp["w_in"])[:, :, cols]
    sh["w_inx"] = wx
    G, PS, HG = 32, 64, 16
    bT = np.zeros((DEPTH, 2, 2, 16, 128, 128), np.float32)
    cL = np.zeros((DEPTH, 128, 2, 16, 2, 16), np.float32)
    lane = np.zeros((DEPTH, 128, 3, 32), np.float32)
    for ri, nm in enumerate(("s5_b_re", "s5_b_im")):
        bsrc = f(inp[nm])
        for lt in range(16):
            for half in range(2):
                g = 2 * lt + half
                gl = g % 8
                bT[:, :, ri, lt, gl * 16:(gl + 1) * 16, half * 64:(half + 1) * 64] = bsrc[:, :, g].transpose(0, 1, 3, 2)
    for ri, nm in enumerate(("s5_c_re", "s5_c_im")):
        csrc = f(inp[nm])
        for lt in range(16):
            for half in range(2):
                g = 2 * lt + half
                cL[:, half * 64:(half + 1) * 64, :, lt, ri, :] = csrc[:, :, g].transpose(0, 3, 1, 2)
    lre, lim, ldt = f(inp["s5_lam_re"]), f(inp["s5_lam_im"]), f(inp["s5_log_dt"])
    for d in range(2):
        for lt in range(16):
            for half in range(2):
                g = 2 * lt + half
                lane[:, half * 64:(half + 1) * 64, 0, d * 16 + lt] = lre[:, d, g, :]
                lane[:, half * 64:(half + 1) * 64, 1, d * 16 + lt] = lim[:, d, g, :]
                lane[:, half * 64:(half + 1) * 64, 2, d * 16 + lt] = ldt[:, d, g][:, None]
    sh["s5_bT"], sh["s5_cL"], sh["s5_lane"] = bT, cL, lane
    dg = np.zeros((DEPTH, 128, 2, 4), np.float32)
    dg[:, :, 0, :] = f(inp["s5_d"]).reshape(DEPTH, 4, 128).transpose(0, 2, 1)
    dg[:, :, 1, :] = f(inp["s5_b_glu"]).reshape(DEPTH, 4, 128).transpose(0, 2, 1)
    sh["s5_dg"] = dg
    sh["s5_wglu"] = f(inp["s5_w_glu"])
    mg = np.zeros((DEPTH, 128, 5), np.float32)
    mg[:, :, 0:3] = f(inp["mla_q_norm"]).reshape(DEPTH, 3, 128).transpose(0, 2, 1)
    mg[:, :, 3:5] = f(inp["mla_kv_norm"]).reshape(DEPTH, 2, 128).transpose(0, 2, 1)
    sh["mla_g"] = mg
    wuq = f(inp["mla_w_uq"]).reshape(DEPTH, 384, 8, 96)
    nope = wuq[..., :64].reshape(DEPTH, 384, 512)
    rp8 = wuq[..., 64:]
    rs8 = wuq[..., 64:].reshape(DEPTH, 384, 8, 2, 16)[:, :, :, ::-1, :].reshape(DEPTH, 384, 8, 32)
    rp = np.zeros((DEPTH, 384, 3, 128), np.float32)
    rs = np.zeros((DEPTH, 384, 3, 128), np.float32)
    for h in range(8):
        rp[:, :, h // 3, (h % 3) * 32:(h % 3) * 32 + 32] = rp8[:, :, h]
        rs[:, :, h // 3, (h % 3) * 32:(h % 3) * 32 + 32] = rs8[:, :, h]
    sh["w_uqx"] = f(np.concatenate([nope, rp.reshape(DEPTH, 384, 384), rs.reshape(DEPTH, 384, 384)], -1))
    wkv = f(inp["mla_w_ukv"]).reshape(DEPTH, 256, 8, 128)
    sh["w_ukvk"] = f(wkv[..., :64].reshape(DEPTH, 256, 512))
    sh["w_ukvv"] = f(wkv[..., 64:].reshape(DEPTH, 256, 512))
    rows = L // 64
    row = np.repeat(np.arange(rows, dtype=np.float64), 64)
    col = np.tile(np.arange(64, dtype=np.float64), rows)

    def rope_tab(d, reps):
        nf = d // 4
        fr = 10000.0 ** (-np.arange(nf) / nf)
        ang = np.concatenate([row[:, None] * fr, col[:, None] * fr], -1)
        c = np.concatenate([np.cos(ang), np.cos(ang)], -1).T
        sn = np.concatenate([-np.sin(ang), np.sin(ang)], -1).T
        return np.stack([np.tile(c, (reps, 1)), np.tile(sn, (reps, 1))], 0).astype(np.float32)
    sh["rope_mla"] = rope_tab(32, 4)
    sh["rope_win"] = rope_tab(64, 2)
    jj = np.arange(128)[:, None]
    rr = np.arange(128)[None, :]
    sh["wmask"] = np.stack([(jj >= rr), (jj <= rr)], 0).astype(np.float32)
    sh["win_sink"] = f(inp["win_sink"])
    sh["w_branch"] = f(inp["w_branch"]).reshape(DEPTH, 1536, D)
    sh["w_out"] = f(inp["w_out"])
    lnp = np.stack([f(inp[n]).reshape(DEPTH, 8, 128).transpose(0, 2, 1) for n in ("ln1_g", "ln1_b", "ln2_g", "ln2_b")], 2)
    sh["lnp"] = f(lnp)
    sh["w_router"] = f(inp["w_router"])
    sh["w_gate"], sh["w_up"], sh["w_down"] = f(inp["w_gate"]), f(inp["w_up"]), f(inp["w_down"])
    sel = np.zeros((16, 16, 128), np.float32)
    for e_ in range(16):
        sel[e_, e_, :] = 1.0
    sh["sel16"] = sel
    sh["tau"] = f(np.broadcast_to(np.arange(NT, dtype=np.float32), (128, NT)))
    return sh


def prep_core(inp, b):
    f = lambda a: np.ascontiguousarray(np.asarray(a, np.float32))
    m = {}
    m["xin"] = f(np.concatenate([inp["ctx"][b], inp["x"][b]], 0))
    cond = np.stack([np.asarray(inp["c"][b]), np.asarray(inp["c_ctx"])], -1)
    m["condT"] = f(cond.reshape(8, 128, 2).transpose(1, 0, 2))
    return m


def kernel(**inputs):
    nc = build()
    sh = prep_shared(inputs)
    in_maps = []
    for b in range(8):
        m = dict(sh)
        m.update(prep_core(inputs, b))
        in_maps.append(m)
    res = run_bass_kernel_spmd(nc, in_maps, core_ids=list(range(8)))
    return np.stack([np.asarray(r["out"], np.float32) for r in res.results], 0)
```

```python
import numpy as np
from contextlib import ExitStack
import concourse.bass as bass
import concourse.mybir as mybir
from concourse.bass_utils import run_bass_kernel_spmd

F32 = mybir.dt.float32
BF16 = mybir.dt.bfloat16
I32 = mybir.dt.int32
AF = mybir.ActivationFunctionType
ALU = mybir.AluOpType

ENGS = ("pe", "dve", "act", "pool", "sp")
D = 1024
NT = 2304
LC = 256
L = 2048
DEPTH = 4
ALPHA = (2 * DEPTH) ** 0.25
EPS = 1e-6
NZT = 53
ZT_QA, ZT_KVA, ZT_WQ, ZT_WK, ZT_WV, ZT_G, ZT_WQS, ZT_WKS, ZT_KRX = 4, 7, 9, 17, 18, 19, 43, 51, 52
TBS = [(0, 256, 1), (256, 512, 0), (768, 512, 0), (1280, 512, 0), (1792, 512, 0)]


class Prog:
    def __init__(self, nc, es, n_dma_sems=24):
        self.nc = nc
        self.q = {e: [] for e in ENGS}
        self.sem = {}
        self.cnt = {}
        for e in ENGS:
            self.sem[e] = es.enter_context(nc.semaphore("s_" + e))
            self.cnt[e] = 0
        self.dma_sems = []
        for i in range(n_dma_sems):
            nm = "d%d" % i
            self.sem[nm] = es.enter_context(nc.semaphore("s_" + nm))
            self.cnt[nm] = 0
            self.dma_sems.append(nm)
        self.dma_rr = 0
        self.seen = {e: {} for e in ENGS}
        self.lastw = {}
        self.readers = {}
        self.nops = 0

    def _deps(self, eng, reads, writes):
        deps = {}

        def need(st, v):
            if st == eng and eng == "pe":
                return
            if deps.get(st, 0) < v:
                deps[st] = v

        for k in reads:
            lw = self.lastw.get(k)
            if lw is not None:
                need(*lw)
            if k.startswith("ps"):
                for r in self.readers.get(k, ()):
                    if r[0] != eng:
                        need(*r)
        for k in writes:
            lw = self.lastw.get(k)
            if lw is not None:
                need(*lw)
            for r in self.readers.get(k, ()):
                need(*r)
        return deps

    def _emit_waits(self, eng, deps):
        for st, v in deps.items():
            if self.seen[eng].get(st, 0) < v:
                self.seen[eng][st] = v
                sem = self.sem[st]
                self.q[eng].append(lambda e, sem=sem, v=v: e.wait_ge(sem, v))

    def _record(self, done, reads, writes):
        for k in reads:
            self.readers.setdefault(k, []).append(done)
        for k in writes:
            self.lastw[k] = done
            self.readers[k] = []

    def op(self, eng, fn, reads=(), writes=()):
        deps = self._deps(eng, reads, writes)
        self._emit_waits(eng, deps)
        self.cnt[eng] += 1
        sem = self.sem[eng]
        self.q[eng].append(lambda e, fn=fn, sem=sem: fn(e).then_inc(sem, 1))
        self._record((eng, self.cnt[eng]), reads, writes)
        self.nops += 1

    def dma(self, qeng, out, in_, reads=(), writes=(), **kw):
        st = self.dma_sems[self.dma_rr % len(self.dma_sems)]
        self.dma_rr += 1
        deps = self._deps(st, reads, writes)
        if self.cnt[st] > 0:
            deps[st] = self.cnt[st]
        self._emit_waits(qeng, deps)
        self.cnt[st] += 16
        sem = self.sem[st]
        self.q[qeng].append(
            lambda e, out=out, in_=in_, sem=sem, kw=kw: e.dma_start(out=out, in_=in_, **kw).then_inc(sem, 16))
        self._record((st, self.cnt[st]), reads, writes)
        self.nops += 1

    def barrier(self):
        for eng in ENGS:
            deps = {}
            for st in self.sem:
                if st != eng and self.cnt[st] > 0:
                    deps[st] = self.cnt[st]
            self._emit_waits(eng, deps)
        self.lastw = {}
        self.readers = {}

    def replay(self):
        nc = self.nc
        q = self.q
        with nc.Block() as block:
            @block.tensor
            def _(e):
                for f in q["pe"]:
                    f(e)

            @block.vector
            def _(e):
                for f in q["dve"]:
                    f(e)

            @block.scalar
            def _(e):
                for f in q["act"]:
                    f(e)

            @block.gpsimd
            def _(e):
                for f in q["pool"]:
                    f(e)

            @block.sync
            def _(e):
                for f in q["sp"]:
                    f(e)
        self.q = {e: [] for e in ENGS}


_UID = [0]


def U(name):
    _UID[0] += 1
    return "%s_u%d" % (name, _UID[0])


class Rot:
    def __init__(self, nc, es, name, n, shape, dtype, psum=False):
        self.name = name
        self.n = n
        self.i = 0
        if psum:
            self.t = [es.enter_context(nc.psum_tensor(U("%s%d" % (name, j)), shape, dtype)) for j in range(n)]
        else:
            self.t = [es.enter_context(nc.sbuf_tensor(U("%s%d" % (name, j)), shape, dtype)) for j in range(n)]

    def next(self):
        j = self.i % self.n
        self.i += 1
        return self.t[j], "%s%d" % (self.name, j)


class K:
    pass


def build(nlayers=DEPTH, dbg=()):
    nc = bass.Bass("TRN2", target_bir_lowering=False)
    k = K()
    k.nc = nc
    k.dbg = dbg
    din = lambda name, shape, dt=F32: nc.dram_tensor(name, list(shape), dt, kind="ExternalInput").ap()

    def dscr(name, shape, dt):
        kind = "ExternalOutput" if name in dbg else "Internal"
        return nc.dram_tensor(name, list(shape), dt, kind=kind).ap()

    k.xin = din("xin", [NT, D])
    k.condT = din("condT", [128, 8, 2])
    k.ident = din("ident", [128, 128])
    k.w_ada = din("w_ada", [DEPTH, D, 6 * D])
    k.bada2 = din("bada2", [DEPTH, 128, 48, 2])
    k.w_inx = din("w_inx", [DEPTH, D, NZT * 128])
    k.s5_bT = din("s5_bT", [DEPTH, 2, 2, 16, 128, 128])
    k.s5_cL = din("s5_cL", [DEPTH, 128, 2, 16, 2, 16])
    k.s5_lane = din("s5_lane", [DEPTH, 128, 3, 32])
    k.s5_dg = din("s5_dg", [DEPTH, 128, 2, 4])
    k.s5_wglu = din("s5_wglu", [DEPTH, 512, 512])
    k.tau = din("tau", [128, NT])
    k.mla_g = din("mla_g", [DEPTH, 128, 5])
    k.w_uqx = din("w_uqx", [DEPTH, 384, 2048])
    k.w_ukvk = din("w_ukvk", [DEPTH, 256, 1024])
    k.w_ukvv = din("w_ukvv", [DEPTH, 256, 512])
    k.rope_mla = din("rope_mla", [2, 128, L])
    k.rope_win = din("rope_win", [2, 128, L])
    k.wmask = din("wmask", [2, 128, 128])
    k.win_sink = din("win_sink", [DEPTH, 128, 8])
    k.w_branch = din("w_branch", [DEPTH, 1536, D])
    k.w_out = din("w_out", [DEPTH, D, D])
    k.lnp = din("lnp", [DEPTH, 128, 4, 8])
    k.w_router = din("w_router", [DEPTH, D, 16])
    k.w_gate = din("w_gate", [DEPTH, 16, D, D])
    k.w_up = din("w_up", [DEPTH, 16, D, D])
    k.w_down = din("w_down", [DEPTH, 16, D, D])
    k.sel16 = din("sel16", [16, 16, 128])
    k.out = nc.dram_tensor("out", [L, D], F32, kind="ExternalOutput").ap()
    k.brT = [dscr(nm, [512, NT], BF16) for nm in ("s5T", "mlaT", "winT")]
    k.xT = dscr("xT", [D, NT], F32)
    k.zT = dscr("zT", [NZT * 128, NT], BF16)
    k.vtok = dscr("vtok", [NT, 128], BF16)
    k.h2T = dscr("h2T", [D, NT], BF16)

    with ExitStack() as es:
        P = Prog(nc, es)
        k.P = P
        k.identf = es.enter_context(nc.sbuf_tensor(U("identf"), [128, 128], F32))
        k.identb = es.enter_context(nc.sbuf_tensor(U("identb"), [128, 128], BF16))
        k.onesm = es.enter_context(nc.sbuf_tensor(U("onesm"), [128, 128], F32))
        k.mod = es.enter_context(nc.sbuf_tensor(U("mod"), [128, DEPTH, 48, 2], F32))
        k.epsc = es.enter_context(nc.sbuf_tensor(U("epsc"), [128, 1], F32))
        k.ps = [es.enter_context(nc.psum_tensor("ps%d" % i, [128, 512], F32)) for i in range(8)]
        k.psi = 0

        stage_init(k)
        stage_ada(k, nlayers)
        k.onesb = es.enter_context(nc.sbuf_tensor(U("onesb"), [128, 512], BF16))
        k.onesf = es.enter_context(nc.sbuf_tensor(U("onesf"), [128, 128], F32))
        P.op("dve", lambda e: e.memset(k.onesb[:], 1.0), writes=["onesb"])
        P.op("dve", lambda e: e.memset(k.onesf[:], 1.0), writes=["onesf"])
        k.halfpi = es.enter_context(nc.sbuf_tensor(U("halfpi"), [128, 1], F32))
        P.op("dve", lambda e: e.memset(k.halfpi[:], float(np.pi / 2)), writes=["halfpi"])
        for li in range(nlayers):
            if "skip_win" not in dbg:
                stage_ln_win(k, li)
            if "mla_first" in dbg:
                stage_mla(k, li)
            if "skip_s5" not in dbg:
                stage_s5(k, li)
            if "skip_mla" not in dbg and "mla_first" not in dbg:
                stage_mla(k, li)
            if "skip_winb" not in dbg:
                stage_win(k, li)
            if "skip_mm" not in dbg:
                stage_merge(k, li)
                stage_moe(k, li)
        stage_out(k)
    return nc


def dump(k, name, ap, shape, dt, reads):
    if name not in k.dbg:
        return
    t = k.nc.dram_tensor(name, list(shape), dt, kind="ExternalOutput").ap()
    k.P.dma("sp", t, ap, reads=reads)


def psn(k, lo=0, hi=8):
    j = lo + (k.psi % (hi - lo))
    k.psi += 1
    return k.ps[j], "ps%d" % j


def stage_end(k):
    k.P.barrier()
    k.P.replay()


def stage_init(k):
    nc, P = k.nc, k.P
    P.dma("sp", k.identf[:], k.ident, writes=["identf"])
    P.dma("pool", k.identb[:], k.ident, writes=["identb"])
    P.op("dve", lambda e: e.memset(k.onesm[:], 1.0 / D), writes=["onesm"])
    P.op("dve", lambda e: e.memset(k.epsc[:], EPS), writes=["epsc"])
    with ExitStack() as st:
        xr = Rot(nc, st, "xr", 2, [128, D], F32)
        xo = Rot(nc, st, "xo", 2, [128, 8, 128], F32)
        xTv = k.xT.rearrange("(k p) t -> p k t", p=128)
        for tt in range(NT // 128):
            xt, xk = xr.next()
            P.dma("sp", xt[:], k.xin[tt * 128:(tt + 1) * 128, :], writes=[xk])
            ot, ok = xo.next()
            for half in range(2):
                pt, pk = psn(k)
                for kk in range(4):
                    kf = half * 4 + kk
                    P.op("pe", lambda e, pt=pt, kk=kk, kf=kf, xt=xt: e.transpose(pt[:, kk * 128:(kk + 1) * 128], xt[:, kf * 128:(kf + 1) * 128], k.identf[:]),
                         reads=[xk, "identf"], writes=[pk])
                eng = "act" if half == 0 else "dve"
                if eng == "act":
                    P.op("act", lambda e, pt=pt, ot=ot, half=half: e.copy(ot[:, half * 4:(half + 1) * 4, :], pt[:].rearrange("p (k t) -> p k t", k=4)),
                         reads=[pk], writes=[ok + "h%d" % half])
                else:
                    P.op("dve", lambda e, pt=pt, ot=ot, half=half: e.tensor_copy(ot[:, half * 4:(half + 1) * 4, :], pt[:].rearrange("p (k t) -> p k t", k=4)),
                         reads=[pk], writes=[ok + "h%d" % half])
            P.dma("sp", xTv[:, :, tt * 128:(tt + 1) * 128], ot[:], reads=[ok + "h0", ok + "h1"], writes=["xT"])
        stage_end(k)


def stage_ada(k, nlayers):
    nc, P = k.nc, k.P
    with ExitStack() as st:
        sc = st.enter_context(nc.sbuf_tensor(U("sc"), [128, 8, 2], F32))
        bt = st.enter_context(nc.sbuf_tensor(U("bt"), [128, DEPTH, 48, 2], F32))
        wa = Rot(nc, st, "wa", 2, [128, 8, 768], F32)
        P.dma("sp", sc[:], k.condT, writes=["sc"])
        P.dma("sp", bt[:], k.bada2.rearrange("l p m s -> p l m s"), writes=["bt"])
        P.op("act", lambda e: e.activation(out=sc[:], in_=sc[:], func=AF.Silu), reads=["sc"], writes=["sc"])
        for li in range(nlayers):
            wv = k.w_ada[li].rearrange("(k p) n -> p k n", p=128)
            for cb in range(8):
                wt, wk = wa.next()
                P.dma("sp", wt[:], wv[:, :, cb * 768:(cb + 1) * 768], writes=[wk])
                pt, pk = psn(k)
                for mt in range(6):
                    for kk in range(8):
                        P.op("pe", lambda e, pt=pt, wt=wt, mt=mt, kk=kk: e.matmul(pt[:, mt * 2:mt * 2 + 2], wt[:, kk, mt * 128:(mt + 1) * 128], sc[:, kk, :], start=(kk == 0), stop=(kk == 7)),
                             reads=[wk, "sc"], writes=[pk])
                P.op("dve", lambda e, pt=pt, li=li, cb=cb: e.tensor_tensor(k.mod[:, li, cb * 6:(cb + 1) * 6, :], pt[:, 0:12].rearrange("p (m s) -> p m s", s=2), bt[:, li, cb * 6:(cb + 1) * 6, :], ALU.add),
                     reads=[pk, "bt"], writes=["mod"])
            for j in (1, 4):
                P.op("dve", lambda e, li=li, j=j: e.tensor_scalar_add(k.mod[:, li, j * 8:(j + 1) * 8, :], k.mod[:, li, j * 8:(j + 1) * 8, :], 1.0),
                     reads=["mod"], writes=["mod"])
        stage_end(k)


def ln_stats(k, xb, xk, w, tmp):
    nc, P = k.nc, k.P
    sq, mean, rstd, m2 = tmp["sq"], tmp["mean"], tmp["rstd"], tmp["m2"]
    P.op("act", lambda e: e.activation(out=sq[:, :, :w], in_=xb[:, :, :w], func=AF.Square), reads=[xk], writes=["sq"])
    p1, k1 = psn(k)
    p2, k2 = psn(k)
    for kk in range(8):
        P.op("pe", lambda e, kk=kk: e.matmul(p1[:, :w], k.onesm[:], xb[:, kk, :w], start=(kk == 0), stop=(kk == 7)), reads=[xk, "onesm"], writes=[k1])
    for kk in range(8):
        P.op("pe", lambda e, kk=kk: e.matmul(p2[:, :w], k.onesm[:], sq[:, kk, :w], start=(kk == 0), stop=(kk == 7)), reads=["sq", "onesm"], writes=[k2])
    P.op("act", lambda e: e.copy(mean[:, :w], p1[:, :w]), reads=[k1], writes=["mean"])
    P.op("dve", lambda e: e.tensor_tensor(m2[:, :w], mean[:, :w], mean[:, :w], ALU.mult), reads=["mean"], writes=["m2"])
    P.op("dve", lambda e: e.tensor_tensor(m2[:, :w], p2[:, :w], m2[:, :w], ALU.subtract), reads=[k2, "m2"], writes=["m2"])
    P.op("act", lambda e: e.activation(out=m2[:, :w], in_=m2[:, :w], func=AF.Sqrt, bias=k.epsc[:], scale=1.0), reads=["m2", "epsc"], writes=["m2"])
    P.op("dve", lambda e: e.reciprocal(rstd[:, :w], m2[:, :w]), reads=["m2"], writes=["rstd"])


def ln_tmp(nc, st):
    return {
        "sq": st.enter_context(nc.sbuf_tensor(U("ln_sq"), [128, 8, 512], F32)),
        "mean": st.enter_context(nc.sbuf_tensor(U("ln_mean"), [128, 512], F32)),
        "rstd": st.enter_context(nc.sbuf_tensor(U("ln_rstd"), [128, 512], F32)),
        "m2": st.enter_context(nc.sbuf_tensor(U("ln_m2"), [128, 512], F32)),
        "t": st.enter_context(nc.sbuf_tensor(U("ln_t"), [128, 512], F32)),
    }


def ln_apply(k, xb, xk, w, tmp, kk, out, okeys, scale_ap, bias_ap, extra_reads=()):
    P = k.P
    t = tmp["t"]
    P.op("dve", lambda e: e.tensor_tensor(t[:, :w], xb[:, kk, :w], tmp["mean"][:, :w], ALU.subtract), reads=[xk, "mean"], writes=["ln_t"])
    P.op("dve", lambda e: e.tensor_tensor(t[:, :w], t[:, :w], tmp["rstd"][:, :w], ALU.mult), reads=["ln_t", "rstd"], writes=["ln_t"])
    P.op("act", lambda e: e.activation(out=out, in_=t[:, :w], func=AF.Identity, scale=scale_ap, bias=bias_ap), reads=["ln_t", "mod"] + list(extra_reads), writes=okeys)


def stage_ln_win(k, li):
    nc, P = k.nc, k.P
    with ExitStack() as st:
        hT = st.enter_context(nc.sbuf_tensor(U("hT"), [128, 8, NT], BF16))
        with ExitStack() as st2:
            tmp = ln_tmp(nc, st2)
            xbr = Rot(nc, st2, "xb", 2, [128, 8, 512], F32)
            xTv = k.xT.rearrange("(k p) t -> p k t", p=128)
            for (t0, w, isc) in TBS:
                xb, xk = xbr.next()
                P.dma("sp", xb[:, :, :w], xTv[:, :, t0:t0 + w], reads=["xT"], writes=[xk])
                ln_stats(k, xb, xk, w, tmp)
                for kk in range(8):
                    ln_apply(k, xb, xk, w, tmp, kk, hT[:, kk, t0:t0 + w], ["hT%d" % kk],
                             k.mod[:, li, 8 + kk, isc:isc + 1], k.mod[:, li, 0 + kk, isc:isc + 1])
            P.barrier()
        wr = Rot(nc, st, "wr", 3, [128, 8, 128], BF16)
        zs = Rot(nc, st, "zs", 3, [128, NT], BF16)
        vt = st.enter_context(nc.sbuf_tensor(U("vt"), [128, 18, 128], BF16))
        wv = k.w_inx[li].rearrange("(k p) n -> p k n", p=128)
        ev = 0
        for m in range(NZT):
            wt, wk = wr.next()
            P.dma("pool", wt[:], wv[:, :, m * 128:(m + 1) * 128], writes=[wk])
            zt, zk = zs.next()
            mrows = 64 if m == ZT_KRX else 128
            for (t0, w, isc) in TBS:
                pt, pk = psn(k)
                for kk in range(8):
                    P.op("pe", lambda e, pt=pt, wt=wt, kk=kk, t0=t0, w=w, mrows=mrows: e.matmul(pt[:mrows, :w], wt[:, kk, :mrows], hT[:, kk, t0:t0 + w], start=(kk == 0), stop=(kk == 7)),
                         reads=[wk, "hT%d" % kk], writes=[pk])
                gate = ZT_G <= m < ZT_G + 24
                if gate:
                    P.op("act", lambda e, pt=pt, zt=zt, t0=t0, w=w: e.activation(out=zt[:, t0:t0 + w], in_=pt[:, :w], func=AF.Sigmoid), reads=[pk], writes=[zk + "_%d" % t0])
                elif ev % 2 == 0:
                    P.op("act", lambda e, pt=pt, zt=zt, t0=t0, w=w, mrows=mrows: e.copy(zt[:mrows, t0:t0 + w], pt[:mrows, :w]), reads=[pk], writes=[zk + "_%d" % t0])
                else:
                    P.op("dve", lambda e, pt=pt, zt=zt, t0=t0, w=w, mrows=mrows: e.tensor_copy(zt[:mrows, t0:t0 + w], pt[:mrows, :w]), reads=[pk], writes=[zk + "_%d" % t0])
                ev += 1
            P.dma("sp", k.zT[m * 128:m * 128 + mrows, :], zt[:mrows, :], reads=[zk + "_%d" % t[0] for t in TBS], writes=["zT"])
            if m == ZT_WV:
                for tt in range(18):
                    pt, pk = psn(k)
                    for kk in range(8):
                        P.op("pe", lambda e, pt=pt, wt=wt, kk=kk, tt=tt: e.matmul(pt[:, :128], hT[:, kk, tt * 128:(tt + 1) * 128], wt[:, kk, :], start=(kk == 0), stop=(kk == 7)),
                             reads=[wk, "hT%d" % kk], writes=[pk])
                    P.op("dve", lambda e, pt=pt, tt=tt: e.tensor_copy(vt[:, tt, :], pt[:, :128]), reads=[pk], writes=["vt"])
                P.dma("sp", k.vtok.rearrange("(t p) c -> p t c", p=128), vt[:], reads=["vt"], writes=["vtok"])
        stage_end(k)


def stage_s5(k, li):
    nc, P = k.nc, k.P
    TWO_PI = float(2 * np.pi)
    with ExitStack() as st:
        sb = lambda name, shape, dt=F32: st.enter_context(nc.sbuf_tensor(U(name), shape, dt))
        lane = sb("lane", [128, 3, 32])
        dg = sb("dg", [128, 2, 4])
        craw = sb("craw", [128, 2, 16, 2, 16])
        BW = sb("BW", [128, 64, 128], BF16)
        CW = sb("CW", [128, 64, 128], BF16)
        wglu = sb("wglu", [128, 4, 512], BF16)
        tau = sb("tau", [128, NT])
        names = ["dt", "rho", "thn", "fr", "sn", "cs", "ar", "ai", "rden", "qr", "qi", "nqr", "nqi", "tA", "tB"]
        lp = {n: sb("lp_" + n, [128, 32]) for n in names}
        lpi = sb("lp_it", [128, 32], I32)
        P.dma("sp", lane[:], k.s5_lane[li], writes=["lane"])
        P.dma("sp", dg[:], k.s5_dg[li], writes=["dg"])
        P.dma("sp", craw[:], k.s5_cL[li], writes=["craw"])
        P.dma("sp", tau[:], k.tau, writes=["tau"])
        bsrc = k.s5_bT[li].rearrange("d r t p c -> p (d r t) c")
        for j in range(8):
            P.dma("pool", BW[:, j * 8:(j + 1) * 8, :], bsrc[:, j * 8:(j + 1) * 8, :], writes=["BW"])
        P.dma("pool", wglu[:], k.s5_wglu[li].rearrange("(k p) n -> p k n", p=128), writes=["wglu"])
        P.op("pool", lambda e: e.memset(CW[:], 0.0), writes=["CW"])
        lre, lim, ldt = lane[:, 0, :], lane[:, 1, :], lane[:, 2, :]
        R = ["lane", "lp"]
        W = ["lp"]
        V = lambda fn: P.op("dve", fn, reads=R, writes=W)
        A = lambda fn: P.op("act", fn, reads=R + ["halfpi"], writes=W)
        A(lambda e: e.activation(out=lp["dt"][:], in_=ldt, func=AF.Exp))
        V(lambda e: e.tensor_tensor(lp["tA"][:], lre, lp["dt"][:], ALU.mult))
        A(lambda e: e.activation(out=lp["rho"][:], in_=lp["tA"][:], func=AF.Exp))
        V(lambda e: e.tensor_tensor(lp["thn"][:], lim, lp["dt"][:], ALU.mult))
        V(lambda e: e.tensor_scalar(lp["thn"][:], lp["thn"][:], float(1.0 / TWO_PI), None, ALU.mult))
        V(lambda e: e.tensor_copy(lpi[:], lp["thn"][:]))
        V(lambda e: e.tensor_copy(lp["tB"][:], lpi[:]))
        V(lambda e: e.tensor_tensor(lp["fr"][:], lp["thn"][:], lp["tB"][:], ALU.subtract))
        A(lambda e: e.activation(out=lp["sn"][:], in_=lp["fr"][:], func=AF.Sin, scale=TWO_PI))
        A(lambda e: e.activation(out=lp["fr"][:], in_=lp["fr"][:], func=AF.Abs))
        A(lambda e: e.activation(out=lp["cs"][:], in_=lp["fr"][:], func=AF.Sin, scale=-TWO_PI, bias=k.halfpi[:]))
        V(lambda e: e.tensor_tensor(lp["ar"][:], lp["rho"][:], lp["cs"][:], ALU.mult))
        V(lambda e: e.tensor_tensor(lp["ai"][:], lp["rho"][:], lp["sn"][:], ALU.mult))
        V(lambda e: e.tensor_tensor(lp["tA"][:], lre, lre, ALU.mult))
        V(lambda e: e.tensor_tensor(lp["tB"][:], lim, lim, ALU.mult))
        V(lambda e: e.tensor_tensor(lp["tA"][:], lp["tA"][:], lp["tB"][:], ALU.add))
        V(lambda e: e.reciprocal(lp["rden"][:], lp["tA"][:]))
        V(lambda e: e.tensor_scalar_add(lp["ar"][:], lp["ar"][:], -1.0))
        V(lambda e: e.tensor_tensor(lp["tA"][:], lp["ar"][:], lre, ALU.mult))
        V(lambda e: e.tensor_tensor(lp["tB"][:], lp["ai"][:], lim, ALU.mult))
        V(lambda e: e.tensor_tensor(lp["tA"][:], lp["tA"][:], lp["tB"][:], ALU.add))
        V(lambda e: e.tensor_tensor(lp["qr"][:], lp["tA"][:], lp["rden"][:], ALU.mult))
        V(lambda e: e.tensor_tensor(lp["tA"][:], lp["ai"][:], lre, ALU.mult))
        V(lambda e: e.tensor_tensor(lp["tB"][:], lp["ar"][:], lim, ALU.mult))
        V(lambda e: e.tensor_tensor(lp["tA"][:], lp["tA"][:], lp["tB"][:], ALU.subtract))
        V(lambda e: e.tensor_tensor(lp["qi"][:], lp["tA"][:], lp["rden"][:], ALU.mult))
        V(lambda e: e.tensor_scalar(lp["nqr"][:], lp["qr"][:], -1.0, None, ALU.mult))
        V(lambda e: e.tensor_scalar(lp["nqi"][:], lp["qi"][:], -1.0, None, ALU.mult))
        for n_ in ("rho", "thn", "sn", "cs", "qr", "qi", "dt"):
            dump(k, "lp_" + n_, lp[n_][:], [128, 32], F32, ["lp"])
        ctmp = sb("ctmp", [128, 16])
        for d in range(2):
            for lt in range(16):
                col = d * 16 + lt
                cr, ci = craw[:, d, lt, 0, :], craw[:, d, lt, 1, :]
                for half in range(2):
                    g = 2 * lt + half
                    gl = g % 8
                    ps_ = slice(half * 64, half * 64 + 64)
                    for ri in range(2):
                        s1 = lp["qi"] if ri == 0 else lp["nqr"]
                        s2 = lp["qr"] if ri == 0 else lp["nqi"]
                        op1 = ALU.subtract if ri == 0 else ALU.add
                        P.op("dve", lambda e, ci=ci, s1=s1, col=col, ps_=ps_: e.tensor_scalar(ctmp[ps_, :], ci[ps_, :], s1[ps_, col:col + 1], None, ALU.mult), reads=["craw", "lp"], writes=["ctmp"])
                        P.op("dve", lambda e, cr=cr, s2=s2, col=col, ps_=ps_, ri=ri, gl=gl, op1=op1: e.scalar_tensor_tensor(CW[ps_, col * 2 + ri, gl * 16:(gl + 1) * 16], cr[ps_, :], s2[ps_, col:col + 1], ctmp[ps_, :], ALU.mult, op1), reads=["craw", "lp", "ctmp"], writes=["CW"])
        ut = sb("ut", [128, NT], BF16)
        it = sb("s5it", [128, NT], I32)
        fr = sb("s5fr", [128, NT])
        Sn = sb("s5S", [128, NT])
        Cs = sb("s5C", [128, NT])
        t1 = sb("s5t1", [128, NT])
        t2 = sb("s5t2", [128, NT])
        t3 = sb("s5t3", [128, NT])
        zr = sb("s5zr", [128, NT])
        zi = sb("s5zi", [128, NT])
        xr = sb("s5xr", [128, NT], BF16)
        xi = sb("s5xi", [128, NT], BF16)
        gT = sb("s5g", [128, 4, NT], BF16)
        ysr = Rot(nc, st, "s5ys", 2, [128, 512], F32)
        segs = [(0, LC), (LC, NT)]
        for gt in range(4):
            P.dma("sp", ut[:], k.zT[gt * 128:(gt + 1) * 128, :], reads=["zT"], writes=["ut"])
            for d in range(2):
                for l4 in range(4):
                    lt = gt * 4 + l4
                    col = d * 16 + lt
                    thn = lp["thn"][:, col:col + 1]
                    first = (d == 0 and l4 == 0)
                    last = (d == 1 and l4 == 3)
                    for (a, b) in segs:
                        src = tau[:, a:b] if d == 0 else (tau[:, b - 1::-1] if a == 0 else tau[:, b - 1:a - 1:-1])
                        P.op("dve", lambda e, src=src, a=a, b=b, thn=thn: e.tensor_scalar(it[:, a:b], src, thn, None, ALU.mult), reads=["tau", "lp"], writes=["it"])
                        P.op("dve", lambda e, src=src, a=a, b=b, thn=thn: e.scalar_tensor_tensor(fr[:, a:b], src, thn, it[:, a:b], ALU.mult, ALU.subtract), reads=["tau", "lp", "it"], writes=["fr"])
                    P.op("act", lambda e: e.activation(out=Sn[:], in_=fr[:], func=AF.Sin, scale=TWO_PI), reads=["fr"], writes=["Sn"])
                    P.op("act", lambda e: e.activation(out=fr[:], in_=fr[:], func=AF.Abs), reads=["fr"], writes=["fr"])
                    P.op("act", lambda e: e.activation(out=Cs[:], in_=fr[:], func=AF.Sin, scale=-TWO_PI, bias=k.halfpi[:]), reads=["fr", "halfpi"], writes=["Cs"])
                    for (t0, w, isc) in TBS:
                        pr, kr = psn(k, 5, 8)
                        pi_, ki = psn(k, 5, 8)
                        sl = slice(t0, t0 + w)
                        P.op("pe", lambda e, pr=pr, sl=sl, w=w, d=d, lt=lt: e.matmul(pr[:, :w], BW[:, (d * 2 + 0) * 16 + lt, :], ut[:, sl], start=True, stop=True), reads=["BW", "ut"], writes=[kr])
                        P.op("pe", lambda e, pi_=pi_, sl=sl, w=w, d=d, lt=lt: e.matmul(pi_[:, :w], BW[:, (d * 2 + 1) * 16 + lt, :], ut[:, sl], start=True, stop=True), reads=["BW", "ut"], writes=[ki])
                        P.op("dve", lambda e, pr=pr, sl=sl, w=w: e.tensor_tensor(t1[:, sl], pr[:, :w], Cs[:, sl], ALU.mult), reads=[kr, "Cs"], writes=["t1"])
                        P.op("dve", lambda e, pi_=pi_, sl=sl, w=w: e.tensor_tensor(t2[:, sl], pi_[:, :w], Sn[:, sl], ALU.mult), reads=[ki, "Sn"], writes=["t2"])
                        P.op("pool", lambda e, sl=sl: e.tensor_tensor(t1[:, sl], t1[:, sl], t2[:, sl], ALU.add), reads=["t1", "t2"], writes=["t1"])
                        P.op("dve", lambda e, pi_=pi_, sl=sl, w=w: e.tensor_tensor(t2[:, sl], pi_[:, :w], Cs[:, sl], ALU.mult), reads=[ki, "Cs", "t1"], writes=["t2"])
                        P.op("dve", lambda e, pr=pr, sl=sl, w=w: e.tensor_tensor(t3[:, sl], pr[:, :w], Sn[:, sl], ALU.mult), reads=[kr, "Sn"], writes=["t3"])
                        P.op("pool", lambda e, sl=sl: e.tensor_tensor(t2[:, sl], t2[:, sl], t3[:, sl], ALU.subtract), reads=["t2", "t3"], writes=["t2"])
                    rho = lp["rho"][:, col:col + 1]
                    for (src, dst) in ((t1, zr), (t2, zi)):
                        if d == 0:
                            P.op("dve", lambda e, src=src, dst=dst, rho=rho: e.tensor_tensor_scan(dst[:, 0:LC], rho.to_broadcast([128, LC]), src[:, 0:LC], 0.0, ALU.mult, ALU.add), reads=["t1", "t2", "lp"], writes=["z"])
                            P.op("dve", lambda e, src=src, dst=dst, rho=rho: e.tensor_tensor_scan(dst[:, LC:NT], rho.to_broadcast([128, L]), src[:, LC:NT], dst[:, LC - 1:LC], ALU.mult, ALU.add), reads=["t1", "t2", "lp", "z"], writes=["z"])
                        else:
                            P.op("dve", lambda e, src=src, dst=dst, rho=rho: e.tensor_tensor_scan(dst[:, LC - 1::-1], rho.to_broadcast([128, LC]), src[:, LC - 1::-1], 0.0, ALU.mult, ALU.add), reads=["t1", "t2", "lp"], writes=["z"])
                            P.op("dve", lambda e, src=src, dst=dst, rho=rho: e.tensor_tensor_scan(dst[:, NT - 1:LC - 1:-1], rho.to_broadcast([128, L]), src[:, NT - 1:LC - 1:-1], dst[:, 0:1], ALU.mult, ALU.add), reads=["t1", "t2", "lp", "z"], writes=["z"])
                    P.op("pool", lambda e: e.tensor_tensor(t1[:], Cs[:], zr[:], ALU.mult), reads=["Cs", "z"], writes=["t1"])
                    P.op("pool", lambda e: e.tensor_tensor(t3[:], Sn[:], zi[:], ALU.mult), reads=["Sn", "z"], writes=["t3"])
                    P.op("pool", lambda e: e.tensor_tensor(xr[:], t1[:], t3[:], ALU.subtract), reads=["t1", "t3"], writes=["xr"])
                    P.op("pool", lambda e: e.tensor_tensor(t2[:], Sn[:], zr[:], ALU.mult), reads=["Sn", "z"], writes=["t2"])
                    P.op("pool", lambda e: e.tensor_tensor(t3[:], Cs[:], zi[:], ALU.mult), reads=["Cs", "z", "xr"], writes=["t3"])
                    P.op("pool", lambda e: e.tensor_tensor(xi[:], t2[:], t3[:], ALU.add), reads=["t2", "t3"], writes=["xi"])
                    if gt == 0 and d == 0 and l4 == 0:
                        dump(k, "d_Sn", Sn[:], [128, NT], F32, ["Sn"])
                        dump(k, "d_Cs", Cs[:], [128, NT], F32, ["Cs"])
                        dump(k, "d_zr", zr[:], [128, NT], F32, ["z"])
                        dump(k, "d_zi", zi[:], [128, NT], F32, ["z"])
                        dump(k, "d_xr", xr[:], [128, NT], BF16, ["xr"])
                        dump(k, "d_xi", xi[:], [128, NT], BF16, ["xi"])
                    for bi, (t0, w, isc) in enumerate(TBS):
                        sl = slice(t0, t0 + w)
                        yk = "ps%d" % bi
                        P.op("pe", lambda e, bi=bi, sl=sl, w=w, col=col, first=first: e.matmul(k.ps[bi][:, :w], CW[:, col * 2 + 0, :], xr[:, sl], start=first, stop=False), reads=["CW", "xr"], writes=[yk])
                        P.op("pe", lambda e, bi=bi, sl=sl, w=w, col=col, last=last: e.matmul(k.ps[bi][:, :w], CW[:, col * 2 + 1, :], xi[:, sl], start=False, stop=last), reads=["CW", "xi"], writes=[yk])
            for bi, (t0, w, isc) in enumerate(TBS):
                sl = slice(t0, t0 + w)
                ys, ysk = ysr.next()
                P.op("dve", lambda e, bi=bi, sl=sl, w=w, ys=ys, gt=gt: e.scalar_tensor_tensor(ys[:, :w], ut[:, sl], dg[:, 0, gt:gt + 1], k.ps[bi][:, :w], ALU.mult, ALU.add), reads=["ut", "dg", "ps%d" % bi], writes=[ysk])
                P.op("act", lambda e, sl=sl, w=w, ys=ys, gt=gt: e.activation(out=gT[:, gt, sl], in_=ys[:, :w], func=AF.Gelu_apprx_tanh), reads=[ysk], writes=["gT%d" % gt])
        so = Rot(nc, st, "s5o", 2, [128, NT], BF16)
        sgr = Rot(nc, st, "s5sg", 2, [128, 512], F32)
        for mo in range(4):
            ot, ok = so.next()
            for (t0, w, isc) in TBS:
                sl = slice(t0, t0 + w)
                pt, pk = psn(k)
                for kk in range(4):
                    P.op("pe", lambda e, pt=pt, kk=kk, sl=sl, w=w, mo=mo: e.matmul(pt[:, :w], wglu[:, kk, mo * 128:(mo + 1) * 128], gT[:, kk, sl], start=(kk == 0), stop=(kk == 3)), reads=["wglu", "gT%d" % kk], writes=[pk])
                sg, sgk = sgr.next()
                P.op("act", lambda e, pt=pt, w=w, sg=sg, mo=mo: e.activation(out=sg[:, :w], in_=pt[:, :w], func=AF.Sigmoid, bias=dg[:, 1, mo:mo + 1], scale=1.0), reads=[pk, "dg"], writes=[sgk])
                P.op("dve", lambda e, sg=sg, sl=sl, w=w, ot=ot, mo=mo: e.tensor_tensor(ot[:, sl], gT[:, mo, sl], sg[:, :w], ALU.mult), reads=[sgk, "gT%d" % mo], writes=[ok + "_%d" % t0])
            P.dma("sp", k.brT[0][mo * 128:(mo + 1) * 128, :], ot[:], reads=[ok + "_%d" % t[0] for t in TBS], writes=["s5T"])
        stage_end(k)


def rms_norm_T(k, st, src, nk, gains, dst, tag):
    nc, P = k.nc, k.P
    sq = st.enter_context(nc.sbuf_tensor(U("rms_sq"), [128, nk, 512], BF16))
    rinv = st.enter_context(nc.sbuf_tensor(U("rms_ri"), [128, 512], F32))
    for (t0, w, isc) in TBS:
        sl = slice(t0, t0 + w)
        P.op("act", lambda e, sl=sl, w=w: e.activation(out=sq[:, :, :w], in_=src[:, :, sl], func=AF.Square), reads=[tag + "src"], writes=[tag + "sq"])
        pt, pk = psn(k)
        for kk in range(nk):
            P.op("pe", lambda e, pt=pt, kk=kk, w=w: e.matmul(pt[:, :w], k.onesb[:, 0:128], sq[:, kk, :w], start=(kk == 0), stop=(kk == nk - 1)), reads=[tag + "sq", "onesb"], writes=[pk])
        P.op("act", lambda e, pt=pt, w=w: e.activation(out=rinv[:, :w], in_=pt[:, :w], func=AF.Sqrt, scale=float(1.0 / (nk * 128)), bias=k.epsc[:]), reads=[pk, "epsc"], writes=[tag + "ri"])
        P.op("dve", lambda e, w=w: e.reciprocal(rinv[:, :w], rinv[:, :w]), reads=[tag + "ri"], writes=[tag + "ri"])
        for kk in range(nk):
            P.op("dve", lambda e, kk=kk, sl=sl, w=w: e.scalar_tensor_tensor(dst[:, kk, sl], src[:, kk, sl], gains[:, kk:kk + 1], rinv[:, :w], ALU.mult, ALU.mult), reads=[tag + "src", tag + "ri", "mg"], writes=[tag + "dst"])


def softmax_pv(k, ost_rot, score_fn, nkc, va_fn, nq, scale, out_dram, tagp, PTr, esk=None, post=None):
    nc, P = k.nc, k.P
    po, pok = psn(k, 0, 3)
    LA = 3
    scr = {}

    def issue_score(kc):
        pscr, psk = psn(k, 3, 8)
        score_fn(kc, pscr, psk)
        scr[kc] = (pscr, psk)

    for kc in range(min(LA, nkc)):
        issue_score(kc)
    for kc in range(nkc):
        if kc + LA < nkc:
            issue_score(kc + LA)
        pscr, psk = scr.pop(kc)
        pt, ptk = PTr.next()
        P.op("act", lambda e, pscr=pscr, pt=pt: e.activation(out=pt[:, :nq], in_=pscr[:, :nq], func=AF.Exp, scale=scale), reads=[psk], writes=[ptk])
        if post is not None:
            post(kc, pt, ptk)
        va, vak = va_fn(kc)
        P.op("pe", lambda e, po=po, va=va, pt=pt, kc=kc: e.matmul(po[:, :nq], va, pt[:, :nq], start=(kc == 0), stop=(kc == nkc - 1)), reads=[vak, ptk], writes=[pok])
    rv, rvk = k.att_rv.next()
    if esk is not None:
        P.op("dve", lambda e, po=po, rv=rv: e.tensor_scalar(rv[0:64, :nq], po[64:128, :nq], esk, None, ALU.add), reads=[pok, "esk"], writes=[rvk])
        P.op("dve", lambda e, rv=rv: e.reciprocal(rv[0:64, :nq], rv[0:64, :nq]), reads=[rvk], writes=[rvk])
    else:
        P.op("dve", lambda e, po=po, rv=rv: e.reciprocal(rv[0:64, :nq], po[64:128, :nq]), reads=[pok], writes=[rvk])
    ot, otk = ost_rot.next()
    P.op("dve", lambda e, po=po, ot=ot, rv=rv: e.tensor_tensor(ot[0:64, :nq], po[0:64, :nq], rv[0:64, :nq], ALU.mult), reads=[pok, rvk], writes=[otk])
    P.dma("sp", out_dram, ot[0:64, :nq], reads=[otk], writes=[tagp])


def stage_mla(k, li):
    nc, P = k.nc, k.P
    SC = float(96 ** -0.5)
    with ExitStack() as st:
        sb = lambda name, shape, dt=F32: st.enter_context(nc.sbuf_tensor(U(name), shape, dt))
        mg = sb("mg", [128, 5])
        P.dma("sp", mg[:], k.mla_g[li], writes=["mg"])
        VA = sb("VA", [128, 18, 8, 128], BF16)
        KRb = sb("KRb", [128, NT], BF16)
        qn = sb("qn", [128, 3, NT], BF16)
        kvn = sb("kvn", [128, 2, NT], BF16)
        rope = sb("ropem", [128, 2, L])
        wuq = sb("wuq", [128, 3, 2048], BF16)
        wkk = sb("wkk", [128, 2, 1024], BF16)
        k.att_rv = Rot(nc, st, "att_rv", 2, [128, 512], F32)
        P.op("pool", lambda e: e.memset(VA[:], 1.0), writes=["VA"])
        P.dma("sp", rope[:], k.rope_mla.rearrange("c p t -> p c t"), writes=["rope"])
        for j in range(3):
            for c_ in range(4):
                P.dma("pool", wuq[:, j, c_ * 512:(c_ + 1) * 512], k.w_uqx[li][j * 128:(j + 1) * 128, c_ * 512:(c_ + 1) * 512], writes=["wuq"])
        for j in range(2):
            for c_ in range(2):
                P.dma("pool", wkk[:, j, c_ * 512:(c_ + 1) * 512], k.w_ukvk[li][j * 128:(j + 1) * 128, c_ * 512:(c_ + 1) * 512], writes=["wkk"])
        with ExitStack() as st2:
            sb2 = lambda name, shape, dt=F32: st2.enter_context(nc.sbuf_tensor(U(name), shape, dt))
            qa = sb2("qa", [128, 3, NT], BF16)
            kva = sb2("kva", [128, 2, NT], BF16)
            KP = sb2("KP", [128, NT], BF16)
            KS = sb2("KS", [128, L], BF16)
            wvv = sb2("wvv", [128, 2, 512], BF16)
            tA = sb2("mtA", [128, 512])
            tB = sb2("mtB", [128, 512])
            P.dma("sp", qa[:], k.zT[ZT_QA * 128:(ZT_QA + 3) * 128, :].rearrange("(k p) t -> p k t", p=128), reads=["zT"], writes=["qsrc"])
            P.dma("sp", kva[:], k.zT[ZT_KVA * 128:(ZT_KVA + 2) * 128, :].rearrange("(k p) t -> p k t", p=128), reads=["zT"], writes=["ksrc"])
            P.op("pool", lambda e: e.memset(KP[:], 0.0), writes=["KP"])
            P.op("pool", lambda e: e.memset(KS[:], 0.0), writes=["KS"])
            P.dma("sp", KP[64:96, :], k.zT[ZT_KRX * 128:ZT_KRX * 128 + 32, :], reads=["zT"], writes=["KP"])
            P.dma("sp", KS[64:96, :], k.zT[ZT_KRX * 128 + 32:ZT_KRX * 128 + 64, LC:NT], reads=["zT"], writes=["KS"])
            P.dma("pool", wvv[:], k.w_ukvv[li].rearrange("(k p) n -> p k n", p=128), writes=["wvv"])
            rms_norm_T(k, st2, qa, 3, mg[:, 0:3], qn, "q")
            rms_norm_T(k, st2, kva, 2, mg[:, 3:5], kvn, "k")
            P.op("dve", lambda e: e.tensor_copy(KRb[:, 0:LC], KP[:, 0:LC]), reads=["KP"], writes=["KRb"])
            for c in range(4):
                sl = slice(c * 512, (c + 1) * 512)
                sln = slice(LC + c * 512, LC + (c + 1) * 512)
                P.op("dve", lambda e, sl=sl, sln=sln: e.tensor_tensor(tA[:, :], KP[:, sln], rope[:, 0, sl], ALU.mult), reads=["KP", "rope"], writes=["mtA"])
                P.op("pool", lambda e, sl=sl: e.tensor_tensor(tB[:, :], KS[:, sl], rope[:, 1, sl], ALU.mult), reads=["KS", "rope"], writes=["mtB"])
                P.op("dve", lambda e, sln=sln: e.tensor_tensor(KRb[:, sln], tA[:, :], tB[:, :], ALU.add), reads=["mtA", "mtB"], writes=["KRb"])
            dump(k, "d_KP", KP[:], [128, NT], BF16, ["KP"])
            dump(k, "d_KS", KS[:], [128, L], BF16, ["KS"])
            dump(k, "d_KRb", KRb[:], [128, NT], BF16, ["KRb"])
            for tt in range(18):
                pt, pk = psn(k)
                for kk in range(2):
                    P.op("pe", lambda e, pt=pt, kk=kk, tt=tt: e.matmul(pt[:, :512], kvn[:, kk, tt * 128:(tt + 1) * 128], wvv[:, kk, :], start=(kk == 0), stop=(kk == 1)), reads=["wvv", "kdst"], writes=[pk])
                P.op("dve", lambda e, pt=pt, tt=tt: e.tensor_copy(VA[:, tt, :, 0:64], pt[:, :512].rearrange("p (h d) -> p h d", h=8)), reads=[pk], writes=["VA"])
            P.barrier()
        if "mla_stop1" in k.dbg:
            stage_end(k)
            return
        PTr = Rot(nc, st, "mPT", 4, [128, 512], BF16)
        ostr = Rot(nc, st, "most", 3, [128, 512], BF16)
        t1r = Rot(nc, st, "mt1", 2, [128, 512], F32)
        t2r = Rot(nc, st, "mt2", 2, [128, 512], F32)
        for grp in range(2):
            with ExitStack() as st3:
                QP = st3.enter_context(nc.sbuf_tensor(U("QP"), [128, 4, NT], BF16))
                QR = st3.enter_context(nc.sbuf_tensor(U("QR"), [128, 4, L], BF16))
                KH = st3.enter_context(nc.sbuf_tensor(U("KH"), [128, 4, NT], BF16))
                for hh in range(4):
                    h = grp * 4 + hh
                    for (t0, w, isc) in TBS:
                        sl = slice(t0, t0 + w)
                        pm, pmk = psn(k, 3, 8)
                        for kk in range(3):
                            P.op("pe", lambda e, pm=pm, kk=kk, sl=sl, w=w, h=h: e.matmul(pm[:, :w], wuq[:, kk, h * 128:(h + 1) * 128], qn[:, kk, sl], start=(kk == 0), stop=(kk == 2)), reads=["wuq", "qdst"], writes=[pmk])
                        P.op("act", lambda e, pm=pm, sl=sl, w=w, hh=hh: e.copy(QP[:, hh, sl], pm[:, :w]), reads=[pmk], writes=["QP%d" % hh])
                        if not isc:
                            ls = slice(t0 - LC, t0 - LC + w)
                            psw, pswk = psn(k, 3, 8)
                            for kk in range(3):
                                P.op("pe", lambda e, psw=psw, kk=kk, sl=sl, w=w, h=h: e.matmul(psw[:, :w], wuq[:, kk, (8 + h) * 128:(9 + h) * 128], qn[:, kk, sl], start=(kk == 0), stop=(kk == 2)), reads=["wuq", "qdst"], writes=[pswk])
                            t1, t1k = t1r.next()
                            t2, t2k = t2r.next()
                            P.op("dve", lambda e, pm=pm, ls=ls, w=w, t1=t1: e.tensor_tensor(t1[:, :w], pm[:, :w], rope[:, 0, ls], ALU.mult), reads=[pmk, "rope"], writes=[t1k])
                            P.op("dve", lambda e, psw=psw, ls=ls, w=w, t2=t2: e.tensor_tensor(t2[:, :w], psw[:, :w], rope[:, 1, ls], ALU.mult), reads=[pswk, "rope"], writes=[t2k])
                            P.op("pool", lambda e, ls=ls, w=w, hh=hh, t1=t1, t2=t2: e.tensor_tensor(QR[:, hh, ls], t1[:, :w], t2[:, :w], ALU.add), reads=[t1k, t2k], writes=["QR%d" % hh])
                        pk_, pkk = psn(k, 3, 8)
                        for kk in range(2):
                            P.op("pe", lambda e, pk_=pk_, kk=kk, sl=sl, w=w, h=h: e.matmul(pk_[:, :w], wkk[:, kk, h * 128:(h + 1) * 128], kvn[:, kk, sl], start=(kk == 0), stop=(kk == 1)), reads=["wkk", "kdst"], writes=[pkk])
                        P.op("dve", lambda e, pk_=pk_, sl=sl, w=w, hh=hh: e.tensor_tensor(KH[:, hh, sl], pk_[:, :w], KRb[:, sl], ALU.add), reads=[pkk, "KRb"], writes=["KH%d" % hh])
                if grp == 0:
                    dump(k, "d_wkk", wkk[:], [128, 2, 1024], BF16, ["wkk"])
                    dump(k, "d_QP", QP[:, 0, :], [128, NT], BF16, ["QP0"])
                    dump(k, "d_QR", QR[:, 0, :], [128, L], BF16, ["QR0"])
                    dump(k, "d_KH", KH[:, 0, :], [128, NT], BF16, ["KH0"])
                    dump(k, "d_VA", VA[:, :, 0, :], [128, 18, 128], BF16, ["VA"])
                if "mla_stop2" in k.dbg:
                    P.barrier()
                    continue
                for hh in range(4):
                    h = grp * 4 + hh
                    for qb in range(5):
                        if qb < 4:
                            q0, nq, nkc = LC + qb * 512, 512, 18
                        else:
                            q0, nq, nkc = 0, LC, 2

                        def score_fn(kc, pscr, psk, q0=q0, nq=nq, hh=hh):
                            ks = slice(kc * 128, (kc + 1) * 128)
                            if kc >= 2:
                                P.op("pe", lambda e: e.matmul(pscr[:, :nq], KH[:, hh, ks], QR[:, hh, q0 - LC:q0 - LC + nq], start=True, stop=True), reads=["KH%d" % hh, "QR%d" % hh], writes=[psk])
                            else:
                                P.op("pe", lambda e: e.matmul(pscr[:, :nq], KH[:, hh, ks], QP[:, hh, q0:q0 + nq], start=True, stop=True), reads=["KH%d" % hh, "QP%d" % hh], writes=[psk])

                        softmax_pv(k, ostr, score_fn, nkc, lambda kc, h=h: (VA[:, kc, h, :], "VA"), nq, SC,
                                   k.brT[1][h * 64:(h + 1) * 64, q0:q0 + nq], "mlaT", PTr)
                P.barrier()
        stage_end(k)


def stage_win(k, li):
    nc, P = k.nc, k.P
    SC = float(64 ** -0.5)
    with ExitStack() as st:
        sb = lambda name, shape, dt=F32: st.enter_context(nc.sbuf_tensor(U(name), shape, dt))
        Qp = sb("wQp", [128, 8, NT], BF16)
        Qr = sb("wQr", [128, 8, L], BF16)
        Kp = sb("wKp", [128, NT], BF16)
        Kr = sb("wKr", [128, L], BF16)
        VW = sb("wVW", [128, 18, 2, 128], BF16)
        rope = sb("wrope", [128, 2, L])
        msk = sb("wmsk", [128, 2, 128], BF16)
        esk = sb("wesk", [128, 8])
        Qsr = Rot(nc, st, "wQs", 2, [128, L], BF16)
        tAr = Rot(nc, st, "wtA", 2, [128, 512], F32)
        tBr = Rot(nc, st, "wtB", 2, [128, 512], F32)
        k.att_rv = Rot(nc, st, "watt_rv", 2, [128, 512], F32)
        P.op("pool", lambda e: e.memset(VW[:], 1.0), writes=["VW"])
        P.dma("sp", esk[:], k.win_sink[li], writes=["esk"])
        P.op("act", lambda e: e.activation(out=esk[:], in_=esk[:], func=AF.Exp), reads=["esk"], writes=["esk"])
        P.dma("sp", Qp[:], k.zT[ZT_WQ * 128:(ZT_WQ + 8) * 128, :].rearrange("(k p) t -> p k t", p=128), reads=["zT"], writes=["Qp"])
        P.dma("sp", Kp[:], k.zT[ZT_WK * 128:(ZT_WK + 1) * 128, :], reads=["zT"], writes=["Kp"])
        P.dma("sp", rope[:], k.rope_win.rearrange("c p t -> p c t"), writes=["rope"])
        P.dma("pool", msk[:], k.wmask.rearrange("c p t -> p c t"), writes=["msk"])
        for c_ in range(2):
            P.dma("sp", VW[:, :, c_, 0:64], k.vtok[:, c_ * 64:(c_ + 1) * 64].rearrange("(t p) d -> p t d", p=128), reads=["vtok", "VW"], writes=["VW"])
        for j in range(9):
            qs_, qsk = Qsr.next()
            srow = (ZT_WQS + j) * 128 if j < 8 else ZT_WKS * 128
            P.dma("sp", qs_[:], k.zT[srow:srow + 128, LC:NT], reads=["zT"], writes=[qsk])
            for c in range(4):
                sl = slice(c * 512, (c + 1) * 512)
                sln = slice(LC + c * 512, LC + (c + 1) * 512)
                src = Qp[:, j, sln] if j < 8 else Kp[:, sln]
                dst = Qr[:, j, sl] if j < 8 else Kr[:, sl]
                tA, tAk = tAr.next()
                tB, tBk = tBr.next()
                P.op("dve", lambda e, sl=sl, src=src, tA=tA: e.tensor_tensor(tA[:], src, rope[:, 0, sl], ALU.mult), reads=["Qp", "Kp", "rope"], writes=[tAk])
                P.op("pool", lambda e, sl=sl, qs_=qs_, tB=tB: e.tensor_tensor(tB[:], qs_[:, sl], rope[:, 1, sl], ALU.mult), reads=[qsk, "rope"], writes=[tBk])
                P.op("dve", lambda e, dst=dst, tA=tA, tB=tB: e.tensor_tensor(dst, tA[:], tB[:], ALU.add), reads=[tAk, tBk], writes=["Qr", "Kr"])
        PTr = Rot(nc, st, "wPT", 4, [128, 512], BF16)
        ostr = Rot(nc, st, "wost", 3, [128, 512], BF16)
        for h in range(8):
            kk = h // 4
            for n in range(17):
                if n < 16:
                    q0, nq = LC + n * 128, 128
                    chunks = [("b", n + d, d) for d in (-1, 0, 1) if 0 <= n + d < 16] + [("c", 0, 0), ("c", 1, 0)]
                else:
                    q0, nq = 0, LC
                    chunks = [("c", 0, 0), ("c", 1, 0)]

                def score_fn(kc, pscr, psk, chunks=chunks, q0=q0, nq=nq, h=h):
                    typ, ci, d = chunks[kc]
                    if typ == "b":
                        P.op("pe", lambda e: e.matmul(pscr[:, :nq], Kr[:, ci * 128:(ci + 1) * 128], Qr[:, h, q0 - LC:q0 - LC + nq], start=True, stop=True), reads=["Kr", "Qr"], writes=[psk])
                    else:
                        P.op("pe", lambda e: e.matmul(pscr[:, :nq], Kp[:, ci * 128:(ci + 1) * 128], Qp[:, h, q0:q0 + nq], start=True, stop=True), reads=["Kp", "Qp"], writes=[psk])

                def post(kc, pt, ptk, chunks=chunks):
                    typ, ci, d = chunks[kc]
                    if typ == "b" and d != 0:
                        mi = 0 if d == -1 else 1
                        P.op("dve", lambda e: e.tensor_tensor(pt[:, :128], pt[:, :128], msk[:, mi, :], ALU.mult), reads=[ptk, "msk"], writes=[ptk])

                def va_fn(kc, chunks=chunks, kk=kk):
                    typ, ci, d = chunks[kc]
                    tt = (2 + ci) if typ == "b" else ci
                    return VW[:, tt, kk, :], "VW"

                softmax_pv(k, ostr, score_fn, len(chunks), va_fn, nq, SC, k.brT[2][h * 64:(h + 1) * 64, q0:q0 + nq], "winT", PTr,
                           esk=esk[64:128, h:h + 1], post=post)
        stage_end(k)


def stage_merge(k, li):
    nc, P = k.nc, k.P
    with ExitStack() as st:
        sb = lambda name, shape, dt=F32: st.enter_context(nc.sbuf_tensor(U(name), shape, dt))
        wbr = sb("wbr", [128, 12, D], BF16)
        wout = sb("wout", [128, 8, D], BF16)
        lnp = sb("lnp", [128, 4, 8])
        k.lnp_t = lnp
        P.dma("sp", lnp[:], k.lnp[li], writes=["mod"])
        for j in range(12):
            P.dma("pool", wbr[:, j, :], k.w_branch[li][j * 128:(j + 1) * 128, :], writes=["wbr"])
        for j in range(8):
            P.dma("pool", wout[:, j, :], k.w_out[li][j * 128:(j + 1) * 128, :], writes=["wout"])
        tmp = ln_tmp(nc, st)
        obr = [sb("ob%d" % b, [128, 4, 512], BF16) for b in range(3)]
        gbr = [sb("gb%d" % b, [128, 8, 512], BF16) for b in range(3)]
        mT = sb("mT", [128, 8, 512], BF16)
        mt1 = sb("mt1", [128, 512])
        mt2 = sb("mt2", [128, 512])
        xb = sb("mxb", [128, 8, 512])
        rb = sb("mrb", [128, 8, 512])
        x1 = sb("mx1", [128, 8, 512])
        h2 = sb("mh2", [128, 8, 512], BF16)
        xTv = k.xT.rearrange("(k p) t -> p k t", p=128)
        h2v = k.h2T.rearrange("(k p) t -> p k t", p=128)
        for (t0, w, isc) in TBS:
            sl = slice(t0, t0 + w)
            for b in range(3):
                P.dma("sp", obr[b][:, :, :w], k.brT[b][:, sl].rearrange("(k p) t -> p k t", p=128), reads=["brT"], writes=["ob%d" % b])
                P.dma("sp", gbr[b][:, :, :w], k.zT[(ZT_G + 8 * b) * 128:(ZT_G + 8 + 8 * b) * 128, sl].rearrange("(k p) t -> p k t", p=128), reads=["zT"], writes=["gb%d" % b])
            P.dma("sp", xb[:, :, :w], xTv[:, :, sl], reads=["xT"], writes=["mxb"])
            for mo in range(8):
                for b in range(3):
                    pt, pk = psn(k)
                    for kk in range(4):
                        P.op("pe", lambda e, pt=pt, kk=kk, b=b, mo=mo, w=w: e.matmul(pt[:, :w], wbr[:, b * 4 + kk, mo * 128:(mo + 1) * 128], obr[b][:, kk, :w], start=(kk == 0), stop=(kk == 3)), reads=["wbr", "ob%d" % b], writes=[pk])
                    if b == 0:
                        P.op("dve", lambda e, pt=pt, mo=mo, w=w: e.tensor_tensor(mt1[:, :w], pt[:, :w], gbr[0][:, mo, :w], ALU.mult), reads=[pk, "gb0"], writes=["mt1"])
                    elif b == 1:
                        P.op("dve", lambda e, pt=pt, mo=mo, w=w: e.tensor_tensor(mt2[:, :w], pt[:, :w], gbr[1][:, mo, :w], ALU.mult), reads=[pk, "gb1"], writes=["mt2"])
                        P.op("pool", lambda e, w=w: e.tensor_tensor(mt1[:, :w], mt1[:, :w], mt2[:, :w], ALU.add), reads=["mt1", "mt2"], writes=["mt1"])
                    else:
                        P.op("dve", lambda e, pt=pt, mo=mo, w=w: e.tensor_tensor(mt2[:, :w], pt[:, :w], gbr[2][:, mo, :w], ALU.mult), reads=[pk, "gb2"], writes=["mt2"])
                        P.op("pool", lambda e, mo=mo, w=w: e.tensor_tensor(mT[:, mo, :w], mt1[:, :w], mt2[:, :w], ALU.add), reads=["mt1", "mt2"], writes=["mT"])
            for mo in range(8):
                pt, pk = psn(k)
                for kk in range(8):
                    P.op("pe", lambda e, pt=pt, kk=kk, mo=mo, w=w: e.matmul(pt[:, :w], wout[:, kk, mo * 128:(mo + 1) * 128], mT[:, kk, :w], start=(kk == 0), stop=(kk == 7)), reads=["wout", "mT"], writes=[pk])
                P.op("act", lambda e, pt=pt, mo=mo, w=w, isc=isc: e.activation(out=mt1[:, :w], in_=pt[:, :w], func=AF.Identity, scale=k.mod[:, li, 16 + mo, isc:isc + 1]), reads=[pk, "mod"], writes=["mt1"])
                P.op("dve", lambda e, mo=mo, w=w: e.scalar_tensor_tensor(rb[:, mo, :w], xb[:, mo, :w], float(ALPHA), mt1[:, :w], ALU.mult, ALU.add), reads=["mxb", "mt1"], writes=["mrb"])
            ln_stats(k, rb, "mrb", w, tmp)
            for kk in range(8):
                ln_apply(k, rb, "mrb", w, tmp, kk, x1[:, kk, :w], ["mx1"], lnp[:, 0, kk:kk + 1], lnp[:, 1, kk:kk + 1])
            P.dma("sp", xTv[:, :, sl], x1[:, :, :w], reads=["mx1"], writes=["xT"])
            ln_stats(k, x1, "mx1", w, tmp)
            for kk in range(8):
                ln_apply(k, x1, "mx1", w, tmp, kk, h2[:, kk, :w], ["mh2"], k.mod[:, li, 32 + kk, isc:isc + 1], k.mod[:, li, 24 + kk, isc:isc + 1])
            P.dma("sp", h2v[:, :, sl], h2[:, :, :w], reads=["mh2"], writes=["h2T"])
        stage_end(k)


def stage_moe(k, li):
    nc, P = k.nc, k.P
    with ExitStack() as st0:
        fT = st0.enter_context(nc.sbuf_tensor(U("efT"), [128, 8, NT], BF16))
        lnp = st0.enter_context(nc.sbuf_tensor(U("elnp"), [128, 4, 8], F32))
        st = st0.enter_context(ExitStack())
        sb = lambda name, shape, dt=F32: st.enter_context(nc.sbuf_tensor(U(name), shape, dt))
        h2 = sb("eh2", [128, 8, NT], BF16)
        wr = sb("ewr", [128, 8, 16], BF16)
        aff = sb("eaff", [16, NT])
        wk_ = sb("ewk", [16, NT])
        gw = sb("egw", [16, NT])
        m8 = sb("em8", [16, 8])
        thr = sb("ethr", [16, 2])
        sel = sb("esel", [16, 16, 128])
        gwb = sb("egwb", [128, NT], BF16)
        P.dma("sp", lnp[:], k.lnp[li], writes=["mod"])
        P.dma("sp", h2[:], k.h2T.rearrange("(k p) t -> p k t", p=128), reads=["h2T"], writes=["eh2"])
        P.dma("pool", wr[:], k.w_router[li].rearrange("(k p) n -> p k n", p=128), writes=["ewr"])
        P.dma("sp", sel[:], k.sel16, writes=["esel"])
        for (t0, w, isc) in TBS:
            sl = slice(t0, t0 + w)
            pt, pk = psn(k)
            for kk in range(8):
                P.op("pe", lambda e, pt=pt, kk=kk, sl=sl, w=w: e.matmul(pt[0:16, :w], wr[:, kk, :], h2[:, kk, sl], start=(kk == 0), stop=(kk == 7)), reads=["ewr", "eh2"], writes=[pk])
            P.op("act", lambda e, pt=pt, sl=sl, w=w: e.activation(out=wk_[:, sl], in_=pt[0:16, :w], func=AF.Exp), reads=[pk], writes=["ewk"])
            p2, k2 = psn(k)
            P.op("pe", lambda e, p2=p2, sl=sl, w=w: e.matmul(p2[0:16, :w], k.onesf[0:16, 0:16], wk_[:, sl], start=True, stop=True), reads=["ewk", "onesf"], writes=[k2])
            P.op("dve", lambda e, p2=p2, sl=sl, w=w: e.reciprocal(gw[:, sl], p2[0:16, :w]), reads=[k2], writes=["egw"])
            P.op("dve", lambda e, sl=sl: e.tensor_tensor(aff[:, sl], wk_[:, sl], gw[:, sl], ALU.mult), reads=["ewk", "egw"], writes=["eaff"])
        P.op("dve", lambda e: e.tensor_copy(wk_[:], aff[:]), reads=["eaff"], writes=["ewk"])
        for si, (a, b, cap) in enumerate(((0, LC, 32), (LC, NT, 256))):
            for r in range(cap // 8):
                P.op("dve", lambda e, a=a, b=b: e.max(out=m8[:], in_=wk_[:, a:b]), reads=["ewk"], writes=["em8"])
                if r < cap // 8 - 1:
                    P.op("dve", lambda e, a=a, b=b: e.match_replace(out=wk_[:, a:b], in_to_replace=m8[:], in_values=wk_[:, a:b], imm_value=-1.0), reads=["ewk", "em8"], writes=["ewk"])
            P.op("dve", lambda e, si=si: e.tensor_copy(thr[:, si:si + 1], m8[:, 7:8]), reads=["em8"], writes=["ethr"])
            P.op("dve", lambda e, a=a, b=b, si=si: e.scalar_tensor_tensor(gw[:, a:b], aff[:, a:b], thr[:, si:si + 1], aff[:, a:b], ALU.is_ge, ALU.mult), reads=["eaff", "ethr"], writes=["egw"])
        mw = Rot(nc, st, "emw", 24, [128, D], BF16)
        act = sb("eact", [128, 8, 512], BF16)
        sa = Rot(nc, st, "esa", 2, [128, 512], F32)
        au = Rot(nc, st, "eau", 2, [128, 512], F32)
        for ex in range(16):
            ws = {}
            for nm, src in (("g", k.w_gate), ("u", k.w_up), ("d", k.w_down)):
                for kk in range(8):
                    t, tk = mw.next()
                    P.dma("pool", t[:], src[li, ex, kk * 128:(kk + 1) * 128, :], writes=[tk])
                    ws[(nm, kk)] = (t, tk)
            for (t0, w, isc) in TBS:
                sl = slice(t0, t0 + w)
                pb, pbk = psn(k, 6, 8)
                P.op("pe", lambda e, pb=pb, sl=sl, w=w, ex=ex: e.matmul(pb[:, :w], sel[:, ex, :], gw[:, sl], start=True, stop=True), reads=["esel", "egw"], writes=[pbk])
                P.op("act", lambda e, pb=pb, sl=sl, w=w: e.copy(gwb[:, sl], pb[:, :w]), reads=[pbk], writes=["egwb"])
            for (t0, w, isc) in TBS:
                sl = slice(t0, t0 + w)
                for mo in range(8):
                    pa, pak = psn(k, 0, 3)
                    pu, puk = psn(k, 3, 6)
                    for kk in range(8):
                        wt, wtk = ws[("g", kk)]
                        P.op("pe", lambda e, pa=pa, wt=wt, kk=kk, mo=mo, sl=sl, w=w: e.matmul(pa[:, :w], wt[:, mo * 128:(mo + 1) * 128], h2[:, kk, sl], start=(kk == 0), stop=(kk == 7)), reads=[wtk, "eh2"], writes=[pak])
                    for kk in range(8):
                        wt, wtk = ws[("u", kk)]
                        P.op("pe", lambda e, pu=pu, wt=wt, kk=kk, mo=mo, sl=sl, w=w: e.matmul(pu[:, :w], wt[:, mo * 128:(mo + 1) * 128], h2[:, kk, sl], start=(kk == 0), stop=(kk == 7)), reads=[wtk, "eh2"], writes=[puk])
                    s_, sk_ = sa.next()
                    a_, ak_ = au.next()
                    P.op("act", lambda e, pa=pa, s_=s_, w=w: e.activation(out=s_[:, :w], in_=pa[:, :w], func=AF.Silu), reads=[pak], writes=[sk_])
                    P.op("dve", lambda e, pu=pu, s_=s_, a_=a_, w=w: e.tensor_tensor(a_[:, :w], pu[:, :w], s_[:, :w], ALU.mult), reads=[puk, sk_], writes=[ak_])
                    P.op("pool", lambda e, a_=a_, mo=mo, sl=sl, w=w: e.tensor_tensor(act[:, mo, :w], a_[:, :w], gwb[:, sl], ALU.mult), reads=[ak_, "egwb"], writes=["eact"])
                for mo in range(8):
                    py, pyk = psn(k, 6, 8)
                    for kk in range(8):
                        wt, wtk = ws[("d", kk)]
                        P.op("pe", lambda e, py=py, wt=wt, kk=kk, mo=mo, w=w: e.matmul(py[:, :w], wt[:, mo * 128:(mo + 1) * 128], act[:, kk, :w], start=(kk == 0), stop=(kk == 7)), reads=[wtk, "eact"], writes=[pyk])
                    if ex == 0:
                        P.op("dve", lambda e, py=py, mo=mo, sl=sl, w=w: e.tensor_copy(fT[:, mo, sl], py[:, :w]), reads=[pyk], writes=["efT"])
                    else:
                        P.op("dve", lambda e, py=py, mo=mo, sl=sl, w=w: e.tensor_tensor(fT[:, mo, sl], fT[:, mo, sl], py[:, :w], ALU.add), reads=[pyk, "efT"], writes=["efT"])
        P.barrier()
        st.close()
        st = st0
        tmp = ln_tmp(nc, st)
        xb = sb("exb", [128, 8, 512])
        rb = sb("erb", [128, 8, 512])
        x2 = sb("ex2", [128, 8, 512])
        xTv = k.xT.rearrange("(k p) t -> p k t", p=128)
        for (t0, w, isc) in TBS:
            sl = slice(t0, t0 + w)
            P.dma("sp", xb[:, :, :w], xTv[:, :, sl], reads=["xT"], writes=["exb"])
            for mo in range(8):
                P.op("act", lambda e, mo=mo, sl=sl, w=w, isc=isc: e.activation(out=tmp["t"][:, :w], in_=fT[:, mo, sl], func=AF.Identity, scale=k.mod[:, li, 40 + mo, isc:isc + 1]), reads=["efT", "mod"], writes=["ln_t"])
                P.op("dve", lambda e, mo=mo, w=w: e.scalar_tensor_tensor(rb[:, mo, :w], xb[:, mo, :w], float(ALPHA), tmp["t"][:, :w], ALU.mult, ALU.add), reads=["exb", "ln_t"], writes=["erb"])
            ln_stats(k, rb, "erb", w, tmp)
            for kk in range(8):
                ln_apply(k, rb, "erb", w, tmp, kk, x2[:, kk, :w], ["ex2"], lnp[:, 2, kk:kk + 1], lnp[:, 3, kk:kk + 1])
            P.dma("sp", xTv[:, :, sl], x2[:, :, :w], reads=["ex2"], writes=["xT"])
        stage_end(k)

def stage_out(k):
    nc, P = k.nc, k.P
    with ExitStack() as st:
        xr = Rot(nc, st, "oxr", 2, [128, 8, 128], F32)
        xo = Rot(nc, st, "oxo", 2, [128, D], F32)
        xTv = k.xT.rearrange("(k p) t -> p k t", p=128)
        for tt in range(L // 128):
            xt, xk = xr.next()
            P.dma("sp", xt[:], xTv[:, :, LC + tt * 128:LC + (tt + 1) * 128], reads=["xT"], writes=[xk])
            ot, ok = xo.next()
            for half in range(2):
                pt, pk = psn(k)
                for kk in range(4):
                    kf = half * 4 + kk
                    P.op("pe", lambda e, pt=pt, kk=kk, kf=kf, xt=xt: e.transpose(pt[:, kk * 128:(kk + 1) * 128], xt[:, kf, :], k.identf[:]),
                         reads=[xk, "identf"], writes=[pk])
                if half == 0:
                    P.op("act", lambda e, pt=pt, ot=ot: e.copy(ot[:, 0:512], pt[:]), reads=[pk], writes=[ok + "h0"])
                else:
                    P.op("dve", lambda e, pt=pt, ot=ot: e.tensor_copy(ot[:, 512:1024], pt[:]), reads=[pk], writes=[ok + "h1"])
            P.dma("sp", k.out[tt * 128:(tt + 1) * 128, :], ot[:], reads=[ok + "h0", ok + "h1"], writes=["out"])
        stage_end(k)


Z_ORDER = None


def _zcols():
    u = np.arange(0, 512)
    qa = np.arange(512, 896)
    kva = np.arange(896, 1152)
    kr = np.arange(1152, 1184)
    wq = np.arange(1184, 1696)
    wk = np.arange(1696, 1824)
    wv = np.arange(1824, 1952)
    gates = np.arange(1952, 5024)
    Z = -np.ones(64, np.int64)

    def padq(cols8):
        out = []
        for h in range(8):
            kk = h // 4
            out.append(np.concatenate([cols8[h], Z]) if kk == 0 else np.concatenate([Z, cols8[h]]))
        return np.concatenate(out)
    wq8 = wq.reshape(8, 64)
    wq8s = wq.reshape(8, 2, 32)[:, ::-1, :].reshape(8, 64)
    wk_sw = wk.reshape(2, 2, 32)[:, ::-1, :].reshape(-1)
    kr_sw = kr.reshape(2, 16)[::-1].reshape(-1)
    cols = np.concatenate([u, qa, kva, padq(wq8), wk, wv, gates, padq(wq8s), wk_sw, kr, kr_sw, Z])
    assert cols.size == NZT * 128, cols.size
    return cols


def prep_shared(inp):
    f = lambda a: np.ascontiguousarray(np.asarray(a, np.float32))
    sh = {}
    sh["ident"] = np.eye(128, dtype=np.float32)
    sh["w_ada"] = f(inp["w_ada"])
    b = f(inp["b_ada"]).reshape(DEPTH, 48, 128).transpose(0, 2, 1)
    sh["bada2"] = f(np.repeat(b[:, :, :, None], 2, axis=3))
    cols = _zcols()
    wx = f(inp["w_in"])[:, :, np.maximum(cols, 0)]
    wx[:, :, cols < 0] = 0.0
    sh["w_inx"] = f(wx)
    G, PS, HG = 32, 64, 16
    bT = np.zeros((DEPTH, 2, 2, 16, 128, 128), np.float32)
    cL = np.zeros((DEPTH, 128, 2, 16, 2, 16), np.float32)
    lane = np.zeros((DEPTH, 128, 3, 32), np.float32)
    for ri, nm in enumerate(("s5_b_re", "s5_b_im")):
        bsrc = f(inp[nm])
        for lt in range(16):
            for half in range(2):
                g = 2 * lt + half
                gl = g % 8
                bT[:, :, ri, lt, gl * 16:(gl + 1) * 16, half * 64:(half + 1) * 64] = bsrc[:, :, g].transpose(0, 1, 3, 2)
    for ri, nm in enumerate(("s5_c_re", "s5_c_im")):
        csrc = f(inp[nm])
        for lt in range(16):
            for half in range(2):
                g = 2 * lt + half
                cL[:, half * 64:(half + 1) * 64, :, lt, ri, :] = csrc[:, :, g].transpose(0, 3, 1, 2)
    lre, lim, ldt = f(inp["s5_lam_re"]), f(inp["s5_lam_im"]), f(inp["s5_log_dt"])
    for d in range(2):
        for lt in range(16):
            for half in range(2):
                g = 2 * lt + half
                lane[:, half * 64:(half + 1) * 64, 0, d * 16 + lt] = lre[:, d, g, :]
                lane[:, half * 64:(half + 1) * 64, 1, d * 16 + lt] = lim[:, d, g, :]
                lane[:, half * 64:(half + 1) * 64, 2, d * 16 + lt] = ldt[:, d, g][:, None]
    sh["s5_bT"], sh["s5_cL"], sh["s5_lane"] = bT, cL, lane
    dg = np.zeros((DEPTH, 128, 2, 4), np.float32)
    dg[:, :, 0, :] = f(inp["s5_d"]).reshape(DEPTH, 4, 128).transpose(0, 2, 1)
    dg[:, :, 1, :] = f(inp["s5_b_glu"]).reshape(DEPTH, 4, 128).transpose(0, 2, 1)
    sh["s5_dg"] = dg
    sh["s5_wglu"] = f(inp["s5_w_glu"])
    mg = np.zeros((DEPTH, 128, 5), np.float32)
    mg[:, :, 0:3] = f(inp["mla_q_norm"]).reshape(DEPTH, 3, 128).transpose(0, 2, 1)
    mg[:, :, 3:5] = f(inp["mla_kv_norm"]).reshape(DEPTH, 2, 128).transpose(0, 2, 1)
    sh["mla_g"] = mg
    wuq = f(inp["mla_w_uq"]).reshape(DEPTH, 384, 8, 96)
    wm = np.zeros((DEPTH, 384, 8, 128), np.float32)
    wsw = np.zeros((DEPTH, 384, 8, 128), np.float32)
    wm[..., 0:96] = wuq
    wsw[..., 64:96] = wuq[..., 64:].reshape(DEPTH, 384, 8, 2, 16)[:, :, :, ::-1, :].reshape(DEPTH, 384, 8, 32)
    sh["w_uqx"] = f(np.concatenate([wm.reshape(DEPTH, 384, 1024), wsw.reshape(DEPTH, 384, 1024)], -1))
    wkv = f(inp["mla_w_ukv"]).reshape(DEPTH, 256, 8, 128)
    wkp = np.zeros((DEPTH, 256, 8, 128), np.float32)
    wkp[..., 0:64] = wkv[..., :64]
    sh["w_ukvk"] = f(wkp.reshape(DEPTH, 256, 1024))
    sh["w_ukvv"] = f(wkv[..., 64:].reshape(DEPTH, 256, 512))
    rows = L // 64
    row = np.repeat(np.arange(rows, dtype=np.float64), 64)
    col = np.tile(np.arange(64, dtype=np.float64), rows)

    def rope_tab(d, reps):
        nf = d // 4
        fr = 10000.0 ** (-np.arange(nf) / nf)
        ang = np.concatenate([row[:, None] * fr, col[:, None] * fr], -1)
        c = np.concatenate([np.cos(ang), np.cos(ang)], -1).T
        sn = np.concatenate([-np.sin(ang), np.sin(ang)], -1).T
        return np.stack([np.tile(c, (reps, 1)), np.tile(sn, (reps, 1))], 0).astype(np.float32)
    rm = np.zeros((2, 128, L), np.float32)
    rm[0] = 1.0
    rm[:, 64:96, :] = rope_tab(32, 1)
    sh["rope_mla"] = rm
    sh["rope_win"] = rope_tab(64, 2)
    jj = np.arange(128)[:, None]
    rr = np.arange(128)[None, :]
    sh["wmask"] = np.stack([(jj >= rr), (jj <= rr)], 0).astype(np.float32)
    sh["win_sink"] = f(np.broadcast_to(f(inp["win_sink"])[:, None, :], (DEPTH, 128, 8)))
    sh["w_branch"] = f(inp["w_branch"]).reshape(DEPTH, 1536, D)
    sh["w_out"] = f(inp["w_out"])
    lnp = np.stack([f(inp[n]).reshape(DEPTH, 8, 128).transpose(0, 2, 1) for n in ("ln1_g", "ln1_b", "ln2_g", "ln2_b")], 2)
    sh["lnp"] = f(lnp)
    sh["w_router"] = f(inp["w_router"])
    sh["w_gate"], sh["w_up"], sh["w_down"] = f(inp["w_gate"]), f(inp["w_up"]), f(inp["w_down"])
    sel = np.zeros((16, 16, 128), np.float32)
    for e_ in range(16):
        sel[e_, e_, :] = 1.0
    sh["sel16"] = sel
    sh["tau"] = f(np.broadcast_to(np.arange(NT, dtype=np.float32), (128, NT)))
    return sh


def prep_core(inp, b):
    f = lambda a: np.ascontiguousarray(np.asarray(a, np.float32))
    m = {}
    m["xin"] = f(np.concatenate([inp["ctx"][b], inp["x"][b]], 0))
    cond = np.stack([np.asarray(inp["c"][b]), np.asarray(inp["c_ctx"])], -1)
    m["condT"] = f(cond.reshape(8, 128, 2).transpose(1, 0, 2))
    return m


def kernel(**inputs):
    nc = build()
    sh = prep_shared(inputs)
    in_maps = []
    for b in range(8):
        m = dict(sh)
        m.update(prep_core(inputs, b))
        in_maps.append(m)
    res = run_bass_kernel_spmd(nc, in_maps, core_ids=list(range(8)))
    return np.stack([np.asarray(r["out"], np.float32) for r in res.results], 0)
```

```python
import numpy as np
from contextlib import ExitStack
import concourse.bass as bass
import concourse.mybir as mybir
from concourse.bass_utils import run_bass_kernel_spmd

F32 = mybir.dt.float32
BF16 = mybir.dt.bfloat16
I32 = mybir.dt.int32
AF = mybir.ActivationFunctionType
ALU = mybir.AluOpType

ENGS = ("pe", "dve", "act", "pool", "sp")
D = 1024
NT = 2304
LC = 256
L = 2048
DEPTH = 4
ALPHA = (2 * DEPTH) ** 0.25
EPS = 1e-6
NZT = 53
ZT_QA, ZT_KVA, ZT_WQ, ZT_WK, ZT_WV, ZT_G, ZT_WQS, ZT_WKS, ZT_KRX = 4, 7, 9, 17, 18, 19, 43, 51, 52
TBS = [(0, 256, 1), (256, 512, 0), (768, 512, 0), (1280, 512, 0), (1792, 512, 0)]


class Prog:
    def __init__(self, nc, es, n_dma_sems=24):
        self.nc = nc
        self.q = {e: [] for e in ENGS}
        self.sem = {}
        self.cnt = {}
        for e in ENGS:
            self.sem[e] = es.enter_context(nc.semaphore("s_" + e))
            self.cnt[e] = 0
        self.dma_sems = []
        for i in range(n_dma_sems):
            nm = "d%d" % i
            self.sem[nm] = es.enter_context(nc.semaphore("s_" + nm))
            self.cnt[nm] = 0
            self.dma_sems.append(nm)
        self.dma_rr = 0
        self.seen = {e: {} for e in ENGS}
        self.lastw = {}
        self.readers = {}
        self.nops = 0

    def _deps(self, eng, reads, writes):
        deps = {}

        def need(st, v):
            if st == eng and eng == "pe":
                return
            if deps.get(st, 0) < v:
                deps[st] = v

        for k in reads:
            lw = self.lastw.get(k)
            if lw is not None:
                need(*lw)
            if k.startswith("ps"):
                for r in self.readers.get(k, ()):
                    if r[0] != eng:
                        need(*r)
        for k in writes:
            lw = self.lastw.get(k)
            if lw is not None:
                need(*lw)
            for r in self.readers.get(k, ()):
                need(*r)
        return deps

    def _emit_waits(self, eng, deps):
        for st, v in deps.items():
            if self.seen[eng].get(st, 0) < v:
                self.seen[eng][st] = v
                sem = self.sem[st]
                self.q[eng].append(lambda e, sem=sem, v=v: e.wait_ge(sem, v))

    def _record(self, done, reads, writes):
        for k in reads:
            self.readers.setdefault(k, []).append(done)
        for k in writes:
            self.lastw[k] = done
            self.readers[k] = []

    def op(self, eng, fn, reads=(), writes=()):
        deps = self._deps(eng, reads, writes)
        self._emit_waits(eng, deps)
        self.cnt[eng] += 1
        sem = self.sem[eng]
        self.q[eng].append(lambda e, fn=fn, sem=sem: fn(e).then_inc(sem, 1))
        self._record((eng, self.cnt[eng]), reads, writes)
        self.nops += 1

    def dma(self, qeng, out, in_, reads=(), writes=(), **kw):
        st = self.dma_sems[self.dma_rr % len(self.dma_sems)]
        self.dma_rr += 1
        deps = self._deps(st, reads, writes)
        if self.cnt[st] > 0:
            deps[st] = self.cnt[st]
        self._emit_waits(qeng, deps)
        self.cnt[st] += 16
        sem = self.sem[st]
        self.q[qeng].append(
            lambda e, out=out, in_=in_, sem=sem, kw=kw: e.dma_start(out=out, in_=in_, **kw).then_inc(sem, 16))
        self._record((st, self.cnt[st]), reads, writes)
        self.nops += 1

    def barrier(self):
        for eng in ENGS:
            deps = {}
            for st in self.sem:
                if st != eng and self.cnt[st] > 0:
                    deps[st] = self.cnt[st]
            self._emit_waits(eng, deps)
        self.lastw = {}
        self.readers = {}

    def replay(self):
        nc = self.nc
        q = self.q
        with nc.Block() as block:
            @block.tensor
            def _(e):
                for f in q["pe"]:
                    f(e)

            @block.vector
            def _(e):
                for f in q["dve"]:
                    f(e)

            @block.scalar
            def _(e):
                for f in q["act"]:
                    f(e)

            @block.gpsimd
            def _(e):
                for f in q["pool"]:
                    f(e)

            @block.sync
            def _(e):
                for f in q["sp"]:
                    f(e)
        self.q = {e: [] for e in ENGS}


_UID = [0]


def U(name):
    _UID[0] += 1
    return "%s_u%d" % (name, _UID[0])


class Rot:
    def __init__(self, nc, es, name, n, shape, dtype, psum=False):
        self.name = name
        self.n = n
        self.i = 0
        if psum:
            self.t = [es.enter_context(nc.psum_tensor(U("%s%d" % (name, j)), shape, dtype)) for j in range(n)]
        else:
            self.t = [es.enter_context(nc.sbuf_tensor(U("%s%d" % (name, j)), shape, dtype)) for j in range(n)]

    def next(self):
        j = self.i % self.n
        self.i += 1
        return self.t[j], "%s%d" % (self.name, j)


class K:
    pass


def build(nlayers=DEPTH, dbg=()):
    nc = bass.Bass("TRN2", target_bir_lowering=False)
    k = K()
    k.nc = nc
    k.dbg = dbg
    din = lambda name, shape, dt=F32: nc.dram_tensor(name, list(shape), dt, kind="ExternalInput").ap()

    def dscr(name, shape, dt):
        kind = "ExternalOutput" if name in dbg else "Internal"
        return nc.dram_tensor(name, list(shape), dt, kind=kind).ap()

    k.xin = din("xin", [NT, D])
    k.condT = din("condT", [128, 8, 2])
    k.ident = din("ident", [128, 128])
    k.w_ada = din("w_ada", [DEPTH, D, 6 * D])
    k.bada2 = din("bada2", [DEPTH, 128, 48, 2])
    k.w_inx = din("w_inx", [DEPTH, D, NZT * 128])
    k.s5_bT = din("s5_bT", [DEPTH, 2, 2, 16, 128, 128])
    k.s5_cL = din("s5_cL", [DEPTH, 128, 2, 16, 2, 16])
    k.s5_lane = din("s5_lane", [DEPTH, 128, 3, 32])
    k.s5_dg = din("s5_dg", [DEPTH, 128, 2, 4])
    k.s5_wglu = din("s5_wglu", [DEPTH, 512, 512])
    k.tau = din("tau", [128, NT])
    k.mla_g = din("mla_g", [DEPTH, 128, 5])
    k.w_uqx = din("w_uqx", [DEPTH, 384, 2048])
    k.w_ukvk = din("w_ukvk", [DEPTH, 256, 1024])
    k.w_ukvv = din("w_ukvv", [DEPTH, 256, 512])
    k.rope_mla = din("rope_mla", [2, 128, L])
    k.rope_win = din("rope_win", [2, 128, L])
    k.wmask = din("wmask", [2, 128, 128])
    k.win_sink = din("win_sink", [DEPTH, 128, 8])
    k.w_branch = din("w_branch", [DEPTH, 1536, D])
    k.w_out = din("w_out", [DEPTH, D, D])
    k.lnp = din("lnp", [DEPTH, 128, 4, 8])
    k.w_router = din("w_router", [DEPTH, D, 16])
    k.w_gate = din("w_gate", [DEPTH, 16, D, D])
    k.w_up = din("w_up", [DEPTH, 16, D, D])
    k.w_down = din("w_down", [DEPTH, 16, D, D])
    k.sel16 = din("sel16", [16, 16, 128])
    k.iota_s = din("iota_s", [128, 384])
    k.iota_p3 = din("iota_p3", [128, 3])
    k.out = nc.dram_tensor("out", [L, D], F32, kind="ExternalOutput").ap()
    k.brT = [dscr(nm, [512, NT], BF16) for nm in ("s5T", "mlaT", "winT")]
    k.xT = dscr("xT", [D, NT], F32)
    k.zT = dscr("zT", [NZT * 128, NT], BF16)
    k.vtok = dscr("vtok", [NT, 128], BF16)
    k.h2T = dscr("h2T", [D, NT], BF16)
    k.yg = dscr("yg", [16, 3, 128, D], BF16)

    with ExitStack() as es:
        P = Prog(nc, es)
        k.P = P
        k.identf = es.enter_context(nc.sbuf_tensor(U("identf"), [128, 128], F32))
        k.identb = es.enter_context(nc.sbuf_tensor(U("identb"), [128, 128], BF16))
        k.onesm = es.enter_context(nc.sbuf_tensor(U("onesm"), [128, 128], F32))
        k.mod = es.enter_context(nc.sbuf_tensor(U("mod"), [128, DEPTH, 48, 2], F32))
        k.epsc = es.enter_context(nc.sbuf_tensor(U("epsc"), [128, 1], F32))
        k.ps = [es.enter_context(nc.psum_tensor("ps%d" % i, [128, 512], F32)) for i in range(8)]
        k.psi = 0

        stage_init(k)
        stage_ada(k, nlayers)
        k.onesb = es.enter_context(nc.sbuf_tensor(U("onesb"), [128, 512], BF16))
        k.onesf = es.enter_context(nc.sbuf_tensor(U("onesf"), [128, 128], F32))
        P.op("dve", lambda e: e.memset(k.onesb[:], 1.0), writes=["onesb"])
        P.op("dve", lambda e: e.memset(k.onesf[:], 1.0), writes=["onesf"])
        k.halfpi = es.enter_context(nc.sbuf_tensor(U("halfpi"), [128, 1], F32))
        P.op("dve", lambda e: e.memset(k.halfpi[:], float(np.pi / 2)), writes=["halfpi"])
        for li in range(nlayers):
            if "skip_win" not in dbg:
                stage_ln_win(k, li)
            if "mla_first" in dbg:
                stage_mla(k, li)
            if "skip_s5" not in dbg:
                stage_s5(k, li)
            if "skip_mla" not in dbg and "mla_first" not in dbg:
                stage_mla(k, li)
            if "skip_winb" not in dbg:
                stage_win(k, li)
            if "skip_mm" not in dbg:
                stage_merge(k, li)
                stage_moe(k, li)
        stage_out(k)
    return nc


def dump(k, name, ap, shape, dt, reads):
    if name not in k.dbg:
        return
    t = k.nc.dram_tensor(name, list(shape), dt, kind="ExternalOutput").ap()
    k.P.dma("sp", t, ap, reads=reads)


def psn(k, lo=0, hi=8):
    j = lo + (k.psi % (hi - lo))
    k.psi += 1
    return k.ps[j], "ps%d" % j


def stage_end(k):
    k.P.barrier()
    k.P.replay()


def stage_init(k):
    nc, P = k.nc, k.P
    P.dma("sp", k.identf[:], k.ident, writes=["identf"])
    P.dma("pool", k.identb[:], k.ident, writes=["identb"])
    P.op("dve", lambda e: e.memset(k.onesm[:], 1.0 / D), writes=["onesm"])
    P.op("dve", lambda e: e.memset(k.epsc[:], EPS), writes=["epsc"])
    with ExitStack() as st:
        xr = Rot(nc, st, "xr", 2, [128, D], F32)
        xo = Rot(nc, st, "xo", 2, [128, 8, 128], F32)
        xTv = k.xT.rearrange("(k p) t -> p k t", p=128)
        for tt in range(NT // 128):
            xt, xk = xr.next()
            P.dma("sp", xt[:], k.xin[tt * 128:(tt + 1) * 128, :], writes=[xk])
            ot, ok = xo.next()
            for half in range(2):
                pt, pk = psn(k)
                for kk in range(4):
                    kf = half * 4 + kk
                    P.op("pe", lambda e, pt=pt, kk=kk, kf=kf, xt=xt: e.transpose(pt[:, kk * 128:(kk + 1) * 128], xt[:, kf * 128:(kf + 1) * 128], k.identf[:]),
                         reads=[xk, "identf"], writes=[pk])
                eng = "act" if half == 0 else "dve"
                if eng == "act":
                    P.op("act", lambda e, pt=pt, ot=ot, half=half: e.copy(ot[:, half * 4:(half + 1) * 4, :], pt[:].rearrange("p (k t) -> p k t", k=4)),
                         reads=[pk], writes=[ok + "h%d" % half])
                else:
                    P.op("dve", lambda e, pt=pt, ot=ot, half=half: e.tensor_copy(ot[:, half * 4:(half + 1) * 4, :], pt[:].rearrange("p (k t) -> p k t", k=4)),
                         reads=[pk], writes=[ok + "h%d" % half])
            P.dma("sp", xTv[:, :, tt * 128:(tt + 1) * 128], ot[:], reads=[ok + "h0", ok + "h1"], writes=["xT"])
        stage_end(k)


def stage_ada(k, nlayers):
    nc, P = k.nc, k.P
    with ExitStack() as st:
        sc = st.enter_context(nc.sbuf_tensor(U("sc"), [128, 8, 2], F32))
        bt = st.enter_context(nc.sbuf_tensor(U("bt"), [128, DEPTH, 48, 2], F32))
        wa = Rot(nc, st, "wa", 2, [128, 8, 768], F32)
        P.dma("sp", sc[:], k.condT, writes=["sc"])
        P.dma("sp", bt[:], k.bada2.rearrange("l p m s -> p l m s"), writes=["bt"])
        P.op("act", lambda e: e.activation(out=sc[:], in_=sc[:], func=AF.Silu), reads=["sc"], writes=["sc"])
        for li in range(nlayers):
            wv = k.w_ada[li].rearrange("(k p) n -> p k n", p=128)
            for cb in range(8):
                wt, wk = wa.next()
                P.dma("sp", wt[:], wv[:, :, cb * 768:(cb + 1) * 768], writes=[wk])
                pt, pk = psn(k)
                for mt in range(6):
                    for kk in range(8):
                        P.op("pe", lambda e, pt=pt, wt=wt, mt=mt, kk=kk: e.matmul(pt[:, mt * 2:mt * 2 + 2], wt[:, kk, mt * 128:(mt + 1) * 128], sc[:, kk, :], start=(kk == 0), stop=(kk == 7)),
                             reads=[wk, "sc"], writes=[pk])
                P.op("dve", lambda e, pt=pt, li=li, cb=cb: e.tensor_tensor(k.mod[:, li, cb * 6:(cb + 1) * 6, :], pt[:, 0:12].rearrange("p (m s) -> p m s", s=2), bt[:, li, cb * 6:(cb + 1) * 6, :], ALU.add),
                     reads=[pk, "bt"], writes=["mod"])
            for j in (1, 4):
                P.op("dve", lambda e, li=li, j=j: e.tensor_scalar_add(k.mod[:, li, j * 8:(j + 1) * 8, :], k.mod[:, li, j * 8:(j + 1) * 8, :], 1.0),
                     reads=["mod"], writes=["mod"])
        stage_end(k)


def ln_stats(k, xb, xk, w, tmp):
    nc, P = k.nc, k.P
    sq, mean, rstd, m2 = tmp["sq"], tmp["mean"], tmp["rstd"], tmp["m2"]
    P.op("act", lambda e: e.activation(out=sq[:, :, :w], in_=xb[:, :, :w], func=AF.Square), reads=[xk], writes=["sq"])
    p1, k1 = psn(k)
    p2, k2 = psn(k)
    for kk in range(8):
        P.op("pe", lambda e, kk=kk: e.matmul(p1[:, :w], k.onesm[:], xb[:, kk, :w], start=(kk == 0), stop=(kk == 7)), reads=[xk, "onesm"], writes=[k1])
    for kk in range(8):
        P.op("pe", lambda e, kk=kk: e.matmul(p2[:, :w], k.onesm[:], sq[:, kk, :w], start=(kk == 0), stop=(kk == 7)), reads=["sq", "onesm"], writes=[k2])
    P.op("act", lambda e: e.copy(mean[:, :w], p1[:, :w]), reads=[k1], writes=["mean"])
    P.op("dve", lambda e: e.tensor_tensor(m2[:, :w], mean[:, :w], mean[:, :w], ALU.mult), reads=["mean"], writes=["m2"])
    P.op("dve", lambda e: e.tensor_tensor(m2[:, :w], p2[:, :w], m2[:, :w], ALU.subtract), reads=[k2, "m2"], writes=["m2"])
    P.op("act", lambda e: e.activation(out=m2[:, :w], in_=m2[:, :w], func=AF.Sqrt, bias=k.epsc[:], scale=1.0), reads=["m2", "epsc"], writes=["m2"])
    P.op("dve", lambda e: e.reciprocal(rstd[:, :w], m2[:, :w]), reads=["m2"], writes=["rstd"])


def ln_tmp(nc, st, W=512):
    return {
        "sq": st.enter_context(nc.sbuf_tensor(U("ln_sq"), [128, 8, W], F32)),
        "mean": st.enter_context(nc.sbuf_tensor(U("ln_mean"), [128, W], F32)),
        "rstd": st.enter_context(nc.sbuf_tensor(U("ln_rstd"), [128, W], F32)),
        "m2": st.enter_context(nc.sbuf_tensor(U("ln_m2"), [128, W], F32)),
        "t": st.enter_context(nc.sbuf_tensor(U("ln_t"), [128, W], F32)),
        "tr": Rot(nc, st, "ln_tr", 3, [128, W], F32),
    }


def ln_apply(k, xb, xk, w, tmp, kk, out, okeys, scale_ap, bias_ap, extra_reads=()):
    P = k.P
    t, tk = tmp["tr"].next()
    P.op("dve", lambda e: e.tensor_tensor(t[:, :w], xb[:, kk, :w], tmp["mean"][:, :w], ALU.subtract), reads=[xk, "mean"], writes=[tk])
    P.op("dve", lambda e: e.tensor_tensor(t[:, :w], t[:, :w], tmp["rstd"][:, :w], ALU.mult), reads=[tk, "rstd"], writes=[tk])
    P.op("act", lambda e: e.activation(out=out, in_=t[:, :w], func=AF.Identity, scale=scale_ap, bias=bias_ap), reads=[tk, "mod"] + list(extra_reads), writes=okeys)


def stage_ln_win(k, li):
    nc, P = k.nc, k.P
    with ExitStack() as st:
        hT = st.enter_context(nc.sbuf_tensor(U("hT"), [128, 8, NT], BF16))
        with ExitStack() as st2:
            tmp = ln_tmp(nc, st2)
            xbr = Rot(nc, st2, "xb", 2, [128, 8, 512], F32)
            xTv = k.xT.rearrange("(k p) t -> p k t", p=128)
            for (t0, w, isc) in TBS:
                xb, xk = xbr.next()
                P.dma("sp", xb[:, :, :w], xTv[:, :, t0:t0 + w], reads=["xT"], writes=[xk])
                ln_stats(k, xb, xk, w, tmp)
                for kk in range(8):
                    ln_apply(k, xb, xk, w, tmp, kk, hT[:, kk, t0:t0 + w], ["hT%d" % kk],
                             k.mod[:, li, 8 + kk, isc:isc + 1], k.mod[:, li, 0 + kk, isc:isc + 1])
            P.barrier()
        wr = Rot(nc, st, "wr", 3, [128, 8, 128], BF16)
        zs = Rot(nc, st, "zs", 3, [128, NT], BF16)
        vt = st.enter_context(nc.sbuf_tensor(U("vt"), [128, 18, 128], BF16))
        wv = k.w_inx[li].rearrange("(k p) n -> p k n", p=128)
        ev = 0
        for m in range(NZT):
            wt, wk = wr.next()
            P.dma("pool", wt[:], wv[:, :, m * 128:(m + 1) * 128], writes=[wk])
            zt, zk = zs.next()
            mrows = 64 if m == ZT_KRX else 128
            for (t0, w, isc) in TBS:
                pt, pk = psn(k)
                for kk in range(8):
                    P.op("pe", lambda e, pt=pt, wt=wt, kk=kk, t0=t0, w=w, mrows=mrows: e.matmul(pt[:mrows, :w], wt[:, kk, :mrows], hT[:, kk, t0:t0 + w], start=(kk == 0), stop=(kk == 7)),
                         reads=[wk, "hT%d" % kk], writes=[pk])
                gate = ZT_G <= m < ZT_G + 24
                if gate:
                    P.op("act", lambda e, pt=pt, zt=zt, t0=t0, w=w: e.activation(out=zt[:, t0:t0 + w], in_=pt[:, :w], func=AF.Sigmoid), reads=[pk], writes=[zk + "_%d" % t0])
                elif ev % 2 == 0:
                    P.op("act", lambda e, pt=pt, zt=zt, t0=t0, w=w, mrows=mrows: e.copy(zt[:mrows, t0:t0 + w], pt[:mrows, :w]), reads=[pk], writes=[zk + "_%d" % t0])
                else:
                    P.op("dve", lambda e, pt=pt, zt=zt, t0=t0, w=w, mrows=mrows: e.tensor_copy(zt[:mrows, t0:t0 + w], pt[:mrows, :w]), reads=[pk], writes=[zk + "_%d" % t0])
                ev += 1
            P.dma("sp", k.zT[m * 128:m * 128 + mrows, :], zt[:mrows, :], reads=[zk + "_%d" % t[0] for t in TBS], writes=["zT"])
            if m == ZT_WV:
                for tt in range(18):
                    pt, pk = psn(k)
                    for kk in range(8):
                        P.op("pe", lambda e, pt=pt, wt=wt, kk=kk, tt=tt: e.matmul(pt[:, :128], hT[:, kk, tt * 128:(tt + 1) * 128], wt[:, kk, :], start=(kk == 0), stop=(kk == 7)),
                             reads=[wk, "hT%d" % kk], writes=[pk])
                    P.op("dve", lambda e, pt=pt, tt=tt: e.tensor_copy(vt[:, tt, :], pt[:, :128]), reads=[pk], writes=["vt"])
                P.dma("sp", k.vtok.rearrange("(t p) c -> p t c", p=128), vt[:], reads=["vt"], writes=["vtok"])
        stage_end(k)


def stage_s5(k, li):
    nc, P = k.nc, k.P
    TWO_PI = float(2 * np.pi)
    with ExitStack() as st:
        sb = lambda name, shape, dt=F32: st.enter_context(nc.sbuf_tensor(U(name), shape, dt))
        lane = sb("lane", [128, 3, 32])
        dg = sb("dg", [128, 2, 4])
        BW = sb("BW", [128, 64, 128], BF16)
        CW = sb("CW", [128, 96, 128], BF16)
        tau = sb("tau", [128, NT])
        names = ["dt", "rho", "thn", "fr", "sn", "cs", "ar", "ai", "rden", "qr", "qi", "nqr", "nqi", "tA", "tB"]
        lp = {n: sb("lp_" + n, [128, 32]) for n in names}
        lpi = sb("lp_it", [128, 32], I32)
        P.dma("sp", lane[:], k.s5_lane[li], writes=["lane"])
        P.dma("sp", dg[:], k.s5_dg[li], writes=["dg"])
        P.dma("sp", tau[:], k.tau, writes=["tau"])
        bsrc = k.s5_bT[li].rearrange("d r t p c -> p (d r t) c")
        for j in range(8):
            P.dma("pool", BW[:, j * 8:(j + 1) * 8, :], bsrc[:, j * 8:(j + 1) * 8, :], writes=["BW"])
        P.op("pool", lambda e: e.memset(CW[:], 0.0), writes=["CW"])
        lre, lim, ldt = lane[:, 0, :], lane[:, 1, :], lane[:, 2, :]
        R = ["lane", "lp"]
        W = ["lp"]
        V = lambda fn: P.op("dve", fn, reads=R, writes=W)
        A = lambda fn: P.op("act", fn, reads=R + ["halfpi"], writes=W)
        A(lambda e: e.activation(out=lp["dt"][:], in_=ldt, func=AF.Exp))
        V(lambda e: e.tensor_tensor(lp["tA"][:], lre, lp["dt"][:], ALU.mult))
        A(lambda e: e.activation(out=lp["rho"][:], in_=lp["tA"][:], func=AF.Exp))
        V(lambda e: e.tensor_tensor(lp["thn"][:], lim, lp["dt"][:], ALU.mult))
        V(lambda e: e.tensor_scalar(lp["thn"][:], lp["thn"][:], float(1.0 / TWO_PI), None, ALU.mult))
        V(lambda e: e.tensor_copy(lpi[:], lp["thn"][:]))
        V(lambda e: e.tensor_copy(lp["tB"][:], lpi[:]))
        V(lambda e: e.tensor_tensor(lp["fr"][:], lp["thn"][:], lp["tB"][:], ALU.subtract))
        A(lambda e: e.activation(out=lp["sn"][:], in_=lp["fr"][:], func=AF.Sin, scale=TWO_PI))
        A(lambda e: e.activation(out=lp["fr"][:], in_=lp["fr"][:], func=AF.Abs))
        A(lambda e: e.activation(out=lp["cs"][:], in_=lp["fr"][:], func=AF.Sin, scale=-TWO_PI, bias=k.halfpi[:]))
        V(lambda e: e.tensor_tensor(lp["ar"][:], lp["rho"][:], lp["cs"][:], ALU.mult))
        V(lambda e: e.tensor_tensor(lp["ai"][:], lp["rho"][:], lp["sn"][:], ALU.mult))
        V(lambda e: e.tensor_tensor(lp["tA"][:], lre, lre, ALU.mult))
        V(lambda e: e.tensor_tensor(lp["tB"][:], lim, lim, ALU.mult))
        V(lambda e: e.tensor_tensor(lp["tA"][:], lp["tA"][:], lp["tB"][:], ALU.add))
        V(lambda e: e.reciprocal(lp["rden"][:], lp["tA"][:]))
        V(lambda e: e.tensor_scalar_add(lp["ar"][:], lp["ar"][:], -1.0))
        V(lambda e: e.tensor_tensor(lp["tA"][:], lp["ar"][:], lre, ALU.mult))
        V(lambda e: e.tensor_tensor(lp["tB"][:], lp["ai"][:], lim, ALU.mult))
        V(lambda e: e.tensor_tensor(lp["tA"][:], lp["tA"][:], lp["tB"][:], ALU.add))
        V(lambda e: e.tensor_tensor(lp["qr"][:], lp["tA"][:], lp["rden"][:], ALU.mult))
        V(lambda e: e.tensor_tensor(lp["tA"][:], lp["ai"][:], lre, ALU.mult))
        V(lambda e: e.tensor_tensor(lp["tB"][:], lp["ar"][:], lim, ALU.mult))
        V(lambda e: e.tensor_tensor(lp["tA"][:], lp["tA"][:], lp["tB"][:], ALU.subtract))
        V(lambda e: e.tensor_tensor(lp["qi"][:], lp["tA"][:], lp["rden"][:], ALU.mult))
        V(lambda e: e.tensor_scalar(lp["nqr"][:], lp["qr"][:], -1.0, None, ALU.mult))
        V(lambda e: e.tensor_scalar(lp["nqi"][:], lp["qi"][:], -1.0, None, ALU.mult))
        for n_ in ("rho", "thn", "sn", "cs", "qr", "qi", "dt"):
            dump(k, "lp_" + n_, lp[n_][:], [128, 32], F32, ["lp"])
        stC = ExitStack()
        craw = stC.enter_context(nc.sbuf_tensor(U("craw"), [128, 2, 16, 2, 16], F32))
        ctmp = stC.enter_context(nc.sbuf_tensor(U("ctmp"), [128, 16], F32))
        P.dma("sp", craw[:], k.s5_cL[li], writes=["craw"])
        for d in range(2):
            for lt in range(16):
                col = d * 16 + lt
                cr, ci = craw[:, d, lt, 0, :], craw[:, d, lt, 1, :]
                for half in range(2):
                    g = 2 * lt + half
                    gl = g % 8
                    ps_ = slice(half * 64, half * 64 + 64)
                    for ri in range(3):
                        s1 = lp["qi"] if ri != 1 else lp["nqr"]
                        s2 = (lp["qr"], lp["nqi"], lp["nqr"])[ri]
                        op1 = (ALU.subtract, ALU.add, ALU.add)[ri]
                        P.op("dve", lambda e, ci=ci, s1=s1, col=col, ps_=ps_: e.tensor_scalar(ctmp[ps_, :], ci[ps_, :], s1[ps_, col:col + 1], None, ALU.mult), reads=["craw", "lp"], writes=["ctmp"])
                        P.op("dve", lambda e, cr=cr, s2=s2, col=col, ps_=ps_, ri=ri, gl=gl, op1=op1: e.scalar_tensor_tensor(CW[ps_, col * 3 + ri, gl * 16:(gl + 1) * 16], cr[ps_, :], s2[ps_, col:col + 1], ctmp[ps_, :], ALU.mult, op1), reads=["craw", "lp", "ctmp"], writes=["CW"])
        P.barrier()
        stC.close()
        ut = sb("ut", [128, NT], BF16)
        gT = sb("s5g", [128, 4, NT], BF16)
        with ExitStack() as stU:
            sbu = lambda name, shape, dt=F32: stU.enter_context(nc.sbuf_tensor(U(name), shape, dt))
            it = sbu("s5it", [128, NT], I32)
            fr = sbu("s5fr", [128, NT])
            SnR = Rot(nc, stU, "s5S", 2, [128, NT], BF16)
            CsR = Rot(nc, stU, "s5C", 2, [128, NT], BF16)
            br = sbu("s5br", [128, NT], BF16)
            bi = sbu("s5bi", [128, NT], BF16)
            p1 = sbu("s5p1", [128, NT], BF16)
            p2 = sbu("s5p2", [128, NT], BF16)
            p3 = sbu("s5p3", [128, NT], BF16)
            wr = sbu("s5wr", [128, NT], BF16)
            wi = sbu("s5wi", [128, NT], BF16)
            zrR = Rot(nc, stU, "s5zr", 2, [128, NT], BF16)
            ziR = Rot(nc, stU, "s5zi", 2, [128, NT], BF16)
            qR = [Rot(nc, stU, "s5q%d" % j, 2, [128, NT], BF16) for j in range(4)]
            ysr = Rot(nc, stU, "s5ys", 2, [128, 512], F32)
            segs = [(0, LC), (LC, NT)]
            for gt in range(4):
                P.dma("sp", ut[:], k.zT[gt * 128:(gt + 1) * 128, :], reads=["zT"], writes=["ut"])
                for d in range(2):
                    for l4 in range(4):
                        lt = gt * 4 + l4
                        col = d * 16 + lt
                        thn = lp["thn"][:, col:col + 1]
                        first = (d == 0 and l4 == 0)
                        last = (d == 1 and l4 == 3)
                        Sn, Snk = SnR.next()
                        Cs, Csk = CsR.next()
                        zr, zrk = zrR.next()
                        zi, zik = ziR.next()
                        qs = [r_.next() for r_ in qR]
                        for (a_, b_) in segs:
                            src = tau[:, a_:b_] if d == 0 else (tau[:, b_ - 1::-1] if a_ == 0 else tau[:, b_ - 1:a_ - 1:-1])
                            P.op("dve", lambda e, src=src, a_=a_, b_=b_, thn=thn: e.tensor_scalar(it[:, a_:b_], src, thn, None, ALU.mult), reads=["tau", "lp"], writes=["it"])
                            P.op("dve", lambda e, src=src, a_=a_, b_=b_, thn=thn: e.scalar_tensor_tensor(fr[:, a_:b_], src, thn, it[:, a_:b_], ALU.mult, ALU.subtract), reads=["tau", "lp", "it"], writes=["fr"])
                        P.op("act", lambda e, Sn=Sn: e.activation(out=Sn[:], in_=fr[:], func=AF.Sin, scale=TWO_PI), reads=["fr"], writes=[Snk])
                        P.op("act", lambda e: e.activation(out=fr[:], in_=fr[:], func=AF.Abs), reads=["fr"], writes=["fr"])
                        P.op("act", lambda e, Cs=Cs: e.activation(out=Cs[:], in_=fr[:], func=AF.Sin, scale=-TWO_PI, bias=k.halfpi[:]), reads=["fr", "halfpi"], writes=[Csk])
                        for (t0, w, isc) in TBS:
                            pr, kr = psn(k, 5, 8)
                            pi_, ki = psn(k, 5, 8)
                            sl = slice(t0, t0 + w)
                            P.op("pe", lambda e, pr=pr, sl=sl, w=w, d=d, lt=lt: e.matmul(pr[:, :w], BW[:, (d * 2 + 0) * 16 + lt, :], ut[:, sl], start=True, stop=True), reads=["BW", "ut"], writes=[kr])
                            P.op("pe", lambda e, pi_=pi_, sl=sl, w=w, d=d, lt=lt: e.matmul(pi_[:, :w], BW[:, (d * 2 + 1) * 16 + lt, :], ut[:, sl], start=True, stop=True), reads=["BW", "ut"], writes=[ki])
                            P.op("act", lambda e, pr=pr, sl=sl, w=w: e.copy(br[:, sl], pr[:, :w]), reads=[kr], writes=["br"])
                            P.op("act", lambda e, pi_=pi_, sl=sl, w=w: e.copy(bi[:, sl], pi_[:, :w]), reads=[ki], writes=["bi"])
                        P.op("dve", lambda e, Cs=Cs: e.tensor_tensor(p1[:], Cs[:], br[:], ALU.mult), reads=[Csk, "br"], writes=["p1"])
                        P.op("pool", lambda e, Sn=Sn: e.tensor_tensor(p2[:], Sn[:], bi[:], ALU.mult), reads=[Snk, "bi"], writes=["p2"])
                        P.op("pool", lambda e, Cs=Cs: e.tensor_tensor(p3[:], Cs[:], bi[:], ALU.mult), reads=[Csk, "bi"], writes=["p3"])
                        P.op("dve", lambda e: e.tensor_tensor(wr[:], p1[:], p2[:], ALU.add), reads=["p1", "p2"], writes=["wr"])
                        P.op("pool", lambda e, Sn=Sn: e.tensor_tensor(p2[:], Sn[:], br[:], ALU.mult), reads=[Snk, "br", "wr"], writes=["p2"])
                        P.op("pool", lambda e: e.tensor_tensor(wi[:], p3[:], p2[:], ALU.subtract), reads=["p3", "p2"], writes=["wi"])
                        rho = lp["rho"][:, col:col + 1]
                        for (src, dst, dk) in ((wr, zr, zrk), (wi, zi, zik)):
                            if d == 0:
                                P.op("dve", lambda e, src=src, dst=dst, rho=rho: e.tensor_tensor_scan(dst[:, 0:LC], rho.to_broadcast([128, LC]), src[:, 0:LC], 0.0, ALU.mult, ALU.add), reads=["wr", "wi", "lp"], writes=[dk])
                                P.op("dve", lambda e, src=src, dst=dst, rho=rho: e.tensor_tensor_scan(dst[:, LC:NT], rho.to_broadcast([128, L]), src[:, LC:NT], dst[:, LC - 1:LC], ALU.mult, ALU.add), reads=["wr", "wi", "lp", dk], writes=[dk])
                            else:
                                P.op("dve", lambda e, src=src, dst=dst, rho=rho: e.tensor_tensor_scan(dst[:, LC - 1::-1], rho.to_broadcast([128, LC]), src[:, LC - 1::-1], 0.0, ALU.mult, ALU.add), reads=["wr", "wi", "lp"], writes=[dk])
                                P.op("dve", lambda e, src=src, dst=dst, rho=rho: e.tensor_tensor_scan(dst[:, NT - 1:LC - 1:-1], rho.to_broadcast([128, L]), src[:, NT - 1:LC - 1:-1], dst[:, 0:1], ALU.mult, ALU.add), reads=["wr", "wi", "lp", dk], writes=[dk])
                        (q1, q1k), (q2, q2k), (q3, q3k), (q4, q4k) = qs
                        P.op("pool", lambda e, Cs=Cs, zr=zr, q1=q1: e.tensor_tensor(q1[:], Cs[:], zr[:], ALU.mult), reads=[Csk, zrk], writes=[q1k])
                        P.op("dve", lambda e, Sn=Sn, zi=zi, q2=q2: e.tensor_tensor(q2[:], Sn[:], zi[:], ALU.mult), reads=[Snk, zik], writes=[q2k])
                        P.op("pool", lambda e, Sn=Sn, zr=zr, q3=q3: e.tensor_tensor(q3[:], Sn[:], zr[:], ALU.mult), reads=[Snk, zrk], writes=[q3k])
                        P.op("dve", lambda e, Cs=Cs, zi=zi, q4=q4: e.tensor_tensor(q4[:], Cs[:], zi[:], ALU.mult), reads=[Csk, zik], writes=[q4k])
                        for bi_, (t0, w, isc) in enumerate(TBS):
                            sl = slice(t0, t0 + w)
                            yk = "ps%d" % bi_
                            for j_, (qq, qk, wsl) in enumerate(((q1, q1k, 0), (q2, q2k, 2), (q3, q3k, 1), (q4, q4k, 1))):
                                P.op("pe", lambda e, bi_=bi_, sl=sl, w=w, col=col, first=first, last=last, qq=qq, wsl=wsl, j_=j_: e.matmul(k.ps[bi_][:, :w], CW[:, col * 3 + wsl, :], qq[:, sl], start=(first and j_ == 0), stop=(last and j_ == 3)), reads=["CW", qk], writes=[yk])
                for bi_, (t0, w, isc) in enumerate(TBS):
                    sl = slice(t0, t0 + w)
                    ys, ysk = ysr.next()
                    P.op("dve", lambda e, bi_=bi_, sl=sl, w=w, ys=ys, gt=gt: e.scalar_tensor_tensor(ys[:, :w], ut[:, sl], dg[:, 0, gt:gt + 1], k.ps[bi_][:, :w], ALU.mult, ALU.add), reads=["ut", "dg", "ps%d" % bi_], writes=[ysk])
                    P.op("act", lambda e, sl=sl, w=w, ys=ys, gt=gt: e.activation(out=gT[:, gt, sl], in_=ys[:, :w], func=AF.Gelu_apprx_tanh), reads=[ysk], writes=["gT%d" % gt])
            P.barrier()
        wglu = sb("wglu", [128, 4, 512], BF16)
        P.dma("pool", wglu[:], k.s5_wglu[li].rearrange("(k p) n -> p k n", p=128), writes=["wglu"])
        so = Rot(nc, st, "s5o", 2, [128, NT], BF16)
        sgr = Rot(nc, st, "s5sg", 2, [128, 512], F32)
        for mo in range(4):
            ot, ok = so.next()
            for (t0, w, isc) in TBS:
                sl = slice(t0, t0 + w)
                pt, pk = psn(k)
                for kk in range(4):
                    P.op("pe", lambda e, pt=pt, kk=kk, sl=sl, w=w, mo=mo: e.matmul(pt[:, :w], wglu[:, kk, mo * 128:(mo + 1) * 128], gT[:, kk, sl], start=(kk == 0), stop=(kk == 3)), reads=["wglu", "gT%d" % kk], writes=[pk])
                sg, sgk = sgr.next()
                P.op("act", lambda e, pt=pt, w=w, sg=sg, mo=mo: e.activation(out=sg[:, :w], in_=pt[:, :w], func=AF.Sigmoid, bias=dg[:, 1, mo:mo + 1], scale=1.0), reads=[pk, "dg"], writes=[sgk])
                P.op("dve", lambda e, sg=sg, sl=sl, w=w, ot=ot, mo=mo: e.tensor_tensor(ot[:, sl], gT[:, mo, sl], sg[:, :w], ALU.mult), reads=[sgk, "gT%d" % mo], writes=[ok + "_%d" % t0])
            P.dma("sp", k.brT[0][mo * 128:(mo + 1) * 128, :], ot[:], reads=[ok + "_%d" % t[0] for t in TBS], writes=["s5T"])
        stage_end(k)


def rms_norm_T(k, st, src, nk, gains, dst, tag):
    nc, P = k.nc, k.P
    sq = st.enter_context(nc.sbuf_tensor(U("rms_sq"), [128, nk, 512], BF16))
    rinv = st.enter_context(nc.sbuf_tensor(U("rms_ri"), [128, 512], F32))
    for (t0, w, isc) in TBS:
        sl = slice(t0, t0 + w)
        P.op("act", lambda e, sl=sl, w=w: e.activation(out=sq[:, :, :w], in_=src[:, :, sl], func=AF.Square), reads=[tag + "src"], writes=[tag + "sq"])
        pt, pk = psn(k)
        for kk in range(nk):
            P.op("pe", lambda e, pt=pt, kk=kk, w=w: e.matmul(pt[:, :w], k.onesb[:, 0:128], sq[:, kk, :w], start=(kk == 0), stop=(kk == nk - 1)), reads=[tag + "sq", "onesb"], writes=[pk])
        P.op("act", lambda e, pt=pt, w=w: e.activation(out=rinv[:, :w], in_=pt[:, :w], func=AF.Sqrt, scale=float(1.0 / (nk * 128)), bias=k.epsc[:]), reads=[pk, "epsc"], writes=[tag + "ri"])
        P.op("dve", lambda e, w=w: e.reciprocal(rinv[:, :w], rinv[:, :w]), reads=[tag + "ri"], writes=[tag + "ri"])
        for kk in range(nk):
            P.op("dve", lambda e, kk=kk, sl=sl, w=w: e.scalar_tensor_tensor(dst[:, kk, sl], src[:, kk, sl], gains[:, kk:kk + 1], rinv[:, :w], ALU.mult, ALU.mult), reads=[tag + "src", tag + "ri", "mg"], writes=[tag + "dst"])


def softmax_pv(k, ost_rot, score_fn, nkc, va_fn, nq, scale, out_dram, tagp, PTr, esk=None, post=None):
    nc, P = k.nc, k.P
    po, pok = psn(k, 0, 3)
    LA = 3
    scr = {}

    def issue_score(kc):
        pscr, psk = psn(k, 3, 8)
        score_fn(kc, pscr, psk)
        scr[kc] = (pscr, psk)

    for kc in range(min(LA, nkc)):
        issue_score(kc)
    for kc in range(nkc):
        if kc + LA < nkc:
            issue_score(kc + LA)
        pscr, psk = scr.pop(kc)
        pt, ptk = PTr.next()
        P.op("act", lambda e, pscr=pscr, pt=pt: e.activation(out=pt[:, :nq], in_=pscr[:, :nq], func=AF.Exp, scale=scale), reads=[psk], writes=[ptk])
        if post is not None:
            post(kc, pt, ptk)
        va, vak = va_fn(kc)
        P.op("pe", lambda e, po=po, va=va, pt=pt, kc=kc: e.matmul(po[:, :nq], va, pt[:, :nq], start=(kc == 0), stop=(kc == nkc - 1)), reads=[vak, ptk], writes=[pok])
    rv, rvk = k.att_rv.next()
    if esk is not None:
        P.op("dve", lambda e, po=po, rv=rv: e.tensor_scalar(rv[0:64, :nq], po[64:128, :nq], esk, None, ALU.add), reads=[pok, "esk"], writes=[rvk])
        P.op("dve", lambda e, rv=rv: e.reciprocal(rv[0:64, :nq], rv[0:64, :nq]), reads=[rvk], writes=[rvk])
    else:
        P.op("dve", lambda e, po=po, rv=rv: e.reciprocal(rv[0:64, :nq], po[64:128, :nq]), reads=[pok], writes=[rvk])
    ot, otk = ost_rot.next()
    P.op("dve", lambda e, po=po, ot=ot, rv=rv: e.tensor_tensor(ot[0:64, :nq], po[0:64, :nq], rv[0:64, :nq], ALU.mult), reads=[pok, rvk], writes=[otk])
    P.dma("sp", out_dram, ot[0:64, :nq], reads=[otk], writes=[tagp])


def stage_mla(k, li):
    nc, P = k.nc, k.P
    SC = float(96 ** -0.5)
    with ExitStack() as st:
        sb = lambda name, shape, dt=F32: st.enter_context(nc.sbuf_tensor(U(name), shape, dt))
        mg = sb("mg", [128, 5])
        P.dma("sp", mg[:], k.mla_g[li], writes=["mg"])
        VA = sb("VA", [128, 18, 8, 128], BF16)
        KRb = sb("KRb", [128, NT], BF16)
        qn = sb("qn", [128, 3, NT], BF16)
        kvn = sb("kvn", [128, 2, NT], BF16)
        rope = sb("ropem", [128, 2, L])
        wuq = sb("wuq", [128, 3, 2048], BF16)
        wkk = sb("wkk", [128, 2, 1024], BF16)
        k.att_rv = Rot(nc, st, "att_rv", 2, [128, 512], F32)
        P.op("pool", lambda e: e.memset(VA[:], 1.0), writes=["VA"])
        P.dma("sp", rope[:], k.rope_mla.rearrange("c p t -> p c t"), writes=["rope"])
        for j in range(3):
            for c_ in range(4):
                P.dma("pool", wuq[:, j, c_ * 512:(c_ + 1) * 512], k.w_uqx[li][j * 128:(j + 1) * 128, c_ * 512:(c_ + 1) * 512], writes=["wuq"])
        for j in range(2):
            for c_ in range(2):
                P.dma("pool", wkk[:, j, c_ * 512:(c_ + 1) * 512], k.w_ukvk[li][j * 128:(j + 1) * 128, c_ * 512:(c_ + 1) * 512], writes=["wkk"])
        with ExitStack() as st2:
            sb2 = lambda name, shape, dt=F32: st2.enter_context(nc.sbuf_tensor(U(name), shape, dt))
            qa = sb2("qa", [128, 3, NT], BF16)
            kva = sb2("kva", [128, 2, NT], BF16)
            KP = sb2("KP", [128, NT], BF16)
            KS = sb2("KS", [128, L], BF16)
            wvv = sb2("wvv", [128, 2, 512], BF16)
            tA = sb2("mtA", [128, 512])
            tB = sb2("mtB", [128, 512])
            P.dma("sp", qa[:], k.zT[ZT_QA * 128:(ZT_QA + 3) * 128, :].rearrange("(k p) t -> p k t", p=128), reads=["zT"], writes=["qsrc"])
            P.dma("sp", kva[:], k.zT[ZT_KVA * 128:(ZT_KVA + 2) * 128, :].rearrange("(k p) t -> p k t", p=128), reads=["zT"], writes=["ksrc"])
            P.op("pool", lambda e: e.memset(KP[:], 0.0), writes=["KP"])
            P.op("pool", lambda e: e.memset(KS[:], 0.0), writes=["KS"])
            P.dma("sp", KP[64:96, :], k.zT[ZT_KRX * 128:ZT_KRX * 128 + 32, :], reads=["zT"], writes=["KP"])
            P.dma("sp", KS[64:96, :], k.zT[ZT_KRX * 128 + 32:ZT_KRX * 128 + 64, LC:NT], reads=["zT"], writes=["KS"])
            P.dma("pool", wvv[:], k.w_ukvv[li].rearrange("(k p) n -> p k n", p=128), writes=["wvv"])
            rms_norm_T(k, st2, qa, 3, mg[:, 0:3], qn, "q")
            rms_norm_T(k, st2, kva, 2, mg[:, 3:5], kvn, "k")
            P.op("dve", lambda e: e.tensor_copy(KRb[:, 0:LC], KP[:, 0:LC]), reads=["KP"], writes=["KRb"])
            for c in range(4):
                sl = slice(c * 512, (c + 1) * 512)
                sln = slice(LC + c * 512, LC + (c + 1) * 512)
                P.op("dve", lambda e, sl=sl, sln=sln: e.tensor_tensor(tA[:, :], KP[:, sln], rope[:, 0, sl], ALU.mult), reads=["KP", "rope"], writes=["mtA"])
                P.op("pool", lambda e, sl=sl: e.tensor_tensor(tB[:, :], KS[:, sl], rope[:, 1, sl], ALU.mult), reads=["KS", "rope"], writes=["mtB"])
                P.op("dve", lambda e, sln=sln: e.tensor_tensor(KRb[:, sln], tA[:, :], tB[:, :], ALU.add), reads=["mtA", "mtB"], writes=["KRb"])
            dump(k, "d_KP", KP[:], [128, NT], BF16, ["KP"])
            dump(k, "d_KS", KS[:], [128, L], BF16, ["KS"])
            dump(k, "d_KRb", KRb[:], [128, NT], BF16, ["KRb"])
            for tt in range(18):
                pt, pk = psn(k)
                for kk in range(2):
                    P.op("pe", lambda e, pt=pt, kk=kk, tt=tt: e.matmul(pt[:, :512], kvn[:, kk, tt * 128:(tt + 1) * 128], wvv[:, kk, :], start=(kk == 0), stop=(kk == 1)), reads=["wvv", "kdst"], writes=[pk])
                P.op("dve", lambda e, pt=pt, tt=tt: e.tensor_copy(VA[:, tt, :, 0:64], pt[:, :512].rearrange("p (h d) -> p h d", h=8)), reads=[pk], writes=["VA"])
            P.barrier()
        if "mla_stop1" in k.dbg:
            stage_end(k)
            return
        PTr = Rot(nc, st, "mPT", 4, [128, 512], BF16)
        ostr = Rot(nc, st, "most", 3, [128, 512], BF16)
        t1r = Rot(nc, st, "mt1", 2, [128, 512], F32)
        t2r = Rot(nc, st, "mt2", 2, [128, 512], F32)
        for grp in range(2):
            with ExitStack() as st3:
                QP = st3.enter_context(nc.sbuf_tensor(U("QP"), [128, 4, NT], BF16))
                QR = st3.enter_context(nc.sbuf_tensor(U("QR"), [128, 4, L], BF16))
                KH = st3.enter_context(nc.sbuf_tensor(U("KH"), [128, 4, NT], BF16))
                for hh in range(4):
                    h = grp * 4 + hh
                    for (t0, w, isc) in TBS:
                        sl = slice(t0, t0 + w)
                        pm, pmk = psn(k, 3, 8)
                        for kk in range(3):
                            P.op("pe", lambda e, pm=pm, kk=kk, sl=sl, w=w, h=h: e.matmul(pm[:, :w], wuq[:, kk, h * 128:(h + 1) * 128], qn[:, kk, sl], start=(kk == 0), stop=(kk == 2)), reads=["wuq", "qdst"], writes=[pmk])
                        P.op("act", lambda e, pm=pm, sl=sl, w=w, hh=hh: e.copy(QP[:, hh, sl], pm[:, :w]), reads=[pmk], writes=["QP%d" % hh])
                        if not isc:
                            ls = slice(t0 - LC, t0 - LC + w)
                            psw, pswk = psn(k, 3, 8)
                            for kk in range(3):
                                P.op("pe", lambda e, psw=psw, kk=kk, sl=sl, w=w, h=h: e.matmul(psw[:, :w], wuq[:, kk, (8 + h) * 128:(9 + h) * 128], qn[:, kk, sl], start=(kk == 0), stop=(kk == 2)), reads=["wuq", "qdst"], writes=[pswk])
                            t1, t1k = t1r.next()
                            t2, t2k = t2r.next()
                            P.op("dve", lambda e, pm=pm, ls=ls, w=w, t1=t1: e.tensor_tensor(t1[:, :w], pm[:, :w], rope[:, 0, ls], ALU.mult), reads=[pmk, "rope"], writes=[t1k])
                            P.op("dve", lambda e, psw=psw, ls=ls, w=w, t2=t2: e.tensor_tensor(t2[:, :w], psw[:, :w], rope[:, 1, ls], ALU.mult), reads=[pswk, "rope"], writes=[t2k])
                            P.op("pool", lambda e, ls=ls, w=w, hh=hh, t1=t1, t2=t2: e.tensor_tensor(QR[:, hh, ls], t1[:, :w], t2[:, :w], ALU.add), reads=[t1k, t2k], writes=["QR%d" % hh])
                        pk_, pkk = psn(k, 3, 8)
                        for kk in range(2):
                            P.op("pe", lambda e, pk_=pk_, kk=kk, sl=sl, w=w, h=h: e.matmul(pk_[:, :w], wkk[:, kk, h * 128:(h + 1) * 128], kvn[:, kk, sl], start=(kk == 0), stop=(kk == 1)), reads=["wkk", "kdst"], writes=[pkk])
                        P.op("dve", lambda e, pk_=pk_, sl=sl, w=w, hh=hh: e.tensor_tensor(KH[:, hh, sl], pk_[:, :w], KRb[:, sl], ALU.add), reads=[pkk, "KRb"], writes=["KH%d" % hh])
                if grp == 0:
                    dump(k, "d_wkk", wkk[:], [128, 2, 1024], BF16, ["wkk"])
                    dump(k, "d_QP", QP[:, 0, :], [128, NT], BF16, ["QP0"])
                    dump(k, "d_QR", QR[:, 0, :], [128, L], BF16, ["QR0"])
                    dump(k, "d_KH", KH[:, 0, :], [128, NT], BF16, ["KH0"])
                    dump(k, "d_VA", VA[:, :, 0, :], [128, 18, 128], BF16, ["VA"])
                if "mla_stop2" in k.dbg:
                    P.barrier()
                    continue
                for hh in range(4):
                    h = grp * 4 + hh
                    for qb in range(5):
                        if qb < 4:
                            q0, nq, nkc = LC + qb * 512, 512, 18
                        else:
                            q0, nq, nkc = 0, LC, 2

                        def score_fn(kc, pscr, psk, q0=q0, nq=nq, hh=hh):
                            ks = slice(kc * 128, (kc + 1) * 128)
                            if kc >= 2:
                                P.op("pe", lambda e: e.matmul(pscr[:, :nq], KH[:, hh, ks], QR[:, hh, q0 - LC:q0 - LC + nq], start=True, stop=True), reads=["KH%d" % hh, "QR%d" % hh], writes=[psk])
                            else:
                                P.op("pe", lambda e: e.matmul(pscr[:, :nq], KH[:, hh, ks], QP[:, hh, q0:q0 + nq], start=True, stop=True), reads=["KH%d" % hh, "QP%d" % hh], writes=[psk])

                        softmax_pv(k, ostr, score_fn, nkc, lambda kc, h=h: (VA[:, kc, h, :], "VA"), nq, SC,
                                   k.brT[1][h * 64:(h + 1) * 64, q0:q0 + nq], "mlaT", PTr)
                P.barrier()
        stage_end(k)


def stage_win(k, li):
    nc, P = k.nc, k.P
    SC = float(64 ** -0.5)
    with ExitStack() as st:
        sb = lambda name, shape, dt=F32: st.enter_context(nc.sbuf_tensor(U(name), shape, dt))
        Qp = sb("wQp", [128, 8, NT], BF16)
        Qr = sb("wQr", [128, 8, L], BF16)
        Kp = sb("wKp", [128, NT], BF16)
        Kr = sb("wKr", [128, L], BF16)
        VW = sb("wVW", [128, 18, 2, 128], BF16)
        rope = sb("wrope", [128, 2, L])
        msk = sb("wmsk", [128, 2, 128], BF16)
        esk = sb("wesk", [128, 8])
        Qsr = Rot(nc, st, "wQs", 2, [128, L], BF16)
        tAr = Rot(nc, st, "wtA", 2, [128, 512], F32)
        tBr = Rot(nc, st, "wtB", 2, [128, 512], F32)
        k.att_rv = Rot(nc, st, "watt_rv", 2, [128, 512], F32)
        P.op("pool", lambda e: e.memset(VW[:], 1.0), writes=["VW"])
        P.dma("sp", esk[:], k.win_sink[li], writes=["esk"])
        P.op("act", lambda e: e.activation(out=esk[:], in_=esk[:], func=AF.Exp), reads=["esk"], writes=["esk"])
        P.dma("sp", Qp[:], k.zT[ZT_WQ * 128:(ZT_WQ + 8) * 128, :].rearrange("(k p) t -> p k t", p=128), reads=["zT"], writes=["Qp"])
        P.dma("sp", Kp[:], k.zT[ZT_WK * 128:(ZT_WK + 1) * 128, :], reads=["zT"], writes=["Kp"])
        P.dma("sp", rope[:], k.rope_win.rearrange("c p t -> p c t"), writes=["rope"])
        P.dma("pool", msk[:], k.wmask.rearrange("c p t -> p c t"), writes=["msk"])
        for c_ in range(2):
            P.dma("sp", VW[:, :, c_, 0:64], k.vtok[:, c_ * 64:(c_ + 1) * 64].rearrange("(t p) d -> p t d", p=128), reads=["vtok", "VW"], writes=["VW"])
        for j in range(9):
            qs_, qsk = Qsr.next()
            srow = (ZT_WQS + j) * 128 if j < 8 else ZT_WKS * 128
            P.dma("sp", qs_[:], k.zT[srow:srow + 128, LC:NT], reads=["zT"], writes=[qsk])
            for c in range(4):
                sl = slice(c * 512, (c + 1) * 512)
                sln = slice(LC + c * 512, LC + (c + 1) * 512)
                src = Qp[:, j, sln] if j < 8 else Kp[:, sln]
                dst = Qr[:, j, sl] if j < 8 else Kr[:, sl]
                tA, tAk = tAr.next()
                tB, tBk = tBr.next()
                P.op("dve", lambda e, sl=sl, src=src, tA=tA: e.tensor_tensor(tA[:], src, rope[:, 0, sl], ALU.mult), reads=["Qp", "Kp", "rope"], writes=[tAk])
                P.op("pool", lambda e, sl=sl, qs_=qs_, tB=tB: e.tensor_tensor(tB[:], qs_[:, sl], rope[:, 1, sl], ALU.mult), reads=[qsk, "rope"], writes=[tBk])
                P.op("dve", lambda e, dst=dst, tA=tA, tB=tB: e.tensor_tensor(dst, tA[:], tB[:], ALU.add), reads=[tAk, tBk], writes=["Qr", "Kr"])
        PTr = Rot(nc, st, "wPT", 4, [128, 512], BF16)
        ostr = Rot(nc, st, "wost", 3, [128, 512], BF16)
        for h in range(8):
            kk = h // 4
            for n in range(17):
                if n < 16:
                    q0, nq = LC + n * 128, 128
                    chunks = [("b", n + d, d) for d in (-1, 0, 1) if 0 <= n + d < 16] + [("c", 0, 0), ("c", 1, 0)]
                else:
                    q0, nq = 0, LC
                    chunks = [("c", 0, 0), ("c", 1, 0)]

                def score_fn(kc, pscr, psk, chunks=chunks, q0=q0, nq=nq, h=h):
                    typ, ci, d = chunks[kc]
                    if typ == "b":
                        P.op("pe", lambda e: e.matmul(pscr[:, :nq], Kr[:, ci * 128:(ci + 1) * 128], Qr[:, h, q0 - LC:q0 - LC + nq], start=True, stop=True), reads=["Kr", "Qr"], writes=[psk])
                    else:
                        P.op("pe", lambda e: e.matmul(pscr[:, :nq], Kp[:, ci * 128:(ci + 1) * 128], Qp[:, h, q0:q0 + nq], start=True, stop=True), reads=["Kp", "Qp"], writes=[psk])

                def post(kc, pt, ptk, chunks=chunks):
                    typ, ci, d = chunks[kc]
                    if typ == "b" and d != 0:
                        mi = 0 if d == -1 else 1
                        P.op("dve", lambda e: e.tensor_tensor(pt[:, :128], pt[:, :128], msk[:, mi, :], ALU.mult), reads=[ptk, "msk"], writes=[ptk])

                def va_fn(kc, chunks=chunks, kk=kk):
                    typ, ci, d = chunks[kc]
                    tt = (2 + ci) if typ == "b" else ci
                    return VW[:, tt, kk, :], "VW"

                softmax_pv(k, ostr, score_fn, len(chunks), va_fn, nq, SC, k.brT[2][h * 64:(h + 1) * 64, q0:q0 + nq], "winT", PTr,
                           esk=esk[64:128, h:h + 1], post=post)
        stage_end(k)


def stage_merge(k, li):
    nc, P = k.nc, k.P
    with ExitStack() as st:
        sb = lambda name, shape, dt=F32: st.enter_context(nc.sbuf_tensor(U(name), shape, dt))
        wbr = sb("wbr", [128, 12, D], BF16)
        wout = sb("wout", [128, 8, D], BF16)
        lnp = sb("lnp", [128, 4, 8])
        k.lnp_t = lnp
        P.dma("sp", lnp[:], k.lnp[li], writes=["mod"])
        for j in range(12):
            P.dma("pool", wbr[:, j, :], k.w_branch[li][j * 128:(j + 1) * 128, :], writes=["wbr"])
        for j in range(8):
            P.dma("pool", wout[:, j, :], k.w_out[li][j * 128:(j + 1) * 128, :], writes=["wout"])
        tmp = ln_tmp(nc, st)
        obr = [sb("ob%d" % b, [128, 4, 512], BF16) for b in range(3)]
        gbr = [sb("gb%d" % b, [128, 8, 512], BF16) for b in range(3)]
        mT = sb("mT", [128, 8, 512], BF16)
        m1r = Rot(nc, st, "mgt1", 2, [128, 512], F32)
        m2r = Rot(nc, st, "mgt2", 3, [128, 512], F32)
        ytr = Rot(nc, st, "mgty", 2, [128, 512], F32)
        xb = sb("mxb", [128, 8, 512])
        rb = sb("mrb", [128, 8, 512])
        x1 = sb("mx1", [128, 8, 512])
        h2 = sb("mh2", [128, 8, 512], BF16)
        xTv = k.xT.rearrange("(k p) t -> p k t", p=128)
        h2v = k.h2T.rearrange("(k p) t -> p k t", p=128)
        for (t0, w, isc) in TBS:
            sl = slice(t0, t0 + w)
            for b in range(3):
                P.dma("sp", obr[b][:, :, :w], k.brT[b][:, sl].rearrange("(k p) t -> p k t", p=128), reads=["brT"], writes=["ob%d" % b])
                P.dma("sp", gbr[b][:, :, :w], k.zT[(ZT_G + 8 * b) * 128:(ZT_G + 8 + 8 * b) * 128, sl].rearrange("(k p) t -> p k t", p=128), reads=["zT"], writes=["gb%d" % b])
            P.dma("sp", xb[:, :, :w], xTv[:, :, sl], reads=["xT"], writes=["mxb"])
            for mo in range(8):
                mt1, m1k = m1r.next()
                for b in range(3):
                    pt, pk = psn(k)
                    for kk in range(4):
                        P.op("pe", lambda e, pt=pt, kk=kk, b=b, mo=mo, w=w: e.matmul(pt[:, :w], wbr[:, b * 4 + kk, mo * 128:(mo + 1) * 128], obr[b][:, kk, :w], start=(kk == 0), stop=(kk == 3)), reads=["wbr", "ob%d" % b], writes=[pk])
                    if b == 0:
                        P.op("dve", lambda e, pt=pt, mo=mo, w=w, mt1=mt1: e.tensor_tensor(mt1[:, :w], pt[:, :w], gbr[0][:, mo, :w], ALU.mult), reads=[pk, "gb0"], writes=[m1k])
                    elif b == 1:
                        mt2, m2k = m2r.next()
                        P.op("dve", lambda e, pt=pt, mo=mo, w=w, mt2=mt2: e.tensor_tensor(mt2[:, :w], pt[:, :w], gbr[1][:, mo, :w], ALU.mult), reads=[pk, "gb1"], writes=[m2k])
                        P.op("pool", lambda e, w=w, mt1=mt1, mt2=mt2: e.tensor_tensor(mt1[:, :w], mt1[:, :w], mt2[:, :w], ALU.add), reads=[m1k, m2k], writes=[m1k])
                    else:
                        mt2, m2k = m2r.next()
                        P.op("dve", lambda e, pt=pt, mo=mo, w=w, mt2=mt2: e.tensor_tensor(mt2[:, :w], pt[:, :w], gbr[2][:, mo, :w], ALU.mult), reads=[pk, "gb2"], writes=[m2k])
                        P.op("pool", lambda e, mo=mo, w=w, mt1=mt1, mt2=mt2: e.tensor_tensor(mT[:, mo, :w], mt1[:, :w], mt2[:, :w], ALU.add), reads=[m1k, m2k], writes=["mT"])
            for mo in range(8):
                pt, pk = psn(k)
                for kk in range(8):
                    P.op("pe", lambda e, pt=pt, kk=kk, mo=mo, w=w: e.matmul(pt[:, :w], wout[:, kk, mo * 128:(mo + 1) * 128], mT[:, kk, :w], start=(kk == 0), stop=(kk == 7)), reads=["wout", "mT"], writes=[pk])
                yt, ytk = ytr.next()
                P.op("act", lambda e, pt=pt, mo=mo, w=w, isc=isc, yt=yt: e.activation(out=yt[:, :w], in_=pt[:, :w], func=AF.Identity, scale=k.mod[:, li, 16 + mo, isc:isc + 1]), reads=[pk, "mod"], writes=[ytk])
                P.op("dve", lambda e, mo=mo, w=w, yt=yt: e.scalar_tensor_tensor(rb[:, mo, :w], xb[:, mo, :w], float(ALPHA), yt[:, :w], ALU.mult, ALU.add), reads=["mxb", ytk], writes=["mrb"])
            ln_stats(k, rb, "mrb", w, tmp)
            for kk in range(8):
                ln_apply(k, rb, "mrb", w, tmp, kk, x1[:, kk, :w], ["mx1"], lnp[:, 0, kk:kk + 1], lnp[:, 1, kk:kk + 1])
            P.dma("sp", xTv[:, :, sl], x1[:, :, :w], reads=["mx1"], writes=["xT"])
            ln_stats(k, x1, "mx1", w, tmp)
            for kk in range(8):
                ln_apply(k, x1, "mx1", w, tmp, kk, h2[:, kk, :w], ["mh2"], k.mod[:, li, 32 + kk, isc:isc + 1], k.mod[:, li, 24 + kk, isc:isc + 1])
            P.dma("sp", h2v[:, :, sl], h2[:, :, :w], reads=["mh2"], writes=["h2T"])
        stage_end(k)


TB9 = [(0, 256, 1)] + [(256 + i * 256, 256, 0) for i in range(8)]


def stage_moe(k, li):
    nc, P = k.nc, k.P
    with ExitStack() as st0:
        sb0 = lambda name, shape, dt=F32: st0.enter_context(nc.sbuf_tensor(U(name), shape, dt))
        lnp = sb0("elnp", [128, 4, 8])
        posm = sb0("eposm", [16, NT])
        sel = sb0("esel", [16, 16, 128])
        iop = sb0("eiop", [128, 3])
        P.dma("sp", lnp[:], k.lnp[li], writes=["mod"])
        P.dma("sp", sel[:], k.sel16, writes=["esel"])
        P.dma("sp", iop[:], k.iota_p3, writes=["eiop"])
        with ExitStack() as st1:
            sb1 = lambda name, shape, dt=F32: st1.enter_context(nc.sbuf_tensor(U(name), shape, dt))
            h2tok = sb1("eh2tok", [128, 18, D], BF16)
            posm_tok = sb1("eposmt", [128, 18, 16])
            gw_tok = sb1("egwt", [128, 18, 16], BF16)
            ios = sb1("eios", [128, 384])
            P.dma("sp", ios[:], k.iota_s, writes=["eios"])
            with ExitStack() as stA:
                sbA = lambda name, shape, dt=F32: stA.enter_context(nc.sbuf_tensor(U(name), shape, dt))
                h2 = sbA("eh2", [128, 8, NT], BF16)
                wr = sbA("ewr", [128, 8, 16], BF16)
                aff = sbA("eaff", [16, NT])
                wk_ = sbA("ewk", [16, NT])
                gw = sbA("egw", [16, NT])
                msk = sbA("emsk", [16, NT])
                m8 = sbA("em8", [16, 8])
                thr = sbA("ethr", [16, 2])
                P.dma("sp", h2[:], k.h2T.rearrange("(k p) t -> p k t", p=128), reads=["h2T"], writes=["eh2"])
                P.dma("pool", wr[:], k.w_router[li].rearrange("(k p) n -> p k n", p=128), writes=["ewr"])
                for (t0, w, isc) in TBS:
                    sl = slice(t0, t0 + w)
                    pt, pk = psn(k)
                    for kk in range(8):
                        P.op("pe", lambda e, pt=pt, kk=kk, sl=sl, w=w: e.matmul(pt[0:16, :w], wr[:, kk, :], h2[:, kk, sl], start=(kk == 0), stop=(kk == 7)), reads=["ewr", "eh2"], writes=[pk])
                    P.op("act", lambda e, pt=pt, sl=sl, w=w: e.activation(out=wk_[:, sl], in_=pt[0:16, :w], func=AF.Exp), reads=[pk], writes=["ewk"])
                    p2, k2 = psn(k)
                    P.op("pe", lambda e, p2=p2, sl=sl, w=w: e.matmul(p2[0:16, :w], k.onesf[0:16, 0:16], wk_[:, sl], start=True, stop=True), reads=["ewk", "onesf"], writes=[k2])
                    P.op("dve", lambda e, p2=p2, sl=sl, w=w: e.reciprocal(gw[:, sl], p2[0:16, :w]), reads=[k2], writes=["egw"])
                    P.op("dve", lambda e, sl=sl: e.tensor_tensor(aff[:, sl], wk_[:, sl], gw[:, sl], ALU.mult), reads=["ewk", "egw"], writes=["eaff"])
                P.op("dve", lambda e: e.tensor_copy(wk_[:], aff[:]), reads=["eaff"], writes=["ewk"])
                for si, (a_, b_, cap, off) in enumerate(((0, LC, 32, 256.0), (LC, NT, 256, 0.0))):
                    for r in range(cap // 8):
                        P.op("dve", lambda e, a_=a_, b_=b_: e.max(out=m8[:], in_=wk_[:, a_:b_]), reads=["ewk"], writes=["em8"])
                        if r < cap // 8 - 1:
                            P.op("dve", lambda e, a_=a_, b_=b_: e.match_replace(out=wk_[:, a_:b_], in_to_replace=m8[:], in_values=wk_[:, a_:b_], imm_value=-1.0), reads=["ewk", "em8"], writes=["ewk"])
                    P.op("dve", lambda e, si=si: e.tensor_copy(thr[:, si:si + 1], m8[:, 7:8]), reads=["em8"], writes=["ethr"])
                    P.op("dve", lambda e, a_=a_, b_=b_, si=si: e.tensor_scalar(msk[:, a_:b_], aff[:, a_:b_], thr[:, si:si + 1], None, ALU.is_ge), reads=["eaff", "ethr"], writes=["emsk"])
                    P.op("dve", lambda e, a_=a_, b_=b_: e.tensor_tensor(gw[:, a_:b_], aff[:, a_:b_], msk[:, a_:b_], ALU.mult), reads=["eaff", "emsk"], writes=["egw"])
                    P.op("dve", lambda e, a_=a_, b_=b_: e.tensor_tensor_scan(wk_[:, a_:b_], k.onesf[0:16, 0:1].to_broadcast([16, b_ - a_]), msk[:, a_:b_], 0.0, ALU.mult, ALU.add), reads=["emsk", "onesf", "ewk"], writes=["ewk"])
                    P.op("dve", lambda e, a_=a_, b_=b_, off=off: e.scalar_tensor_tensor(posm[:, a_:b_], wk_[:, a_:b_], float(off), msk[:, a_:b_], ALU.add, ALU.mult), reads=["ewk", "emsk"], writes=["eposm"])
                    P.op("dve", lambda e, a_=a_, b_=b_: e.tensor_scalar_add(posm[:, a_:b_], posm[:, a_:b_], -1.0), reads=["eposm"], writes=["eposm"])
                for (src, dst, skey, dkey) in ((posm, posm_tok, "eposm", "eposmt"), (gw, gw_tok, "egw", "egwt")):
                    pt, pk = psn(k)
                    for tt in range(18):
                        P.op("pe", lambda e, pt=pt, tt=tt, src=src: e.transpose(pt[:, tt * 16:(tt + 1) * 16], src[0:16, tt * 128:(tt + 1) * 128], k.identf[0:16, 0:16]), reads=[skey, "identf"], writes=[pk])
                    P.op("dve", lambda e, pt=pt, dst=dst: e.tensor_copy(dst[:], pt[:, 0:288].rearrange("p (t e) -> p t e", e=16)), reads=[pk], writes=[dkey])
                for tt in range(18):
                    pt, pk = psn(k)
                    ptb = pt[:].bitcast(BF16)
                    for kf in range(8):
                        P.op("pe", lambda e, ptb=ptb, kf=kf, tt=tt: e.transpose(ptb[:, kf * 128:(kf + 1) * 128], h2[:, kf, tt * 128:(tt + 1) * 128], k.identb[:]), reads=["eh2", "identb"], writes=[pk])
                    if tt % 2 == 0:
                        P.op("act", lambda e, ptb=ptb, tt=tt: e.copy(h2tok[:, tt, :], ptb[:, 0:1024]), reads=[pk], writes=["eh2tok"])
                    else:
                        P.op("dve", lambda e, ptb=ptb, tt=tt: e.tensor_copy(h2tok[:, tt, :], ptb[:, 0:1024]), reads=[pk], writes=["eh2tok"])
                P.barrier()
            mw = Rot(nc, st1, "emw", 32, [128, D], BF16)
            Sr = Rot(nc, st1, "eS", 2, [128, 18, 384], BF16)
            Xr = Rot(nc, st1, "eX", 2, [128, 8, 288], BF16)
            Ar = Rot(nc, st1, "eA", 2, [128, 8, 384], BF16)
            Yr = Rot(nc, st1, "eY", 2, [128, 3, D], BF16)
            sar = Rot(nc, st1, "esa", 2, [128, 288], F32)
            gsr = Rot(nc, st1, "egs", 2, [128, 3], F32)
            for j in range(2):
                At, Ak = Ar.next()
                P.op("pool", lambda e, At=At: e.memset(At[:], 0.0), writes=[Ak])
            ev = 0
            for ex in range(16):
                ws = {}
                for nm, src in (("g", k.w_gate), ("u", k.w_up), ("d", k.w_down)):
                    for kk in range(8):
                        t, tk = mw.next()
                        P.dma("pool", t[:], src[li, ex, kk * 128:(kk + 1) * 128, :], writes=[tk])
                        ws[(nm, kk)] = (t, tk)
                S, Sk = Sr.next()
                P.op("dve", lambda e, S=S, ex=ex: e.tensor_tensor(S[:], ios[:].unsqueeze(1).to_broadcast([128, 18, 384]), posm_tok[:, :, ex:ex + 1].to_broadcast([128, 18, 384]), ALU.is_equal), reads=["eios", "eposmt"], writes=[Sk])
                X, Xk = Xr.next()
                for ft in range(8):
                    pt, pk = psn(k, 0, 4)
                    for tt in range(18):
                        P.op("pe", lambda e, pt=pt, tt=tt, ft=ft, S=S: e.matmul(pt[:, :288], h2tok[:, tt, ft * 128:(ft + 1) * 128], S[:, tt, 0:288], start=(tt == 0), stop=(tt == 17)), reads=["eh2tok", Sk], writes=[pk])
                    if ev % 2 == 0:
                        P.op("act", lambda e, pt=pt, ft=ft, X=X: e.copy(X[:, ft, :], pt[:, :288]), reads=[pk], writes=[Xk])
                    else:
                        P.op("dve", lambda e, pt=pt, ft=ft, X=X: e.tensor_copy(X[:, ft, :], pt[:, :288]), reads=[pk], writes=[Xk])
                    ev += 1
                pg, pgk = psn(k, 0, 4)
                for st_ in range(3):
                    for tt in range(18):
                        P.op("pe", lambda e, pg=pg, tt=tt, st_=st_, S=S, ex=ex: e.matmul(pg[:, st_:st_ + 1], S[:, tt, st_ * 128:(st_ + 1) * 128], gw_tok[:, tt, ex:ex + 1], start=(tt == 0), stop=(tt == 17)), reads=[Sk, "egwt"], writes=[pgk])
                gs, gsk = gsr.next()
                P.op("dve", lambda e, pg=pg, gs=gs: e.tensor_copy(gs[:], pg[:, 0:3]), reads=[pgk], writes=[gsk])
                At, Ak = Ar.next()
                for fo in range(8):
                    pa, pak = psn(k, 4, 6)
                    pu, puk = psn(k, 6, 8)
                    for kk in range(8):
                        wt, wtk = ws[("g", kk)]
                        P.op("pe", lambda e, pa=pa, wt=wt, kk=kk, fo=fo, X=X: e.matmul(pa[:, :288], wt[:, fo * 128:(fo + 1) * 128], X[:, kk, :], start=(kk == 0), stop=(kk == 7)), reads=[wtk, Xk], writes=[pak])
                    for kk in range(8):
                        wt, wtk = ws[("u", kk)]
                        P.op("pe", lambda e, pu=pu, wt=wt, kk=kk, fo=fo, X=X: e.matmul(pu[:, :288], wt[:, fo * 128:(fo + 1) * 128], X[:, kk, :], start=(kk == 0), stop=(kk == 7)), reads=[wtk, Xk], writes=[puk])
                    s_, sk_ = sar.next()
                    P.op("act", lambda e, pa=pa, s_=s_: e.activation(out=s_[:], in_=pa[:, :288], func=AF.Silu), reads=[pak], writes=[sk_])
                    P.op("dve", lambda e, pu=pu, s_=s_, At=At, fo=fo: e.tensor_tensor(At[:, fo, 0:288], pu[:, :288], s_[:], ALU.mult), reads=[puk, sk_], writes=[Ak])
                Y, Yk = Yr.next()
                for st_ in range(3):
                    for half in range(2):
                        py, pyk = psn(k, 0, 4)
                        for kk in range(8):
                            wt, wtk = ws[("d", kk)]
                            P.op("pe", lambda e, py=py, wt=wt, kk=kk, st_=st_, half=half, At=At: e.matmul(py[:, :512], At[:, kk, st_ * 128:(st_ + 1) * 128], wt[:, half * 512:(half + 1) * 512], start=(kk == 0), stop=(kk == 7)), reads=[wtk, Ak], writes=[pyk])
                        P.op("act", lambda e, py=py, st_=st_, half=half, Y=Y, gs=gs: e.activation(out=Y[:, st_, half * 512:(half + 1) * 512], in_=py[:, :512], func=AF.Identity, scale=gs[:, st_:st_ + 1]), reads=[pyk, gsk], writes=[Yk])
                P.dma("sp", k.yg[ex].rearrange("s p d -> p s d"), Y[:], reads=[Yk], writes=["yg"])
            P.barrier()
        with ExitStack() as st2:
            sb2 = lambda name, shape, dt=F32: st2.enter_context(nc.sbuf_tensor(U(name), shape, dt))
            Yall = sb2("eYall", [128, 48, D], BF16)
            for ex in range(16):
                P.dma("sp", Yall[:, ex * 3:(ex + 1) * 3, :], k.yg[ex].rearrange("s p d -> p s d"), reads=["yg"], writes=["eYall"])
            STr = Rot(nc, st2, "eST", 1, [128, 48, 256], BF16)
            tmp = ln_tmp(nc, st2, 256)
            xbr = Rot(nc, st2, "exb", 2, [128, 8, 256], F32)
            rb = sb2("erb", [128, 8, 256])
            xTv = k.xT.rearrange("(k p) t -> p k t", p=128)
            for (t0, w, isc) in TB9:
                sl = slice(t0, t0 + w)
                xb, xbk = xbr.next()
                P.dma("sp", xb[:], xTv[:, :, sl], reads=["xT"], writes=[xbk])
                ST, STk = STr.next()
                for ex in range(16):
                    pb, pbk = psn(k, 4, 8)
                    P.op("pe", lambda e, pb=pb, sl=sl, ex=ex: e.matmul(pb[:, :256], sel[:, ex, :], posm[:, sl], start=True, stop=True), reads=["esel", "eposm"], writes=[pbk])
                    P.op("dve", lambda e, pb=pb, ex=ex, ST=ST: e.tensor_tensor(ST[:, ex * 3:(ex + 1) * 3, :], pb[:, 0:256].unsqueeze(1).to_broadcast([128, 3, 256]), iop[:].unsqueeze(2).to_broadcast([128, 3, 256]), ALU.is_equal), reads=[pbk, "eiop"], writes=[STk])
                for mo in range(8):
                    pf, pfk = psn(k, 0, 4)
                    for j in range(48):
                        P.op("pe", lambda e, pf=pf, j=j, mo=mo, ST=ST: e.matmul(pf[:, :256], Yall[:, j, mo * 128:(mo + 1) * 128], ST[:, j, :], start=(j == 0), stop=(j == 47)), reads=["eYall", STk], writes=[pfk])
                    ft_, ftk = tmp["tr"].next()
                    P.op("act", lambda e, pf=pf, mo=mo, isc=isc, ft_=ft_: e.activation(out=ft_[:, :256], in_=pf[:, :256], func=AF.Identity, scale=k.mod[:, li, 40 + mo, isc:isc + 1]), reads=[pfk, "mod"], writes=[ftk])
                    P.op("dve", lambda e, mo=mo, xb=xb, ft_=ft_: e.scalar_tensor_tensor(rb[:, mo, :], xb[:, mo, :], float(ALPHA), ft_[:, :256], ALU.mult, ALU.add), reads=[xbk, ftk], writes=["erb"])
                ln_stats(k, rb, "erb", w, tmp)
                for kk in range(8):
                    ln_apply(k, rb, "erb", w, tmp, kk, xb[:, kk, :], [xbk], lnp[:, 2, kk:kk + 1], lnp[:, 3, kk:kk + 1])
                P.dma("sp", xTv[:, :, sl], xb[:], reads=[xbk], writes=["xT"])
        stage_end(k)


def stage_out(k):
    nc, P = k.nc, k.P
    with ExitStack() as st:
        xr = Rot(nc, st, "oxr", 2, [128, 8, 128], F32)
        xo = Rot(nc, st, "oxo", 2, [128, D], F32)
        xTv = k.xT.rearrange("(k p) t -> p k t", p=128)
        for tt in range(L // 128):
            xt, xk = xr.next()
            P.dma("sp", xt[:], xTv[:, :, LC + tt * 128:LC + (tt + 1) * 128], reads=["xT"], writes=[xk])
            ot, ok = xo.next()
            for half in range(2):
                pt, pk = psn(k)
                for kk in range(4):
                    kf = half * 4 + kk
                    P.op("pe", lambda e, pt=pt, kk=kk, kf=kf, xt=xt: e.transpose(pt[:, kk * 128:(kk + 1) * 128], xt[:, kf, :], k.identf[:]),
                         reads=[xk, "identf"], writes=[pk])
                if half == 0:
                    P.op("act", lambda e, pt=pt, ot=ot: e.copy(ot[:, 0:512], pt[:]), reads=[pk], writes=[ok + "h0"])
                else:
                    P.op("dve", lambda e, pt=pt, ot=ot: e.tensor_copy(ot[:, 512:1024], pt[:]), reads=[pk], writes=[ok + "h1"])
            P.dma("sp", k.out[tt * 128:(tt + 1) * 128, :], ot[:], reads=[ok + "h0", ok + "h1"], writes=["out"])
        stage_end(k)


Z_ORDER = None


def _zcols():
    u = np.arange(0, 512)
    qa = np.arange(512, 896)
    kva = np.arange(896, 1152)
    kr = np.arange(1152, 1184)
    wq = np.arange(1184, 1696)
    wk = np.arange(1696, 1824)
    wv = np.arange(1824, 1952)
    gates = np.arange(1952, 5024)
    Z = -np.ones(64, np.int64)

    def padq(cols8):
        out = []
        for h in range(8):
            kk = h // 4
            out.append(np.concatenate([cols8[h], Z]) if kk == 0 else np.concatenate([Z, cols8[h]]))
        return np.concatenate(out)
    wq8 = wq.reshape(8, 64)
    wq8s = wq.reshape(8, 2, 32)[:, ::-1, :].reshape(8, 64)
    wk_sw = wk.reshape(2, 2, 32)[:, ::-1, :].reshape(-1)
    kr_sw = kr.reshape(2, 16)[::-1].reshape(-1)
    cols = np.concatenate([u, qa, kva, padq(wq8), wk, wv, gates, padq(wq8s), wk_sw, kr, kr_sw, Z])
    assert cols.size == NZT * 128, cols.size
    return cols


def prep_shared(inp):
    f = lambda a: np.ascontiguousarray(np.asarray(a, np.float32))
    sh = {}
    sh["ident"] = np.eye(128, dtype=np.float32)
    sh["w_ada"] = f(inp["w_ada"])
    b = f(inp["b_ada"]).reshape(DEPTH, 48, 128).transpose(0, 2, 1)
    sh["bada2"] = f(np.repeat(b[:, :, :, None], 2, axis=3))
    cols = _zcols()
    wx = f(inp["w_in"])[:, :, np.maximum(cols, 0)]
    wx[:, :, cols < 0] = 0.0
    sh["w_inx"] = f(wx)
    G, PS, HG = 32, 64, 16
    bT = np.zeros((DEPTH, 2, 2, 16, 128, 128), np.float32)
    cL = np.zeros((DEPTH, 128, 2, 16, 2, 16), np.float32)
    lane = np.zeros((DEPTH, 128, 3, 32), np.float32)
    for ri, nm in enumerate(("s5_b_re", "s5_b_im")):
        bsrc = f(inp[nm])
        for lt in range(16):
            for half in range(2):
                g = 2 * lt + half
                gl = g % 8
                bT[:, :, ri, lt, gl * 16:(gl + 1) * 16, half * 64:(half + 1) * 64] = bsrc[:, :, g].transpose(0, 1, 3, 2)
    for ri, nm in enumerate(("s5_c_re", "s5_c_im")):
        csrc = f(inp[nm])
        for lt in range(16):
            for half in range(2):
                g = 2 * lt + half
                cL[:, half * 64:(half + 1) * 64, :, lt, ri, :] = csrc[:, :, g].transpose(0, 3, 1, 2)
    lre, lim, ldt = f(inp["s5_lam_re"]), f(inp["s5_lam_im"]), f(inp["s5_log_dt"])
    for d in range(2):
        for lt in range(16):
            for half in range(2):
                g = 2 * lt + half
                lane[:, half * 64:(half + 1) * 64, 0, d * 16 + lt] = lre[:, d, g, :]
                lane[:, half * 64:(half + 1) * 64, 1, d * 16 + lt] = lim[:, d, g, :]
                lane[:, half * 64:(half + 1) * 64, 2, d * 16 + lt] = ldt[:, d, g][:, None]
    sh["s5_bT"], sh["s5_cL"], sh["s5_lane"] = bT, cL, lane
    dg = np.zeros((DEPTH, 128, 2, 4), np.float32)
    dg[:, :, 0, :] = f(inp["s5_d"]).reshape(DEPTH, 4, 128).transpose(0, 2, 1)
    dg[:, :, 1, :] = f(inp["s5_b_glu"]).reshape(DEPTH, 4, 128).transpose(0, 2, 1)
    sh["s5_dg"] = dg
    sh["s5_wglu"] = f(inp["s5_w_glu"])
    mg = np.zeros((DEPTH, 128, 5), np.float32)
    mg[:, :, 0:3] = f(inp["mla_q_norm"]).reshape(DEPTH, 3, 128).transpose(0, 2, 1)
    mg[:, :, 3:5] = f(inp["mla_kv_norm"]).reshape(DEPTH, 2, 128).transpose(0, 2, 1)
    sh["mla_g"] = mg
    wuq = f(inp["mla_w_uq"]).reshape(DEPTH, 384, 8, 96)
    wm = np.zeros((DEPTH, 384, 8, 128), np.float32)
    wsw = np.zeros((DEPTH, 384, 8, 128), np.float32)
    wm[..., 0:96] = wuq
    wsw[..., 64:96] = wuq[..., 64:].reshape(DEPTH, 384, 8, 2, 16)[:, :, :, ::-1, :].reshape(DEPTH, 384, 8, 32)
    sh["w_uqx"] = f(np.concatenate([wm.reshape(DEPTH, 384, 1024), wsw.reshape(DEPTH, 384, 1024)], -1))
    wkv = f(inp["mla_w_ukv"]).reshape(DEPTH, 256, 8, 128)
    wkp = np.zeros((DEPTH, 256, 8, 128), np.float32)
    wkp[..., 0:64] = wkv[..., :64]
    sh["w_ukvk"] = f(wkp.reshape(DEPTH, 256, 1024))
    sh["w_ukvv"] = f(wkv[..., 64:].reshape(DEPTH, 256, 512))
    rows = L // 64
    row = np.repeat(np.arange(rows, dtype=np.float64), 64)
    col = np.tile(np.arange(64, dtype=np.float64), rows)

    def rope_tab(d, reps):
        nf = d // 4
        fr = 10000.0 ** (-np.arange(nf) / nf)
        ang = np.concatenate([row[:, None] * fr, col[:, None] * fr], -1)
        c = np.concatenate([np.cos(ang), np.cos(ang)], -1).T
        sn = np.concatenate([-np.sin(ang), np.sin(ang)], -1).T
        return np.stack([np.tile(c, (reps, 1)), np.tile(sn, (reps, 1))], 0).astype(np.float32)
    rm = np.zeros((2, 128, L), np.float32)
    rm[0] = 1.0
    rm[:, 64:96, :] = rope_tab(32, 1)
    sh["rope_mla"] = rm
    sh["rope_win"] = rope_tab(64, 2)
    jj = np.arange(128)[:, None]
    rr = np.arange(128)[None, :]
    sh["wmask"] = np.stack([(jj >= rr), (jj <= rr)], 0).astype(np.float32)
    sh["win_sink"] = f(np.broadcast_to(f(inp["win_sink"])[:, None, :], (DEPTH, 128, 8)))
    sh["w_branch"] = f(inp["w_branch"]).reshape(DEPTH, 1536, D)
    sh["w_out"] = f(inp["w_out"])
    lnp = np.stack([f(inp[n]).reshape(DEPTH, 8, 128).transpose(0, 2, 1) for n in ("ln1_g", "ln1_b", "ln2_g", "ln2_b")], 2)
    sh["lnp"] = f(lnp)
    sh["w_router"] = f(inp["w_router"])
    sh["w_gate"], sh["w_up"], sh["w_down"] = f(inp["w_gate"]), f(inp["w_up"]), f(inp["w_down"])
    sel = np.zeros((16, 16, 128), np.float32)
    for e_ in range(16):
        sel[e_, e_, :] = 1.0
    sh["sel16"] = sel
    sh["iota_s"] = f(np.broadcast_to(np.arange(384, dtype=np.float32), (128, 384)))
    sh["iota_p3"] = f(np.arange(128, dtype=np.float32)[:, None] + np.array([0.0, 128.0, 256.0], np.float32)[None, :])
    sh["tau"] = f(np.broadcast_to(np.arange(NT, dtype=np.float32), (128, NT)))
    return sh


def prep_core(inp, b):
    f = lambda a: np.ascontiguousarray(np.asarray(a, np.float32))
    m = {}
    m["xin"] = f(np.concatenate([inp["ctx"][b], inp["x"][b]], 0))
    cond = np.stack([np.asarray(inp["c"][b]), np.asarray(inp["c_ctx"])], -1)
    m["condT"] = f(cond.reshape(8, 128, 2).transpose(1, 0, 2))
    return m


def kernel(**inputs):
    nc = build()
    sh = prep_shared(inputs)
    in_maps = []
    for b in range(8):
        m = dict(sh)
        m.update(prep_core(inputs, b))
        in_maps.append(m)
    res = run_bass_kernel_spmd(nc, in_maps, core_ids=list(range(8)))
    return np.stack([np.asarray(r["out"], np.float32) for r in res.results], 0)
```

```python
import numpy as np
from contextlib import ExitStack
import concourse.bass as bass
import concourse.mybir as mybir
from concourse.bass_utils import run_bass_kernel_spmd

F32 = mybir.dt.float32
BF16 = mybir.dt.bfloat16
I32 = mybir.dt.int32
AF = mybir.ActivationFunctionType
ALU = mybir.AluOpType

ENGS = ("pe", "dve", "act", "pool", "sp")
D = 1024
NT = 2304
LC = 256
L = 2048
DEPTH = 4
ALPHA = (2 * DEPTH) ** 0.25
EPS = 1e-6
NZT = 53
ZT_QA, ZT_KVA, ZT_WQ, ZT_WK, ZT_WV, ZT_G, ZT_WQS, ZT_WKS, ZT_KRX = 4, 7, 9, 17, 18, 19, 43, 51, 52
TBS = [(0, 256, 1), (256, 512, 0), (768, 512, 0), (1280, 512, 0), (1792, 512, 0)]


class Prog:
    def __init__(self, nc, es, n_dma_sems=24):
        self.nc = nc
        self.q = {e: [] for e in ENGS}
        self.sem = {}
        self.cnt = {}
        for e in ENGS:
            self.sem[e] = es.enter_context(nc.semaphore("s_" + e))
            self.cnt[e] = 0
        self.dma_sems = []
        for i in range(n_dma_sems):
            nm = "d%d" % i
            self.sem[nm] = es.enter_context(nc.semaphore("s_" + nm))
            self.cnt[nm] = 0
            self.dma_sems.append(nm)
        self.dma_rr = 0
        self.seen = {e: {} for e in ENGS}
        self.lastw = {}
        self.readers = {}
        self.nops = 0

    def _deps(self, eng, reads, writes):
        deps = {}

        def need(st, v):
            if st == eng and eng == "pe":
                return
            if deps.get(st, 0) < v:
                deps[st] = v

        for k in reads:
            lw = self.lastw.get(k)
            if lw is not None:
                need(*lw)
            if k.startswith("ps"):
                for r in self.readers.get(k, ()):
                    if r[0] != eng:
                        need(*r)
        for k in writes:
            lw = self.lastw.get(k)
            if lw is not None:
                need(*lw)
            for r in self.readers.get(k, ()):
                need(*r)
        return deps

    def _emit_waits(self, eng, deps):
        for st, v in deps.items():
            if self.seen[eng].get(st, 0) < v:
                self.seen[eng][st] = v
                sem = self.sem[st]
                self.q[eng].append(lambda e, sem=sem, v=v: e.wait_ge(sem, v))

    def _record(self, done, reads, writes):
        for k in reads:
            self.readers.setdefault(k, []).append(done)
        for k in writes:
            self.lastw[k] = done
            self.readers[k] = []

    def op(self, eng, fn, reads=(), writes=()):
        deps = self._deps(eng, reads, writes)
        self._emit_waits(eng, deps)
        self.cnt[eng] += 1
        sem = self.sem[eng]
        self.q[eng].append(lambda e, fn=fn, sem=sem: fn(e).then_inc(sem, 1))
        self._record((eng, self.cnt[eng]), reads, writes)
        self.nops += 1

    def dma(self, qeng, out, in_, reads=(), writes=(), **kw):
        st = self.dma_sems[self.dma_rr % len(self.dma_sems)]
        self.dma_rr += 1
        deps = self._deps(st, reads, writes)
        if self.cnt[st] > 0:
            deps[st] = self.cnt[st]
        self._emit_waits(qeng, deps)
        self.cnt[st] += 16
        sem = self.sem[st]
        self.q[qeng].append(
            lambda e, out=out, in_=in_, sem=sem, kw=kw: e.dma_start(out=out, in_=in_, **kw).then_inc(sem, 16))
        self._record((st, self.cnt[st]), reads, writes)
        self.nops += 1

    def barrier(self):
        for eng in ENGS:
            deps = {}
            for st in self.sem:
                if st != eng and self.cnt[st] > 0:
                    deps[st] = self.cnt[st]
            self._emit_waits(eng, deps)
        self.lastw = {}
        self.readers = {}

    def replay(self):
        nc = self.nc
        q = self.q
        with nc.Block() as block:
            @block.tensor
            def _(e):
                for f in q["pe"]:
                    f(e)

            @block.vector
            def _(e):
                for f in q["dve"]:
                    f(e)

            @block.scalar
            def _(e):
                for f in q["act"]:
                    f(e)

            @block.gpsimd
            def _(e):
                for f in q["pool"]:
                    f(e)

            @block.sync
            def _(e):
                for f in q["sp"]:
                    f(e)
        self.q = {e: [] for e in ENGS}


_UID = [0]


def U(name):
    _UID[0] += 1
    return "%s_u%d" % (name, _UID[0])


class Rot:
    def __init__(self, nc, es, name, n, shape, dtype, psum=False):
        self.name = name
        self.n = n
        self.i = 0
        if psum:
            self.t = [es.enter_context(nc.psum_tensor(U("%s%d" % (name, j)), shape, dtype)) for j in range(n)]
        else:
            self.t = [es.enter_context(nc.sbuf_tensor(U("%s%d" % (name, j)), shape, dtype)) for j in range(n)]

    def next(self):
        j = self.i % self.n
        self.i += 1
        return self.t[j], "%s%d" % (self.name, j)


class K:
    pass


def build(nlayers=DEPTH, dbg=()):
    nc = bass.Bass("TRN2", target_bir_lowering=False)
    k = K()
    k.nc = nc
    k.dbg = dbg
    din = lambda name, shape, dt=F32: nc.dram_tensor(name, list(shape), dt, kind="ExternalInput").ap()

    def dscr(name, shape, dt):
        kind = "ExternalOutput" if name in dbg else "Internal"
        return nc.dram_tensor(name, list(shape), dt, kind=kind).ap()

    k.xin = din("xin", [NT, D])
    k.condT = din("condT", [128, 8, 2])
    k.ident = din("ident", [128, 128])
    k.w_ada = din("w_ada", [DEPTH, D, 6 * D])
    k.bada2 = din("bada2", [DEPTH, 128, 48, 2])
    k.w_inx = din("w_inx", [DEPTH, D, NZT * 128])
    k.s5_bT = din("s5_bT", [DEPTH, 2, 2, 16, 128, 128])
    k.s5_cL = din("s5_cL", [DEPTH, 128, 2, 16, 2, 16])
    k.s5_lane = din("s5_lane", [DEPTH, 128, 3, 32])
    k.s5_dg = din("s5_dg", [DEPTH, 128, 2, 4])
    k.s5_wglu = din("s5_wglu", [DEPTH, 512, 512])
    k.tau = din("tau", [128, NT])
    k.mla_g = din("mla_g", [DEPTH, 128, 5])
    k.w_uqx = din("w_uqx", [DEPTH, 384, 2048])
    k.w_ukvk = din("w_ukvk", [DEPTH, 256, 1024])
    k.w_ukvv = din("w_ukvv", [DEPTH, 256, 512])
    k.rope_mla = din("rope_mla", [2, 128, L])
    k.rope_win = din("rope_win", [2, 128, L])
    k.wmask = din("wmask", [2, 128, 128])
    k.win_sink = din("win_sink", [DEPTH, 128, 8])
    k.w_branch = din("w_branch", [DEPTH, 1536, D])
    k.w_out = din("w_out", [DEPTH, D, D])
    k.lnp = din("lnp", [DEPTH, 128, 4, 8])
    k.w_router = din("w_router", [DEPTH, D, 16])
    k.w_gate = din("w_gate", [DEPTH, 16, D, D])
    k.w_up = din("w_up", [DEPTH, 16, D, D])
    k.w_down = din("w_down", [DEPTH, 16, D, D])
    k.sel16 = din("sel16", [16, 16, 128])
    k.iota_s = din("iota_s", [128, 384])
    k.iota_p3 = din("iota_p3", [128, 3])
    k.out = nc.dram_tensor("out", [L, D], F32, kind="ExternalOutput").ap()
    k.brT = [dscr(nm, [512, NT], BF16) for nm in ("s5T", "mlaT", "winT")]
    k.xT = dscr("xT", [D, NT], F32)
    k.zT = dscr("zT", [NZT * 128, NT], BF16)
    k.vtok = dscr("vtok", [NT, 128], BF16)
    k.h2T = dscr("h2T", [D, NT], BF16)
    k.yg = dscr("yg", [16, 3, 128, D], BF16)

    with ExitStack() as es:
        P = Prog(nc, es)
        k.P = P
        k.identf = es.enter_context(nc.sbuf_tensor(U("identf"), [128, 128], F32))
        k.identb = es.enter_context(nc.sbuf_tensor(U("identb"), [128, 128], BF16))
        k.onesm = es.enter_context(nc.sbuf_tensor(U("onesm"), [128, 128], F32))
        k.mod = es.enter_context(nc.sbuf_tensor(U("mod"), [128, DEPTH, 48, 2], F32))
        k.epsc = es.enter_context(nc.sbuf_tensor(U("epsc"), [128, 1], F32))
        k.ps = [es.enter_context(nc.psum_tensor("ps%d" % i, [128, 512], F32)) for i in range(8)]
        k.psi = 0

        stage_init(k)
        stage_ada(k, nlayers)
        k.onesb = es.enter_context(nc.sbuf_tensor(U("onesb"), [128, 512], BF16))
        k.onesf = es.enter_context(nc.sbuf_tensor(U("onesf"), [128, 128], F32))
        P.op("dve", lambda e: e.memset(k.onesb[:], 1.0), writes=["onesb"])
        P.op("dve", lambda e: e.memset(k.onesf[:], 1.0), writes=["onesf"])
        k.halfpi = es.enter_context(nc.sbuf_tensor(U("halfpi"), [128, 1], F32))
        P.op("dve", lambda e: e.memset(k.halfpi[:], float(np.pi / 2)), writes=["halfpi"])
        for li in range(nlayers):
            if "skip_win" not in dbg:
                stage_ln_win(k, li)
            if "mla_first" in dbg:
                stage_mla(k, li)
            if "skip_s5" not in dbg:
                stage_s5(k, li)
            if "skip_mla" not in dbg and "mla_first" not in dbg:
                stage_mla(k, li)
            if "skip_winb" not in dbg:
                stage_win(k, li)
            if "skip_mm" not in dbg:
                stage_merge(k, li)
                stage_moe(k, li)
        stage_out(k)
    return nc


def dump(k, name, ap, shape, dt, reads):
    if name not in k.dbg:
        return
    t = k.nc.dram_tensor(name, list(shape), dt, kind="ExternalOutput").ap()
    k.P.dma("sp", t, ap, reads=reads)


def psn(k, lo=0, hi=8):
    j = lo + (k.psi % (hi - lo))
    k.psi += 1
    return k.ps[j], "ps%d" % j


def stage_end(k):
    k.P.barrier()
    k.P.replay()


def stage_init(k):
    nc, P = k.nc, k.P
    P.dma("sp", k.identf[:], k.ident, writes=["identf"])
    P.dma("pool", k.identb[:], k.ident, writes=["identb"])
    P.op("dve", lambda e: e.memset(k.onesm[:], 1.0 / D), writes=["onesm"])
    P.op("dve", lambda e: e.memset(k.epsc[:], EPS), writes=["epsc"])
    with ExitStack() as st:
        xr = Rot(nc, st, "xr", 2, [128, D], F32)
        xo = Rot(nc, st, "xo", 2, [128, 8, 128], F32)
        xTv = k.xT.rearrange("(k p) t -> p k t", p=128)
        for tt in range(NT // 128):
            xt, xk = xr.next()
            P.dma("sp", xt[:], k.xin[tt * 128:(tt + 1) * 128, :], writes=[xk])
            ot, ok = xo.next()
            for half in range(2):
                pt, pk = psn(k)
                for kk in range(4):
                    kf = half * 4 + kk
                    P.op("pe", lambda e, pt=pt, kk=kk, kf=kf, xt=xt: e.transpose(pt[:, kk * 128:(kk + 1) * 128], xt[:, kf * 128:(kf + 1) * 128], k.identf[:]),
                         reads=[xk, "identf"], writes=[pk])
                eng = "act" if half == 0 else "dve"
                if eng == "act":
                    P.op("act", lambda e, pt=pt, ot=ot, half=half: e.copy(ot[:, half * 4:(half + 1) * 4, :], pt[:].rearrange("p (k t) -> p k t", k=4)),
                         reads=[pk], writes=[ok + "h%d" % half])
                else:
                    P.op("dve", lambda e, pt=pt, ot=ot, half=half: e.tensor_copy(ot[:, half * 4:(half + 1) * 4, :], pt[:].rearrange("p (k t) -> p k t", k=4)),
                         reads=[pk], writes=[ok + "h%d" % half])
            P.dma("sp", xTv[:, :, tt * 128:(tt + 1) * 128], ot[:], reads=[ok + "h0", ok + "h1"], writes=["xT"])
        stage_end(k)


def stage_ada(k, nlayers):
    nc, P = k.nc, k.P
    with ExitStack() as st:
        sc = st.enter_context(nc.sbuf_tensor(U("sc"), [128, 8, 2], F32))
        bt = st.enter_context(nc.sbuf_tensor(U("bt"), [128, DEPTH, 48, 2], F32))
        wa = Rot(nc, st, "wa", 2, [128, 8, 768], F32)
        P.dma("sp", sc[:], k.condT, writes=["sc"])
        P.dma("sp", bt[:], k.bada2.rearrange("l p m s -> p l m s"), writes=["bt"])
        P.op("act", lambda e: e.activation(out=sc[:], in_=sc[:], func=AF.Silu), reads=["sc"], writes=["sc"])
        for li in range(nlayers):
            wv = k.w_ada[li].rearrange("(k p) n -> p k n", p=128)
            for cb in range(8):
                wt, wk = wa.next()
                P.dma("sp", wt[:], wv[:, :, cb * 768:(cb + 1) * 768], writes=[wk])
                pt, pk = psn(k)
                for mt in range(6):
                    for kk in range(8):
                        P.op("pe", lambda e, pt=pt, wt=wt, mt=mt, kk=kk: e.matmul(pt[:, mt * 2:mt * 2 + 2], wt[:, kk, mt * 128:(mt + 1) * 128], sc[:, kk, :], start=(kk == 0), stop=(kk == 7)),
                             reads=[wk, "sc"], writes=[pk])
                P.op("dve", lambda e, pt=pt, li=li, cb=cb: e.tensor_tensor(k.mod[:, li, cb * 6:(cb + 1) * 6, :], pt[:, 0:12].rearrange("p (m s) -> p m s", s=2), bt[:, li, cb * 6:(cb + 1) * 6, :], ALU.add),
                     reads=[pk, "bt"], writes=["mod"])
            for j in (1, 4):
                P.op("dve", lambda e, li=li, j=j: e.tensor_scalar_add(k.mod[:, li, j * 8:(j + 1) * 8, :], k.mod[:, li, j * 8:(j + 1) * 8, :], 1.0),
                     reads=["mod"], writes=["mod"])
        stage_end(k)


def ln_stats(k, xb, xk, w, tmp):
    nc, P = k.nc, k.P
    sq, mean, rstd, m2 = tmp["sq"], tmp["mean"], tmp["rstd"], tmp["m2"]
    P.op("act", lambda e: e.activation(out=sq[:, :, :w], in_=xb[:, :, :w], func=AF.Square), reads=[xk], writes=["sq"])
    p1, k1 = psn(k)
    p2, k2 = psn(k)
    for kk in range(8):
        P.op("pe", lambda e, kk=kk: e.matmul(p1[:, :w], k.onesm[:], xb[:, kk, :w], start=(kk == 0), stop=(kk == 7)), reads=[xk, "onesm"], writes=[k1])
    for kk in range(8):
        P.op("pe", lambda e, kk=kk: e.matmul(p2[:, :w], k.onesm[:], sq[:, kk, :w], start=(kk == 0), stop=(kk == 7)), reads=["sq", "onesm"], writes=[k2])
    P.op("act", lambda e: e.copy(mean[:, :w], p1[:, :w]), reads=[k1], writes=["mean"])
    P.op("dve", lambda e: e.tensor_tensor(m2[:, :w], mean[:, :w], mean[:, :w], ALU.mult), reads=["mean"], writes=["m2"])
    P.op("dve", lambda e: e.tensor_tensor(m2[:, :w], p2[:, :w], m2[:, :w], ALU.subtract), reads=[k2, "m2"], writes=["m2"])
    P.op("act", lambda e: e.activation(out=m2[:, :w], in_=m2[:, :w], func=AF.Sqrt, bias=k.epsc[:], scale=1.0), reads=["m2", "epsc"], writes=["m2"])
    P.op("dve", lambda e: e.reciprocal(rstd[:, :w], m2[:, :w]), reads=["m2"], writes=["rstd"])


def ln_tmp(nc, st, W=512):
    return {
        "sq": st.enter_context(nc.sbuf_tensor(U("ln_sq"), [128, 8, W], F32)),
        "mean": st.enter_context(nc.sbuf_tensor(U("ln_mean"), [128, W], F32)),
        "rstd": st.enter_context(nc.sbuf_tensor(U("ln_rstd"), [128, W], F32)),
        "m2": st.enter_context(nc.sbuf_tensor(U("ln_m2"), [128, W], F32)),
        "t": st.enter_context(nc.sbuf_tensor(U("ln_t"), [128, W], F32)),
        "tr": Rot(nc, st, "ln_tr", 3, [128, W], F32),
    }


def ln_apply(k, xb, xk, w, tmp, kk, out, okeys, scale_ap, bias_ap, extra_reads=()):
    P = k.P
    t, tk = tmp["tr"].next()
    P.op("dve", lambda e: e.tensor_tensor(t[:, :w], xb[:, kk, :w], tmp["mean"][:, :w], ALU.subtract), reads=[xk, "mean"], writes=[tk])
    P.op("dve", lambda e: e.tensor_tensor(t[:, :w], t[:, :w], tmp["rstd"][:, :w], ALU.mult), reads=[tk, "rstd"], writes=[tk])
    P.op("act", lambda e: e.activation(out=out, in_=t[:, :w], func=AF.Identity, scale=scale_ap, bias=bias_ap), reads=[tk, "mod"] + list(extra_reads), writes=okeys)


def stage_ln_win(k, li):
    nc, P = k.nc, k.P
    with ExitStack() as st:
        hT = st.enter_context(nc.sbuf_tensor(U("hT"), [128, 8, NT], BF16))
        with ExitStack() as st2:
            tmp = ln_tmp(nc, st2)
            xbr = Rot(nc, st2, "xb", 2, [128, 8, 512], F32)
            xTv = k.xT.rearrange("(k p) t -> p k t", p=128)
            for (t0, w, isc) in TBS:
                xb, xk = xbr.next()
                P.dma("sp", xb[:, :, :w], xTv[:, :, t0:t0 + w], reads=["xT"], writes=[xk])
                ln_stats(k, xb, xk, w, tmp)
                for kk in range(8):
                    ln_apply(k, xb, xk, w, tmp, kk, hT[:, kk, t0:t0 + w], ["hT%d" % kk],
                             k.mod[:, li, 8 + kk, isc:isc + 1], k.mod[:, li, 0 + kk, isc:isc + 1])
            P.barrier()
        wr = Rot(nc, st, "wr", 3, [128, 8, 128], BF16)
        zs = Rot(nc, st, "zs", 3, [128, NT], BF16)
        vt = st.enter_context(nc.sbuf_tensor(U("vt"), [128, 18, 128], BF16))
        wv = k.w_inx[li].rearrange("(k p) n -> p k n", p=128)
        ev = 0
        for m in range(NZT):
            wt, wk = wr.next()
            P.dma("pool", wt[:], wv[:, :, m * 128:(m + 1) * 128], writes=[wk])
            zt, zk = zs.next()
            mrows = 64 if m == ZT_KRX else 128
            for (t0, w, isc) in TBS:
                pt, pk = psn(k)
                for kk in range(8):
                    P.op("pe", lambda e, pt=pt, wt=wt, kk=kk, t0=t0, w=w, mrows=mrows: e.matmul(pt[:mrows, :w], wt[:, kk, :mrows], hT[:, kk, t0:t0 + w], start=(kk == 0), stop=(kk == 7)),
                         reads=[wk, "hT%d" % kk], writes=[pk])
                gate = ZT_G <= m < ZT_G + 24
                if gate:
                    P.op("act", lambda e, pt=pt, zt=zt, t0=t0, w=w: e.activation(out=zt[:, t0:t0 + w], in_=pt[:, :w], func=AF.Sigmoid), reads=[pk], writes=[zk + "_%d" % t0])
                elif ev % 2 == 0:
                    P.op("act", lambda e, pt=pt, zt=zt, t0=t0, w=w, mrows=mrows: e.copy(zt[:mrows, t0:t0 + w], pt[:mrows, :w]), reads=[pk], writes=[zk + "_%d" % t0])
                else:
                    P.op("dve", lambda e, pt=pt, zt=zt, t0=t0, w=w, mrows=mrows: e.tensor_copy(zt[:mrows, t0:t0 + w], pt[:mrows, :w]), reads=[pk], writes=[zk + "_%d" % t0])
                ev += 1
            P.dma("sp", k.zT[m * 128:m * 128 + mrows, :], zt[:mrows, :], reads=[zk + "_%d" % t[0] for t in TBS], writes=["zT"])
            if m == ZT_WV:
                for tt in range(18):
                    pt, pk = psn(k)
                    for kk in range(8):
                        P.op("pe", lambda e, pt=pt, wt=wt, kk=kk, tt=tt: e.matmul(pt[:, :128], hT[:, kk, tt * 128:(tt + 1) * 128], wt[:, kk, :], start=(kk == 0), stop=(kk == 7)),
                             reads=[wk, "hT%d" % kk], writes=[pk])
                    P.op("dve", lambda e, pt=pt, tt=tt: e.tensor_copy(vt[:, tt, :], pt[:, :128]), reads=[pk], writes=["vt"])
                P.dma("sp", k.vtok.rearrange("(t p) c -> p t c", p=128), vt[:], reads=["vt"], writes=["vtok"])
        stage_end(k)


def stage_s5(k, li):
    S5E = "dve" if "s5pool" not in k.dbg else "pool"
    nc, P = k.nc, k.P
    TWO_PI = float(2 * np.pi)
    with ExitStack() as st:
        sb = lambda name, shape, dt=F32: st.enter_context(nc.sbuf_tensor(U(name), shape, dt))
        lane = sb("lane", [128, 3, 32])
        dg = sb("dg", [128, 2, 4])
        BW = sb("BW", [128, 64, 128], BF16)
        CW = sb("CW", [128, 96, 128], BF16)
        tau = sb("tau", [128, NT])
        names = ["dt", "rho", "thn", "fr", "sn", "cs", "ar", "ai", "rden", "qr", "qi", "nqr", "nqi", "tA", "tB"]
        lp = {n: sb("lp_" + n, [128, 32]) for n in names}
        lpi = sb("lp_it", [128, 32], I32)
        P.dma("sp", lane[:], k.s5_lane[li], writes=["lane"])
        P.dma("sp", dg[:], k.s5_dg[li], writes=["dg"])
        P.dma("sp", tau[:], k.tau, writes=["tau"])
        bsrc = k.s5_bT[li].rearrange("d r t p c -> p (d r t) c")
        for j in range(8):
            P.dma("pool", BW[:, j * 8:(j + 1) * 8, :], bsrc[:, j * 8:(j + 1) * 8, :], writes=["BW"])
        P.op("pool", lambda e: e.memset(CW[:], 0.0), writes=["CW"])
        lre, lim, ldt = lane[:, 0, :], lane[:, 1, :], lane[:, 2, :]
        R = ["lane", "lp"]
        W = ["lp"]
        V = lambda fn: P.op("dve", fn, reads=R, writes=W)
        A = lambda fn: P.op("act", fn, reads=R + ["halfpi"], writes=W)
        A(lambda e: e.activation(out=lp["dt"][:], in_=ldt, func=AF.Exp))
        V(lambda e: e.tensor_tensor(lp["tA"][:], lre, lp["dt"][:], ALU.mult))
        A(lambda e: e.activation(out=lp["rho"][:], in_=lp["tA"][:], func=AF.Exp))
        V(lambda e: e.tensor_tensor(lp["thn"][:], lim, lp["dt"][:], ALU.mult))
        V(lambda e: e.tensor_scalar(lp["thn"][:], lp["thn"][:], float(1.0 / TWO_PI), None, ALU.mult))
        V(lambda e: e.tensor_copy(lpi[:], lp["thn"][:]))
        V(lambda e: e.tensor_copy(lp["tB"][:], lpi[:]))
        V(lambda e: e.tensor_tensor(lp["fr"][:], lp["thn"][:], lp["tB"][:], ALU.subtract))
        A(lambda e: e.activation(out=lp["sn"][:], in_=lp["fr"][:], func=AF.Sin, scale=TWO_PI))
        A(lambda e: e.activation(out=lp["fr"][:], in_=lp["fr"][:], func=AF.Abs))
        A(lambda e: e.activation(out=lp["cs"][:], in_=lp["fr"][:], func=AF.Sin, scale=-TWO_PI, bias=k.halfpi[:]))
        V(lambda e: e.tensor_tensor(lp["ar"][:], lp["rho"][:], lp["cs"][:], ALU.mult))
        V(lambda e: e.tensor_tensor(lp["ai"][:], lp["rho"][:], lp["sn"][:], ALU.mult))
        V(lambda e: e.tensor_tensor(lp["tA"][:], lre, lre, ALU.mult))
        V(lambda e: e.tensor_tensor(lp["tB"][:], lim, lim, ALU.mult))
        V(lambda e: e.tensor_tensor(lp["tA"][:], lp["tA"][:], lp["tB"][:], ALU.add))
        V(lambda e: e.reciprocal(lp["rden"][:], lp["tA"][:]))
        V(lambda e: e.tensor_scalar_add(lp["ar"][:], lp["ar"][:], -1.0))
        V(lambda e: e.tensor_tensor(lp["tA"][:], lp["ar"][:], lre, ALU.mult))
        V(lambda e: e.tensor_tensor(lp["tB"][:], lp["ai"][:], lim, ALU.mult))
        V(lambda e: e.tensor_tensor(lp["tA"][:], lp["tA"][:], lp["tB"][:], ALU.add))
        V(lambda e: e.tensor_tensor(lp["qr"][:], lp["tA"][:], lp["rden"][:], ALU.mult))
        V(lambda e: e.tensor_tensor(lp["tA"][:], lp["ai"][:], lre, ALU.mult))
        V(lambda e: e.tensor_tensor(lp["tB"][:], lp["ar"][:], lim, ALU.mult))
        V(lambda e: e.tensor_tensor(lp["tA"][:], lp["tA"][:], lp["tB"][:], ALU.subtract))
        V(lambda e: e.tensor_tensor(lp["qi"][:], lp["tA"][:], lp["rden"][:], ALU.mult))
        V(lambda e: e.tensor_scalar(lp["nqr"][:], lp["qr"][:], -1.0, None, ALU.mult))
        V(lambda e: e.tensor_scalar(lp["nqi"][:], lp["qi"][:], -1.0, None, ALU.mult))
        for n_ in ("rho", "thn", "sn", "cs", "qr", "qi", "dt"):
            dump(k, "lp_" + n_, lp[n_][:], [128, 32], F32, ["lp"])
        stC = ExitStack()
        craw = stC.enter_context(nc.sbuf_tensor(U("craw"), [128, 2, 16, 2, 16], F32))
        ctmp = stC.enter_context(nc.sbuf_tensor(U("ctmp"), [128, 16], F32))
        P.dma("sp", craw[:], k.s5_cL[li], writes=["craw"])
        for d in range(2):
            for lt in range(16):
                col = d * 16 + lt
                cr, ci = craw[:, d, lt, 0, :], craw[:, d, lt, 1, :]
                for half in range(2):
                    g = 2 * lt + half
                    gl = g % 8
                    ps_ = slice(half * 64, half * 64 + 64)
                    for ri in range(3):
                        s1 = lp["qi"] if ri != 1 else lp["nqr"]
                        s2 = (lp["qr"], lp["nqi"], lp["nqr"])[ri]
                        op1 = (ALU.subtract, ALU.add, ALU.add)[ri]
                        P.op("dve", lambda e, ci=ci, s1=s1, col=col, ps_=ps_: e.tensor_scalar(ctmp[ps_, :], ci[ps_, :], s1[ps_, col:col + 1], None, ALU.mult), reads=["craw", "lp"], writes=["ctmp"])
                        P.op("dve", lambda e, cr=cr, s2=s2, col=col, ps_=ps_, ri=ri, gl=gl, op1=op1: e.scalar_tensor_tensor(CW[ps_, col * 3 + ri, gl * 16:(gl + 1) * 16], cr[ps_, :], s2[ps_, col:col + 1], ctmp[ps_, :], ALU.mult, op1), reads=["craw", "lp", "ctmp"], writes=["CW"])
        P.barrier()
        stC.close()
        ut = sb("ut", [128, NT], BF16)
        gT = sb("s5g", [128, 4, NT], BF16)
        with ExitStack() as stU:
            sbu = lambda name, shape, dt=F32: stU.enter_context(nc.sbuf_tensor(U(name), shape, dt))
            it = sbu("s5it", [128, NT], I32)
            fr = sbu("s5fr", [128, NT])
            SnR = Rot(nc, stU, "s5S", 2, [128, NT], BF16)
            CsR = Rot(nc, stU, "s5C", 2, [128, NT], BF16)
            br = sbu("s5br", [128, NT], BF16)
            bi = sbu("s5bi", [128, NT], BF16)
            p1 = sbu("s5p1", [128, NT], BF16)
            p2 = sbu("s5p2", [128, NT], BF16)
            p3 = sbu("s5p3", [128, NT], BF16)
            wr = sbu("s5wr", [128, NT], BF16)
            wi = sbu("s5wi", [128, NT], BF16)
            zrR = Rot(nc, stU, "s5zr", 2, [128, NT], BF16)
            ziR = Rot(nc, stU, "s5zi", 2, [128, NT], BF16)
            qR = [Rot(nc, stU, "s5q%d" % j, 2, [128, NT], BF16) for j in range(4)]
            ysr = Rot(nc, stU, "s5ys", 2, [128, 512], F32)
            segs = [(0, LC), (LC, NT)]
            for gt in range(4):
                P.dma("sp", ut[:], k.zT[gt * 128:(gt + 1) * 128, :], reads=["zT"], writes=["ut"])
                for d in range(2):
                    for l4 in range(4):
                        lt = gt * 4 + l4
                        col = d * 16 + lt
                        thn = lp["thn"][:, col:col + 1]
                        first = (d == 0 and l4 == 0)
                        last = (d == 1 and l4 == 3)
                        Sn, Snk = SnR.next()
                        Cs, Csk = CsR.next()
                        zr, zrk = zrR.next()
                        zi, zik = ziR.next()
                        qs = [r_.next() for r_ in qR]
                        for (a_, b_) in segs:
                            src = tau[:, a_:b_] if d == 0 else (tau[:, b_ - 1::-1] if a_ == 0 else tau[:, b_ - 1:a_ - 1:-1])
                            P.op("dve", lambda e, src=src, a_=a_, b_=b_, thn=thn: e.tensor_scalar(it[:, a_:b_], src, thn, None, ALU.mult), reads=["tau", "lp"], writes=["it"])
                            P.op("dve", lambda e, src=src, a_=a_, b_=b_, thn=thn: e.scalar_tensor_tensor(fr[:, a_:b_], src, thn, it[:, a_:b_], ALU.mult, ALU.subtract), reads=["tau", "lp", "it"], writes=["fr"])
                        P.op("act", lambda e, Sn=Sn: e.activation(out=Sn[:], in_=fr[:], func=AF.Sin, scale=TWO_PI), reads=["fr"], writes=[Snk])
                        P.op("act", lambda e: e.activation(out=fr[:], in_=fr[:], func=AF.Abs), reads=["fr"], writes=["fr"])
                        P.op("act", lambda e, Cs=Cs: e.activation(out=Cs[:], in_=fr[:], func=AF.Sin, scale=-TWO_PI, bias=k.halfpi[:]), reads=["fr", "halfpi"], writes=[Csk])
                        for (t0, w, isc) in TBS:
                            pr, kr = psn(k, 5, 8)
                            pi_, ki = psn(k, 5, 8)
                            sl = slice(t0, t0 + w)
                            P.op("pe", lambda e, pr=pr, sl=sl, w=w, d=d, lt=lt: e.matmul(pr[:, :w], BW[:, (d * 2 + 0) * 16 + lt, :], ut[:, sl], start=True, stop=True), reads=["BW", "ut"], writes=[kr])
                            P.op("pe", lambda e, pi_=pi_, sl=sl, w=w, d=d, lt=lt: e.matmul(pi_[:, :w], BW[:, (d * 2 + 1) * 16 + lt, :], ut[:, sl], start=True, stop=True), reads=["BW", "ut"], writes=[ki])
                            P.op("act", lambda e, pr=pr, sl=sl, w=w: e.copy(br[:, sl], pr[:, :w]), reads=[kr], writes=["br"])
                            P.op("act", lambda e, pi_=pi_, sl=sl, w=w: e.copy(bi[:, sl], pi_[:, :w]), reads=[ki], writes=["bi"])
                        P.op("dve", lambda e, Cs=Cs: e.tensor_tensor(p1[:], Cs[:], br[:], ALU.mult), reads=[Csk, "br"], writes=["p1"])
                        P.op(S5E, lambda e, Sn=Sn: e.tensor_tensor(p2[:], Sn[:], bi[:], ALU.mult), reads=[Snk, "bi"], writes=["p2"])
                        P.op(S5E, lambda e, Cs=Cs: e.tensor_tensor(p3[:], Cs[:], bi[:], ALU.mult), reads=[Csk, "bi"], writes=["p3"])
                        P.op("dve", lambda e: e.tensor_tensor(wr[:], p1[:], p2[:], ALU.add), reads=["p1", "p2"], writes=["wr"])
                        P.op(S5E, lambda e, Sn=Sn: e.tensor_tensor(p2[:], Sn[:], br[:], ALU.mult), reads=[Snk, "br", "wr"], writes=["p2"])
                        P.op(S5E, lambda e: e.tensor_tensor(wi[:], p3[:], p2[:], ALU.subtract), reads=["p3", "p2"], writes=["wi"])
                        rho = lp["rho"][:, col:col + 1]
                        for (src, dst, dk) in ((wr, zr, zrk), (wi, zi, zik)):
                            if d == 0:
                                P.op("dve", lambda e, src=src, dst=dst, rho=rho: e.tensor_tensor_scan(dst[:, 0:LC], rho.to_broadcast([128, LC]), src[:, 0:LC], 0.0, ALU.mult, ALU.add), reads=["wr", "wi", "lp"], writes=[dk])
                                P.op("dve", lambda e, src=src, dst=dst, rho=rho: e.tensor_tensor_scan(dst[:, LC:NT], rho.to_broadcast([128, L]), src[:, LC:NT], dst[:, LC - 1:LC], ALU.mult, ALU.add), reads=["wr", "wi", "lp", dk], writes=[dk])
                            else:
                                P.op("dve", lambda e, src=src, dst=dst, rho=rho: e.tensor_tensor_scan(dst[:, LC - 1::-1], rho.to_broadcast([128, LC]), src[:, LC - 1::-1], 0.0, ALU.mult, ALU.add), reads=["wr", "wi", "lp"], writes=[dk])
                                P.op("dve", lambda e, src=src, dst=dst, rho=rho: e.tensor_tensor_scan(dst[:, NT - 1:LC - 1:-1], rho.to_broadcast([128, L]), src[:, NT - 1:LC - 1:-1], dst[:, 0:1], ALU.mult, ALU.add), reads=["wr", "wi", "lp", dk], writes=[dk])
                        (q1, q1k), (q2, q2k), (q3, q3k), (q4, q4k) = qs
                        P.op(S5E, lambda e, Cs=Cs, zr=zr, q1=q1: e.tensor_tensor(q1[:], Cs[:], zr[:], ALU.mult), reads=[Csk, zrk], writes=[q1k])
                        P.op("dve", lambda e, Sn=Sn, zi=zi, q2=q2: e.tensor_tensor(q2[:], Sn[:], zi[:], ALU.mult), reads=[Snk, zik], writes=[q2k])
                        P.op(S5E, lambda e, Sn=Sn, zr=zr, q3=q3: e.tensor_tensor(q3[:], Sn[:], zr[:], ALU.mult), reads=[Snk, zrk], writes=[q3k])
                        P.op("dve", lambda e, Cs=Cs, zi=zi, q4=q4: e.tensor_tensor(q4[:], Cs[:], zi[:], ALU.mult), reads=[Csk, zik], writes=[q4k])
                        for bi_, (t0, w, isc) in enumerate(TBS):
                            sl = slice(t0, t0 + w)
                            yk = "ps%d" % bi_
                            for j_, (qq, qk, wsl) in enumerate(((q1, q1k, 0), (q2, q2k, 2), (q3, q3k, 1), (q4, q4k, 1))):
                                P.op("pe", lambda e, bi_=bi_, sl=sl, w=w, col=col, first=first, last=last, qq=qq, wsl=wsl, j_=j_: e.matmul(k.ps[bi_][:, :w], CW[:, col * 3 + wsl, :], qq[:, sl], start=(first and j_ == 0), stop=(last and j_ == 3)), reads=["CW", qk], writes=[yk])
                for bi_, (t0, w, isc) in enumerate(TBS):
                    sl = slice(t0, t0 + w)
                    ys, ysk = ysr.next()
                    P.op("dve", lambda e, bi_=bi_, sl=sl, w=w, ys=ys, gt=gt: e.scalar_tensor_tensor(ys[:, :w], ut[:, sl], dg[:, 0, gt:gt + 1], k.ps[bi_][:, :w], ALU.mult, ALU.add), reads=["ut", "dg", "ps%d" % bi_], writes=[ysk])
                    P.op("act", lambda e, sl=sl, w=w, ys=ys, gt=gt: e.activation(out=gT[:, gt, sl], in_=ys[:, :w], func=AF.Gelu_apprx_tanh), reads=[ysk], writes=["gT%d" % gt])
            P.barrier()
        wglu = sb("wglu", [128, 4, 512], BF16)
        P.dma("pool", wglu[:], k.s5_wglu[li].rearrange("(k p) n -> p k n", p=128), writes=["wglu"])
        so = Rot(nc, st, "s5o", 2, [128, NT], BF16)
        sgr = Rot(nc, st, "s5sg", 2, [128, 512], F32)
        for mo in range(4):
            ot, ok = so.next()
            for (t0, w, isc) in TBS:
                sl = slice(t0, t0 + w)
                pt, pk = psn(k)
                for kk in range(4):
                    P.op("pe", lambda e, pt=pt, kk=kk, sl=sl, w=w, mo=mo: e.matmul(pt[:, :w], wglu[:, kk, mo * 128:(mo + 1) * 128], gT[:, kk, sl], start=(kk == 0), stop=(kk == 3)), reads=["wglu", "gT%d" % kk], writes=[pk])
                sg, sgk = sgr.next()
                P.op("act", lambda e, pt=pt, w=w, sg=sg, mo=mo: e.activation(out=sg[:, :w], in_=pt[:, :w], func=AF.Sigmoid, bias=dg[:, 1, mo:mo + 1], scale=1.0), reads=[pk, "dg"], writes=[sgk])
                P.op("dve", lambda e, sg=sg, sl=sl, w=w, ot=ot, mo=mo: e.tensor_tensor(ot[:, sl], gT[:, mo, sl], sg[:, :w], ALU.mult), reads=[sgk, "gT%d" % mo], writes=[ok + "_%d" % t0])
            P.dma("sp", k.brT[0][mo * 128:(mo + 1) * 128, :], ot[:], reads=[ok + "_%d" % t[0] for t in TBS], writes=["s5T"])
        stage_end(k)


def rms_norm_T(k, st, src, nk, gains, dst, tag):
    nc, P = k.nc, k.P
    sq = st.enter_context(nc.sbuf_tensor(U("rms_sq"), [128, nk, 512], BF16))
    rinv = st.enter_context(nc.sbuf_tensor(U("rms_ri"), [128, 512], F32))
    for (t0, w, isc) in TBS:
        sl = slice(t0, t0 + w)
        P.op("act", lambda e, sl=sl, w=w: e.activation(out=sq[:, :, :w], in_=src[:, :, sl], func=AF.Square), reads=[tag + "src"], writes=[tag + "sq"])
        pt, pk = psn(k)
        for kk in range(nk):
            P.op("pe", lambda e, pt=pt, kk=kk, w=w: e.matmul(pt[:, :w], k.onesb[:, 0:128], sq[:, kk, :w], start=(kk == 0), stop=(kk == nk - 1)), reads=[tag + "sq", "onesb"], writes=[pk])
        P.op("act", lambda e, pt=pt, w=w: e.activation(out=rinv[:, :w], in_=pt[:, :w], func=AF.Sqrt, scale=float(1.0 / (nk * 128)), bias=k.epsc[:]), reads=[pk, "epsc"], writes=[tag + "ri"])
        P.op("dve", lambda e, w=w: e.reciprocal(rinv[:, :w], rinv[:, :w]), reads=[tag + "ri"], writes=[tag + "ri"])
        for kk in range(nk):
            P.op("dve", lambda e, kk=kk, sl=sl, w=w: e.scalar_tensor_tensor(dst[:, kk, sl], src[:, kk, sl], gains[:, kk:kk + 1], rinv[:, :w], ALU.mult, ALU.mult), reads=[tag + "src", tag + "ri", "mg"], writes=[tag + "dst"])


def softmax_pv(k, ost_rot, score_fn, nkc, va_fn, nq, scale, out_dram, tagp, PTr, esk=None, post=None):
    nc, P = k.nc, k.P
    po, pok = psn(k, 0, 3)
    LA = 3
    scr = {}

    def issue_score(kc):
        pscr, psk = psn(k, 3, 8)
        score_fn(kc, pscr, psk)
        scr[kc] = (pscr, psk)

    for kc in range(min(LA, nkc)):
        issue_score(kc)
    for kc in range(nkc):
        if kc + LA < nkc:
            issue_score(kc + LA)
        pscr, psk = scr.pop(kc)
        pt, ptk = PTr.next()
        P.op("act", lambda e, pscr=pscr, pt=pt: e.activation(out=pt[:, :nq], in_=pscr[:, :nq], func=AF.Exp, scale=scale), reads=[psk], writes=[ptk])
        if post is not None:
            post(kc, pt, ptk)
        va, vak = va_fn(kc)
        P.op("pe", lambda e, po=po, va=va, pt=pt, kc=kc: e.matmul(po[:, :nq], va, pt[:, :nq], start=(kc == 0), stop=(kc == nkc - 1)), reads=[vak, ptk], writes=[pok])
    rv, rvk = k.att_rv.next()
    if esk is not None:
        P.op("dve", lambda e, po=po, rv=rv: e.tensor_scalar(rv[0:64, :nq], po[64:128, :nq], esk, None, ALU.add), reads=[pok, "esk"], writes=[rvk])
        P.op("dve", lambda e, rv=rv: e.reciprocal(rv[0:64, :nq], rv[0:64, :nq]), reads=[rvk], writes=[rvk])
    else:
        P.op("dve", lambda e, po=po, rv=rv: e.reciprocal(rv[0:64, :nq], po[64:128, :nq]), reads=[pok], writes=[rvk])
    ot, otk = ost_rot.next()
    P.op("dve", lambda e, po=po, ot=ot, rv=rv: e.tensor_tensor(ot[0:64, :nq], po[0:64, :nq], rv[0:64, :nq], ALU.mult), reads=[pok, rvk], writes=[otk])
    P.dma("sp", out_dram, ot[0:64, :nq], reads=[otk], writes=[tagp])


def stage_mla(k, li):
    nc, P = k.nc, k.P
    SC = float(96 ** -0.5)
    with ExitStack() as st:
        sb = lambda name, shape, dt=F32: st.enter_context(nc.sbuf_tensor(U(name), shape, dt))
        mg = sb("mg", [128, 5])
        P.dma("sp", mg[:], k.mla_g[li], writes=["mg"])
        VA = sb("VA", [128, 18, 8, 128], BF16)
        KRb = sb("KRb", [128, NT], BF16)
        qn = sb("qn", [128, 3, NT], BF16)
        kvn = sb("kvn", [128, 2, NT], BF16)
        rope = sb("ropem", [128, 2, L])
        wuq = sb("wuq", [128, 3, 2048], BF16)
        wkk = sb("wkk", [128, 2, 1024], BF16)
        k.att_rv = Rot(nc, st, "att_rv", 2, [128, 512], F32)
        P.op("pool", lambda e: e.memset(VA[:], 1.0), writes=["VA"])
        P.dma("sp", rope[:], k.rope_mla.rearrange("c p t -> p c t"), writes=["rope"])
        for j in range(3):
            for c_ in range(4):
                P.dma("pool", wuq[:, j, c_ * 512:(c_ + 1) * 512], k.w_uqx[li][j * 128:(j + 1) * 128, c_ * 512:(c_ + 1) * 512], writes=["wuq"])
        for j in range(2):
            for c_ in range(2):
                P.dma("pool", wkk[:, j, c_ * 512:(c_ + 1) * 512], k.w_ukvk[li][j * 128:(j + 1) * 128, c_ * 512:(c_ + 1) * 512], writes=["wkk"])
        with ExitStack() as st2:
            sb2 = lambda name, shape, dt=F32: st2.enter_context(nc.sbuf_tensor(U(name), shape, dt))
            qa = sb2("qa", [128, 3, NT], BF16)
            kva = sb2("kva", [128, 2, NT], BF16)
            KP = sb2("KP", [128, NT], BF16)
            KS = sb2("KS", [128, L], BF16)
            wvv = sb2("wvv", [128, 2, 512], BF16)
            tA = sb2("mtA", [128, 512])
            tB = sb2("mtB", [128, 512])
            P.dma("sp", qa[:], k.zT[ZT_QA * 128:(ZT_QA + 3) * 128, :].rearrange("(k p) t -> p k t", p=128), reads=["zT"], writes=["qsrc"])
            P.dma("sp", kva[:], k.zT[ZT_KVA * 128:(ZT_KVA + 2) * 128, :].rearrange("(k p) t -> p k t", p=128), reads=["zT"], writes=["ksrc"])
            P.op("pool", lambda e: e.memset(KP[:], 0.0), writes=["KP"])
            P.op("pool", lambda e: e.memset(KS[:], 0.0), writes=["KS"])
            P.dma("sp", KP[64:96, :], k.zT[ZT_KRX * 128:ZT_KRX * 128 + 32, :], reads=["zT"], writes=["KP"])
            P.dma("sp", KS[64:96, :], k.zT[ZT_KRX * 128 + 32:ZT_KRX * 128 + 64, LC:NT], reads=["zT"], writes=["KS"])
            P.dma("pool", wvv[:], k.w_ukvv[li].rearrange("(k p) n -> p k n", p=128), writes=["wvv"])
            rms_norm_T(k, st2, qa, 3, mg[:, 0:3], qn, "q")
            rms_norm_T(k, st2, kva, 2, mg[:, 3:5], kvn, "k")
            P.op("dve", lambda e: e.tensor_copy(KRb[:, 0:LC], KP[:, 0:LC]), reads=["KP"], writes=["KRb"])
            for c in range(4):
                sl = slice(c * 512, (c + 1) * 512)
                sln = slice(LC + c * 512, LC + (c + 1) * 512)
                P.op("dve", lambda e, sl=sl, sln=sln: e.tensor_tensor(tA[:, :], KP[:, sln], rope[:, 0, sl], ALU.mult), reads=["KP", "rope"], writes=["mtA"])
                P.op("dve", lambda e, sl=sl: e.tensor_tensor(tB[:, :], KS[:, sl], rope[:, 1, sl], ALU.mult), reads=["KS", "rope"], writes=["mtB"])
                P.op("dve", lambda e, sln=sln: e.tensor_tensor(KRb[:, sln], tA[:, :], tB[:, :], ALU.add), reads=["mtA", "mtB"], writes=["KRb"])
            dump(k, "d_KP", KP[:], [128, NT], BF16, ["KP"])
            dump(k, "d_KS", KS[:], [128, L], BF16, ["KS"])
            dump(k, "d_KRb", KRb[:], [128, NT], BF16, ["KRb"])
            for tt in range(18):
                pt, pk = psn(k)
                for kk in range(2):
                    P.op("pe", lambda e, pt=pt, kk=kk, tt=tt: e.matmul(pt[:, :512], kvn[:, kk, tt * 128:(tt + 1) * 128], wvv[:, kk, :], start=(kk == 0), stop=(kk == 1)), reads=["wvv", "kdst"], writes=[pk])
                P.op("dve", lambda e, pt=pt, tt=tt: e.tensor_copy(VA[:, tt, :, 0:64], pt[:, :512].rearrange("p (h d) -> p h d", h=8)), reads=[pk], writes=["VA"])
            P.barrier()
        if "mla_stop1" in k.dbg:
            stage_end(k)
            return
        PTr = Rot(nc, st, "mPT", 4, [128, 512], BF16)
        ostr = Rot(nc, st, "most", 3, [128, 512], BF16)
        t1r = Rot(nc, st, "mt1", 2, [128, 512], F32)
        t2r = Rot(nc, st, "mt2", 2, [128, 512], F32)
        for grp in range(2):
            with ExitStack() as st3:
                QP = st3.enter_context(nc.sbuf_tensor(U("QP"), [128, 4, NT], BF16))
                QR = st3.enter_context(nc.sbuf_tensor(U("QR"), [128, 4, L], BF16))
                KH = st3.enter_context(nc.sbuf_tensor(U("KH"), [128, 4, NT], BF16))
                for hh in range(4):
                    h = grp * 4 + hh
                    for (t0, w, isc) in TBS:
                        sl = slice(t0, t0 + w)
                        pm, pmk = psn(k, 3, 8)
                        for kk in range(3):
                            P.op("pe", lambda e, pm=pm, kk=kk, sl=sl, w=w, h=h: e.matmul(pm[:, :w], wuq[:, kk, h * 128:(h + 1) * 128], qn[:, kk, sl], start=(kk == 0), stop=(kk == 2)), reads=["wuq", "qdst"], writes=[pmk])
                        P.op("act", lambda e, pm=pm, sl=sl, w=w, hh=hh: e.copy(QP[:, hh, sl], pm[:, :w]), reads=[pmk], writes=["QP%d" % hh])
                        if not isc:
                            ls = slice(t0 - LC, t0 - LC + w)
                            psw, pswk = psn(k, 3, 8)
                            for kk in range(3):
                                P.op("pe", lambda e, psw=psw, kk=kk, sl=sl, w=w, h=h: e.matmul(psw[:, :w], wuq[:, kk, (8 + h) * 128:(9 + h) * 128], qn[:, kk, sl], start=(kk == 0), stop=(kk == 2)), reads=["wuq", "qdst"], writes=[pswk])
                            t1, t1k = t1r.next()
                            t2, t2k = t2r.next()
                            P.op("dve", lambda e, pm=pm, ls=ls, w=w, t1=t1: e.tensor_tensor(t1[:, :w], pm[:, :w], rope[:, 0, ls], ALU.mult), reads=[pmk, "rope"], writes=[t1k])
                            P.op("dve", lambda e, psw=psw, ls=ls, w=w, t2=t2: e.tensor_tensor(t2[:, :w], psw[:, :w], rope[:, 1, ls], ALU.mult), reads=[pswk, "rope"], writes=[t2k])
                            P.op("dve", lambda e, ls=ls, w=w, hh=hh, t1=t1, t2=t2: e.tensor_tensor(QR[:, hh, ls], t1[:, :w], t2[:, :w], ALU.add), reads=[t1k, t2k], writes=["QR%d" % hh])
                        pk_, pkk = psn(k, 3, 8)
                        for kk in range(2):
                            P.op("pe", lambda e, pk_=pk_, kk=kk, sl=sl, w=w, h=h: e.matmul(pk_[:, :w], wkk[:, kk, h * 128:(h + 1) * 128], kvn[:, kk, sl], start=(kk == 0), stop=(kk == 1)), reads=["wkk", "kdst"], writes=[pkk])
                        P.op("dve", lambda e, pk_=pk_, sl=sl, w=w, hh=hh: e.tensor_tensor(KH[:, hh, sl], pk_[:, :w], KRb[:, sl], ALU.add), reads=[pkk, "KRb"], writes=["KH%d" % hh])
                if grp == 0:
                    dump(k, "d_wkk", wkk[:], [128, 2, 1024], BF16, ["wkk"])
                    dump(k, "d_QP", QP[:, 0, :], [128, NT], BF16, ["QP0"])
                    dump(k, "d_QR", QR[:, 0, :], [128, L], BF16, ["QR0"])
                    dump(k, "d_KH", KH[:, 0, :], [128, NT], BF16, ["KH0"])
                    dump(k, "d_VA", VA[:, :, 0, :], [128, 18, 128], BF16, ["VA"])
                if "mla_stop2" in k.dbg:
                    P.barrier()
                    continue
                for hh in range(4):
                    h = grp * 4 + hh
                    for qb in range(5):
                        if qb < 4:
                            q0, nq, nkc = LC + qb * 512, 512, 18
                        else:
                            q0, nq, nkc = 0, LC, 2

                        def score_fn(kc, pscr, psk, q0=q0, nq=nq, hh=hh):
                            ks = slice(kc * 128, (kc + 1) * 128)
                            if kc >= 2:
                                P.op("pe", lambda e: e.matmul(pscr[:, :nq], KH[:, hh, ks], QR[:, hh, q0 - LC:q0 - LC + nq], start=True, stop=True), reads=["KH%d" % hh, "QR%d" % hh], writes=[psk])
                            else:
                                P.op("pe", lambda e: e.matmul(pscr[:, :nq], KH[:, hh, ks], QP[:, hh, q0:q0 + nq], start=True, stop=True), reads=["KH%d" % hh, "QP%d" % hh], writes=[psk])

                        softmax_pv(k, ostr, score_fn, nkc, lambda kc, h=h: (VA[:, kc, h, :], "VA"), nq, SC,
                                   k.brT[1][h * 64:(h + 1) * 64, q0:q0 + nq], "mlaT", PTr)
                P.barrier()
        stage_end(k)


def stage_win(k, li):
    nc, P = k.nc, k.P
    SC = float(64 ** -0.5)
    with ExitStack() as st:
        sb = lambda name, shape, dt=F32: st.enter_context(nc.sbuf_tensor(U(name), shape, dt))
        Qp = sb("wQp", [128, 8, NT], BF16)
        Qr = sb("wQr", [128, 8, L], BF16)
        Kp = sb("wKp", [128, NT], BF16)
        Kr = sb("wKr", [128, L], BF16)
        VW = sb("wVW", [128, 18, 2, 128], BF16)
        rope = sb("wrope", [128, 2, L])
        msk = sb("wmsk", [128, 2, 128], BF16)
        esk = sb("wesk", [128, 8])
        Qsr = Rot(nc, st, "wQs", 2, [128, L], BF16)
        tAr = Rot(nc, st, "wtA", 2, [128, 512], F32)
        tBr = Rot(nc, st, "wtB", 2, [128, 512], F32)
        k.att_rv = Rot(nc, st, "watt_rv", 2, [128, 512], F32)
        P.op("pool", lambda e: e.memset(VW[:], 1.0), writes=["VW"])
        P.dma("sp", esk[:], k.win_sink[li], writes=["esk"])
        P.op("act", lambda e: e.activation(out=esk[:], in_=esk[:], func=AF.Exp), reads=["esk"], writes=["esk"])
        P.dma("sp", Qp[:], k.zT[ZT_WQ * 128:(ZT_WQ + 8) * 128, :].rearrange("(k p) t -> p k t", p=128), reads=["zT"], writes=["Qp"])
        P.dma("sp", Kp[:], k.zT[ZT_WK * 128:(ZT_WK + 1) * 128, :], reads=["zT"], writes=["Kp"])
        P.dma("sp", rope[:], k.rope_win.rearrange("c p t -> p c t"), writes=["rope"])
        P.dma("pool", msk[:], k.wmask.rearrange("c p t -> p c t"), writes=["msk"])
        for c_ in range(2):
            P.dma("sp", VW[:, :, c_, 0:64], k.vtok[:, c_ * 64:(c_ + 1) * 64].rearrange("(t p) d -> p t d", p=128), reads=["vtok", "VW"], writes=["VW"])
        for j in range(9):
            qs_, qsk = Qsr.next()
            srow = (ZT_WQS + j) * 128 if j < 8 else ZT_WKS * 128
            P.dma("sp", qs_[:], k.zT[srow:srow + 128, LC:NT], reads=["zT"], writes=[qsk])
            for c in range(4):
                sl = slice(c * 512, (c + 1) * 512)
                sln = slice(LC + c * 512, LC + (c + 1) * 512)
                src = Qp[:, j, sln] if j < 8 else Kp[:, sln]
                dst = Qr[:, j, sl] if j < 8 else Kr[:, sl]
                tA, tAk = tAr.next()
                tB, tBk = tBr.next()
                P.op("dve", lambda e, sl=sl, src=src, tA=tA: e.tensor_tensor(tA[:], src, rope[:, 0, sl], ALU.mult), reads=["Qp", "Kp", "rope"], writes=[tAk])
                P.op("dve", lambda e, sl=sl, qs_=qs_, tB=tB: e.tensor_tensor(tB[:], qs_[:, sl], rope[:, 1, sl], ALU.mult), reads=[qsk, "rope"], writes=[tBk])
                P.op("dve", lambda e, dst=dst, tA=tA, tB=tB: e.tensor_tensor(dst, tA[:], tB[:], ALU.add), reads=[tAk, tBk], writes=["Qr", "Kr"])
        PTr = Rot(nc, st, "wPT", 4, [128, 512], BF16)
        ostr = Rot(nc, st, "wost", 3, [128, 512], BF16)
        for h in range(8):
            kk = h // 4
            for n in range(17):
                if n < 16:
                    q0, nq = LC + n * 128, 128
                    chunks = [("b", n + d, d) for d in (-1, 0, 1) if 0 <= n + d < 16] + [("c", 0, 0), ("c", 1, 0)]
                else:
                    q0, nq = 0, LC
                    chunks = [("c", 0, 0), ("c", 1, 0)]

                def score_fn(kc, pscr, psk, chunks=chunks, q0=q0, nq=nq, h=h):
                    typ, ci, d = chunks[kc]
                    if typ == "b":
                        P.op("pe", lambda e: e.matmul(pscr[:, :nq], Kr[:, ci * 128:(ci + 1) * 128], Qr[:, h, q0 - LC:q0 - LC + nq], start=True, stop=True), reads=["Kr", "Qr"], writes=[psk])
                    else:
                        P.op("pe", lambda e: e.matmul(pscr[:, :nq], Kp[:, ci * 128:(ci + 1) * 128], Qp[:, h, q0:q0 + nq], start=True, stop=True), reads=["Kp", "Qp"], writes=[psk])

                def post(kc, pt, ptk, chunks=chunks):
                    typ, ci, d = chunks[kc]
                    if typ == "b" and d != 0:
                        mi = 0 if d == -1 else 1
                        P.op("dve", lambda e: e.tensor_tensor(pt[:, :128], pt[:, :128], msk[:, mi, :], ALU.mult), reads=[ptk, "msk"], writes=[ptk])

                def va_fn(kc, chunks=chunks, kk=kk):
                    typ, ci, d = chunks[kc]
                    tt = (2 + ci) if typ == "b" else ci
                    return VW[:, tt, kk, :], "VW"

                softmax_pv(k, ostr, score_fn, len(chunks), va_fn, nq, SC, k.brT[2][h * 64:(h + 1) * 64, q0:q0 + nq], "winT", PTr,
                           esk=esk[64:128, h:h + 1], post=post)
        stage_end(k)


def stage_merge(k, li):
    nc, P = k.nc, k.P
    with ExitStack() as st:
        sb = lambda name, shape, dt=F32: st.enter_context(nc.sbuf_tensor(U(name), shape, dt))
        wbr = sb("wbr", [128, 12, D], BF16)
        wout = sb("wout", [128, 8, D], BF16)
        lnp = sb("lnp", [128, 4, 8])
        k.lnp_t = lnp
        P.dma("sp", lnp[:], k.lnp[li], writes=["mod"])
        for j in range(12):
            P.dma("pool", wbr[:, j, :], k.w_branch[li][j * 128:(j + 1) * 128, :], writes=["wbr"])
        for j in range(8):
            P.dma("pool", wout[:, j, :], k.w_out[li][j * 128:(j + 1) * 128, :], writes=["wout"])
        tmp = ln_tmp(nc, st)
        obr = [sb("ob%d" % b, [128, 4, 512], BF16) for b in range(3)]
        gbr = [sb("gb%d" % b, [128, 8, 512], BF16) for b in range(3)]
        mT = sb("mT", [128, 8, 512], BF16)
        m1r = Rot(nc, st, "mgt1", 2, [128, 512], F32)
        m2r = Rot(nc, st, "mgt2", 3, [128, 512], F32)
        ytr = Rot(nc, st, "mgty", 2, [128, 512], F32)
        xb = sb("mxb", [128, 8, 512])
        rb = sb("mrb", [128, 8, 512])
        x1 = sb("mx1", [128, 8, 512])
        h2 = sb("mh2", [128, 8, 512], BF16)
        xTv = k.xT.rearrange("(k p) t -> p k t", p=128)
        h2v = k.h2T.rearrange("(k p) t -> p k t", p=128)
        for (t0, w, isc) in TBS:
            sl = slice(t0, t0 + w)
            for b in range(3):
                P.dma("sp", obr[b][:, :, :w], k.brT[b][:, sl].rearrange("(k p) t -> p k t", p=128), reads=["brT"], writes=["ob%d" % b])
                P.dma("sp", gbr[b][:, :, :w], k.zT[(ZT_G + 8 * b) * 128:(ZT_G + 8 + 8 * b) * 128, sl].rearrange("(k p) t -> p k t", p=128), reads=["zT"], writes=["gb%d" % b])
            P.dma("sp", xb[:, :, :w], xTv[:, :, sl], reads=["xT"], writes=["mxb"])
            for mo in range(8):
                mt1, m1k = m1r.next()
                for b in range(3):
                    pt, pk = psn(k)
                    for kk in range(4):
                        P.op("pe", lambda e, pt=pt, kk=kk, b=b, mo=mo, w=w: e.matmul(pt[:, :w], wbr[:, b * 4 + kk, mo * 128:(mo + 1) * 128], obr[b][:, kk, :w], start=(kk == 0), stop=(kk == 3)), reads=["wbr", "ob%d" % b], writes=[pk])
                    if b == 0:
                        P.op("dve", lambda e, pt=pt, mo=mo, w=w, mt1=mt1: e.tensor_tensor(mt1[:, :w], pt[:, :w], gbr[0][:, mo, :w], ALU.mult), reads=[pk, "gb0"], writes=[m1k])
                    elif b == 1:
                        mt2, m2k = m2r.next()
                        P.op("dve", lambda e, pt=pt, mo=mo, w=w, mt2=mt2: e.tensor_tensor(mt2[:, :w], pt[:, :w], gbr[1][:, mo, :w], ALU.mult), reads=[pk, "gb1"], writes=[m2k])
                        P.op("dve", lambda e, w=w, mt1=mt1, mt2=mt2: e.tensor_tensor(mt1[:, :w], mt1[:, :w], mt2[:, :w], ALU.add), reads=[m1k, m2k], writes=[m1k])
                    else:
                        mt2, m2k = m2r.next()
                        P.op("dve", lambda e, pt=pt, mo=mo, w=w, mt2=mt2: e.tensor_tensor(mt2[:, :w], pt[:, :w], gbr[2][:, mo, :w], ALU.mult), reads=[pk, "gb2"], writes=[m2k])
                        P.op("dve", lambda e, mo=mo, w=w, mt1=mt1, mt2=mt2: e.tensor_tensor(mT[:, mo, :w], mt1[:, :w], mt2[:, :w], ALU.add), reads=[m1k, m2k], writes=["mT"])
            for mo in range(8):
                pt, pk = psn(k)
                for kk in range(8):
                    P.op("pe", lambda e, pt=pt, kk=kk, mo=mo, w=w: e.matmul(pt[:, :w], wout[:, kk, mo * 128:(mo + 1) * 128], mT[:, kk, :w], start=(kk == 0), stop=(kk == 7)), reads=["wout", "mT"], writes=[pk])
                yt, ytk = ytr.next()
                P.op("act", lambda e, pt=pt, mo=mo, w=w, isc=isc, yt=yt: e.activation(out=yt[:, :w], in_=pt[:, :w], func=AF.Identity, scale=k.mod[:, li, 16 + mo, isc:isc + 1]), reads=[pk, "mod"], writes=[ytk])
                P.op("dve", lambda e, mo=mo, w=w, yt=yt: e.scalar_tensor_tensor(rb[:, mo, :w], xb[:, mo, :w], float(ALPHA), yt[:, :w], ALU.mult, ALU.add), reads=["mxb", ytk], writes=["mrb"])
            ln_stats(k, rb, "mrb", w, tmp)
            for kk in range(8):
                ln_apply(k, rb, "mrb", w, tmp, kk, x1[:, kk, :w], ["mx1"], lnp[:, 0, kk:kk + 1], lnp[:, 1, kk:kk + 1])
            P.dma("sp", xTv[:, :, sl], x1[:, :, :w], reads=["mx1"], writes=["xT"])
            ln_stats(k, x1, "mx1", w, tmp)
            for kk in range(8):
                ln_apply(k, x1, "mx1", w, tmp, kk, h2[:, kk, :w], ["mh2"], k.mod[:, li, 32 + kk, isc:isc + 1], k.mod[:, li, 24 + kk, isc:isc + 1])
            P.dma("sp", h2v[:, :, sl], h2[:, :, :w], reads=["mh2"], writes=["h2T"])
        stage_end(k)


TB9 = [(0, 256, 1)] + [(256 + i * 256, 256, 0) for i in range(8)]


def stage_moe(k, li):
    nc, P = k.nc, k.P
    with ExitStack() as st0:
        sb0 = lambda name, shape, dt=F32: st0.enter_context(nc.sbuf_tensor(U(name), shape, dt))
        lnp = sb0("elnp", [128, 4, 8])
        posm = sb0("eposm", [16, NT])
        sel = sb0("esel", [16, 16, 128])
        iop = sb0("eiop", [128, 3])
        P.dma("sp", lnp[:], k.lnp[li], writes=["mod"])
        P.dma("sp", sel[:], k.sel16, writes=["esel"])
        P.dma("sp", iop[:], k.iota_p3, writes=["eiop"])
        with ExitStack() as st1:
            sb1 = lambda name, shape, dt=F32: st1.enter_context(nc.sbuf_tensor(U(name), shape, dt))
            h2tok = sb1("eh2tok", [128, 18, D], BF16)
            posm_tok = sb1("eposmt", [128, 18, 16])
            gw_tok = sb1("egwt", [128, 18, 16], BF16)
            ios = sb1("eios", [128, 384])
            P.dma("sp", ios[:], k.iota_s, writes=["eios"])
            with ExitStack() as stA:
                sbA = lambda name, shape, dt=F32: stA.enter_context(nc.sbuf_tensor(U(name), shape, dt))
                h2 = sbA("eh2", [128, 8, NT], BF16)
                wr = sbA("ewr", [128, 8, 16], BF16)
                aff = sbA("eaff", [16, NT])
                wk_ = sbA("ewk", [16, NT])
                gw = sbA("egw", [16, NT])
                msk = sbA("emsk", [16, NT])
                m8 = sbA("em8", [16, 8])
                thr = sbA("ethr", [16, 2])
                P.dma("sp", h2[:], k.h2T.rearrange("(k p) t -> p k t", p=128), reads=["h2T"], writes=["eh2"])
                P.dma("pool", wr[:], k.w_router[li].rearrange("(k p) n -> p k n", p=128), writes=["ewr"])
                for (t0, w, isc) in TBS:
                    sl = slice(t0, t0 + w)
                    pt, pk = psn(k)
                    for kk in range(8):
                        P.op("pe", lambda e, pt=pt, kk=kk, sl=sl, w=w: e.matmul(pt[0:16, :w], wr[:, kk, :], h2[:, kk, sl], start=(kk == 0), stop=(kk == 7)), reads=["ewr", "eh2"], writes=[pk])
                    P.op("act", lambda e, pt=pt, sl=sl, w=w: e.activation(out=wk_[:, sl], in_=pt[0:16, :w], func=AF.Exp), reads=[pk], writes=["ewk"])
                    p2, k2 = psn(k)
                    P.op("pe", lambda e, p2=p2, sl=sl, w=w: e.matmul(p2[0:16, :w], k.onesf[0:16, 0:16], wk_[:, sl], start=True, stop=True), reads=["ewk", "onesf"], writes=[k2])
                    P.op("dve", lambda e, p2=p2, sl=sl, w=w: e.reciprocal(gw[:, sl], p2[0:16, :w]), reads=[k2], writes=["egw"])
                    P.op("dve", lambda e, sl=sl: e.tensor_tensor(aff[:, sl], wk_[:, sl], gw[:, sl], ALU.mult), reads=["ewk", "egw"], writes=["eaff"])
                P.op("dve", lambda e: e.tensor_copy(wk_[:], aff[:]), reads=["eaff"], writes=["ewk"])
                for si, (a_, b_, cap, off) in enumerate(((0, LC, 32, 256.0), (LC, NT, 256, 0.0))):
                    for r in range(cap // 8):
                        P.op("dve", lambda e, a_=a_, b_=b_: e.max(out=m8[:], in_=wk_[:, a_:b_]), reads=["ewk"], writes=["em8"])
                        if r < cap // 8 - 1:
                            P.op("dve", lambda e, a_=a_, b_=b_: e.match_replace(out=wk_[:, a_:b_], in_to_replace=m8[:], in_values=wk_[:, a_:b_], imm_value=-1.0), reads=["ewk", "em8"], writes=["ewk"])
                    P.op("dve", lambda e, si=si: e.tensor_copy(thr[:, si:si + 1], m8[:, 7:8]), reads=["em8"], writes=["ethr"])
                    P.op("dve", lambda e, a_=a_, b_=b_, si=si: e.tensor_scalar(msk[:, a_:b_], aff[:, a_:b_], thr[:, si:si + 1], None, ALU.is_ge), reads=["eaff", "ethr"], writes=["emsk"])
                    P.op("dve", lambda e, a_=a_, b_=b_: e.tensor_tensor(gw[:, a_:b_], aff[:, a_:b_], msk[:, a_:b_], ALU.mult), reads=["eaff", "emsk"], writes=["egw"])
                    P.op("dve", lambda e, a_=a_, b_=b_: e.tensor_tensor_scan(wk_[:, a_:b_], k.onesf[0:16, 0:1].to_broadcast([16, b_ - a_]), msk[:, a_:b_], 0.0, ALU.mult, ALU.add), reads=["emsk", "onesf", "ewk"], writes=["ewk"])
                    P.op("dve", lambda e, a_=a_, b_=b_, off=off: e.scalar_tensor_tensor(posm[:, a_:b_], wk_[:, a_:b_], float(off), msk[:, a_:b_], ALU.add, ALU.mult), reads=["ewk", "emsk"], writes=["eposm"])
                    P.op("dve", lambda e, a_=a_, b_=b_: e.tensor_scalar_add(posm[:, a_:b_], posm[:, a_:b_], -1.0), reads=["eposm"], writes=["eposm"])
                for (src, dst, skey, dkey) in ((posm, posm_tok, "eposm", "eposmt"), (gw, gw_tok, "egw", "egwt")):
                    pt, pk = psn(k)
                    for tt in range(18):
                        P.op("pe", lambda e, pt=pt, tt=tt, src=src: e.transpose(pt[:, tt * 16:(tt + 1) * 16], src[0:16, tt * 128:(tt + 1) * 128], k.identf[0:16, 0:16]), reads=[skey, "identf"], writes=[pk])
                    P.op("dve", lambda e, pt=pt, dst=dst: e.tensor_copy(dst[:], pt[:, 0:288].rearrange("p (t e) -> p t e", e=16)), reads=[pk], writes=[dkey])
                for tt in range(18):
                    pt, pk = psn(k)
                    ptb = pt[:].bitcast(BF16)
                    for kf in range(8):
                        P.op("pe", lambda e, ptb=ptb, kf=kf, tt=tt: e.transpose(ptb[:, kf * 128:(kf + 1) * 128], h2[:, kf, tt * 128:(tt + 1) * 128], k.identb[:]), reads=["eh2", "identb"], writes=[pk])
                    if tt % 2 == 0:
                        P.op("act", lambda e, ptb=ptb, tt=tt: e.copy(h2tok[:, tt, :], ptb[:, 0:1024]), reads=[pk], writes=["eh2tok"])
                    else:
                        P.op("dve", lambda e, ptb=ptb, tt=tt: e.tensor_copy(h2tok[:, tt, :], ptb[:, 0:1024]), reads=[pk], writes=["eh2tok"])
                P.barrier()
            mw = Rot(nc, st1, "emw", 32, [128, D], BF16)
            Sr = Rot(nc, st1, "eS", 2, [128, 18, 384], BF16)
            Xr = Rot(nc, st1, "eX", 2, [128, 8, 288], BF16)
            Ar = Rot(nc, st1, "eA", 2, [128, 8, 384], BF16)
            Yr = Rot(nc, st1, "eY", 2, [128, 3, D], BF16)
            sar = Rot(nc, st1, "esa", 2, [128, 288], F32)
            gsr = Rot(nc, st1, "egs", 2, [128, 3], F32)
            for j in range(2):
                At, Ak = Ar.next()
                P.op("pool", lambda e, At=At: e.memset(At[:], 0.0), writes=[Ak])
            ev = 0
            for ex in range(16):
                ws = {}
                for nm, src in (("g", k.w_gate), ("u", k.w_up), ("d", k.w_down)):
                    for kk in range(8):
                        t, tk = mw.next()
                        P.dma("pool", t[:], src[li, ex, kk * 128:(kk + 1) * 128, :], writes=[tk])
                        ws[(nm, kk)] = (t, tk)
                S, Sk = Sr.next()
                P.op("dve", lambda e, S=S, ex=ex: e.tensor_tensor(S[:], ios[:].unsqueeze(1).to_broadcast([128, 18, 384]), posm_tok[:, :, ex:ex + 1].to_broadcast([128, 18, 384]), ALU.is_equal), reads=["eios", "eposmt"], writes=[Sk])
                X, Xk = Xr.next()
                for ft in range(8):
                    pt, pk = psn(k, 0, 4)
                    for tt in range(2, 18):
                        P.op("pe", lambda e, pt=pt, tt=tt, ft=ft, S=S: e.matmul(pt[:, 0:256], h2tok[:, tt, ft * 128:(ft + 1) * 128], S[:, tt, 0:256], start=(tt == 2), stop=(tt == 17)), reads=["eh2tok", Sk], writes=[pk])
                    for tt in range(2):
                        P.op("pe", lambda e, pt=pt, tt=tt, ft=ft, S=S: e.matmul(pt[:, 256:288], h2tok[:, tt, ft * 128:(ft + 1) * 128], S[:, tt, 256:288], start=(tt == 0), stop=(tt == 1)), reads=["eh2tok", Sk], writes=[pk])
                    if ev % 2 == 0:
                        P.op("act", lambda e, pt=pt, ft=ft, X=X: e.copy(X[:, ft, :], pt[:, :288]), reads=[pk], writes=[Xk])
                    else:
                        P.op("dve", lambda e, pt=pt, ft=ft, X=X: e.tensor_copy(X[:, ft, :], pt[:, :288]), reads=[pk], writes=[Xk])
                    ev += 1
                pg, pgk = psn(k, 0, 4)
                for st_ in range(3):
                    tts = list(range(2, 18)) if st_ < 2 else [0, 1]
                    for tt in tts:
                        P.op("pe", lambda e, pg=pg, tt=tt, st_=st_, S=S, ex=ex, tts=tts: e.matmul(pg[:, st_:st_ + 1], S[:, tt, st_ * 128:(st_ + 1) * 128], gw_tok[:, tt, ex:ex + 1], start=(tt == tts[0]), stop=(tt == tts[-1])), reads=[Sk, "egwt"], writes=[pgk])
                gs, gsk = gsr.next()
                P.op("dve", lambda e, pg=pg, gs=gs: e.tensor_copy(gs[:], pg[:, 0:3]), reads=[pgk], writes=[gsk])
                At, Ak = Ar.next()
                for fo in range(8):
                    pa, pak = psn(k, 4, 6)
                    pu, puk = psn(k, 6, 8)
                    for kk in range(8):
                        wt, wtk = ws[("g", kk)]
                        P.op("pe", lambda e, pa=pa, wt=wt, kk=kk, fo=fo, X=X: e.matmul(pa[:, :288], wt[:, fo * 128:(fo + 1) * 128], X[:, kk, :], start=(kk == 0), stop=(kk == 7)), reads=[wtk, Xk], writes=[pak])
                    for kk in range(8):
                        wt, wtk = ws[("u", kk)]
                        P.op("pe", lambda e, pu=pu, wt=wt, kk=kk, fo=fo, X=X: e.matmul(pu[:, :288], wt[:, fo * 128:(fo + 1) * 128], X[:, kk, :], start=(kk == 0), stop=(kk == 7)), reads=[wtk, Xk], writes=[puk])
                    s_, sk_ = sar.next()
                    P.op("act", lambda e, pa=pa, s_=s_: e.activation(out=s_[:], in_=pa[:, :288], func=AF.Silu), reads=[pak], writes=[sk_])
                    P.op("dve", lambda e, pu=pu, s_=s_, At=At, fo=fo: e.tensor_tensor(At[:, fo, 0:288], pu[:, :288], s_[:], ALU.mult), reads=[puk, sk_], writes=[Ak])
                Y, Yk = Yr.next()
                for st_ in range(3):
                    for half in range(2):
                        py, pyk = psn(k, 0, 4)
                        for kk in range(8):
                            wt, wtk = ws[("d", kk)]
                            P.op("pe", lambda e, py=py, wt=wt, kk=kk, st_=st_, half=half, At=At: e.matmul(py[:, :512], At[:, kk, st_ * 128:(st_ + 1) * 128], wt[:, half * 512:(half + 1) * 512], start=(kk == 0), stop=(kk == 7)), reads=[wtk, Ak], writes=[pyk])
                        P.op("act", lambda e, py=py, st_=st_, half=half, Y=Y, gs=gs: e.activation(out=Y[:, st_, half * 512:(half + 1) * 512], in_=py[:, :512], func=AF.Identity, scale=gs[:, st_:st_ + 1]), reads=[pyk, gsk], writes=[Yk])
                P.dma("sp", k.yg[ex].rearrange("s p d -> p s d"), Y[:], reads=[Yk], writes=["yg"])
            P.barrier()
        with ExitStack() as st2:
            sb2 = lambda name, shape, dt=F32: st2.enter_context(nc.sbuf_tensor(U(name), shape, dt))
            Yall = sb2("eYall", [128, 48, D], BF16)
            for ex in range(16):
                P.dma("sp", Yall[:, ex * 3:(ex + 1) * 3, :], k.yg[ex].rearrange("s p d -> p s d"), reads=["yg"], writes=["eYall"])
            STr = Rot(nc, st2, "eST", 1, [128, 48, 256], BF16)
            tmp = ln_tmp(nc, st2, 256)
            xbr = Rot(nc, st2, "exb", 2, [128, 8, 256], F32)
            rb = sb2("erb", [128, 8, 256])
            xTv = k.xT.rearrange("(k p) t -> p k t", p=128)
            for (t0, w, isc) in TB9:
                sl = slice(t0, t0 + w)
                xb, xbk = xbr.next()
                P.dma("sp", xb[:], xTv[:, :, sl], reads=["xT"], writes=[xbk])
                ST, STk = STr.next()
                sts = [2] if isc else [0, 1]
                for ex in range(16):
                    pb, pbk = psn(k, 4, 8)
                    P.op("pe", lambda e, pb=pb, sl=sl, ex=ex: e.matmul(pb[:, :256], sel[:, ex, :], posm[:, sl], start=True, stop=True), reads=["esel", "eposm"], writes=[pbk])
                    n_ = len(sts)
                    P.op("dve", lambda e, pb=pb, ex=ex, ST=ST, sts=sts, n_=n_: e.tensor_tensor(ST[:, ex * 3 + sts[0]:ex * 3 + sts[0] + n_, :], pb[:, 0:256].unsqueeze(1).to_broadcast([128, n_, 256]), iop[:, sts[0]:sts[0] + n_].unsqueeze(2).to_broadcast([128, n_, 256]), ALU.is_equal), reads=[pbk, "eiop"], writes=[STk])
                js = [ex * 3 + st_ for ex in range(16) for st_ in sts]
                for mo in range(8):
                    pf, pfk = psn(k, 0, 4)
                    for j in js:
                        P.op("pe", lambda e, pf=pf, j=j, mo=mo, ST=ST, js=js: e.matmul(pf[:, :256], Yall[:, j, mo * 128:(mo + 1) * 128], ST[:, j, :], start=(j == js[0]), stop=(j == js[-1])), reads=["eYall", STk], writes=[pfk])
                    ft_, ftk = tmp["tr"].next()
                    P.op("act", lambda e, pf=pf, mo=mo, isc=isc, ft_=ft_: e.activation(out=ft_[:, :256], in_=pf[:, :256], func=AF.Identity, scale=k.mod[:, li, 40 + mo, isc:isc + 1]), reads=[pfk, "mod"], writes=[ftk])
                    P.op("dve", lambda e, mo=mo, xb=xb, ft_=ft_: e.scalar_tensor_tensor(rb[:, mo, :], xb[:, mo, :], float(ALPHA), ft_[:, :256], ALU.mult, ALU.add), reads=[xbk, ftk], writes=["erb"])
                ln_stats(k, rb, "erb", w, tmp)
                for kk in range(8):
                    ln_apply(k, rb, "erb", w, tmp, kk, xb[:, kk, :], [xbk], lnp[:, 2, kk:kk + 1], lnp[:, 3, kk:kk + 1])
                P.dma("sp", xTv[:, :, sl], xb[:], reads=[xbk], writes=["xT"])
        stage_end(k)


def stage_out(k):
    nc, P = k.nc, k.P
    with ExitStack() as st:
        xr = Rot(nc, st, "oxr", 2, [128, 8, 128], F32)
        xo = Rot(nc, st, "oxo", 2, [128, D], F32)
        xTv = k.xT.rearrange("(k p) t -> p k t", p=128)
        for tt in range(L // 128):
            xt, xk = xr.next()
            P.dma("sp", xt[:], xTv[:, :, LC + tt * 128:LC + (tt + 1) * 128], reads=["xT"], writes=[xk])
            ot, ok = xo.next()
            for half in range(2):
                pt, pk = psn(k)
                for kk in range(4):
                    kf = half * 4 + kk
                    P.op("pe", lambda e, pt=pt, kk=kk, kf=kf, xt=xt: e.transpose(pt[:, kk * 128:(kk + 1) * 128], xt[:, kf, :], k.identf[:]),
                         reads=[xk, "identf"], writes=[pk])
                if half == 0:
                    P.op("act", lambda e, pt=pt, ot=ot: e.copy(ot[:, 0:512], pt[:]), reads=[pk], writes=[ok + "h0"])
                else:
                    P.op("dve", lambda e, pt=pt, ot=ot: e.tensor_copy(ot[:, 512:1024], pt[:]), reads=[pk], writes=[ok + "h1"])
            P.dma("sp", k.out[tt * 128:(tt + 1) * 128, :], ot[:], reads=[ok + "h0", ok + "h1"], writes=["out"])
        stage_end(k)


Z_ORDER = None


def _zcols():
    u = np.arange(0, 512)
    qa = np.arange(512, 896)
    kva = np.arange(896, 1152)
    kr = np.arange(1152, 1184)
    wq = np.arange(1184, 1696)
    wk = np.arange(1696, 1824)
    wv = np.arange(1824, 1952)
    gates = np.arange(1952, 5024)
    Z = -np.ones(64, np.int64)

    def padq(cols8):
        out = []
        for h in range(8):
            kk = h // 4
            out.append(np.concatenate([cols8[h], Z]) if kk == 0 else np.concatenate([Z, cols8[h]]))
        return np.concatenate(out)
    wq8 = wq.reshape(8, 64)
    wq8s = wq.reshape(8, 2, 32)[:, ::-1, :].reshape(8, 64)
    wk_sw = wk.reshape(2, 2, 32)[:, ::-1, :].reshape(-1)
    kr_sw = kr.reshape(2, 16)[::-1].reshape(-1)
    cols = np.concatenate([u, qa, kva, padq(wq8), wk, wv, gates, padq(wq8s), wk_sw, kr, kr_sw, Z])
    assert cols.size == NZT * 128, cols.size
    return cols


def prep_shared(inp):
    f = lambda a: np.ascontiguousarray(np.asarray(a, np.float32))
    sh = {}
    sh["ident"] = np.eye(128, dtype=np.float32)
    sh["w_ada"] = f(inp["w_ada"])
    b = f(inp["b_ada"]).reshape(DEPTH, 48, 128).transpose(0, 2, 1)
    sh["bada2"] = f(np.repeat(b[:, :, :, None], 2, axis=3))
    cols = _zcols()
    wx = f(inp["w_in"])[:, :, np.maximum(cols, 0)]
    wx[:, :, cols < 0] = 0.0
    sh["w_inx"] = f(wx)
    G, PS, HG = 32, 64, 16
    bT = np.zeros((DEPTH, 2, 2, 16, 128, 128), np.float32)
    cL = np.zeros((DEPTH, 128, 2, 16, 2, 16), np.float32)
    lane = np.zeros((DEPTH, 128, 3, 32), np.float32)
    for ri, nm in enumerate(("s5_b_re", "s5_b_im")):
        bsrc = f(inp[nm])
        for lt in range(16):
            for half in range(2):
                g = 2 * lt + half
                gl = g % 8
                bT[:, :, ri, lt, gl * 16:(gl + 1) * 16, half * 64:(half + 1) * 64] = bsrc[:, :, g].transpose(0, 1, 3, 2)
    for ri, nm in enumerate(("s5_c_re", "s5_c_im")):
        csrc = f(inp[nm])
        for lt in range(16):
            for half in range(2):
                g = 2 * lt + half
                cL[:, half * 64:(half + 1) * 64, :, lt, ri, :] = csrc[:, :, g].transpose(0, 3, 1, 2)
    lre, lim, ldt = f(inp["s5_lam_re"]), f(inp["s5_lam_im"]), f(inp["s5_log_dt"])
    for d in range(2):
        for lt in range(16):
            for half in range(2):
                g = 2 * lt + half
                lane[:, half * 64:(half + 1) * 64, 0, d * 16 + lt] = lre[:, d, g, :]
                lane[:, half * 64:(half + 1) * 64, 1, d * 16 + lt] = lim[:, d, g, :]
                lane[:, half * 64:(half + 1) * 64, 2, d * 16 + lt] = ldt[:, d, g][:, None]
    sh["s5_bT"], sh["s5_cL"], sh["s5_lane"] = bT, cL, lane
    dg = np.zeros((DEPTH, 128, 2, 4), np.float32)
    dg[:, :, 0, :] = f(inp["s5_d"]).reshape(DEPTH, 4, 128).transpose(0, 2, 1)
    dg[:, :, 1, :] = f(inp["s5_b_glu"]).reshape(DEPTH, 4, 128).transpose(0, 2, 1)
    sh["s5_dg"] = dg
    sh["s5_wglu"] = f(inp["s5_w_glu"])
    mg = np.zeros((DEPTH, 128, 5), np.float32)
    mg[:, :, 0:3] = f(inp["mla_q_norm"]).reshape(DEPTH, 3, 128).transpose(0, 2, 1)
    mg[:, :, 3:5] = f(inp["mla_kv_norm"]).reshape(DEPTH, 2, 128).transpose(0, 2, 1)
    sh["mla_g"] = mg
    wuq = f(inp["mla_w_uq"]).reshape(DEPTH, 384, 8, 96)
    wm = np.zeros((DEPTH, 384, 8, 128), np.float32)
    wsw = np.zeros((DEPTH, 384, 8, 128), np.float32)
    wm[..., 0:96] = wuq
    wsw[..., 64:96] = wuq[..., 64:].reshape(DEPTH, 384, 8, 2, 16)[:, :, :, ::-1, :].reshape(DEPTH, 384, 8, 32)
    sh["w_uqx"] = f(np.concatenate([wm.reshape(DEPTH, 384, 1024), wsw.reshape(DEPTH, 384, 1024)], -1))
    wkv = f(inp["mla_w_ukv"]).reshape(DEPTH, 256, 8, 128)
    wkp = np.zeros((DEPTH, 256, 8, 128), np.float32)
    wkp[..., 0:64] = wkv[..., :64]
    sh["w_ukvk"] = f(wkp.reshape(DEPTH, 256, 1024))
    sh["w_ukvv"] = f(wkv[..., 64:].reshape(DEPTH, 256, 512))
    rows = L // 64
    row = np.repeat(np.arange(rows, dtype=np.float64), 64)
    col = np.tile(np.arange(64, dtype=np.float64), rows)

    def rope_tab(d, reps):
        nf = d // 4
        fr = 10000.0 ** (-np.arange(nf) / nf)
        ang = np.concatenate([row[:, None] * fr, col[:, None] * fr], -1)
        c = np.concatenate([np.cos(ang), np.cos(ang)], -1).T
        sn = np.concatenate([-np.sin(ang), np.sin(ang)], -1).T
        return np.stack([np.tile(c, (reps, 1)), np.tile(sn, (reps, 1))], 0).astype(np.float32)
    rm = np.zeros((2, 128, L), np.float32)
    rm[0] = 1.0
    rm[:, 64:96, :] = rope_tab(32, 1)
    sh["rope_mla"] = rm
    sh["rope_win"] = rope_tab(64, 2)
    jj = np.arange(128)[:, None]
    rr = np.arange(128)[None, :]
    sh["wmask"] = np.stack([(jj >= rr), (jj <= rr)], 0).astype(np.float32)
    sh["win_sink"] = f(np.broadcast_to(f(inp["win_sink"])[:, None, :], (DEPTH, 128, 8)))
    sh["w_branch"] = f(inp["w_branch"]).reshape(DEPTH, 1536, D)
    sh["w_out"] = f(inp["w_out"])
    lnp = np.stack([f(inp[n]).reshape(DEPTH, 8, 128).transpose(0, 2, 1) for n in ("ln1_g", "ln1_b", "ln2_g", "ln2_b")], 2)
    sh["lnp"] = f(lnp)
    sh["w_router"] = f(inp["w_router"])
    sh["w_gate"], sh["w_up"], sh["w_down"] = f(inp["w_gate"]), f(inp["w_up"]), f(inp["w_down"])
    sel = np.zeros((16, 16, 128), np.float32)
    for e_ in range(16):
        sel[e_, e_, :] = 1.0
    sh["sel16"] = sel
    sh["iota_s"] = f(np.broadcast_to(np.arange(384, dtype=np.float32), (128, 384)))
    sh["iota_p3"] = f(np.arange(128, dtype=np.float32)[:, None] + np.array([0.0, 128.0, 256.0], np.float32)[None, :])
    sh["tau"] = f(np.broadcast_to(np.arange(NT, dtype=np.float32), (128, NT)))
    return sh


def prep_core(inp, b):
    f = lambda a: np.ascontiguousarray(np.asarray(a, np.float32))
    m = {}
    m["xin"] = f(np.concatenate([inp["ctx"][b], inp["x"][b]], 0))
    cond = np.stack([np.asarray(inp["c"][b]), np.asarray(inp["c_ctx"])], -1)
    m["condT"] = f(cond.reshape(8, 128, 2).transpose(1, 0, 2))
    return m


def kernel(**inputs):
    nc = build()
    sh = prep_shared(inputs)
    in_maps = []
    for b in range(8):
        m = dict(sh)
        m.update(prep_core(inputs, b))
        in_maps.append(m)
    res = run_bass_kernel_spmd(nc, in_maps, core_ids=list(range(8)))
    return np.stack([np.asarray(r["out"], np.float32) for r in res.results], 0)
```

```python
import numpy as np
from contextlib import ExitStack
import concourse.bass as bass
import concourse.mybir as mybir
from concourse.bass_utils import run_bass_kernel_spmd

F32 = mybir.dt.float32
BF16 = mybir.dt.bfloat16
I32 = mybir.dt.int32
F32R = mybir.dt.float32r
AF = mybir.ActivationFunctionType
ALU = mybir.AluOpType

ENGS = ("pe", "dve", "act", "pool", "sp")
D = 1024
NT = 2304
LC = 256
L = 2048
DEPTH = 4
ALPHA = (2 * DEPTH) ** 0.25
EPS = 1e-6
NZT = 53
ZT_QA, ZT_KVA, ZT_WQ, ZT_WK, ZT_WV, ZT_G, ZT_WQS, ZT_WKS, ZT_KRX = 4, 7, 9, 17, 18, 19, 43, 51, 52
TBS = [(0, 256, 1), (256, 512, 0), (768, 512, 0), (1280, 512, 0), (1792, 512, 0)]


class Prog:
    def __init__(self, nc, es, n_dma_sems=24):
        self.nc = nc
        self.q = {e: [] for e in ENGS}
        self.sem = {}
        self.cnt = {}
        for e in ENGS:
            self.sem[e] = es.enter_context(nc.semaphore("s_" + e))
            self.cnt[e] = 0
        self.dma_sems = []
        for i in range(n_dma_sems):
            nm = "d%d" % i
            self.sem[nm] = es.enter_context(nc.semaphore("s_" + nm))
            self.cnt[nm] = 0
            self.dma_sems.append(nm)
        self.dma_rr = 0
        self.seen = {e: {} for e in ENGS}
        self.lastw = {}
        self.readers = {}
        self.nops = 0

    def _deps(self, eng, reads, writes):
        deps = {}

        def need(st, v):
            if st == eng and eng == "pe":
                return
            if deps.get(st, 0) < v:
                deps[st] = v

        for k in reads:
            lw = self.lastw.get(k)
            if lw is not None:
                need(*lw)
            if k.startswith("ps"):
                for r in self.readers.get(k, ()):
                    if r[0] != eng:
                        need(*r)
        for k in writes:
            lw = self.lastw.get(k)
            if lw is not None:
                need(*lw)
            for r in self.readers.get(k, ()):
                need(*r)
        return deps

    def _emit_waits(self, eng, deps):
        for st, v in deps.items():
            if self.seen[eng].get(st, 0) < v:
                self.seen[eng][st] = v
                sem = self.sem[st]
                self.q[eng].append(lambda e, sem=sem, v=v: e.wait_ge(sem, v))

    def _record(self, done, reads, writes):
        for k in reads:
            self.readers.setdefault(k, []).append(done)
        for k in writes:
            self.lastw[k] = done
            self.readers[k] = []

    def op(self, eng, fn, reads=(), writes=()):
        deps = self._deps(eng, reads, writes)
        self._emit_waits(eng, deps)
        self.cnt[eng] += 1
        sem = self.sem[eng]
        self.q[eng].append(lambda e, fn=fn, sem=sem: fn(e).then_inc(sem, 1))
        self._record((eng, self.cnt[eng]), reads, writes)
        self.nops += 1

    def dma(self, qeng, out, in_, reads=(), writes=(), **kw):
        st = self.dma_sems[self.dma_rr % len(self.dma_sems)]
        self.dma_rr += 1
        deps = self._deps(st, reads, writes)
        if self.cnt[st] > 0:
            deps[st] = self.cnt[st]
        self._emit_waits(qeng, deps)
        self.cnt[st] += 16
        sem = self.sem[st]
        self.q[qeng].append(
            lambda e, out=out, in_=in_, sem=sem, kw=kw: e.dma_start(out=out, in_=in_, **kw).then_inc(sem, 16))
        self._record((st, self.cnt[st]), reads, writes)
        self.nops += 1

    def barrier(self):
        for eng in ENGS:
            deps = {}
            for st in self.sem:
                if st != eng and self.cnt[st] > 0:
                    deps[st] = self.cnt[st]
            self._emit_waits(eng, deps)
        self.lastw = {}
        self.readers = {}

    def replay(self):
        nc = self.nc
        q = self.q
        with nc.Block() as block:
            @block.tensor
            def _(e):
                for f in q["pe"]:
                    f(e)

            @block.vector
            def _(e):
                for f in q["dve"]:
                    f(e)

            @block.scalar
            def _(e):
                for f in q["act"]:
                    f(e)

            @block.gpsimd
            def _(e):
                for f in q["pool"]:
                    f(e)

            @block.sync
            def _(e):
                for f in q["sp"]:
                    f(e)
        self.q = {e: [] for e in ENGS}


_UID = [0]


def U(name):
    _UID[0] += 1
    return "%s_u%d" % (name, _UID[0])


class Rot:
    def __init__(self, nc, es, name, n, shape, dtype, psum=False):
        self.name = name
        self.n = n
        self.i = 0
        if psum:
            self.t = [es.enter_context(nc.psum_tensor(U("%s%d" % (name, j)), shape, dtype)) for j in range(n)]
        else:
            self.t = [es.enter_context(nc.sbuf_tensor(U("%s%d" % (name, j)), shape, dtype)) for j in range(n)]

    def next(self):
        j = self.i % self.n
        self.i += 1
        return self.t[j], "%s%d" % (self.name, j)


class K:
    pass


def build(nlayers=DEPTH, dbg=()):
    nc = bass.Bass("TRN2", target_bir_lowering=False)
    k = K()
    k.nc = nc
    k.dbg = dbg
    din = lambda name, shape, dt=F32: nc.dram_tensor(name, list(shape), dt, kind="ExternalInput").ap()

    def dscr(name, shape, dt):
        kind = "ExternalOutput" if name in dbg else "Internal"
        return nc.dram_tensor(name, list(shape), dt, kind=kind).ap()

    k.xin = din("xin", [NT, D])
    k.condT = din("condT", [128, 8, 2])
    k.ident = din("ident", [128, 128])
    k.w_ada = din("w_ada", [DEPTH, D, 6 * D])
    k.bada2 = din("bada2", [DEPTH, 128, 48, 2])
    k.w_inx = din("w_inx", [DEPTH, D, NZT * 128])
    k.s5_bT = din("s5_bT", [DEPTH, 2, 2, 16, 128, 128])
    k.s5_cL = din("s5_cL", [DEPTH, 128, 2, 16, 2, 16])
    k.s5_lane = din("s5_lane", [DEPTH, 128, 3, 32])
    k.s5_dg = din("s5_dg", [DEPTH, 128, 2, 4])
    k.s5_wglu = din("s5_wglu", [DEPTH, 512, 512])
    k.tau = din("tau", [128, NT])
    k.mla_g = din("mla_g", [DEPTH, 128, 5])
    k.w_uqx = din("w_uqx", [DEPTH, 384, 2048])
    k.w_ukvk = din("w_ukvk", [DEPTH, 256, 1024])
    k.w_ukvv = din("w_ukvv", [DEPTH, 256, 512])
    k.rope_mla = din("rope_mla", [2, 128, L])
    k.rope_win = din("rope_win", [2, 128, L])
    k.wmask = din("wmask", [2, 128, 128])
    k.win_sink = din("win_sink", [DEPTH, 128, 8])
    k.w_branch = din("w_branch", [DEPTH, 1536, D])
    k.w_out = din("w_out", [DEPTH, D, D])
    k.lnp = din("lnp", [DEPTH, 128, 4, 8])
    k.w_router = din("w_router", [DEPTH, D, 16])
    k.w_gate = din("w_gate", [DEPTH, 16, D, D])
    k.w_up = din("w_up", [DEPTH, 16, D, D])
    k.w_down = din("w_down", [DEPTH, 16, D, D])
    k.sel16 = din("sel16", [16, 16, 128])
    k.iota_s = din("iota_s", [128, 384])
    k.iota_p3 = din("iota_p3", [128, 3])
    k.out = nc.dram_tensor("out", [L, D], F32, kind="ExternalOutput").ap()
    k.brT = [dscr(nm, [512, NT], BF16) for nm in ("s5T", "mlaT", "winT")]
    k.xT = dscr("xT", [D, NT], F32)
    k.zT = dscr("zT", [NZT * 128, NT], BF16)
    k.vtok = dscr("vtok", [NT, 128], BF16)
    k.h2T = dscr("h2T", [D, NT], BF16)
    k.yg = dscr("yg", [16, 3, 128, D], BF16)

    with ExitStack() as es:
        P = Prog(nc, es)
        k.P = P
        k.identf = es.enter_context(nc.sbuf_tensor(U("identf"), [128, 128], F32))
        k.identb = es.enter_context(nc.sbuf_tensor(U("identb"), [128, 128], BF16))
        k.onesm = es.enter_context(nc.sbuf_tensor(U("onesm"), [128, 128], F32))
        k.mod = es.enter_context(nc.sbuf_tensor(U("mod"), [128, DEPTH, 48, 2], F32))
        k.epsc = es.enter_context(nc.sbuf_tensor(U("epsc"), [128, 1], F32))
        k.ps = [es.enter_context(nc.psum_tensor("ps%d" % i, [128, 512], F32)) for i in range(8)]
        k.psi = 0

        stage_init(k)
        stage_ada(k, nlayers)
        k.onesmb = es.enter_context(nc.sbuf_tensor(U("onesmb"), [128, 128], BF16))
        P.op("dve", lambda e: e.memset(k.onesmb[:], 1.0 / D), writes=["onesmb"])
        k.onesb = es.enter_context(nc.sbuf_tensor(U("onesb"), [128, 512], BF16))
        k.onesf = es.enter_context(nc.sbuf_tensor(U("onesf"), [128, 128], F32))
        P.op("dve", lambda e: e.memset(k.onesb[:], 1.0), writes=["onesb"])
        P.op("dve", lambda e: e.memset(k.onesf[:], 1.0), writes=["onesf"])
        k.halfpi = es.enter_context(nc.sbuf_tensor(U("halfpi"), [128, 1], F32))
        P.op("dve", lambda e: e.memset(k.halfpi[:], float(np.pi / 2)), writes=["halfpi"])
        for li in range(nlayers):
            if "skip_win" not in dbg:
                stage_ln_win(k, li)
            if "mla_first" in dbg:
                stage_mla(k, li)
            if "skip_s5" not in dbg:
                stage_s5(k, li)
            if "skip_mla" not in dbg and "mla_first" not in dbg:
                stage_mla(k, li)
            if "skip_winb" not in dbg:
                stage_win(k, li)
            if "skip_mm" not in dbg:
                stage_merge(k, li)
                stage_moe(k, li)
        stage_out(k)
    return nc


def dump(k, name, ap, shape, dt, reads):
    if name not in k.dbg:
        return
    t = k.nc.dram_tensor(name, list(shape), dt, kind="ExternalOutput").ap()
    k.P.dma("sp", t, ap, reads=reads)


def psn(k, lo=0, hi=8):
    j = lo + (k.psi % (hi - lo))
    k.psi += 1
    return k.ps[j], "ps%d" % j


def stage_end(k):
    k.P.barrier()
    k.P.replay()


def stage_init(k):
    nc, P = k.nc, k.P
    P.dma("sp", k.identf[:], k.ident, writes=["identf"])
    P.dma("pool", k.identb[:], k.ident, writes=["identb"])
    P.op("dve", lambda e: e.memset(k.onesm[:], 1.0 / D), writes=["onesm"])
    P.op("dve", lambda e: e.memset(k.epsc[:], EPS), writes=["epsc"])
    with ExitStack() as st:
        xr = Rot(nc, st, "xr", 2, [128, D], F32)
        xo = Rot(nc, st, "xo", 2, [128, 8, 128], F32)
        xTv = k.xT.rearrange("(k p) t -> p k t", p=128)
        for tt in range(NT // 128):
            xt, xk = xr.next()
            P.dma("sp", xt[:], k.xin[tt * 128:(tt + 1) * 128, :], writes=[xk])
            ot, ok = xo.next()
            for half in range(2):
                pt, pk = psn(k)
                for kk in range(4):
                    kf = half * 4 + kk
                    P.op("pe", lambda e, pt=pt, kk=kk, kf=kf, xt=xt: e.transpose(pt[:, kk * 128:(kk + 1) * 128], xt[:, kf * 128:(kf + 1) * 128], k.identf[:]),
                         reads=[xk, "identf"], writes=[pk])
                eng = "act" if half == 0 else "dve"
                if eng == "act":
                    P.op("act", lambda e, pt=pt, ot=ot, half=half: e.copy(ot[:, half * 4:(half + 1) * 4, :], pt[:].rearrange("p (k t) -> p k t", k=4)),
                         reads=[pk], writes=[ok + "h%d" % half])
                else:
                    P.op("dve", lambda e, pt=pt, ot=ot, half=half: e.tensor_copy(ot[:, half * 4:(half + 1) * 4, :], pt[:].rearrange("p (k t) -> p k t", k=4)),
                         reads=[pk], writes=[ok + "h%d" % half])
            P.dma("sp", xTv[:, :, tt * 128:(tt + 1) * 128], ot[:], reads=[ok + "h0", ok + "h1"], writes=["xT"])
        stage_end(k)


def stage_ada(k, nlayers):
    nc, P = k.nc, k.P
    with ExitStack() as st:
        sc = st.enter_context(nc.sbuf_tensor(U("sc"), [128, 8, 2], F32))
        bt = st.enter_context(nc.sbuf_tensor(U("bt"), [128, DEPTH, 48, 2], F32))
        wa = Rot(nc, st, "wa", 2, [128, 8, 768], F32)
        P.dma("sp", sc[:], k.condT, writes=["sc"])
        P.dma("sp", bt[:], k.bada2.rearrange("l p m s -> p l m s"), writes=["bt"])
        P.op("act", lambda e: e.activation(out=sc[:], in_=sc[:], func=AF.Silu), reads=["sc"], writes=["sc"])
        for li in range(nlayers):
            wv = k.w_ada[li].rearrange("(k p) n -> p k n", p=128)
            for cb in range(8):
                wt, wk = wa.next()
                P.dma("sp", wt[:], wv[:, :, cb * 768:(cb + 1) * 768], writes=[wk])
                pt, pk = psn(k)
                for mt in range(6):
                    for kk in range(8):
                        P.op("pe", lambda e, pt=pt, wt=wt, mt=mt, kk=kk: e.matmul(pt[:, mt * 2:mt * 2 + 2], wt[:, kk, mt * 128:(mt + 1) * 128], sc[:, kk, :], start=(kk == 0), stop=(kk == 7)),
                             reads=[wk, "sc"], writes=[pk])
                P.op("dve", lambda e, pt=pt, li=li, cb=cb: e.tensor_tensor(k.mod[:, li, cb * 6:(cb + 1) * 6, :], pt[:, 0:12].rearrange("p (m s) -> p m s", s=2), bt[:, li, cb * 6:(cb + 1) * 6, :], ALU.add),
                     reads=[pk, "bt"], writes=["mod"])
            for j in (1, 4):
                P.op("dve", lambda e, li=li, j=j: e.tensor_scalar_add(k.mod[:, li, j * 8:(j + 1) * 8, :], k.mod[:, li, j * 8:(j + 1) * 8, :], 1.0),
                     reads=["mod"], writes=["mod"])
        stage_end(k)


def ln_stats(k, xb, xk, w, tmp, tag=""):
    nc, P = k.nc, k.P
    sq, mean, rstd, m2, xbf = tmp["sq"], tmp["mean"], tmp["rstd"], tmp["m2"], tmp["xbf"]
    P.op("act", lambda e: e.activation(out=sq[:, :, :w], in_=xb[:, :, :w], func=AF.Square), reads=[xk], writes=["sq" + tag])
    P.op("act", lambda e: e.copy(xbf[:, :, :w], xb[:, :, :w]), reads=[xk], writes=["xbf" + tag])
    p1, k1 = psn(k)
    p2, k2 = psn(k)
    for kk in range(8):
        P.op("pe", lambda e, kk=kk: e.matmul(p1[:, :w], k.onesmb[:], xbf[:, kk, :w], start=(kk == 0), stop=(kk == 7)), reads=["xbf" + tag, "onesmb"], writes=[k1])
    for kk in range(8):
        P.op("pe", lambda e, kk=kk: e.matmul(p2[:, :w], k.onesmb[:], sq[:, kk, :w], start=(kk == 0), stop=(kk == 7)), reads=["sq" + tag, "onesmb"], writes=[k2])
    P.op("act", lambda e: e.copy(mean[:, :w], p1[:, :w]), reads=[k1], writes=["mean" + tag])
    P.op("dve", lambda e: e.tensor_tensor(m2[:, :w], mean[:, :w], mean[:, :w], ALU.mult), reads=["mean" + tag], writes=["m2" + tag])
    P.op("dve", lambda e: e.tensor_tensor(m2[:, :w], p2[:, :w], m2[:, :w], ALU.subtract), reads=[k2, "m2" + tag], writes=["m2" + tag])
    P.op("act", lambda e: e.activation(out=m2[:, :w], in_=m2[:, :w], func=AF.Sqrt, bias=k.epsc[:], scale=1.0), reads=["m2" + tag, "epsc"], writes=["m2" + tag])
    P.op("dve", lambda e: e.reciprocal(rstd[:, :w], m2[:, :w]), reads=["m2" + tag], writes=["rstd" + tag])


def ln_tmp(nc, st, W=512):
    return {
        "sq": st.enter_context(nc.sbuf_tensor(U("ln_sq"), [128, 8, W], BF16)),
        "xbf": st.enter_context(nc.sbuf_tensor(U("ln_xbf"), [128, 8, W], BF16)),
        "mean": st.enter_context(nc.sbuf_tensor(U("ln_mean"), [128, W], F32)),
        "rstd": st.enter_context(nc.sbuf_tensor(U("ln_rstd"), [128, W], F32)),
        "m2": st.enter_context(nc.sbuf_tensor(U("ln_m2"), [128, W], F32)),
        "t": st.enter_context(nc.sbuf_tensor(U("ln_t"), [128, W], F32)),
        "tr": Rot(nc, st, U("ln_tr"), 3, [128, W], F32),
    }


def ln_apply(k, xb, xk, w, tmp, kk, out, okeys, scale_ap, bias_ap, extra_reads=(), tag=""):
    P = k.P
    t, tk = tmp["tr"].next()
    P.op("dve", lambda e: e.tensor_tensor(t[:, :w], xb[:, kk, :w], tmp["mean"][:, :w], ALU.subtract), reads=[xk, "mean" + tag], writes=[tk])
    P.op("dve", lambda e: e.tensor_tensor(t[:, :w], t[:, :w], tmp["rstd"][:, :w], ALU.mult), reads=[tk, "rstd" + tag], writes=[tk])
    P.op("act", lambda e: e.activation(out=out, in_=t[:, :w], func=AF.Identity, scale=scale_ap, bias=bias_ap), reads=[tk, "mod"] + list(extra_reads), writes=okeys)


def stage_ln_win(k, li):
    nc, P = k.nc, k.P
    with ExitStack() as st:
        hT = st.enter_context(nc.sbuf_tensor(U("hT"), [128, 8, NT], BF16))
        with ExitStack() as st2:
            tmp = ln_tmp(nc, st2)
            xbr = Rot(nc, st2, "xb", 2, [128, 8, 512], F32)
            xTv = k.xT.rearrange("(k p) t -> p k t", p=128)
            for (t0, w, isc) in TBS:
                xb, xk = xbr.next()
                P.dma("sp", xb[:, :, :w], xTv[:, :, t0:t0 + w], reads=["xT"], writes=[xk])
                ln_stats(k, xb, xk, w, tmp)
                for kk in range(8):
                    ln_apply(k, xb, xk, w, tmp, kk, hT[:, kk, t0:t0 + w], ["hT%d" % kk],
                             k.mod[:, li, 8 + kk, isc:isc + 1], k.mod[:, li, 0 + kk, isc:isc + 1])
            P.barrier()
        wr = Rot(nc, st, "wr", 3, [128, 8, 128], BF16)
        zs = Rot(nc, st, "zs", 3, [128, NT], BF16)
        vt = st.enter_context(nc.sbuf_tensor(U("vt"), [128, 18, 128], BF16))
        wv = k.w_inx[li].rearrange("(k p) n -> p k n", p=128)
        ev = 0
        for m in range(NZT):
            wt, wk = wr.next()
            P.dma("pool", wt[:], wv[:, :, m * 128:(m + 1) * 128], writes=[wk])
            zt, zk = zs.next()
            mrows = 64 if m == ZT_KRX else 128
            for (t0, w, isc) in TBS:
                pt, pk = psn(k)
                for kk in range(8):
                    P.op("pe", lambda e, pt=pt, wt=wt, kk=kk, t0=t0, w=w, mrows=mrows: e.matmul(pt[:mrows, :w], wt[:, kk, :mrows], hT[:, kk, t0:t0 + w], start=(kk == 0), stop=(kk == 7)),
                         reads=[wk, "hT%d" % kk], writes=[pk])
                gate = ZT_G <= m < ZT_G + 24
                if gate:
                    P.op("act", lambda e, pt=pt, zt=zt, t0=t0, w=w: e.activation(out=zt[:, t0:t0 + w], in_=pt[:, :w], func=AF.Sigmoid), reads=[pk], writes=[zk + "_%d" % t0])
                elif ev % 2 == 0:
                    P.op("act", lambda e, pt=pt, zt=zt, t0=t0, w=w, mrows=mrows: e.copy(zt[:mrows, t0:t0 + w], pt[:mrows, :w]), reads=[pk], writes=[zk + "_%d" % t0])
                else:
                    P.op("dve", lambda e, pt=pt, zt=zt, t0=t0, w=w, mrows=mrows: e.tensor_copy(zt[:mrows, t0:t0 + w], pt[:mrows, :w]), reads=[pk], writes=[zk + "_%d" % t0])
                ev += 1
            P.dma("sp", k.zT[m * 128:m * 128 + mrows, :], zt[:mrows, :], reads=[zk + "_%d" % t[0] for t in TBS], writes=["zT"])
            if m == ZT_WV:
                for tt in range(18):
                    pt, pk = psn(k)
                    for kk in range(8):
                        P.op("pe", lambda e, pt=pt, wt=wt, kk=kk, tt=tt: e.matmul(pt[:, :128], hT[:, kk, tt * 128:(tt + 1) * 128], wt[:, kk, :], start=(kk == 0), stop=(kk == 7)),
                             reads=[wk, "hT%d" % kk], writes=[pk])
                    P.op("dve", lambda e, pt=pt, tt=tt: e.tensor_copy(vt[:, tt, :], pt[:, :128]), reads=[pk], writes=["vt"])
                P.dma("sp", k.vtok.rearrange("(t p) c -> p t c", p=128), vt[:], reads=["vt"], writes=["vtok"])
        stage_end(k)


def stage_s5(k, li):
    S5E = "dve" if "s5pool" not in k.dbg else "pool"
    nc, P = k.nc, k.P
    TWO_PI = float(2 * np.pi)
    with ExitStack() as st:
        sb = lambda name, shape, dt=F32: st.enter_context(nc.sbuf_tensor(U(name), shape, dt))
        lane = sb("lane", [128, 3, 32])
        dg = sb("dg", [128, 2, 4])
        BW = sb("BW", [128, 64, 128], BF16)
        CW = sb("CW", [128, 96, 128], BF16)
        tau = sb("tau", [128, NT])
        names = ["dt", "rho", "thn", "fr", "sn", "cs", "ar", "ai", "rden", "qr", "qi", "nqr", "nqi", "tA", "tB"]
        lp = {n: sb("lp_" + n, [128, 32]) for n in names}
        lpi = sb("lp_it", [128, 32], I32)
        P.dma("sp", lane[:], k.s5_lane[li], writes=["lane"])
        P.dma("sp", dg[:], k.s5_dg[li], writes=["dg"])
        P.dma("sp", tau[:], k.tau, writes=["tau"])
        bsrc = k.s5_bT[li].rearrange("d r t p c -> p (d r t) c")
        for j in range(8):
            P.dma("pool", BW[:, j * 8:(j + 1) * 8, :], bsrc[:, j * 8:(j + 1) * 8, :], writes=["BW"])
        P.op("pool", lambda e: e.memset(CW[:], 0.0), writes=["CW"])
        lre, lim, ldt = lane[:, 0, :], lane[:, 1, :], lane[:, 2, :]
        R = ["lane", "lp"]
        W = ["lp"]
        V = lambda fn: P.op("dve", fn, reads=R, writes=W)
        A = lambda fn: P.op("act", fn, reads=R + ["halfpi"], writes=W)
        A(lambda e: e.activation(out=lp["dt"][:], in_=ldt, func=AF.Exp))
        V(lambda e: e.tensor_tensor(lp["tA"][:], lre, lp["dt"][:], ALU.mult))
        A(lambda e: e.activation(out=lp["rho"][:], in_=lp["tA"][:], func=AF.Exp))
        V(lambda e: e.tensor_tensor(lp["thn"][:], lim, lp["dt"][:], ALU.mult))
        V(lambda e: e.tensor_scalar(lp["thn"][:], lp["thn"][:], float(1.0 / TWO_PI), None, ALU.mult))
        V(lambda e: e.tensor_copy(lpi[:], lp["thn"][:]))
        V(lambda e: e.tensor_copy(lp["tB"][:], lpi[:]))
        V(lambda e: e.tensor_tensor(lp["fr"][:], lp["thn"][:], lp["tB"][:], ALU.subtract))
        A(lambda e: e.activation(out=lp["sn"][:], in_=lp["fr"][:], func=AF.Sin, scale=TWO_PI))
        A(lambda e: e.activation(out=lp["fr"][:], in_=lp["fr"][:], func=AF.Abs))
        A(lambda e: e.activation(out=lp["cs"][:], in_=lp["fr"][:], func=AF.Sin, scale=-TWO_PI, bias=k.halfpi[:]))
        V(lambda e: e.tensor_tensor(lp["ar"][:], lp["rho"][:], lp["cs"][:], ALU.mult))
        V(lambda e: e.tensor_tensor(lp["ai"][:], lp["rho"][:], lp["sn"][:], ALU.mult))
        V(lambda e: e.tensor_tensor(lp["tA"][:], lre, lre, ALU.mult))
        V(lambda e: e.tensor_tensor(lp["tB"][:], lim, lim, ALU.mult))
        V(lambda e: e.tensor_tensor(lp["tA"][:], lp["tA"][:], lp["tB"][:], ALU.add))
        V(lambda e: e.reciprocal(lp["rden"][:], lp["tA"][:]))
        V(lambda e: e.tensor_scalar_add(lp["ar"][:], lp["ar"][:], -1.0))
        V(lambda e: e.tensor_tensor(lp["tA"][:], lp["ar"][:], lre, ALU.mult))
        V(lambda e: e.tensor_tensor(lp["tB"][:], lp["ai"][:], lim, ALU.mult))
        V(lambda e: e.tensor_tensor(lp["tA"][:], lp["tA"][:], lp["tB"][:], ALU.add))
        V(lambda e: e.tensor_tensor(lp["qr"][:], lp["tA"][:], lp["rden"][:], ALU.mult))
        V(lambda e: e.tensor_tensor(lp["tA"][:], lp["ai"][:], lre, ALU.mult))
        V(lambda e: e.tensor_tensor(lp["tB"][:], lp["ar"][:], lim, ALU.mult))
        V(lambda e: e.tensor_tensor(lp["tA"][:], lp["tA"][:], lp["tB"][:], ALU.subtract))
        V(lambda e: e.tensor_tensor(lp["qi"][:], lp["tA"][:], lp["rden"][:], ALU.mult))
        V(lambda e: e.tensor_scalar(lp["nqr"][:], lp["qr"][:], -1.0, None, ALU.mult))
        V(lambda e: e.tensor_scalar(lp["nqi"][:], lp["qi"][:], -1.0, None, ALU.mult))
        for n_ in ("rho", "thn", "sn", "cs", "qr", "qi", "dt"):
            dump(k, "lp_" + n_, lp[n_][:], [128, 32], F32, ["lp"])
        stC = ExitStack()
        craw = stC.enter_context(nc.sbuf_tensor(U("craw"), [128, 2, 16, 2, 16], F32))
        ctmp = stC.enter_context(nc.sbuf_tensor(U("ctmp"), [128, 16], F32))
        P.dma("sp", craw[:], k.s5_cL[li], writes=["craw"])
        for d in range(2):
            for lt in range(16):
                col = d * 16 + lt
                cr, ci = craw[:, d, lt, 0, :], craw[:, d, lt, 1, :]
                for half in range(2):
                    g = 2 * lt + half
                    gl = g % 8
                    ps_ = slice(half * 64, half * 64 + 64)
                    for ri in range(3):
                        s1 = lp["qi"] if ri != 1 else lp["nqr"]
                        s2 = (lp["qr"], lp["nqi"], lp["nqr"])[ri]
                        op1 = (ALU.subtract, ALU.add, ALU.add)[ri]
                        P.op("dve", lambda e, ci=ci, s1=s1, col=col, ps_=ps_: e.tensor_scalar(ctmp[ps_, :], ci[ps_, :], s1[ps_, col:col + 1], None, ALU.mult), reads=["craw", "lp"], writes=["ctmp"])
                        P.op("dve", lambda e, cr=cr, s2=s2, col=col, ps_=ps_, ri=ri, gl=gl, op1=op1: e.scalar_tensor_tensor(CW[ps_, col * 3 + ri, gl * 16:(gl + 1) * 16], cr[ps_, :], s2[ps_, col:col + 1], ctmp[ps_, :], ALU.mult, op1), reads=["craw", "lp", "ctmp"], writes=["CW"])
        P.barrier()
        stC.close()
        gT = sb("s5g", [128, 4, NT], BF16)
        with ExitStack() as stU:
            sbu = lambda name, shape, dt=F32: stU.enter_context(nc.sbuf_tensor(U(name), shape, dt))
            utR = Rot(nc, stU, "s5ut", 2, [128, NT], BF16)
            it = sbu("s5it", [128, NT], I32)
            fr = sbu("s5fr", [128, NT])
            SnR = Rot(nc, stU, "s5S", 2, [128, NT], BF16)
            CsR = Rot(nc, stU, "s5C", 2, [128, NT], BF16)
            br = sbu("s5br", [128, NT], BF16)
            bi = sbu("s5bi", [128, NT], BF16)
            p1 = sbu("s5p1", [128, NT], BF16)
            p2 = sbu("s5p2", [128, NT], BF16)
            p3 = sbu("s5p3", [128, NT], BF16)
            wr = sbu("s5wr", [128, NT], BF16)
            wi = sbu("s5wi", [128, NT], BF16)
            zrR = Rot(nc, stU, "s5zr", 2, [128, NT], BF16)
            ziR = Rot(nc, stU, "s5zi", 2, [128, NT], BF16)
            qR = [Rot(nc, stU, "s5q%d" % j, 2, [128, NT], BF16) for j in range(4)]
            ysr = Rot(nc, stU, "s5ys", 1, [128, 512], F32)
            segs = [(0, LC), (LC, NT)]
            units = [(gt, d, l4) for gt in range(4) for d in range(2) for l4 in range(4)]
            uts = {}
            ctx_ = {}

            def alpha(u):
                gt, d, l4 = units[u]
                lt = gt * 4 + l4
                col = d * 16 + lt
                thn = lp["thn"][:, col:col + 1]
                if (d, l4) == (0, 0):
                    ut, utk = utR.next()
                    P.dma("sp", ut[:], k.zT[gt * 128:(gt + 1) * 128, :], reads=["zT"], writes=[utk])
                    uts[gt] = (ut, utk)
                Sn, Snk = SnR.next()
                Cs, Csk = CsR.next()
                ctx_[u] = dict(Sn=Sn, Snk=Snk, Cs=Cs, Csk=Csk, col=col, lt=lt)
                for (a_, b_) in segs:
                    src = tau[:, a_:b_] if d == 0 else (tau[:, b_ - 1::-1] if a_ == 0 else tau[:, b_ - 1:a_ - 1:-1])
                    P.op("dve", lambda e, src=src, a_=a_, b_=b_, thn=thn: e.tensor_scalar(it[:, a_:b_], src, thn, None, ALU.mult), reads=["tau", "lp"], writes=["it"])
                    P.op("dve", lambda e, src=src, a_=a_, b_=b_, thn=thn: e.scalar_tensor_tensor(fr[:, a_:b_], src, thn, it[:, a_:b_], ALU.mult, ALU.subtract), reads=["tau", "lp", "it"], writes=["fr"])
                P.op("act", lambda e, Sn=Sn: e.activation(out=Sn[:], in_=fr[:], func=AF.Sin, scale=TWO_PI), reads=["fr"], writes=[Snk])
                P.op("act", lambda e: e.activation(out=fr[:], in_=fr[:], func=AF.Abs), reads=["fr"], writes=["fr"])
                P.op("act", lambda e, Cs=Cs: e.activation(out=Cs[:], in_=fr[:], func=AF.Sin, scale=-TWO_PI, bias=k.halfpi[:]), reads=["fr", "halfpi"], writes=[Csk])

            def bu(u):
                gt, d, l4 = units[u]
                lt = ctx_[u]["lt"]
                ut, utk = uts[gt]
                for (t0, w, isc) in TBS:
                    pr, kr = psn(k, 5, 8)
                    pi_, ki = psn(k, 5, 8)
                    sl = slice(t0, t0 + w)
                    P.op("pe", lambda e, pr=pr, sl=sl, w=w, d=d, lt=lt, ut=ut: e.matmul(pr[:, :w], BW[:, (d * 2 + 0) * 16 + lt, :], ut[:, sl], start=True, stop=True), reads=["BW", utk], writes=[kr])
                    P.op("pe", lambda e, pi_=pi_, sl=sl, w=w, d=d, lt=lt, ut=ut: e.matmul(pi_[:, :w], BW[:, (d * 2 + 1) * 16 + lt, :], ut[:, sl], start=True, stop=True), reads=["BW", utk], writes=[ki])
                    P.op("act", lambda e, pr=pr, sl=sl, w=w: e.copy(br[:, sl], pr[:, :w]), reads=[kr], writes=["br"])
                    P.op("act", lambda e, pi_=pi_, sl=sl, w=w: e.copy(bi[:, sl], pi_[:, :w]), reads=[ki], writes=["bi"])

            def beta(u):
                c = ctx_[u]
                Sn, Snk, Cs, Csk = c["Sn"], c["Snk"], c["Cs"], c["Csk"]
                P.op("dve", lambda e: e.tensor_tensor(p1[:], Cs[:], br[:], ALU.mult), reads=[Csk, "br"], writes=["p1"])
                P.op("dve", lambda e: e.tensor_tensor(p2[:], Sn[:], bi[:], ALU.mult), reads=[Snk, "bi"], writes=["p2"])
                P.op("dve", lambda e: e.tensor_tensor(p3[:], Cs[:], bi[:], ALU.mult), reads=[Csk, "bi"], writes=["p3"])
                P.op("dve", lambda e: e.tensor_tensor(wr[:], p1[:], p2[:], ALU.add), reads=["p1", "p2"], writes=["wr"])
                P.op("dve", lambda e: e.tensor_tensor(p2[:], Sn[:], br[:], ALU.mult), reads=[Snk, "br", "wr"], writes=["p2"])
                P.op("dve", lambda e: e.tensor_tensor(wi[:], p3[:], p2[:], ALU.subtract), reads=["p3", "p2"], writes=["wi"])

            def gamma_delta(u):
                gt, d, l4 = units[u]
                c = ctx_.pop(u)
                Sn, Snk, Cs, Csk, col = c["Sn"], c["Snk"], c["Cs"], c["Csk"], c["col"]
                first = (d == 0 and l4 == 0)
                last = (d == 1 and l4 == 3)
                zr, zrk = zrR.next()
                zi, zik = ziR.next()
                qs = [r_.next() for r_ in qR]
                rho = lp["rho"][:, col:col + 1]
                for (src, dst, dk, sk_) in ((wr, zr, zrk, "wr"), (wi, zi, zik, "wi")):
                    if d == 0:
                        P.op("dve", lambda e, src=src, dst=dst: e.tensor_tensor_scan(dst[:, 0:LC], rho.to_broadcast([128, LC]), src[:, 0:LC], 0.0, ALU.mult, ALU.add), reads=[sk_, "lp"], writes=[dk])
                        P.op("dve", lambda e, src=src, dst=dst: e.tensor_tensor_scan(dst[:, LC:NT], rho.to_broadcast([128, L]), src[:, LC:NT], dst[:, LC - 1:LC], ALU.mult, ALU.add), reads=[sk_, "lp", dk], writes=[dk])
                    else:
                        P.op("dve", lambda e, src=src, dst=dst: e.tensor_tensor_scan(dst[:, LC - 1::-1], rho.to_broadcast([128, LC]), src[:, LC - 1::-1], 0.0, ALU.mult, ALU.add), reads=[sk_, "lp"], writes=[dk])
                        P.op("dve", lambda e, src=src, dst=dst: e.tensor_tensor_scan(dst[:, NT - 1:LC - 1:-1], rho.to_broadcast([128, L]), src[:, NT - 1:LC - 1:-1], dst[:, 0:1], ALU.mult, ALU.add), reads=[sk_, "lp", dk], writes=[dk])
                (q1, q1k), (q2, q2k), (q3, q3k), (q4, q4k) = qs
                P.op("dve", lambda e: e.tensor_tensor(q1[:], Cs[:], zr[:], ALU.mult), reads=[Csk, zrk], writes=[q1k])
                P.op("dve", lambda e: e.tensor_tensor(q2[:], Sn[:], zi[:], ALU.mult), reads=[Snk, zik], writes=[q2k])
                P.op("dve", lambda e: e.tensor_tensor(q3[:], Sn[:], zr[:], ALU.mult), reads=[Snk, zrk], writes=[q3k])
                P.op("dve", lambda e: e.tensor_tensor(q4[:], Cs[:], zi[:], ALU.mult), reads=[Csk, zik], writes=[q4k])
                for bi_, (t0, w, isc) in enumerate(TBS):
                    sl = slice(t0, t0 + w)
                    yk = "ps%d" % bi_
                    for j_, (qq, qk, wsl) in enumerate(((q1, q1k, 0), (q2, q2k, 2), (q3, q3k, 1), (q4, q4k, 1))):
                        P.op("pe", lambda e, bi_=bi_, sl=sl, w=w, qq=qq, wsl=wsl, j_=j_: e.matmul(k.ps[bi_][:, :w], CW[:, col * 3 + wsl, :], qq[:, sl], start=(first and j_ == 0), stop=(last and j_ == 3)), reads=["CW", qk], writes=[yk])
                if last:
                    ut, utk = uts[gt]
                    for bi_, (t0, w, isc) in enumerate(TBS):
                        sl = slice(t0, t0 + w)
                        ys, ysk = ysr.next()
                        P.op("dve", lambda e, bi_=bi_, sl=sl, w=w, ys=ys: e.scalar_tensor_tensor(ys[:, :w], ut[:, sl], dg[:, 0, gt:gt + 1], k.ps[bi_][:, :w], ALU.mult, ALU.add), reads=[utk, "dg", "ps%d" % bi_], writes=[ysk])
                        P.op("act", lambda e, sl=sl, w=w, ys=ys: e.activation(out=gT[:, gt, sl], in_=ys[:, :w], func=AF.Gelu_apprx_tanh), reads=[ysk], writes=["gT%d" % gt])

            alpha(0)
            bu(0)
            for u in range(len(units)):
                beta(u)
                if u + 1 < len(units):
                    alpha(u + 1)
                    bu(u + 1)
                gamma_delta(u)
            P.barrier()
        wglu = sb("wglu", [128, 4, 512], BF16)
        P.dma("pool", wglu[:], k.s5_wglu[li].rearrange("(k p) n -> p k n", p=128), writes=["wglu"])
        so = Rot(nc, st, "s5o", 2, [128, NT], BF16)
        sgr = Rot(nc, st, "s5sg", 2, [128, 512], F32)
        for mo in range(4):
            ot, ok = so.next()
            for (t0, w, isc) in TBS:
                sl = slice(t0, t0 + w)
                pt, pk = psn(k)
                for kk in range(4):
                    P.op("pe", lambda e, pt=pt, kk=kk, sl=sl, w=w, mo=mo: e.matmul(pt[:, :w], wglu[:, kk, mo * 128:(mo + 1) * 128], gT[:, kk, sl], start=(kk == 0), stop=(kk == 3)), reads=["wglu", "gT%d" % kk], writes=[pk])
                sg, sgk = sgr.next()
                P.op("act", lambda e, pt=pt, w=w, sg=sg, mo=mo: e.activation(out=sg[:, :w], in_=pt[:, :w], func=AF.Sigmoid, bias=dg[:, 1, mo:mo + 1], scale=1.0), reads=[pk, "dg"], writes=[sgk])
                P.op("dve", lambda e, sg=sg, sl=sl, w=w, ot=ot, mo=mo: e.tensor_tensor(ot[:, sl], gT[:, mo, sl], sg[:, :w], ALU.mult), reads=[sgk, "gT%d" % mo], writes=[ok + "_%d" % t0])
            P.dma("sp", k.brT[0][mo * 128:(mo + 1) * 128, :], ot[:], reads=[ok + "_%d" % t[0] for t in TBS], writes=["s5T"])
        stage_end(k)


def rms_norm_T(k, st, src, nk, gains, dst, tag):
    nc, P = k.nc, k.P
    sq = st.enter_context(nc.sbuf_tensor(U("rms_sq"), [128, nk, 512], BF16))
    rinv = st.enter_context(nc.sbuf_tensor(U("rms_ri"), [128, 512], F32))
    for (t0, w, isc) in TBS:
        sl = slice(t0, t0 + w)
        P.op("act", lambda e, sl=sl, w=w: e.activation(out=sq[:, :, :w], in_=src[:, :, sl], func=AF.Square), reads=[tag + "src"], writes=[tag + "sq"])
        pt, pk = psn(k)
        for kk in range(nk):
            P.op("pe", lambda e, pt=pt, kk=kk, w=w: e.matmul(pt[:, :w], k.onesb[:, 0:128], sq[:, kk, :w], start=(kk == 0), stop=(kk == nk - 1)), reads=[tag + "sq", "onesb"], writes=[pk])
        P.op("act", lambda e, pt=pt, w=w: e.activation(out=rinv[:, :w], in_=pt[:, :w], func=AF.Sqrt, scale=float(1.0 / (nk * 128)), bias=k.epsc[:]), reads=[pk, "epsc"], writes=[tag + "ri"])
        P.op("dve", lambda e, w=w: e.reciprocal(rinv[:, :w], rinv[:, :w]), reads=[tag + "ri"], writes=[tag + "ri"])
        for kk in range(nk):
            P.op("dve", lambda e, kk=kk, sl=sl, w=w: e.scalar_tensor_tensor(dst[:, kk, sl], src[:, kk, sl], gains[:, kk:kk + 1], rinv[:, :w], ALU.mult, ALU.mult), reads=[tag + "src", tag + "ri", "mg"], writes=[tag + "dst"])


def softmax_pv(k, ost_rot, score_fn, nkc, va_fn, nq, scale, out_dram, tagp, PTr, esk=None, post=None):
    nc, P = k.nc, k.P
    po, pok = psn(k, 0, 3)
    LA = 3
    scr = {}

    def issue_score(kc):
        pscr, psk = psn(k, 3, 8)
        score_fn(kc, pscr, psk)
        scr[kc] = (pscr, psk)

    for kc in range(min(LA, nkc)):
        issue_score(kc)
    for kc in range(nkc):
        if kc + LA < nkc:
            issue_score(kc + LA)
        pscr, psk = scr.pop(kc)
        pt, ptk = PTr.next()
        P.op("act", lambda e, pscr=pscr, pt=pt: e.activation(out=pt[:, :nq], in_=pscr[:, :nq], func=AF.Exp, scale=scale), reads=[psk], writes=[ptk])
        if post is not None:
            post(kc, pt, ptk)
        va, vak = va_fn(kc)
        P.op("pe", lambda e, po=po, va=va, pt=pt, kc=kc: e.matmul(po[:, :nq], va, pt[:, :nq], start=(kc == 0), stop=(kc == nkc - 1)), reads=[vak, ptk], writes=[pok])
    rv, rvk = k.att_rv.next()
    if esk is not None:
        P.op("dve", lambda e, po=po, rv=rv: e.tensor_scalar(rv[0:64, :nq], po[64:128, :nq], esk, None, ALU.add), reads=[pok, "esk"], writes=[rvk])
        P.op("dve", lambda e, rv=rv: e.reciprocal(rv[0:64, :nq], rv[0:64, :nq]), reads=[rvk], writes=[rvk])
    else:
        P.op("dve", lambda e, po=po, rv=rv: e.reciprocal(rv[0:64, :nq], po[64:128, :nq]), reads=[pok], writes=[rvk])
    ot, otk = ost_rot.next()
    P.op("dve", lambda e, po=po, ot=ot, rv=rv: e.tensor_tensor(ot[0:64, :nq], po[0:64, :nq], rv[0:64, :nq], ALU.mult), reads=[pok, rvk], writes=[otk])
    P.dma("sp", out_dram, ot[0:64, :nq], reads=[otk], writes=[tagp])


def stage_mla(k, li):
    nc, P = k.nc, k.P
    SC = float(96 ** -0.5)
    with ExitStack() as st:
        sb = lambda name, shape, dt=F32: st.enter_context(nc.sbuf_tensor(U(name), shape, dt))
        mg = sb("mg", [128, 5])
        P.dma("sp", mg[:], k.mla_g[li], writes=["mg"])
        VA = sb("VA", [128, 18, 8, 128], BF16)
        KRb = sb("KRb", [128, NT], BF16)
        qn = sb("qn", [128, 3, NT], BF16)
        kvn = sb("kvn", [128, 2, NT], BF16)
        rope = sb("ropem", [128, 2, L])
        wuq = sb("wuq", [128, 3, 2048], BF16)
        wkk = sb("wkk", [128, 2, 1024], BF16)
        k.att_rv = Rot(nc, st, "att_rv", 2, [128, 512], F32)
        P.op("pool", lambda e: e.memset(VA[:], 1.0), writes=["VA"])
        P.dma("sp", rope[:], k.rope_mla.rearrange("c p t -> p c t"), writes=["rope"])
        for j in range(3):
            for c_ in range(4):
                P.dma("pool", wuq[:, j, c_ * 512:(c_ + 1) * 512], k.w_uqx[li][j * 128:(j + 1) * 128, c_ * 512:(c_ + 1) * 512], writes=["wuq"])
        for j in range(2):
            for c_ in range(2):
                P.dma("pool", wkk[:, j, c_ * 512:(c_ + 1) * 512], k.w_ukvk[li][j * 128:(j + 1) * 128, c_ * 512:(c_ + 1) * 512], writes=["wkk"])
        with ExitStack() as st2:
            sb2 = lambda name, shape, dt=F32: st2.enter_context(nc.sbuf_tensor(U(name), shape, dt))
            qa = sb2("qa", [128, 3, NT], BF16)
            kva = sb2("kva", [128, 2, NT], BF16)
            KP = sb2("KP", [128, NT], BF16)
            KS = sb2("KS", [128, L], BF16)
            wvv = sb2("wvv", [128, 2, 512], BF16)
            tA = sb2("mtA", [128, 512])
            tB = sb2("mtB", [128, 512])
            P.dma("sp", qa[:], k.zT[ZT_QA * 128:(ZT_QA + 3) * 128, :].rearrange("(k p) t -> p k t", p=128), reads=["zT"], writes=["qsrc"])
            P.dma("sp", kva[:], k.zT[ZT_KVA * 128:(ZT_KVA + 2) * 128, :].rearrange("(k p) t -> p k t", p=128), reads=["zT"], writes=["ksrc"])
            P.op("pool", lambda e: e.memset(KP[:], 0.0), writes=["KP"])
            P.op("pool", lambda e: e.memset(KS[:], 0.0), writes=["KS"])
            P.dma("sp", KP[64:96, :], k.zT[ZT_KRX * 128:ZT_KRX * 128 + 32, :], reads=["zT"], writes=["KP"])
            P.dma("sp", KS[64:96, :], k.zT[ZT_KRX * 128 + 32:ZT_KRX * 128 + 64, LC:NT], reads=["zT"], writes=["KS"])
            P.dma("pool", wvv[:], k.w_ukvv[li].rearrange("(k p) n -> p k n", p=128), writes=["wvv"])
            rms_norm_T(k, st2, qa, 3, mg[:, 0:3], qn, "q")
            rms_norm_T(k, st2, kva, 2, mg[:, 3:5], kvn, "k")
            P.op("dve", lambda e: e.tensor_copy(KRb[:, 0:LC], KP[:, 0:LC]), reads=["KP"], writes=["KRb"])
            for c in range(4):
                sl = slice(c * 512, (c + 1) * 512)
                sln = slice(LC + c * 512, LC + (c + 1) * 512)
                P.op("dve", lambda e, sl=sl, sln=sln: e.tensor_tensor(tA[:, :], KP[:, sln], rope[:, 0, sl], ALU.mult), reads=["KP", "rope"], writes=["mtA"])
                P.op("dve", lambda e, sl=sl: e.tensor_tensor(tB[:, :], KS[:, sl], rope[:, 1, sl], ALU.mult), reads=["KS", "rope"], writes=["mtB"])
                P.op("dve", lambda e, sln=sln: e.tensor_tensor(KRb[:, sln], tA[:, :], tB[:, :], ALU.add), reads=["mtA", "mtB"], writes=["KRb"])
            dump(k, "d_KP", KP[:], [128, NT], BF16, ["KP"])
            dump(k, "d_KS", KS[:], [128, L], BF16, ["KS"])
            dump(k, "d_KRb", KRb[:], [128, NT], BF16, ["KRb"])
            for tt in range(18):
                pt, pk = psn(k)
                for kk in range(2):
                    P.op("pe", lambda e, pt=pt, kk=kk, tt=tt: e.matmul(pt[:, :512], kvn[:, kk, tt * 128:(tt + 1) * 128], wvv[:, kk, :], start=(kk == 0), stop=(kk == 1)), reads=["wvv", "kdst"], writes=[pk])
                P.op("dve", lambda e, pt=pt, tt=tt: e.tensor_copy(VA[:, tt, :, 0:64], pt[:, :512].rearrange("p (h d) -> p h d", h=8)), reads=[pk], writes=["VA"])
            P.barrier()
        if "mla_stop1" in k.dbg:
            stage_end(k)
            return
        PTr = Rot(nc, st, "mPT", 4, [128, 512], BF16)
        ostr = Rot(nc, st, "most", 3, [128, 512], BF16)
        t1r = Rot(nc, st, "mt1", 2, [128, 512], F32)
        t2r = Rot(nc, st, "mt2", 2, [128, 512], F32)
        for grp in range(2):
            with ExitStack() as st3:
                QP = st3.enter_context(nc.sbuf_tensor(U("QP"), [128, 4, NT], BF16))
                QR = st3.enter_context(nc.sbuf_tensor(U("QR"), [128, 4, L], BF16))
                KH = st3.enter_context(nc.sbuf_tensor(U("KH"), [128, 4, NT], BF16))
                for hh in range(4):
                    h = grp * 4 + hh
                    for (t0, w, isc) in TBS:
                        sl = slice(t0, t0 + w)
                        pm, pmk = psn(k, 3, 8)
                        for kk in range(3):
                            P.op("pe", lambda e, pm=pm, kk=kk, sl=sl, w=w, h=h: e.matmul(pm[:, :w], wuq[:, kk, h * 128:(h + 1) * 128], qn[:, kk, sl], start=(kk == 0), stop=(kk == 2)), reads=["wuq", "qdst"], writes=[pmk])
                        P.op("act", lambda e, pm=pm, sl=sl, w=w, hh=hh: e.copy(QP[:, hh, sl], pm[:, :w]), reads=[pmk], writes=["QP%d" % hh])
                        if not isc:
                            ls = slice(t0 - LC, t0 - LC + w)
                            psw, pswk = psn(k, 3, 8)
                            for kk in range(3):
                                P.op("pe", lambda e, psw=psw, kk=kk, sl=sl, w=w, h=h: e.matmul(psw[:, :w], wuq[:, kk, (8 + h) * 128:(9 + h) * 128], qn[:, kk, sl], start=(kk == 0), stop=(kk == 2)), reads=["wuq", "qdst"], writes=[pswk])
                            t1, t1k = t1r.next()
                            t2, t2k = t2r.next()
                            P.op("dve", lambda e, pm=pm, ls=ls, w=w, t1=t1: e.tensor_tensor(t1[:, :w], pm[:, :w], rope[:, 0, ls], ALU.mult), reads=[pmk, "rope"], writes=[t1k])
                            P.op("dve", lambda e, psw=psw, ls=ls, w=w, t2=t2: e.tensor_tensor(t2[:, :w], psw[:, :w], rope[:, 1, ls], ALU.mult), reads=[pswk, "rope"], writes=[t2k])
                            P.op("dve", lambda e, ls=ls, w=w, hh=hh, t1=t1, t2=t2: e.tensor_tensor(QR[:, hh, ls], t1[:, :w], t2[:, :w], ALU.add), reads=[t1k, t2k], writes=["QR%d" % hh])
                        pk_, pkk = psn(k, 3, 8)
                        for kk in range(2):
                            P.op("pe", lambda e, pk_=pk_, kk=kk, sl=sl, w=w, h=h: e.matmul(pk_[:, :w], wkk[:, kk, h * 128:(h + 1) * 128], kvn[:, kk, sl], start=(kk == 0), stop=(kk == 1)), reads=["wkk", "kdst"], writes=[pkk])
                        P.op("dve", lambda e, pk_=pk_, sl=sl, w=w, hh=hh: e.tensor_tensor(KH[:, hh, sl], pk_[:, :w], KRb[:, sl], ALU.add), reads=[pkk, "KRb"], writes=["KH%d" % hh])
                if grp == 0:
                    dump(k, "d_wkk", wkk[:], [128, 2, 1024], BF16, ["wkk"])
                    dump(k, "d_QP", QP[:, 0, :], [128, NT], BF16, ["QP0"])
                    dump(k, "d_QR", QR[:, 0, :], [128, L], BF16, ["QR0"])
                    dump(k, "d_KH", KH[:, 0, :], [128, NT], BF16, ["KH0"])
                    dump(k, "d_VA", VA[:, :, 0, :], [128, 18, 128], BF16, ["VA"])
                if "mla_stop2" in k.dbg:
                    P.barrier()
                    continue
                for hh in range(4):
                    h = grp * 4 + hh
                    for qb in range(5):
                        if qb < 4:
                            q0, nq, nkc = LC + qb * 512, 512, 18
                        else:
                            q0, nq, nkc = 0, LC, 2

                        def score_fn(kc, pscr, psk, q0=q0, nq=nq, hh=hh):
                            ks = slice(kc * 128, (kc + 1) * 128)
                            if kc >= 2:
                                P.op("pe", lambda e: e.matmul(pscr[:, :nq], KH[:, hh, ks], QR[:, hh, q0 - LC:q0 - LC + nq], start=True, stop=True), reads=["KH%d" % hh, "QR%d" % hh], writes=[psk])
                            else:
                                P.op("pe", lambda e: e.matmul(pscr[:, :nq], KH[:, hh, ks], QP[:, hh, q0:q0 + nq], start=True, stop=True), reads=["KH%d" % hh, "QP%d" % hh], writes=[psk])

                        softmax_pv(k, ostr, score_fn, nkc, lambda kc, h=h: (VA[:, kc, h, :], "VA"), nq, SC,
                                   k.brT[1][h * 64:(h + 1) * 64, q0:q0 + nq], "mlaT", PTr)
                P.barrier()
        stage_end(k)


def stage_win(k, li):
    nc, P = k.nc, k.P
    SC = float(64 ** -0.5)
    with ExitStack() as st:
        sb = lambda name, shape, dt=F32: st.enter_context(nc.sbuf_tensor(U(name), shape, dt))
        Qp = sb("wQp", [128, 8, NT], BF16)
        Qr = sb("wQr", [128, 8, L], BF16)
        Kp = sb("wKp", [128, NT], BF16)
        Kr = sb("wKr", [128, L], BF16)
        VW = sb("wVW", [128, 18, 2, 128], BF16)
        rope = sb("wrope", [128, 2, L])
        msk = sb("wmsk", [128, 2, 128], BF16)
        esk = sb("wesk", [128, 8])
        Qsr = Rot(nc, st, "wQs", 2, [128, L], BF16)
        tAr = Rot(nc, st, "wtA", 2, [128, 512], F32)
        tBr = Rot(nc, st, "wtB", 2, [128, 512], F32)
        k.att_rv = Rot(nc, st, "watt_rv", 2, [128, 512], F32)
        P.op("pool", lambda e: e.memset(VW[:], 1.0), writes=["VW"])
        P.dma("sp", esk[:], k.win_sink[li], writes=["esk"])
        P.op("act", lambda e: e.activation(out=esk[:], in_=esk[:], func=AF.Exp), reads=["esk"], writes=["esk"])
        P.dma("sp", Qp[:], k.zT[ZT_WQ * 128:(ZT_WQ + 8) * 128, :].rearrange("(k p) t -> p k t", p=128), reads=["zT"], writes=["Qp"])
        P.dma("sp", Kp[:], k.zT[ZT_WK * 128:(ZT_WK + 1) * 128, :], reads=["zT"], writes=["Kp"])
        P.dma("sp", rope[:], k.rope_win.rearrange("c p t -> p c t"), writes=["rope"])
        P.dma("pool", msk[:], k.wmask.rearrange("c p t -> p c t"), writes=["msk"])
        for c_ in range(2):
            P.dma("sp", VW[:, :, c_, 0:64], k.vtok[:, c_ * 64:(c_ + 1) * 64].rearrange("(t p) d -> p t d", p=128), reads=["vtok", "VW"], writes=["VW"])
        for j in range(9):
            qs_, qsk = Qsr.next()
            srow = (ZT_WQS + j) * 128 if j < 8 else ZT_WKS * 128
            P.dma("sp", qs_[:], k.zT[srow:srow + 128, LC:NT], reads=["zT"], writes=[qsk])
            for c in range(4):
                sl = slice(c * 512, (c + 1) * 512)
                sln = slice(LC + c * 512, LC + (c + 1) * 512)
                src = Qp[:, j, sln] if j < 8 else Kp[:, sln]
                dst = Qr[:, j, sl] if j < 8 else Kr[:, sl]
                tA, tAk = tAr.next()
                tB, tBk = tBr.next()
                P.op("dve", lambda e, sl=sl, src=src, tA=tA: e.tensor_tensor(tA[:], src, rope[:, 0, sl], ALU.mult), reads=["Qp", "Kp", "rope"], writes=[tAk])
                P.op("dve", lambda e, sl=sl, qs_=qs_, tB=tB: e.tensor_tensor(tB[:], qs_[:, sl], rope[:, 1, sl], ALU.mult), reads=[qsk, "rope"], writes=[tBk])
                P.op("dve", lambda e, dst=dst, tA=tA, tB=tB: e.tensor_tensor(dst, tA[:], tB[:], ALU.add), reads=[tAk, tBk], writes=["Qr", "Kr"])
        PTr = Rot(nc, st, "wPT", 4, [128, 512], BF16)
        ostr = Rot(nc, st, "wost", 3, [128, 512], BF16)
        for h in range(8):
            kk = h // 4
            for n in range(17):
                if n < 16:
                    q0, nq = LC + n * 128, 128
                    chunks = [("b", n + d, d) for d in (-1, 0, 1) if 0 <= n + d < 16] + [("c", 0, 0), ("c", 1, 0)]
                else:
                    q0, nq = 0, LC
                    chunks = [("c", 0, 0), ("c", 1, 0)]

                def score_fn(kc, pscr, psk, chunks=chunks, q0=q0, nq=nq, h=h):
                    typ, ci, d = chunks[kc]
                    if typ == "b":
                        P.op("pe", lambda e: e.matmul(pscr[:, :nq], Kr[:, ci * 128:(ci + 1) * 128], Qr[:, h, q0 - LC:q0 - LC + nq], start=True, stop=True), reads=["Kr", "Qr"], writes=[psk])
                    else:
                        P.op("pe", lambda e: e.matmul(pscr[:, :nq], Kp[:, ci * 128:(ci + 1) * 128], Qp[:, h, q0:q0 + nq], start=True, stop=True), reads=["Kp", "Qp"], writes=[psk])

                def post(kc, pt, ptk, chunks=chunks):
                    typ, ci, d = chunks[kc]
                    if typ == "b" and d != 0:
                        mi = 0 if d == -1 else 1
                        P.op("dve", lambda e: e.tensor_tensor(pt[:, :128], pt[:, :128], msk[:, mi, :], ALU.mult), reads=[ptk, "msk"], writes=[ptk])

                def va_fn(kc, chunks=chunks, kk=kk):
                    typ, ci, d = chunks[kc]
                    tt = (2 + ci) if typ == "b" else ci
                    return VW[:, tt, kk, :], "VW"

                softmax_pv(k, ostr, score_fn, len(chunks), va_fn, nq, SC, k.brT[2][h * 64:(h + 1) * 64, q0:q0 + nq], "winT", PTr,
                           esk=esk[64:128, h:h + 1], post=post)
        stage_end(k)


def stage_merge(k, li):
    nc, P = k.nc, k.P
    with ExitStack() as st:
        sb = lambda name, shape, dt=F32: st.enter_context(nc.sbuf_tensor(U(name), shape, dt))
        wbr = sb("wbr", [128, 12, D], BF16)
        wout = sb("wout", [128, 8, D], BF16)
        lnp = sb("lnp", [128, 4, 8])
        k.lnp_t = lnp
        P.dma("sp", lnp[:], k.lnp[li], writes=["mod"])
        for j in range(12):
            P.dma("pool", wbr[:, j, :], k.w_branch[li][j * 128:(j + 1) * 128, :], writes=["wbr"])
        for j in range(8):
            P.dma("pool", wout[:, j, :], k.w_out[li][j * 128:(j + 1) * 128, :], writes=["wout"])
        tmp = ln_tmp(nc, st)
        obr = [sb("ob%d" % b, [128, 4, 512], BF16) for b in range(3)]
        gbr = [sb("gb%d" % b, [128, 8, 512], BF16) for b in range(3)]
        mT = sb("mT", [128, 8, 512], BF16)
        m1r = Rot(nc, st, "mgt1", 2, [128, 512], F32)
        m2r = Rot(nc, st, "mgt2", 3, [128, 512], F32)
        ytr = Rot(nc, st, "mgty", 2, [128, 512], F32)
        xb = sb("mxb", [128, 8, 512])
        rb = sb("mrb", [128, 8, 512])
        x1 = sb("mx1", [128, 8, 512])
        h2 = sb("mh2", [128, 8, 512], BF16)
        xTv = k.xT.rearrange("(k p) t -> p k t", p=128)
        h2v = k.h2T.rearrange("(k p) t -> p k t", p=128)
        for (t0, w, isc) in TBS:
            sl = slice(t0, t0 + w)
            for b in range(3):
                P.dma("sp", obr[b][:, :, :w], k.brT[b][:, sl].rearrange("(k p) t -> p k t", p=128), reads=["brT"], writes=["ob%d" % b])
                P.dma("sp", gbr[b][:, :, :w], k.zT[(ZT_G + 8 * b) * 128:(ZT_G + 8 + 8 * b) * 128, sl].rearrange("(k p) t -> p k t", p=128), reads=["zT"], writes=["gb%d" % b])
            P.dma("sp", xb[:, :, :w], xTv[:, :, sl], reads=["xT"], writes=["mxb"])
            for mo in range(8):
                mt1, m1k = m1r.next()
                for b in range(3):
                    pt, pk = psn(k)
                    for kk in range(4):
                        P.op("pe", lambda e, pt=pt, kk=kk, b=b, mo=mo, w=w: e.matmul(pt[:, :w], wbr[:, b * 4 + kk, mo * 128:(mo + 1) * 128], obr[b][:, kk, :w], start=(kk == 0), stop=(kk == 3)), reads=["wbr", "ob%d" % b], writes=[pk])
                    if b == 0:
                        P.op("dve", lambda e, pt=pt, mo=mo, w=w, mt1=mt1: e.tensor_tensor(mt1[:, :w], pt[:, :w], gbr[0][:, mo, :w], ALU.mult), reads=[pk, "gb0"], writes=[m1k])
                    elif b == 1:
                        mt2, m2k = m2r.next()
                        P.op("dve", lambda e, pt=pt, mo=mo, w=w, mt2=mt2: e.tensor_tensor(mt2[:, :w], pt[:, :w], gbr[1][:, mo, :w], ALU.mult), reads=[pk, "gb1"], writes=[m2k])
                        P.op("dve", lambda e, w=w, mt1=mt1, mt2=mt2: e.tensor_tensor(mt1[:, :w], mt1[:, :w], mt2[:, :w], ALU.add), reads=[m1k, m2k], writes=[m1k])
                    else:
                        mt2, m2k = m2r.next()
                        P.op("dve", lambda e, pt=pt, mo=mo, w=w, mt2=mt2: e.tensor_tensor(mt2[:, :w], pt[:, :w], gbr[2][:, mo, :w], ALU.mult), reads=[pk, "gb2"], writes=[m2k])
                        P.op("dve", lambda e, mo=mo, w=w, mt1=mt1, mt2=mt2: e.tensor_tensor(mT[:, mo, :w], mt1[:, :w], mt2[:, :w], ALU.add), reads=[m1k, m2k], writes=["mT"])
            for mo in range(8):
                pt, pk = psn(k)
                for kk in range(8):
                    P.op("pe", lambda e, pt=pt, kk=kk, mo=mo, w=w: e.matmul(pt[:, :w], wout[:, kk, mo * 128:(mo + 1) * 128], mT[:, kk, :w], start=(kk == 0), stop=(kk == 7)), reads=["wout", "mT"], writes=[pk])
                yt, ytk = ytr.next()
                P.op("act", lambda e, pt=pt, mo=mo, w=w, isc=isc, yt=yt: e.activation(out=yt[:, :w], in_=pt[:, :w], func=AF.Identity, scale=k.mod[:, li, 16 + mo, isc:isc + 1]), reads=[pk, "mod"], writes=[ytk])
                P.op("dve", lambda e, mo=mo, w=w, yt=yt: e.scalar_tensor_tensor(rb[:, mo, :w], xb[:, mo, :w], float(ALPHA), yt[:, :w], ALU.mult, ALU.add), reads=["mxb", ytk], writes=["mrb"])
            ln_stats(k, rb, "mrb", w, tmp)
            for kk in range(8):
                ln_apply(k, rb, "mrb", w, tmp, kk, x1[:, kk, :w], ["mx1"], lnp[:, 0, kk:kk + 1], lnp[:, 1, kk:kk + 1])
            P.dma("sp", xTv[:, :, sl], x1[:, :, :w], reads=["mx1"], writes=["xT"])
            ln_stats(k, x1, "mx1", w, tmp)
            for kk in range(8):
                ln_apply(k, x1, "mx1", w, tmp, kk, h2[:, kk, :w], ["mh2"], k.mod[:, li, 32 + kk, isc:isc + 1], k.mod[:, li, 24 + kk, isc:isc + 1])
            P.dma("sp", h2v[:, :, sl], h2[:, :, :w], reads=["mh2"], writes=["h2T"])
        stage_end(k)


TB9 = [(0, 256, 1)] + [(256 + i * 256, 256, 0) for i in range(8)]


def stage_moe(k, li):
    nc, P = k.nc, k.P
    with ExitStack() as st0:
        sb0 = lambda name, shape, dt=F32: st0.enter_context(nc.sbuf_tensor(U(name), shape, dt))
        lnp = sb0("elnp", [128, 4, 8])
        posm = sb0("eposm", [16, NT])
        sel = sb0("esel", [16, 16, 128])
        iop = sb0("eiop", [128, 3])
        P.dma("sp", lnp[:], k.lnp[li], writes=["mod"])
        P.dma("sp", sel[:], k.sel16, writes=["esel"])
        P.dma("sp", iop[:], k.iota_p3, writes=["eiop"])
        with ExitStack() as st1:
            sb1 = lambda name, shape, dt=F32: st1.enter_context(nc.sbuf_tensor(U(name), shape, dt))
            h2tok = sb1("eh2tok", [128, 18, D], BF16)
            posm_tok = sb1("eposmt", [128, 18, 16])
            gw_tok = sb1("egwt", [128, 18, 16], BF16)
            ios = sb1("eios", [128, 384])
            P.dma("sp", ios[:], k.iota_s, writes=["eios"])
            with ExitStack() as stA:
                sbA = lambda name, shape, dt=F32: stA.enter_context(nc.sbuf_tensor(U(name), shape, dt))
                h2 = sbA("eh2", [128, 8, NT], BF16)
                wr = sbA("ewr", [128, 8, 16], BF16)
                aff = sbA("eaff", [16, NT])
                wk_ = sbA("ewk", [16, NT])
                gw = sbA("egw", [16, NT])
                msk = sbA("emsk", [16, NT])
                m8 = sbA("em8", [16, 8])
                thr = sbA("ethr", [16, 2])
                P.dma("sp", h2[:], k.h2T.rearrange("(k p) t -> p k t", p=128), reads=["h2T"], writes=["eh2"])
                P.dma("pool", wr[:], k.w_router[li].rearrange("(k p) n -> p k n", p=128), writes=["ewr"])
                for (t0, w, isc) in TBS:
                    sl = slice(t0, t0 + w)
                    pt, pk = psn(k)
                    for kk in range(8):
                        P.op("pe", lambda e, pt=pt, kk=kk, sl=sl, w=w: e.matmul(pt[0:16, :w], wr[:, kk, :], h2[:, kk, sl], start=(kk == 0), stop=(kk == 7)), reads=["ewr", "eh2"], writes=[pk])
                    P.op("act", lambda e, pt=pt, sl=sl, w=w: e.activation(out=wk_[:, sl], in_=pt[0:16, :w], func=AF.Exp), reads=[pk], writes=["ewk"])
                    p2, k2 = psn(k)
                    P.op("pe", lambda e, p2=p2, sl=sl, w=w: e.matmul(p2[0:16, :w], k.onesf[0:16, 0:16], wk_[:, sl], start=True, stop=True), reads=["ewk", "onesf"], writes=[k2])
                    P.op("dve", lambda e, p2=p2, sl=sl, w=w: e.reciprocal(gw[:, sl], p2[0:16, :w]), reads=[k2], writes=["egw"])
                    P.op("dve", lambda e, sl=sl: e.tensor_tensor(aff[:, sl], wk_[:, sl], gw[:, sl], ALU.mult), reads=["ewk", "egw"], writes=["eaff"])
                P.op("dve", lambda e: e.tensor_copy(wk_[:], aff[:]), reads=["eaff"], writes=["ewk"])
                for si, (a_, b_, cap, off) in enumerate(((0, LC, 32, 256.0), (LC, NT, 256, 0.0))):
                    for r in range(cap // 8):
                        P.op("dve", lambda e, a_=a_, b_=b_: e.max(out=m8[:], in_=wk_[:, a_:b_]), reads=["ewk"], writes=["em8"])
                        if r < cap // 8 - 1:
                            P.op("dve", lambda e, a_=a_, b_=b_: e.match_replace(out=wk_[:, a_:b_], in_to_replace=m8[:], in_values=wk_[:, a_:b_], imm_value=-1.0), reads=["ewk", "em8"], writes=["ewk"])
                    P.op("dve", lambda e, si=si: e.tensor_copy(thr[:, si:si + 1], m8[:, 7:8]), reads=["em8"], writes=["ethr"])
                    P.op("dve", lambda e, a_=a_, b_=b_, si=si: e.tensor_scalar(msk[:, a_:b_], aff[:, a_:b_], thr[:, si:si + 1], None, ALU.is_ge), reads=["eaff", "ethr"], writes=["emsk"])
                    P.op("dve", lambda e, a_=a_, b_=b_: e.tensor_tensor(gw[:, a_:b_], aff[:, a_:b_], msk[:, a_:b_], ALU.mult), reads=["eaff", "emsk"], writes=["egw"])
                    P.op("dve", lambda e, a_=a_, b_=b_: e.tensor_tensor_scan(wk_[:, a_:b_], k.onesf[0:16, 0:1].to_broadcast([16, b_ - a_]), msk[:, a_:b_], 0.0, ALU.mult, ALU.add), reads=["emsk", "onesf", "ewk"], writes=["ewk"])
                    P.op("dve", lambda e, a_=a_, b_=b_, off=off: e.scalar_tensor_tensor(posm[:, a_:b_], wk_[:, a_:b_], float(off), msk[:, a_:b_], ALU.add, ALU.mult), reads=["ewk", "emsk"], writes=["eposm"])
                    P.op("dve", lambda e, a_=a_, b_=b_: e.tensor_scalar_add(posm[:, a_:b_], posm[:, a_:b_], -1.0), reads=["eposm"], writes=["eposm"])
                for (src, dst, skey, dkey) in ((posm, posm_tok, "eposm", "eposmt"), (gw, gw_tok, "egw", "egwt")):
                    pt, pk = psn(k)
                    for tt in range(18):
                        P.op("pe", lambda e, pt=pt, tt=tt, src=src: e.transpose(pt[:, tt * 16:(tt + 1) * 16], src[0:16, tt * 128:(tt + 1) * 128], k.identf[0:16, 0:16]), reads=[skey, "identf"], writes=[pk])
                    P.op("dve", lambda e, pt=pt, dst=dst: e.tensor_copy(dst[:], pt[:, 0:288].rearrange("p (t e) -> p t e", e=16)), reads=[pk], writes=[dkey])
                for tt in range(18):
                    pt, pk = psn(k)
                    ptb = pt[:].bitcast(BF16)
                    for kf in range(8):
                        P.op("pe", lambda e, ptb=ptb, kf=kf, tt=tt: e.transpose(ptb[:, kf * 128:(kf + 1) * 128], h2[:, kf, tt * 128:(tt + 1) * 128], k.identb[:]), reads=["eh2", "identb"], writes=[pk])
                    if tt % 2 == 0:
                        P.op("act", lambda e, ptb=ptb, tt=tt: e.copy(h2tok[:, tt, :], ptb[:, 0:1024]), reads=[pk], writes=["eh2tok"])
                    else:
                        P.op("dve", lambda e, ptb=ptb, tt=tt: e.tensor_copy(h2tok[:, tt, :], ptb[:, 0:1024]), reads=[pk], writes=["eh2tok"])
                P.barrier()
            mw = Rot(nc, st1, "emw", 32, [128, D], BF16)
            Sr = Rot(nc, st1, "eS", 2, [128, 18, 384], BF16)
            Xr = Rot(nc, st1, "eX", 2, [128, 8, 288], BF16)
            Ar = Rot(nc, st1, "eA", 2, [128, 8, 384], BF16)
            Yr = Rot(nc, st1, "eY", 2, [128, 3, D], BF16)
            sar = Rot(nc, st1, "esa", 2, [128, 288], F32)
            gsr = Rot(nc, st1, "egs", 2, [128, 3], F32)
            for j in range(2):
                At, Ak = Ar.next()
                P.op("pool", lambda e, At=At: e.memset(At[:], 0.0), writes=[Ak])
            ev = 0
            for ex in range(16):
                ws = {}
                for nm, src in (("g", k.w_gate), ("u", k.w_up), ("d", k.w_down)):
                    for kk in range(8):
                        t, tk = mw.next()
                        P.dma("pool", t[:], src[li, ex, kk * 128:(kk + 1) * 128, :], writes=[tk])
                        ws[(nm, kk)] = (t, tk)
                S, Sk = Sr.next()
                P.op("dve", lambda e, S=S, ex=ex: e.tensor_tensor(S[:], ios[:].unsqueeze(1).to_broadcast([128, 18, 384]), posm_tok[:, :, ex:ex + 1].to_broadcast([128, 18, 384]), ALU.is_equal), reads=["eios", "eposmt"], writes=[Sk])
                X, Xk = Xr.next()
                for ft in range(8):
                    pt, pk = psn(k, 0, 4)
                    for tt in range(2, 18):
                        P.op("pe", lambda e, pt=pt, tt=tt, ft=ft, S=S: e.matmul(pt[:, 0:256], h2tok[:, tt, ft * 128:(ft + 1) * 128], S[:, tt, 0:256], start=(tt == 2), stop=(tt == 17)), reads=["eh2tok", Sk], writes=[pk])
                    for tt in range(2):
                        P.op("pe", lambda e, pt=pt, tt=tt, ft=ft, S=S: e.matmul(pt[:, 256:288], h2tok[:, tt, ft * 128:(ft + 1) * 128], S[:, tt, 256:288], start=(tt == 0), stop=(tt == 1)), reads=["eh2tok", Sk], writes=[pk])
                    if ev % 2 == 0:
                        P.op("act", lambda e, pt=pt, ft=ft, X=X: e.copy(X[:, ft, :], pt[:, :288]), reads=[pk], writes=[Xk])
                    else:
                        P.op("dve", lambda e, pt=pt, ft=ft, X=X: e.tensor_copy(X[:, ft, :], pt[:, :288]), reads=[pk], writes=[Xk])
                    ev += 1
                pg, pgk = psn(k, 0, 4)
                for st_ in range(3):
                    tts = list(range(2, 18)) if st_ < 2 else [0, 1]
                    for tt in tts:
                        P.op("pe", lambda e, pg=pg, tt=tt, st_=st_, S=S, ex=ex, tts=tts: e.matmul(pg[:, st_:st_ + 1], S[:, tt, st_ * 128:(st_ + 1) * 128], gw_tok[:, tt, ex:ex + 1], start=(tt == tts[0]), stop=(tt == tts[-1])), reads=[Sk, "egwt"], writes=[pgk])
                gs, gsk = gsr.next()
                P.op("dve", lambda e, pg=pg, gs=gs: e.tensor_copy(gs[:], pg[:, 0:3]), reads=[pgk], writes=[gsk])
                At, Ak = Ar.next()
                for fo in range(8):
                    pa, pak = psn(k, 4, 6)
                    pu, puk = psn(k, 6, 8)
                    for kk in range(8):
                        wt, wtk = ws[("g", kk)]
                        P.op("pe", lambda e, pa=pa, wt=wt, kk=kk, fo=fo, X=X: e.matmul(pa[:, :288], wt[:, fo * 128:(fo + 1) * 128], X[:, kk, :], start=(kk == 0), stop=(kk == 7)), reads=[wtk, Xk], writes=[pak])
                    for kk in range(8):
                        wt, wtk = ws[("u", kk)]
                        P.op("pe", lambda e, pu=pu, wt=wt, kk=kk, fo=fo, X=X: e.matmul(pu[:, :288], wt[:, fo * 128:(fo + 1) * 128], X[:, kk, :], start=(kk == 0), stop=(kk == 7)), reads=[wtk, Xk], writes=[puk])
                    s_, sk_ = sar.next()
                    P.op("act", lambda e, pa=pa, s_=s_: e.activation(out=s_[:], in_=pa[:, :288], func=AF.Silu), reads=[pak], writes=[sk_])
                    P.op("dve", lambda e, pu=pu, s_=s_, At=At, fo=fo: e.tensor_tensor(At[:, fo, 0:288], pu[:, :288], s_[:], ALU.mult), reads=[puk, sk_], writes=[Ak])
                Y, Yk = Yr.next()
                for st_ in range(3):
                    for half in range(2):
                        py, pyk = psn(k, 0, 4)
                        for kk in range(8):
                            wt, wtk = ws[("d", kk)]
                            P.op("pe", lambda e, py=py, wt=wt, kk=kk, st_=st_, half=half, At=At: e.matmul(py[:, :512], At[:, kk, st_ * 128:(st_ + 1) * 128], wt[:, half * 512:(half + 1) * 512], start=(kk == 0), stop=(kk == 7)), reads=[wtk, Ak], writes=[pyk])
                        P.op("act", lambda e, py=py, st_=st_, half=half, Y=Y, gs=gs: e.activation(out=Y[:, st_, half * 512:(half + 1) * 512], in_=py[:, :512], func=AF.Identity, scale=gs[:, st_:st_ + 1]), reads=[pyk, gsk], writes=[Yk])
                P.dma("sp", k.yg[ex].rearrange("s p d -> p s d"), Y[:], reads=[Yk], writes=["yg"])
            P.barrier()
        with ExitStack() as st2:
            sb2 = lambda name, shape, dt=F32: st2.enter_context(nc.sbuf_tensor(U(name), shape, dt))
            Yall = sb2("eYall", [128, 48, D], BF16)
            for ex in range(16):
                P.dma("sp", Yall[:, ex * 3:(ex + 1) * 3, :], k.yg[ex].rearrange("s p d -> p s d"), reads=["yg"], writes=["eYall"])
            STr = Rot(nc, st2, "eST", 2, [128, 32, 256], BF16)
            tmp = ln_tmp(nc, st2, 256)
            xbr = Rot(nc, st2, "exb", 2, [128, 8, 256], F32)
            rbR = Rot(nc, st2, "erb", 2, [128, 8, 256], F32)
            xTv = k.xT.rearrange("(k p) t -> p k t", p=128)
            for (t0, w, isc) in TB9:
                sl = slice(t0, t0 + w)
                xb, xbk = xbr.next()
                rb, rbk = rbR.next()
                P.dma("sp", xb[:], xTv[:, :, sl], reads=["xT"], writes=[xbk])
                ST, STk = STr.next()
                sts = [2] if isc else [0, 1]
                n_ = len(sts)
                for ex in range(16):
                    pb, pbk = psn(k, 4, 8)
                    P.op("pe", lambda e, pb=pb, sl=sl, ex=ex: e.matmul(pb[:, :256], sel[:, ex, :], posm[:, sl], start=True, stop=True), reads=["esel", "eposm"], writes=[pbk])
                    P.op("dve", lambda e, pb=pb, ex=ex, ST=ST, sts=sts, n_=n_: e.tensor_tensor(ST[:, ex * n_:(ex + 1) * n_, :], pb[:, 0:256].unsqueeze(1).to_broadcast([128, n_, 256]), iop[:, sts[0]:sts[0] + n_].unsqueeze(2).to_broadcast([128, n_, 256]), ALU.is_equal), reads=[pbk, "eiop"], writes=[STk])
                js = [(ex * 3 + st_, ex * n_ + i_) for ex in range(16) for i_, st_ in enumerate(sts)]
                for mo in range(8):
                    pf, pfk = psn(k, 0, 4)
                    for (jy, jt) in js:
                        P.op("pe", lambda e, pf=pf, jy=jy, jt=jt, mo=mo, ST=ST, js=js: e.matmul(pf[:, :256], Yall[:, jy, mo * 128:(mo + 1) * 128], ST[:, jt, :], start=(jy == js[0][0]), stop=(jy == js[-1][0])), reads=["eYall", STk], writes=[pfk])
                    ft_, ftk = tmp["tr"].next()
                    P.op("act", lambda e, pf=pf, mo=mo, isc=isc, ft_=ft_: e.activation(out=ft_[:, :256], in_=pf[:, :256], func=AF.Identity, scale=k.mod[:, li, 40 + mo, isc:isc + 1]), reads=[pfk, "mod"], writes=[ftk])
                    P.op("dve", lambda e, mo=mo, xb=xb, ft_=ft_, rb=rb: e.scalar_tensor_tensor(rb[:, mo, :], xb[:, mo, :], float(ALPHA), ft_[:, :256], ALU.mult, ALU.add), reads=[xbk, ftk], writes=[rbk])
                ln_stats(k, rb, rbk, w, tmp)
                for kk in range(8):
                    ln_apply(k, rb, rbk, w, tmp, kk, xb[:, kk, :], [xbk], lnp[:, 2, kk:kk + 1], lnp[:, 3, kk:kk + 1])
                P.dma("sp", xTv[:, :, sl], xb[:], reads=[xbk], writes=["xT"])
        stage_end(k)


def stage_out(k):
    nc, P = k.nc, k.P
    with ExitStack() as st:
        xr = Rot(nc, st, "oxr", 2, [128, 8, 128], F32)
        xo = Rot(nc, st, "oxo", 2, [128, D], F32)
        xTv = k.xT.rearrange("(k p) t -> p k t", p=128)
        for tt in range(L // 128):
            xt, xk = xr.next()
            P.dma("sp", xt[:], xTv[:, :, LC + tt * 128:LC + (tt + 1) * 128], reads=["xT"], writes=[xk])
            ot, ok = xo.next()
            for half in range(2):
                pt, pk = psn(k)
                for kk in range(4):
                    kf = half * 4 + kk
                    P.op("pe", lambda e, pt=pt, kk=kk, kf=kf, xt=xt: e.transpose(pt[:, kk * 128:(kk + 1) * 128], xt[:, kf, :], k.identf[:]),
                         reads=[xk, "identf"], writes=[pk])
                if half == 0:
                    P.op("act", lambda e, pt=pt, ot=ot: e.copy(ot[:, 0:512], pt[:]), reads=[pk], writes=[ok + "h0"])
                else:
                    P.op("dve", lambda e, pt=pt, ot=ot: e.tensor_copy(ot[:, 512:1024], pt[:]), reads=[pk], writes=[ok + "h1"])
            P.dma("sp", k.out[tt * 128:(tt + 1) * 128, :], ot[:], reads=[ok + "h0", ok + "h1"], writes=["out"])
        stage_end(k)


Z_ORDER = None


def _zcols():
    u = np.arange(0, 512)
    qa = np.arange(512, 896)
    kva = np.arange(896, 1152)
    kr = np.arange(1152, 1184)
    wq = np.arange(1184, 1696)
    wk = np.arange(1696, 1824)
    wv = np.arange(1824, 1952)
    gates = np.arange(1952, 5024)
    Z = -np.ones(64, np.int64)

    def padq(cols8):
        out = []
        for h in range(8):
            kk = h // 4
            out.append(np.concatenate([cols8[h], Z]) if kk == 0 else np.concatenate([Z, cols8[h]]))
        return np.concatenate(out)
    wq8 = wq.reshape(8, 64)
    wq8s = wq.reshape(8, 2, 32)[:, ::-1, :].reshape(8, 64)
    wk_sw = wk.reshape(2, 2, 32)[:, ::-1, :].reshape(-1)
    kr_sw = kr.reshape(2, 16)[::-1].reshape(-1)
    cols = np.concatenate([u, qa, kva, padq(wq8), wk, wv, gates, padq(wq8s), wk_sw, kr, kr_sw, Z])
    assert cols.size == NZT * 128, cols.size
    return cols


def prep_shared(inp):
    f = lambda a: np.ascontiguousarray(np.asarray(a, np.float32))
    sh = {}
    sh["ident"] = np.eye(128, dtype=np.float32)
    sh["w_ada"] = f(inp["w_ada"])
    b = f(inp["b_ada"]).reshape(DEPTH, 48, 128).transpose(0, 2, 1)
    sh["bada2"] = f(np.repeat(b[:, :, :, None], 2, axis=3))
    cols = _zcols()
    wx = f(inp["w_in"])[:, :, np.maximum(cols, 0)]
    wx[:, :, cols < 0] = 0.0
    sh["w_inx"] = f(wx)
    G, PS, HG = 32, 64, 16
    bT = np.zeros((DEPTH, 2, 2, 16, 128, 128), np.float32)
    cL = np.zeros((DEPTH, 128, 2, 16, 2, 16), np.float32)
    lane = np.zeros((DEPTH, 128, 3, 32), np.float32)
    for ri, nm in enumerate(("s5_b_re", "s5_b_im")):
        bsrc = f(inp[nm])
        for lt in range(16):
            for half in range(2):
                g = 2 * lt + half
                gl = g % 8
                bT[:, :, ri, lt, gl * 16:(gl + 1) * 16, half * 64:(half + 1) * 64] = bsrc[:, :, g].transpose(0, 1, 3, 2)
    for ri, nm in enumerate(("s5_c_re", "s5_c_im")):
        csrc = f(inp[nm])
        for lt in range(16):
            for half in range(2):
                g = 2 * lt + half
                cL[:, half * 64:(half + 1) * 64, :, lt, ri, :] = csrc[:, :, g].transpose(0, 3, 1, 2)
    lre, lim, ldt = f(inp["s5_lam_re"]), f(inp["s5_lam_im"]), f(inp["s5_log_dt"])
    for d in range(2):
        for lt in range(16):
            for half in range(2):
                g = 2 * lt + half
                lane[:, half * 64:(half + 1) * 64, 0, d * 16 + lt] = lre[:, d, g, :]
                lane[:, half * 64:(half + 1) * 64, 1, d * 16 + lt] = lim[:, d, g, :]
                lane[:, half * 64:(half + 1) * 64, 2, d * 16 + lt] = ldt[:, d, g][:, None]
    sh["s5_bT"], sh["s5_cL"], sh["s5_lane"] = bT, cL, lane
    dg = np.zeros((DEPTH, 128, 2, 4), np.float32)
    dg[:, :, 0, :] = f(inp["s5_d"]).reshape(DEPTH, 4, 128).transpose(0, 2, 1)
    dg[:, :, 1, :] = f(inp["s5_b_glu"]).reshape(DEPTH, 4, 128).transpose(0, 2, 1)
    sh["s5_dg"] = dg
    sh["s5_wglu"] = f(inp["s5_w_glu"])
    mg = np.zeros((DEPTH, 128, 5), np.float32)
    mg[:, :, 0:3] = f(inp["mla_q_norm"]).reshape(DEPTH, 3, 128).transpose(0, 2, 1)
    mg[:, :, 3:5] = f(inp["mla_kv_norm"]).reshape(DEPTH, 2, 128).transpose(0, 2, 1)
    sh["mla_g"] = mg
    wuq = f(inp["mla_w_uq"]).reshape(DEPTH, 384, 8, 96)
    wm = np.zeros((DEPTH, 384, 8, 128), np.float32)
    wsw = np.zeros((DEPTH, 384, 8, 128), np.float32)
    wm[..., 0:96] = wuq
    wsw[..., 64:96] = wuq[..., 64:].reshape(DEPTH, 384, 8, 2, 16)[:, :, :, ::-1, :].reshape(DEPTH, 384, 8, 32)
    sh["w_uqx"] = f(np.concatenate([wm.reshape(DEPTH, 384, 1024), wsw.reshape(DEPTH, 384, 1024)], -1))
    wkv = f(inp["mla_w_ukv"]).reshape(DEPTH, 256, 8, 128)
    wkp = np.zeros((DEPTH, 256, 8, 128), np.float32)
    wkp[..., 0:64] = wkv[..., :64]
    sh["w_ukvk"] = f(wkp.reshape(DEPTH, 256, 1024))
    sh["w_ukvv"] = f(wkv[..., 64:].reshape(DEPTH, 256, 512))
    rows = L // 64
    row = np.repeat(np.arange(rows, dtype=np.float64), 64)
    col = np.tile(np.arange(64, dtype=np.float64), rows)

    def rope_tab(d, reps):
        nf = d // 4
        fr = 10000.0 ** (-np.arange(nf) / nf)
        ang = np.concatenate([row[:, None] * fr, col[:, None] * fr], -1)
        c = np.concatenate([np.cos(ang), np.cos(ang)], -1).T
        sn = np.concatenate([-np.sin(ang), np.sin(ang)], -1).T
        return np.stack([np.tile(c, (reps, 1)), np.tile(sn, (reps, 1))], 0).astype(np.float32)
    rm = np.zeros((2, 128, L), np.float32)
    rm[0] = 1.0
    rm[:, 64:96, :] = rope_tab(32, 1)
    sh["rope_mla"] = rm
    sh["rope_win"] = rope_tab(64, 2)
    jj = np.arange(128)[:, None]
    rr = np.arange(128)[None, :]
    sh["wmask"] = np.stack([(jj >= rr), (jj <= rr)], 0).astype(np.float32)
    sh["win_sink"] = f(np.broadcast_to(f(inp["win_sink"])[:, None, :], (DEPTH, 128, 8)))
    sh["w_branch"] = f(inp["w_branch"]).reshape(DEPTH, 1536, D)
    sh["w_out"] = f(inp["w_out"])
    lnp = np.stack([f(inp[n]).reshape(DEPTH, 8, 128).transpose(0, 2, 1) for n in ("ln1_g", "ln1_b", "ln2_g", "ln2_b")], 2)
    sh["lnp"] = f(lnp)
    sh["w_router"] = f(inp["w_router"])
    sh["w_gate"], sh["w_up"], sh["w_down"] = f(inp["w_gate"]), f(inp["w_up"]), f(inp["w_down"])
    sel = np.zeros((16, 16, 128), np.float32)
    for e_ in range(16):
        sel[e_, e_, :] = 1.0
    sh["sel16"] = sel
    sh["iota_s"] = f(np.broadcast_to(np.arange(384, dtype=np.float32), (128, 384)))
    sh["iota_p3"] = f(np.arange(128, dtype=np.float32)[:, None] + np.array([0.0, 128.0, 256.0], np.float32)[None, :])
    sh["tau"] = f(np.broadcast_to(np.arange(NT, dtype=np.float32), (128, NT)))
    return sh


def prep_core(inp, b):
    f = lambda a: np.ascontiguousarray(np.asarray(a, np.float32))
    m = {}
    m["xin"] = f(np.concatenate([inp["ctx"][b], inp["x"][b]], 0))
    cond = np.stack([np.asarray(inp["c"][b]), np.asarray(inp["c_ctx"])], -1)
    m["condT"] = f(cond.reshape(8, 128, 2).transpose(1, 0, 2))
    return m


def kernel(**inputs):
    nc = build()
    sh = prep_shared(inputs)
    in_maps = []
    for b in range(8):
        m = dict(sh)
        m.update(prep_core(inputs, b))
        in_maps.append(m)
    res = run_bass_kernel_spmd(nc, in_maps, core_ids=list(range(8)))
    return np.stack([np.asarray(r["out"], np.float32) for r in res.results], 0)
```

```python
import numpy as np
from contextlib import ExitStack
import concourse.bass as bass
import concourse.mybir as mybir
from concourse.bass_utils import run_bass_kernel_spmd

F32 = mybir.dt.float32
BF16 = mybir.dt.bfloat16
I32 = mybir.dt.int32
F32R = mybir.dt.float32r
AF = mybir.ActivationFunctionType
ALU = mybir.AluOpType

ENGS = ("pe", "dve", "act", "pool", "sp")
D = 1024
NT = 2304
LC = 256
L = 2048
DEPTH = 4
ALPHA = (2 * DEPTH) ** 0.25
EPS = 1e-6
NZT = 53
ZT_QA, ZT_KVA, ZT_WQ, ZT_WK, ZT_WV, ZT_G, ZT_WQS, ZT_WKS, ZT_KRX = 4, 7, 9, 17, 18, 19, 43, 51, 52
TBS = [(0, 256, 1), (256, 512, 0), (768, 512, 0), (1280, 512, 0), (1792, 512, 0)]


class Prog:
    def __init__(self, nc, es, n_dma_sems=24):
        self.nc = nc
        self.q = {e: [] for e in ENGS}
        self.sem = {}
        self.cnt = {}
        for e in ENGS:
            self.sem[e] = es.enter_context(nc.semaphore("s_" + e))
            self.cnt[e] = 0
        self.dma_sems = []
        for i in range(n_dma_sems):
            nm = "d%d" % i
            self.sem[nm] = es.enter_context(nc.semaphore("s_" + nm))
            self.cnt[nm] = 0
            self.dma_sems.append(nm)
        self.dma_rr = 0
        self.seen = {e: {} for e in ENGS}
        self.lastw = {}
        self.readers = {}
        self.nops = 0

    def _deps(self, eng, reads, writes):
        deps = {}

        def need(st, v):
            if st == eng and eng == "pe":
                return
            if deps.get(st, 0) < v:
                deps[st] = v

        for k in reads:
            lw = self.lastw.get(k)
            if lw is not None:
                need(*lw)
            if k.startswith("ps"):
                for r in self.readers.get(k, ()):
                    if r[0] != eng:
                        need(*r)
        for k in writes:
            lw = self.lastw.get(k)
            if lw is not None:
                need(*lw)
            for r in self.readers.get(k, ()):
                need(*r)
        return deps

    def _emit_waits(self, eng, deps):
        for st, v in deps.items():
            if self.seen[eng].get(st, 0) < v:
                self.seen[eng][st] = v
                sem = self.sem[st]
                self.q[eng].append(lambda e, sem=sem, v=v: e.wait_ge(sem, v))

    def _record(self, done, reads, writes):
        for k in reads:
            self.readers.setdefault(k, []).append(done)
        for k in writes:
            self.lastw[k] = done
            self.readers[k] = []

    def op(self, eng, fn, reads=(), writes=()):
        deps = self._deps(eng, reads, writes)
        self._emit_waits(eng, deps)
        self.cnt[eng] += 1
        sem = self.sem[eng]
        self.q[eng].append(lambda e, fn=fn, sem=sem: fn(e).then_inc(sem, 1))
        self._record((eng, self.cnt[eng]), reads, writes)
        self.nops += 1

    def dma(self, qeng, out, in_, reads=(), writes=(), **kw):
        st = self.dma_sems[self.dma_rr % len(self.dma_sems)]
        self.dma_rr += 1
        deps = self._deps(st, reads, writes)
        if self.cnt[st] > 0:
            deps[st] = self.cnt[st]
        self._emit_waits(qeng, deps)
        self.cnt[st] += 16
        sem = self.sem[st]
        self.q[qeng].append(
            lambda e, out=out, in_=in_, sem=sem, kw=kw: e.dma_start(out=out, in_=in_, **kw).then_inc(sem, 16))
        self._record((st, self.cnt[st]), reads, writes)
        self.nops += 1

    def barrier(self):
        for eng in ENGS:
            deps = {}
            for st in self.sem:
                if st != eng and self.cnt[st] > 0:
                    deps[st] = self.cnt[st]
            self._emit_waits(eng, deps)
        self.lastw = {}
        self.readers = {}

    def replay(self):
        nc = self.nc
        q = self.q
        with nc.Block() as block:
            @block.tensor
            def _(e):
                for f in q["pe"]:
                    f(e)

            @block.vector
            def _(e):
                for f in q["dve"]:
                    f(e)

            @block.scalar
            def _(e):
                for f in q["act"]:
                    f(e)

            @block.gpsimd
            def _(e):
                for f in q["pool"]:
                    f(e)

            @block.sync
            def _(e):
                for f in q["sp"]:
                    f(e)
        self.q = {e: [] for e in ENGS}


_UID = [0]


def U(name):
    _UID[0] += 1
    return "%s_u%d" % (name, _UID[0])


class Rot:
    def __init__(self, nc, es, name, n, shape, dtype, psum=False):
        self.name = name
        self.n = n
        self.i = 0
        if psum:
            self.t = [es.enter_context(nc.psum_tensor(U("%s%d" % (name, j)), shape, dtype)) for j in range(n)]
        else:
            self.t = [es.enter_context(nc.sbuf_tensor(U("%s%d" % (name, j)), shape, dtype)) for j in range(n)]

    def next(self):
        j = self.i % self.n
        self.i += 1
        return self.t[j], "%s%d" % (self.name, j)


class K:
    pass


def build(nlayers=DEPTH, dbg=()):
    nc = bass.Bass("TRN2", target_bir_lowering=False)
    k = K()
    k.nc = nc
    k.dbg = dbg
    din = lambda name, shape, dt=F32: nc.dram_tensor(name, list(shape), dt, kind="ExternalInput").ap()

    def dscr(name, shape, dt):
        kind = "ExternalOutput" if name in dbg else "Internal"
        return nc.dram_tensor(name, list(shape), dt, kind=kind).ap()

    k.xin = din("xin", [NT, D])
    k.condT = din("condT", [128, 8, 2])
    k.ident = din("ident", [128, 128])
    k.w_ada = din("w_ada", [DEPTH, D, 6 * D])
    k.bada2 = din("bada2", [DEPTH, 128, 48, 2])
    k.w_inx = din("w_inx", [DEPTH, D, NZT * 128])
    k.s5_bT = din("s5_bT", [DEPTH, 2, 2, 16, 128, 128])
    k.s5_cL = din("s5_cL", [DEPTH, 128, 2, 16, 2, 16])
    k.s5_lane = din("s5_lane", [DEPTH, 128, 3, 32])
    k.s5_dg = din("s5_dg", [DEPTH, 128, 2, 4])
    k.s5_wglu = din("s5_wglu", [DEPTH, 512, 512])
    k.tau = din("tau", [128, NT])
    k.mla_g = din("mla_g", [DEPTH, 128, 5])
    k.w_uqx = din("w_uqx", [DEPTH, 384, 2048])
    k.w_ukvk = din("w_ukvk", [DEPTH, 256, 1024])
    k.w_ukvv = din("w_ukvv", [DEPTH, 256, 512])
    k.rope_mla = din("rope_mla", [2, 128, L])
    k.rope_win = din("rope_win", [2, 128, L])
    k.wmask = din("wmask", [2, 128, 128])
    k.win_sink = din("win_sink", [DEPTH, 128, 8])
    k.w_branch = din("w_branch", [DEPTH, 1536, D])
    k.w_out = din("w_out", [DEPTH, D, D])
    k.lnp = din("lnp", [DEPTH, 128, 4, 8])
    k.w_router = din("w_router", [DEPTH, D, 16])
    k.w_gate = din("w_gate", [DEPTH, 16, D, D])
    k.w_up = din("w_up", [DEPTH, 16, D, D])
    k.w_down = din("w_down", [DEPTH, 16, D, D])
    k.sel16 = din("sel16", [16, 16, 128])
    k.iota_s = din("iota_s", [128, 384])
    k.iota_p3 = din("iota_p3", [128, 3])
    k.out = nc.dram_tensor("out", [L, D], F32, kind="ExternalOutput").ap()
    k.brT = [dscr(nm, [512, NT], BF16) for nm in ("s5T", "mlaT", "winT")]
    k.xT = dscr("xT", [D, NT], F32)
    k.zT = dscr("zT", [NZT * 128, NT], BF16)
    k.vtok = dscr("vtok", [NT, 128], BF16)
    k.h2T = dscr("h2T", [D, NT], BF16)
    k.yg = dscr("yg", [16, 3, 128, D], BF16)

    with ExitStack() as es:
        P = Prog(nc, es)
        k.P = P
        k.identf = es.enter_context(nc.sbuf_tensor(U("identf"), [128, 128], F32))
        k.identb = es.enter_context(nc.sbuf_tensor(U("identb"), [128, 128], BF16))
        k.onesm = es.enter_context(nc.sbuf_tensor(U("onesm"), [128, 128], F32))
        k.mod = es.enter_context(nc.sbuf_tensor(U("mod"), [128, DEPTH, 48, 2], F32))
        k.epsc = es.enter_context(nc.sbuf_tensor(U("epsc"), [128, 1], F32))
        k.ps = [es.enter_context(nc.psum_tensor("ps%d" % i, [128, 512], F32)) for i in range(8)]
        k.psi = 0

        stage_init(k)
        stage_ada(k, nlayers)
        k.onesmb = es.enter_context(nc.sbuf_tensor(U("onesmb"), [128, 128], BF16))
        P.op("dve", lambda e: e.memset(k.onesmb[:], 1.0 / D), writes=["onesmb"])
        k.onesb = es.enter_context(nc.sbuf_tensor(U("onesb"), [128, 512], BF16))
        k.onesf = es.enter_context(nc.sbuf_tensor(U("onesf"), [128, 128], F32))
        P.op("dve", lambda e: e.memset(k.onesb[:], 1.0), writes=["onesb"])
        P.op("dve", lambda e: e.memset(k.onesf[:], 1.0), writes=["onesf"])
        k.halfpi = es.enter_context(nc.sbuf_tensor(U("halfpi"), [128, 1], F32))
        P.op("dve", lambda e: e.memset(k.halfpi[:], float(np.pi / 2)), writes=["halfpi"])
        for li in range(nlayers):
            if "skip_win" not in dbg:
                stage_ln_win(k, li)
            if "mla_first" in dbg:
                stage_mla(k, li)
            if "skip_s5" not in dbg:
                stage_s5(k, li)
            if "skip_mla" not in dbg and "mla_first" not in dbg:
                stage_mla(k, li)
            if "skip_winb" not in dbg:
                stage_win(k, li)
            if "skip_mm" not in dbg:
                stage_merge(k, li)
                stage_moe(k, li)
        stage_out(k)
    return nc


def dump(k, name, ap, shape, dt, reads):
    if name not in k.dbg:
        return
    t = k.nc.dram_tensor(name, list(shape), dt, kind="ExternalOutput").ap()
    k.P.dma("sp", t, ap, reads=reads)


def psn(k, lo=0, hi=8):
    j = lo + (k.psi % (hi - lo))
    k.psi += 1
    return k.ps[j], "ps%d" % j


def stage_end(k):
    k.P.barrier()
    k.P.replay()


def stage_init(k):
    nc, P = k.nc, k.P
    P.dma("sp", k.identf[:], k.ident, writes=["identf"])
    P.dma("pool", k.identb[:], k.ident, writes=["identb"])
    P.op("dve", lambda e: e.memset(k.onesm[:], 1.0 / D), writes=["onesm"])
    P.op("dve", lambda e: e.memset(k.epsc[:], EPS), writes=["epsc"])
    with ExitStack() as st:
        xr = Rot(nc, st, "xr", 2, [128, D], F32)
        xo = Rot(nc, st, "xo", 2, [128, 8, 128], F32)
        xTv = k.xT.rearrange("(k p) t -> p k t", p=128)
        for tt in range(NT // 128):
            xt, xk = xr.next()
            P.dma("sp", xt[:], k.xin[tt * 128:(tt + 1) * 128, :], writes=[xk])
            ot, ok = xo.next()
            for half in range(2):
                pt, pk = psn(k)
                for kk in range(4):
                    kf = half * 4 + kk
                    P.op("pe", lambda e, pt=pt, kk=kk, kf=kf, xt=xt: e.transpose(pt[:, kk * 128:(kk + 1) * 128], xt[:, kf * 128:(kf + 1) * 128], k.identf[:]),
                         reads=[xk, "identf"], writes=[pk])
                eng = "act" if half == 0 else "dve"
                if eng == "act":
                    P.op("act", lambda e, pt=pt, ot=ot, half=half: e.copy(ot[:, half * 4:(half + 1) * 4, :], pt[:].rearrange("p (k t) -> p k t", k=4)),
                         reads=[pk], writes=[ok + "h%d" % half])
                else:
                    P.op("dve", lambda e, pt=pt, ot=ot, half=half: e.tensor_copy(ot[:, half * 4:(half + 1) * 4, :], pt[:].rearrange("p (k t) -> p k t", k=4)),
                         reads=[pk], writes=[ok + "h%d" % half])
            P.dma("sp", xTv[:, :, tt * 128:(tt + 1) * 128], ot[:], reads=[ok + "h0", ok + "h1"], writes=["xT"])
        stage_end(k)


def stage_ada(k, nlayers):
    nc, P = k.nc, k.P
    with ExitStack() as st:
        sc = st.enter_context(nc.sbuf_tensor(U("sc"), [128, 8, 2], F32))
        bt = st.enter_context(nc.sbuf_tensor(U("bt"), [128, DEPTH, 48, 2], F32))
        wa = Rot(nc, st, "wa", 2, [128, 8, 768], F32)
        P.dma("sp", sc[:], k.condT, writes=["sc"])
        P.dma("sp", bt[:], k.bada2.rearrange("l p m s -> p l m s"), writes=["bt"])
        P.op("act", lambda e: e.activation(out=sc[:], in_=sc[:], func=AF.Silu), reads=["sc"], writes=["sc"])
        for li in range(nlayers):
            wv = k.w_ada[li].rearrange("(k p) n -> p k n", p=128)
            for cb in range(8):
                wt, wk = wa.next()
                P.dma("sp", wt[:], wv[:, :, cb * 768:(cb + 1) * 768], writes=[wk])
                pt, pk = psn(k)
                for mt in range(6):
                    for kk in range(8):
                        P.op("pe", lambda e, pt=pt, wt=wt, mt=mt, kk=kk: e.matmul(pt[:, mt * 2:mt * 2 + 2], wt[:, kk, mt * 128:(mt + 1) * 128], sc[:, kk, :], start=(kk == 0), stop=(kk == 7)),
                             reads=[wk, "sc"], writes=[pk])
                P.op("dve", lambda e, pt=pt, li=li, cb=cb: e.tensor_tensor(k.mod[:, li, cb * 6:(cb + 1) * 6, :], pt[:, 0:12].rearrange("p (m s) -> p m s", s=2), bt[:, li, cb * 6:(cb + 1) * 6, :], ALU.add),
                     reads=[pk, "bt"], writes=["mod"])
            for j in (1, 4):
                P.op("dve", lambda e, li=li, j=j: e.tensor_scalar_add(k.mod[:, li, j * 8:(j + 1) * 8, :], k.mod[:, li, j * 8:(j + 1) * 8, :], 1.0),
                     reads=["mod"], writes=["mod"])
        stage_end(k)


def ln_stats(k, xb, xk, w, tmp, tag=""):
    nc, P = k.nc, k.P
    sq, mean, rstd, m2, xbf = tmp["sq"], tmp["mean"], tmp["rstd"], tmp["m2"], tmp["xbf"]
    P.op("act", lambda e: e.activation(out=sq[:, :, :w], in_=xb[:, :, :w], func=AF.Square), reads=[xk], writes=["sq" + tag])
    P.op("act", lambda e: e.copy(xbf[:, :, :w], xb[:, :, :w]), reads=[xk], writes=["xbf" + tag])
    p1, k1 = psn(k)
    p2, k2 = psn(k)
    for kk in range(8):
        P.op("pe", lambda e, kk=kk: e.matmul(p1[:, :w], k.onesmb[:], xbf[:, kk, :w], start=(kk == 0), stop=(kk == 7)), reads=["xbf" + tag, "onesmb"], writes=[k1])
    for kk in range(8):
        P.op("pe", lambda e, kk=kk: e.matmul(p2[:, :w], k.onesmb[:], sq[:, kk, :w], start=(kk == 0), stop=(kk == 7)), reads=["sq" + tag, "onesmb"], writes=[k2])
    P.op("act", lambda e: e.copy(mean[:, :w], p1[:, :w]), reads=[k1], writes=["mean" + tag])
    P.op("dve", lambda e: e.tensor_tensor(m2[:, :w], mean[:, :w], mean[:, :w], ALU.mult), reads=["mean" + tag], writes=["m2" + tag])
    P.op("dve", lambda e: e.tensor_tensor(m2[:, :w], p2[:, :w], m2[:, :w], ALU.subtract), reads=[k2, "m2" + tag], writes=["m2" + tag])
    P.op("act", lambda e: e.activation(out=m2[:, :w], in_=m2[:, :w], func=AF.Sqrt, bias=k.epsc[:], scale=1.0), reads=["m2" + tag, "epsc"], writes=["m2" + tag])
    P.op("dve", lambda e: e.reciprocal(rstd[:, :w], m2[:, :w]), reads=["m2" + tag], writes=["rstd" + tag])


def ln_tmp(nc, st, W=512):
    return {
        "sq": st.enter_context(nc.sbuf_tensor(U("ln_sq"), [128, 8, W], BF16)),
        "xbf": st.enter_context(nc.sbuf_tensor(U("ln_xbf"), [128, 8, W], BF16)),
        "mean": st.enter_context(nc.sbuf_tensor(U("ln_mean"), [128, W], F32)),
        "rstd": st.enter_context(nc.sbuf_tensor(U("ln_rstd"), [128, W], F32)),
        "m2": st.enter_context(nc.sbuf_tensor(U("ln_m2"), [128, W], F32)),
        "t": st.enter_context(nc.sbuf_tensor(U("ln_t"), [128, W], F32)),
        "tr": Rot(nc, st, U("ln_tr"), 3, [128, W], F32),
    }


def ln_apply(k, xb, xk, w, tmp, kk, out, okeys, scale_ap, bias_ap, extra_reads=(), tag=""):
    P = k.P
    t, tk = tmp["tr"].next()
    P.op("dve", lambda e: e.tensor_tensor(t[:, :w], xb[:, kk, :w], tmp["mean"][:, :w], ALU.subtract), reads=[xk, "mean" + tag], writes=[tk])
    P.op("dve", lambda e: e.tensor_tensor(t[:, :w], t[:, :w], tmp["rstd"][:, :w], ALU.mult), reads=[tk, "rstd" + tag], writes=[tk])
    P.op("act", lambda e: e.activation(out=out, in_=t[:, :w], func=AF.Identity, scale=scale_ap, bias=bias_ap), reads=[tk, "mod"] + list(extra_reads), writes=okeys)


def stage_ln_win(k, li):
    nc, P = k.nc, k.P
    with ExitStack() as st:
        hT = st.enter_context(nc.sbuf_tensor(U("hT"), [128, 8, NT], BF16))
        with ExitStack() as st2:
            tmp = ln_tmp(nc, st2)
            xbr = Rot(nc, st2, "xb", 2, [128, 8, 512], F32)
            xTv = k.xT.rearrange("(k p) t -> p k t", p=128)
            for (t0, w, isc) in TBS:
                xb, xk = xbr.next()
                P.dma("sp", xb[:, :, :w], xTv[:, :, t0:t0 + w], reads=["xT"], writes=[xk])
                ln_stats(k, xb, xk, w, tmp)
                for kk in range(8):
                    ln_apply(k, xb, xk, w, tmp, kk, hT[:, kk, t0:t0 + w], ["hT%d" % kk],
                             k.mod[:, li, 8 + kk, isc:isc + 1], k.mod[:, li, 0 + kk, isc:isc + 1])
            P.barrier()
        wr = Rot(nc, st, "wr", 3, [128, 8, 128], BF16)
        zs = Rot(nc, st, "zs", 3, [128, NT], BF16)
        vt = st.enter_context(nc.sbuf_tensor(U("vt"), [128, 18, 128], BF16))
        wv = k.w_inx[li].rearrange("(k p) n -> p k n", p=128)
        ev = 0
        for m in range(NZT):
            wt, wk = wr.next()
            P.dma("pool", wt[:], wv[:, :, m * 128:(m + 1) * 128], writes=[wk])
            zt, zk = zs.next()
            mrows = 64 if m == ZT_KRX else 128
            for (t0, w, isc) in TBS:
                pt, pk = psn(k)
                for kk in range(8):
                    P.op("pe", lambda e, pt=pt, wt=wt, kk=kk, t0=t0, w=w, mrows=mrows: e.matmul(pt[:mrows, :w], wt[:, kk, :mrows], hT[:, kk, t0:t0 + w], start=(kk == 0), stop=(kk == 7)),
                         reads=[wk, "hT%d" % kk], writes=[pk])
                gate = ZT_G <= m < ZT_G + 24
                if gate:
                    P.op("act", lambda e, pt=pt, zt=zt, t0=t0, w=w: e.activation(out=zt[:, t0:t0 + w], in_=pt[:, :w], func=AF.Sigmoid), reads=[pk], writes=[zk + "_%d" % t0])
                elif ev % 2 == 0:
                    P.op("act", lambda e, pt=pt, zt=zt, t0=t0, w=w, mrows=mrows: e.copy(zt[:mrows, t0:t0 + w], pt[:mrows, :w]), reads=[pk], writes=[zk + "_%d" % t0])
                else:
                    P.op("dve", lambda e, pt=pt, zt=zt, t0=t0, w=w, mrows=mrows: e.tensor_copy(zt[:mrows, t0:t0 + w], pt[:mrows, :w]), reads=[pk], writes=[zk + "_%d" % t0])
                ev += 1
            P.dma("sp", k.zT[m * 128:m * 128 + mrows, :], zt[:mrows, :], reads=[zk + "_%d" % t[0] for t in TBS], writes=["zT"])
            if m == ZT_WV:
                for tt in range(18):
                    pt, pk = psn(k)
                    for kk in range(8):
                        P.op("pe", lambda e, pt=pt, wt=wt, kk=kk, tt=tt: e.matmul(pt[:, :128], hT[:, kk, tt * 128:(tt + 1) * 128], wt[:, kk, :], start=(kk == 0), stop=(kk == 7)),
                             reads=[wk, "hT%d" % kk], writes=[pk])
                    P.op("dve", lambda e, pt=pt, tt=tt: e.tensor_copy(vt[:, tt, :], pt[:, :128]), reads=[pk], writes=["vt"])
                P.dma("sp", k.vtok.rearrange("(t p) c -> p t c", p=128), vt[:], reads=["vt"], writes=["vtok"])
        stage_end(k)


def stage_s5(k, li):
    S5E = "dve" if "s5pool" not in k.dbg else "pool"
    nc, P = k.nc, k.P
    TWO_PI = float(2 * np.pi)
    with ExitStack() as st:
        sb = lambda name, shape, dt=F32: st.enter_context(nc.sbuf_tensor(U(name), shape, dt))
        lane = sb("lane", [128, 3, 32])
        dg = sb("dg", [128, 2, 4])
        BW = sb("BW", [128, 64, 128], BF16)
        CW = sb("CW", [128, 96, 128], BF16)
        tau = sb("tau", [128, NT])
        names = ["dt", "rho", "thn", "fr", "sn", "cs", "ar", "ai", "rden", "qr", "qi", "nqr", "nqi", "tA", "tB"]
        lp = {n: sb("lp_" + n, [128, 32]) for n in names}
        lpi = sb("lp_it", [128, 32], I32)
        P.dma("sp", lane[:], k.s5_lane[li], writes=["lane"])
        P.dma("sp", dg[:], k.s5_dg[li], writes=["dg"])
        P.dma("sp", tau[:], k.tau, writes=["tau"])
        bsrc = k.s5_bT[li].rearrange("d r t p c -> p (d r t) c")
        for j in range(8):
            P.dma("pool", BW[:, j * 8:(j + 1) * 8, :], bsrc[:, j * 8:(j + 1) * 8, :], writes=["BW"])
        P.op("pool", lambda e: e.memset(CW[:], 0.0), writes=["CW"])
        lre, lim, ldt = lane[:, 0, :], lane[:, 1, :], lane[:, 2, :]
        R = ["lane", "lp"]
        W = ["lp"]
        V = lambda fn: P.op("dve", fn, reads=R, writes=W)
        A = lambda fn: P.op("act", fn, reads=R + ["halfpi"], writes=W)
        A(lambda e: e.activation(out=lp["dt"][:], in_=ldt, func=AF.Exp))
        V(lambda e: e.tensor_tensor(lp["tA"][:], lre, lp["dt"][:], ALU.mult))
        A(lambda e: e.activation(out=lp["rho"][:], in_=lp["tA"][:], func=AF.Exp))
        V(lambda e: e.tensor_tensor(lp["thn"][:], lim, lp["dt"][:], ALU.mult))
        V(lambda e: e.tensor_scalar(lp["thn"][:], lp["thn"][:], float(1.0 / TWO_PI), None, ALU.mult))
        V(lambda e: e.tensor_copy(lpi[:], lp["thn"][:]))
        V(lambda e: e.tensor_copy(lp["tB"][:], lpi[:]))
        V(lambda e: e.tensor_tensor(lp["fr"][:], lp["thn"][:], lp["tB"][:], ALU.subtract))
        A(lambda e: e.activation(out=lp["sn"][:], in_=lp["fr"][:], func=AF.Sin, scale=TWO_PI))
        A(lambda e: e.activation(out=lp["fr"][:], in_=lp["fr"][:], func=AF.Abs))
        A(lambda e: e.activation(out=lp["cs"][:], in_=lp["fr"][:], func=AF.Sin, scale=-TWO_PI, bias=k.halfpi[:]))
        V(lambda e: e.tensor_tensor(lp["ar"][:], lp["rho"][:], lp["cs"][:], ALU.mult))
        V(lambda e: e.tensor_tensor(lp["ai"][:], lp["rho"][:], lp["sn"][:], ALU.mult))
        V(lambda e: e.tensor_tensor(lp["tA"][:], lre, lre, ALU.mult))
        V(lambda e: e.tensor_tensor(lp["tB"][:], lim, lim, ALU.mult))
        V(lambda e: e.tensor_tensor(lp["tA"][:], lp["tA"][:], lp["tB"][:], ALU.add))
        V(lambda e: e.reciprocal(lp["rden"][:], lp["tA"][:]))
        V(lambda e: e.tensor_scalar_add(lp["ar"][:], lp["ar"][:], -1.0))
        V(lambda e: e.tensor_tensor(lp["tA"][:], lp["ar"][:], lre, ALU.mult))
        V(lambda e: e.tensor_tensor(lp["tB"][:], lp["ai"][:], lim, ALU.mult))
        V(lambda e: e.tensor_tensor(lp["tA"][:], lp["tA"][:], lp["tB"][:], ALU.add))
        V(lambda e: e.tensor_tensor(lp["qr"][:], lp["tA"][:], lp["rden"][:], ALU.mult))
        V(lambda e: e.tensor_tensor(lp["tA"][:], lp["ai"][:], lre, ALU.mult))
        V(lambda e: e.tensor_tensor(lp["tB"][:], lp["ar"][:], lim, ALU.mult))
        V(lambda e: e.tensor_tensor(lp["tA"][:], lp["tA"][:], lp["tB"][:], ALU.subtract))
        V(lambda e: e.tensor_tensor(lp["qi"][:], lp["tA"][:], lp["rden"][:], ALU.mult))
        V(lambda e: e.tensor_scalar(lp["nqr"][:], lp["qr"][:], -1.0, None, ALU.mult))
        V(lambda e: e.tensor_scalar(lp["nqi"][:], lp["qi"][:], -1.0, None, ALU.mult))
        for n_ in ("rho", "thn", "sn", "cs", "qr", "qi", "dt"):
            dump(k, "lp_" + n_, lp[n_][:], [128, 32], F32, ["lp"])
        stC = ExitStack()
        craw = stC.enter_context(nc.sbuf_tensor(U("craw"), [128, 2, 16, 2, 16], F32))
        ctmp = stC.enter_context(nc.sbuf_tensor(U("ctmp"), [128, 16], F32))
        P.dma("sp", craw[:], k.s5_cL[li], writes=["craw"])
        for d in range(2):
            for lt in range(16):
                col = d * 16 + lt
                cr, ci = craw[:, d, lt, 0, :], craw[:, d, lt, 1, :]
                for half in range(2):
                    g = 2 * lt + half
                    gl = g % 8
                    ps_ = slice(half * 64, half * 64 + 64)
                    for ri in range(3):
                        s1 = lp["qi"] if ri != 1 else lp["nqr"]
                        s2 = (lp["qr"], lp["nqi"], lp["nqr"])[ri]
                        op1 = (ALU.subtract, ALU.add, ALU.add)[ri]
                        P.op("dve", lambda e, ci=ci, s1=s1, col=col, ps_=ps_: e.tensor_scalar(ctmp[ps_, :], ci[ps_, :], s1[ps_, col:col + 1], None, ALU.mult), reads=["craw", "lp"], writes=["ctmp"])
                        P.op("dve", lambda e, cr=cr, s2=s2, col=col, ps_=ps_, ri=ri, gl=gl, op1=op1: e.scalar_tensor_tensor(CW[ps_, col * 3 + ri, gl * 16:(gl + 1) * 16], cr[ps_, :], s2[ps_, col:col + 1], ctmp[ps_, :], ALU.mult, op1), reads=["craw", "lp", "ctmp"], writes=["CW"])
        P.barrier()
        stC.close()
        gT = sb("s5g", [128, 4, NT], BF16)
        with ExitStack() as stU:
            sbu = lambda name, shape, dt=F32: stU.enter_context(nc.sbuf_tensor(U(name), shape, dt))
            utR = Rot(nc, stU, "s5ut", 2, [128, NT], BF16)
            it = sbu("s5it", [128, NT], I32)
            fr = sbu("s5fr", [128, NT])
            SnR = Rot(nc, stU, "s5S", 2, [128, NT], BF16)
            CsR = Rot(nc, stU, "s5C", 2, [128, NT], BF16)
            br = sbu("s5br", [128, NT], BF16)
            bi = sbu("s5bi", [128, NT], BF16)
            p1 = sbu("s5p1", [128, NT], BF16)
            p2 = sbu("s5p2", [128, NT], BF16)
            p3 = sbu("s5p3", [128, NT], BF16)
            wr = sbu("s5wr", [128, NT], BF16)
            wi = sbu("s5wi", [128, NT], BF16)
            zrR = Rot(nc, stU, "s5zr", 2, [128, NT], BF16)
            ziR = Rot(nc, stU, "s5zi", 2, [128, NT], BF16)
            qR = [Rot(nc, stU, "s5q%d" % j, 2, [128, NT], BF16) for j in range(4)]
            ysr = Rot(nc, stU, "s5ys", 1, [128, 512], F32)
            segs = [(0, LC), (LC, NT)]
            units = [(gt, d, l4) for gt in range(4) for d in range(2) for l4 in range(4)]
            uts = {}
            ctx_ = {}

            def alpha(u):
                gt, d, l4 = units[u]
                lt = gt * 4 + l4
                col = d * 16 + lt
                thn = lp["thn"][:, col:col + 1]
                if (d, l4) == (0, 0):
                    ut, utk = utR.next()
                    P.dma("sp", ut[:], k.zT[gt * 128:(gt + 1) * 128, :], reads=["zT"], writes=[utk])
                    uts[gt] = (ut, utk)
                Sn, Snk = SnR.next()
                Cs, Csk = CsR.next()
                ctx_[u] = dict(Sn=Sn, Snk=Snk, Cs=Cs, Csk=Csk, col=col, lt=lt)
                for (a_, b_) in segs:
                    src = tau[:, a_:b_] if d == 0 else (tau[:, b_ - 1::-1] if a_ == 0 else tau[:, b_ - 1:a_ - 1:-1])
                    P.op("dve", lambda e, src=src, a_=a_, b_=b_, thn=thn: e.tensor_scalar(it[:, a_:b_], src, thn, None, ALU.mult), reads=["tau", "lp"], writes=["it"])
                    P.op("dve", lambda e, src=src, a_=a_, b_=b_, thn=thn: e.scalar_tensor_tensor(fr[:, a_:b_], src, thn, it[:, a_:b_], ALU.mult, ALU.subtract), reads=["tau", "lp", "it"], writes=["fr"])
                P.op("act", lambda e, Sn=Sn: e.activation(out=Sn[:], in_=fr[:], func=AF.Sin, scale=TWO_PI), reads=["fr"], writes=[Snk])
                P.op("act", lambda e: e.activation(out=fr[:], in_=fr[:], func=AF.Abs), reads=["fr"], writes=["fr"])
                P.op("act", lambda e, Cs=Cs: e.activation(out=Cs[:], in_=fr[:], func=AF.Sin, scale=-TWO_PI, bias=k.halfpi[:]), reads=["fr", "halfpi"], writes=[Csk])

            def bu(u):
                gt, d, l4 = units[u]
                lt = ctx_[u]["lt"]
                ut, utk = uts[gt]
                for (t0, w, isc) in TBS:
                    pr, kr = psn(k, 5, 8)
                    pi_, ki = psn(k, 5, 8)
                    sl = slice(t0, t0 + w)
                    P.op("pe", lambda e, pr=pr, sl=sl, w=w, d=d, lt=lt, ut=ut: e.matmul(pr[:, :w], BW[:, (d * 2 + 0) * 16 + lt, :], ut[:, sl], start=True, stop=True), reads=["BW", utk], writes=[kr])
                    P.op("pe", lambda e, pi_=pi_, sl=sl, w=w, d=d, lt=lt, ut=ut: e.matmul(pi_[:, :w], BW[:, (d * 2 + 1) * 16 + lt, :], ut[:, sl], start=True, stop=True), reads=["BW", utk], writes=[ki])
                    P.op("act", lambda e, pr=pr, sl=sl, w=w: e.copy(br[:, sl], pr[:, :w]), reads=[kr], writes=["br"])
                    P.op("act", lambda e, pi_=pi_, sl=sl, w=w: e.copy(bi[:, sl], pi_[:, :w]), reads=[ki], writes=["bi"])

            def beta(u):
                c = ctx_[u]
                Sn, Snk, Cs, Csk = c["Sn"], c["Snk"], c["Cs"], c["Csk"]
                P.op("dve", lambda e: e.tensor_tensor(p1[:], Cs[:], br[:], ALU.mult), reads=[Csk, "br"], writes=["p1"])
                P.op("dve", lambda e: e.tensor_tensor(p2[:], Sn[:], bi[:], ALU.mult), reads=[Snk, "bi"], writes=["p2"])
                P.op("dve", lambda e: e.tensor_tensor(p3[:], Cs[:], bi[:], ALU.mult), reads=[Csk, "bi"], writes=["p3"])
                P.op("dve", lambda e: e.tensor_tensor(wr[:], p1[:], p2[:], ALU.add), reads=["p1", "p2"], writes=["wr"])
                P.op("dve", lambda e: e.tensor_tensor(p2[:], Sn[:], br[:], ALU.mult), reads=[Snk, "br", "wr"], writes=["p2"])
                P.op("dve", lambda e: e.tensor_tensor(wi[:], p3[:], p2[:], ALU.subtract), reads=["p3", "p2"], writes=["wi"])

            def gamma_delta(u):
                gt, d, l4 = units[u]
                c = ctx_.pop(u)
                Sn, Snk, Cs, Csk, col = c["Sn"], c["Snk"], c["Cs"], c["Csk"], c["col"]
                first = (d == 0 and l4 == 0)
                last = (d == 1 and l4 == 3)
                zr, zrk = zrR.next()
                zi, zik = ziR.next()
                qs = [r_.next() for r_ in qR]
                rho = lp["rho"][:, col:col + 1]
                for (src, dst, dk, sk_) in ((wr, zr, zrk, "wr"), (wi, zi, zik, "wi")):
                    if d == 0:
                        P.op("dve", lambda e, src=src, dst=dst: e.tensor_tensor_scan(dst[:, 0:LC], rho.to_broadcast([128, LC]), src[:, 0:LC], 0.0, ALU.mult, ALU.add), reads=[sk_, "lp"], writes=[dk])
                        P.op("dve", lambda e, src=src, dst=dst: e.tensor_tensor_scan(dst[:, LC:NT], rho.to_broadcast([128, L]), src[:, LC:NT], dst[:, LC - 1:LC], ALU.mult, ALU.add), reads=[sk_, "lp", dk], writes=[dk])
                    else:
                        P.op("dve", lambda e, src=src, dst=dst: e.tensor_tensor_scan(dst[:, LC - 1::-1], rho.to_broadcast([128, LC]), src[:, LC - 1::-1], 0.0, ALU.mult, ALU.add), reads=[sk_, "lp"], writes=[dk])
                        P.op("dve", lambda e, src=src, dst=dst: e.tensor_tensor_scan(dst[:, NT - 1:LC - 1:-1], rho.to_broadcast([128, L]), src[:, NT - 1:LC - 1:-1], dst[:, 0:1], ALU.mult, ALU.add), reads=[sk_, "lp", dk], writes=[dk])
                (q1, q1k), (q2, q2k), (q3, q3k), (q4, q4k) = qs
                P.op("dve", lambda e: e.tensor_tensor(q1[:], Cs[:], zr[:], ALU.mult), reads=[Csk, zrk], writes=[q1k])
                P.op("dve", lambda e: e.tensor_tensor(q2[:], Sn[:], zi[:], ALU.mult), reads=[Snk, zik], writes=[q2k])
                P.op("dve", lambda e: e.tensor_tensor(q3[:], Sn[:], zr[:], ALU.mult), reads=[Snk, zrk], writes=[q3k])
                P.op("dve", lambda e: e.tensor_tensor(q4[:], Cs[:], zi[:], ALU.mult), reads=[Csk, zik], writes=[q4k])
                for bi_, (t0, w, isc) in enumerate(TBS):
                    sl = slice(t0, t0 + w)
                    yk = "ps%d" % bi_
                    for j_, (qq, qk, wsl) in enumerate(((q1, q1k, 0), (q2, q2k, 2), (q3, q3k, 1), (q4, q4k, 1))):
                        P.op("pe", lambda e, bi_=bi_, sl=sl, w=w, qq=qq, wsl=wsl, j_=j_: e.matmul(k.ps[bi_][:, :w], CW[:, col * 3 + wsl, :], qq[:, sl], start=(first and j_ == 0), stop=(last and j_ == 3)), reads=["CW", qk], writes=[yk])
                if last:
                    ut, utk = uts[gt]
                    for bi_, (t0, w, isc) in enumerate(TBS):
                        sl = slice(t0, t0 + w)
                        ys, ysk = ysr.next()
                        P.op("dve", lambda e, bi_=bi_, sl=sl, w=w, ys=ys: e.scalar_tensor_tensor(ys[:, :w], ut[:, sl], dg[:, 0, gt:gt + 1], k.ps[bi_][:, :w], ALU.mult, ALU.add), reads=[utk, "dg", "ps%d" % bi_], writes=[ysk])
                        P.op("act", lambda e, sl=sl, w=w, ys=ys: e.activation(out=gT[:, gt, sl], in_=ys[:, :w], func=AF.Gelu_apprx_tanh), reads=[ysk], writes=["gT%d" % gt])

            alpha(0)
            bu(0)
            for u in range(len(units)):
                beta(u)
                if u + 1 < len(units):
                    alpha(u + 1)
                    bu(u + 1)
                gamma_delta(u)
            P.barrier()
        wglu = sb("wglu", [128, 4, 512], BF16)
        P.dma("pool", wglu[:], k.s5_wglu[li].rearrange("(k p) n -> p k n", p=128), writes=["wglu"])
        so = Rot(nc, st, "s5o", 2, [128, NT], BF16)
        sgr = Rot(nc, st, "s5sg", 2, [128, 512], F32)
        for mo in range(4):
            ot, ok = so.next()
            for (t0, w, isc) in TBS:
                sl = slice(t0, t0 + w)
                pt, pk = psn(k)
                for kk in range(4):
                    P.op("pe", lambda e, pt=pt, kk=kk, sl=sl, w=w, mo=mo: e.matmul(pt[:, :w], wglu[:, kk, mo * 128:(mo + 1) * 128], gT[:, kk, sl], start=(kk == 0), stop=(kk == 3)), reads=["wglu", "gT%d" % kk], writes=[pk])
                sg, sgk = sgr.next()
                P.op("act", lambda e, pt=pt, w=w, sg=sg, mo=mo: e.activation(out=sg[:, :w], in_=pt[:, :w], func=AF.Sigmoid, bias=dg[:, 1, mo:mo + 1], scale=1.0), reads=[pk, "dg"], writes=[sgk])
                P.op("dve", lambda e, sg=sg, sl=sl, w=w, ot=ot, mo=mo: e.tensor_tensor(ot[:, sl], gT[:, mo, sl], sg[:, :w], ALU.mult), reads=[sgk, "gT%d" % mo], writes=[ok + "_%d" % t0])
            P.dma("sp", k.brT[0][mo * 128:(mo + 1) * 128, :], ot[:], reads=[ok + "_%d" % t[0] for t in TBS], writes=["s5T"])
        stage_end(k)


def rms_norm_T(k, st, src, nk, gains, dst, tag):
    nc, P = k.nc, k.P
    sq = st.enter_context(nc.sbuf_tensor(U("rms_sq"), [128, nk, 512], BF16))
    rinv = st.enter_context(nc.sbuf_tensor(U("rms_ri"), [128, 512], F32))
    for (t0, w, isc) in TBS:
        sl = slice(t0, t0 + w)
        P.op("act", lambda e, sl=sl, w=w: e.activation(out=sq[:, :, :w], in_=src[:, :, sl], func=AF.Square), reads=[tag + "src"], writes=[tag + "sq"])
        pt, pk = psn(k)
        for kk in range(nk):
            P.op("pe", lambda e, pt=pt, kk=kk, w=w: e.matmul(pt[:, :w], k.onesb[:, 0:128], sq[:, kk, :w], start=(kk == 0), stop=(kk == nk - 1)), reads=[tag + "sq", "onesb"], writes=[pk])
        P.op("act", lambda e, pt=pt, w=w: e.activation(out=rinv[:, :w], in_=pt[:, :w], func=AF.Sqrt, scale=float(1.0 / (nk * 128)), bias=k.epsc[:]), reads=[pk, "epsc"], writes=[tag + "ri"])
        P.op("dve", lambda e, w=w: e.reciprocal(rinv[:, :w], rinv[:, :w]), reads=[tag + "ri"], writes=[tag + "ri"])
        for kk in range(nk):
            P.op("dve", lambda e, kk=kk, sl=sl, w=w: e.scalar_tensor_tensor(dst[:, kk, sl], src[:, kk, sl], gains[:, kk:kk + 1], rinv[:, :w], ALU.mult, ALU.mult), reads=[tag + "src", tag + "ri", "mg"], writes=[tag + "dst"])


def softmax_pv(k, ost_rot, score_fn, nkc, va_fn, nq, scale, out_dram, tagp, PTr, esk=None, post=None):
    nc, P = k.nc, k.P
    po, pok = psn(k, 0, 3)
    LA = 3
    scr = {}

    def issue_score(kc):
        pscr, psk = psn(k, 3, 8)
        score_fn(kc, pscr, psk)
        scr[kc] = (pscr, psk)

    for kc in range(min(LA, nkc)):
        issue_score(kc)
    for kc in range(nkc):
        if kc + LA < nkc:
            issue_score(kc + LA)
        pscr, psk = scr.pop(kc)
        pt, ptk = PTr.next()
        P.op("act", lambda e, pscr=pscr, pt=pt: e.activation(out=pt[:, :nq], in_=pscr[:, :nq], func=AF.Exp, scale=scale), reads=[psk], writes=[ptk])
        if post is not None:
            post(kc, pt, ptk)
        va, vak = va_fn(kc)
        P.op("pe", lambda e, po=po, va=va, pt=pt, kc=kc: e.matmul(po[:, :nq], va, pt[:, :nq], start=(kc == 0), stop=(kc == nkc - 1)), reads=[vak, ptk], writes=[pok])
    rv, rvk = k.att_rv.next()
    if esk is not None:
        P.op("dve", lambda e, po=po, rv=rv: e.tensor_scalar(rv[0:64, :nq], po[64:128, :nq], esk, None, ALU.add), reads=[pok, "esk"], writes=[rvk])
        P.op("dve", lambda e, rv=rv: e.reciprocal(rv[0:64, :nq], rv[0:64, :nq]), reads=[rvk], writes=[rvk])
    else:
        P.op("dve", lambda e, po=po, rv=rv: e.reciprocal(rv[0:64, :nq], po[64:128, :nq]), reads=[pok], writes=[rvk])
    ot, otk = ost_rot.next()
    P.op("dve", lambda e, po=po, ot=ot, rv=rv: e.tensor_tensor(ot[0:64, :nq], po[0:64, :nq], rv[0:64, :nq], ALU.mult), reads=[pok, rvk], writes=[otk])
    P.dma("sp", out_dram, ot[0:64, :nq], reads=[otk], writes=[tagp])


def stage_mla(k, li):
    nc, P = k.nc, k.P
    SC = float(96 ** -0.5)
    with ExitStack() as st:
        sb = lambda name, shape, dt=F32: st.enter_context(nc.sbuf_tensor(U(name), shape, dt))
        mg = sb("mg", [128, 5])
        P.dma("sp", mg[:], k.mla_g[li], writes=["mg"])
        VA = sb("VA", [128, 18, 8, 128], BF16)
        KRb = sb("KRb", [128, NT], BF16)
        qn = sb("qn", [128, 3, NT], BF16)
        kvn = sb("kvn", [128, 2, NT], BF16)
        rope = sb("ropem", [128, 2, L])
        wuq = sb("wuq", [128, 3, 2048], BF16)
        wkk = sb("wkk", [128, 2, 1024], BF16)
        k.att_rv = Rot(nc, st, "att_rv", 2, [128, 512], F32)
        P.op("pool", lambda e: e.memset(VA[:], 1.0), writes=["VA"])
        P.dma("sp", rope[:], k.rope_mla.rearrange("c p t -> p c t"), writes=["rope"])
        for j in range(3):
            for c_ in range(4):
                P.dma("pool", wuq[:, j, c_ * 512:(c_ + 1) * 512], k.w_uqx[li][j * 128:(j + 1) * 128, c_ * 512:(c_ + 1) * 512], writes=["wuq"])
        for j in range(2):
            for c_ in range(2):
                P.dma("pool", wkk[:, j, c_ * 512:(c_ + 1) * 512], k.w_ukvk[li][j * 128:(j + 1) * 128, c_ * 512:(c_ + 1) * 512], writes=["wkk"])
        with ExitStack() as st2:
            sb2 = lambda name, shape, dt=F32: st2.enter_context(nc.sbuf_tensor(U(name), shape, dt))
            qa = sb2("qa", [128, 3, NT], BF16)
            kva = sb2("kva", [128, 2, NT], BF16)
            KP = sb2("KP", [128, NT], BF16)
            KS = sb2("KS", [128, L], BF16)
            wvv = sb2("wvv", [128, 2, 512], BF16)
            tA = sb2("mtA", [128, 512])
            tB = sb2("mtB", [128, 512])
            P.dma("sp", qa[:], k.zT[ZT_QA * 128:(ZT_QA + 3) * 128, :].rearrange("(k p) t -> p k t", p=128), reads=["zT"], writes=["qsrc"])
            P.dma("sp", kva[:], k.zT[ZT_KVA * 128:(ZT_KVA + 2) * 128, :].rearrange("(k p) t -> p k t", p=128), reads=["zT"], writes=["ksrc"])
            P.op("pool", lambda e: e.memset(KP[:], 0.0), writes=["KP"])
            P.op("pool", lambda e: e.memset(KS[:], 0.0), writes=["KS"])
            P.dma("sp", KP[64:96, :], k.zT[ZT_KRX * 128:ZT_KRX * 128 + 32, :], reads=["zT"], writes=["KP"])
            P.dma("sp", KS[64:96, :], k.zT[ZT_KRX * 128 + 32:ZT_KRX * 128 + 64, LC:NT], reads=["zT"], writes=["KS"])
            P.dma("pool", wvv[:], k.w_ukvv[li].rearrange("(k p) n -> p k n", p=128), writes=["wvv"])
            rms_norm_T(k, st2, qa, 3, mg[:, 0:3], qn, "q")
            rms_norm_T(k, st2, kva, 2, mg[:, 3:5], kvn, "k")
            P.op("dve", lambda e: e.tensor_copy(KRb[:, 0:LC], KP[:, 0:LC]), reads=["KP"], writes=["KRb"])
            for c in range(4):
                sl = slice(c * 512, (c + 1) * 512)
                sln = slice(LC + c * 512, LC + (c + 1) * 512)
                P.op("dve", lambda e, sl=sl, sln=sln: e.tensor_tensor(tA[:, :], KP[:, sln], rope[:, 0, sl], ALU.mult), reads=["KP", "rope"], writes=["mtA"])
                P.op("dve", lambda e, sl=sl: e.tensor_tensor(tB[:, :], KS[:, sl], rope[:, 1, sl], ALU.mult), reads=["KS", "rope"], writes=["mtB"])
                P.op("dve", lambda e, sln=sln: e.tensor_tensor(KRb[:, sln], tA[:, :], tB[:, :], ALU.add), reads=["mtA", "mtB"], writes=["KRb"])
            dump(k, "d_KP", KP[:], [128, NT], BF16, ["KP"])
            dump(k, "d_KS", KS[:], [128, L], BF16, ["KS"])
            dump(k, "d_KRb", KRb[:], [128, NT], BF16, ["KRb"])
            for tt in range(18):
                pt, pk = psn(k)
                for kk in range(2):
                    P.op("pe", lambda e, pt=pt, kk=kk, tt=tt: e.matmul(pt[:, :512], kvn[:, kk, tt * 128:(tt + 1) * 128], wvv[:, kk, :], start=(kk == 0), stop=(kk == 1)), reads=["wvv", "kdst"], writes=[pk])
                P.op("dve", lambda e, pt=pt, tt=tt: e.tensor_copy(VA[:, tt, :, 0:64], pt[:, :512].rearrange("p (h d) -> p h d", h=8)), reads=[pk], writes=["VA"])
            P.barrier()
        if "mla_stop1" in k.dbg:
            stage_end(k)
            return
        PTr = Rot(nc, st, "mPT", 4, [128, 512], BF16)
        ostr = Rot(nc, st, "most", 3, [128, 512], BF16)
        t1r = Rot(nc, st, "mt1", 2, [128, 512], F32)
        t2r = Rot(nc, st, "mt2", 2, [128, 512], F32)
        for grp in range(2):
            with ExitStack() as st3:
                QP = st3.enter_context(nc.sbuf_tensor(U("QP"), [128, 4, NT], BF16))
                QR = st3.enter_context(nc.sbuf_tensor(U("QR"), [128, 4, L], BF16))
                KH = st3.enter_context(nc.sbuf_tensor(U("KH"), [128, 4, NT], BF16))
                for hh in range(4):
                    h = grp * 4 + hh
                    for (t0, w, isc) in TBS:
                        sl = slice(t0, t0 + w)
                        pm, pmk = psn(k, 3, 8)
                        for kk in range(3):
                            P.op("pe", lambda e, pm=pm, kk=kk, sl=sl, w=w, h=h: e.matmul(pm[:, :w], wuq[:, kk, h * 128:(h + 1) * 128], qn[:, kk, sl], start=(kk == 0), stop=(kk == 2)), reads=["wuq", "qdst"], writes=[pmk])
                        P.op("act", lambda e, pm=pm, sl=sl, w=w, hh=hh: e.copy(QP[:, hh, sl], pm[:, :w]), reads=[pmk], writes=["QP%d" % hh])
                        if not isc:
                            ls = slice(t0 - LC, t0 - LC + w)
                            psw, pswk = psn(k, 3, 8)
                            for kk in range(3):
                                P.op("pe", lambda e, psw=psw, kk=kk, sl=sl, w=w, h=h: e.matmul(psw[:, :w], wuq[:, kk, (8 + h) * 128:(9 + h) * 128], qn[:, kk, sl], start=(kk == 0), stop=(kk == 2)), reads=["wuq", "qdst"], writes=[pswk])
                            t1, t1k = t1r.next()
                            t2, t2k = t2r.next()
                            P.op("dve", lambda e, pm=pm, ls=ls, w=w, t1=t1: e.tensor_tensor(t1[:, :w], pm[:, :w], rope[:, 0, ls], ALU.mult), reads=[pmk, "rope"], writes=[t1k])
                            P.op("dve", lambda e, psw=psw, ls=ls, w=w, t2=t2: e.tensor_tensor(t2[:, :w], psw[:, :w], rope[:, 1, ls], ALU.mult), reads=[pswk, "rope"], writes=[t2k])
                            P.op("dve", lambda e, ls=ls, w=w, hh=hh, t1=t1, t2=t2: e.tensor_tensor(QR[:, hh, ls], t1[:, :w], t2[:, :w], ALU.add), reads=[t1k, t2k], writes=["QR%d" % hh])
                        pk_, pkk = psn(k, 3, 8)
                        for kk in range(2):
                            P.op("pe", lambda e, pk_=pk_, kk=kk, sl=sl, w=w, h=h: e.matmul(pk_[:, :w], wkk[:, kk, h * 128:(h + 1) * 128], kvn[:, kk, sl], start=(kk == 0), stop=(kk == 1)), reads=["wkk", "kdst"], writes=[pkk])
                        P.op("dve", lambda e, pk_=pk_, sl=sl, w=w, hh=hh: e.tensor_tensor(KH[:, hh, sl], pk_[:, :w], KRb[:, sl], ALU.add), reads=[pkk, "KRb"], writes=["KH%d" % hh])
                if grp == 0:
                    dump(k, "d_wkk", wkk[:], [128, 2, 1024], BF16, ["wkk"])
                    dump(k, "d_QP", QP[:, 0, :], [128, NT], BF16, ["QP0"])
                    dump(k, "d_QR", QR[:, 0, :], [128, L], BF16, ["QR0"])
                    dump(k, "d_KH", KH[:, 0, :], [128, NT], BF16, ["KH0"])
                    dump(k, "d_VA", VA[:, :, 0, :], [128, 18, 128], BF16, ["VA"])
                if "mla_stop2" in k.dbg:
                    P.barrier()
                    continue
                for hh in range(4):
                    h = grp * 4 + hh
                    for qb in range(5):
                        if qb < 4:
                            q0, nq, nkc = LC + qb * 512, 512, 18
                        else:
                            q0, nq, nkc = 0, LC, 2

                        def score_fn(kc, pscr, psk, q0=q0, nq=nq, hh=hh):
                            ks = slice(kc * 128, (kc + 1) * 128)
                            if kc >= 2:
                                P.op("pe", lambda e: e.matmul(pscr[:, :nq], KH[:, hh, ks], QR[:, hh, q0 - LC:q0 - LC + nq], start=True, stop=True), reads=["KH%d" % hh, "QR%d" % hh], writes=[psk])
                            else:
                                P.op("pe", lambda e: e.matmul(pscr[:, :nq], KH[:, hh, ks], QP[:, hh, q0:q0 + nq], start=True, stop=True), reads=["KH%d" % hh, "QP%d" % hh], writes=[psk])

                        softmax_pv(k, ostr, score_fn, nkc, lambda kc, h=h: (VA[:, kc, h, :], "VA"), nq, SC,
                                   k.brT[1][h * 64:(h + 1) * 64, q0:q0 + nq], "mlaT", PTr)
                P.barrier()
        stage_end(k)


def stage_win(k, li):
    nc, P = k.nc, k.P
    SC = float(64 ** -0.5)
    with ExitStack() as st:
        sb = lambda name, shape, dt=F32: st.enter_context(nc.sbuf_tensor(U(name), shape, dt))
        Qp = sb("wQp", [128, 8, NT], BF16)
        Qr = sb("wQr", [128, 8, L], BF16)
        Kp = sb("wKp", [128, NT], BF16)
        Kr = sb("wKr", [128, L], BF16)
        VW = sb("wVW", [128, 18, 2, 128], BF16)
        rope = sb("wrope", [128, 2, L])
        msk = sb("wmsk", [128, 2, 128], BF16)
        esk = sb("wesk", [128, 8])
        Qsr = Rot(nc, st, "wQs", 2, [128, L], BF16)
        tAr = Rot(nc, st, "wtA", 2, [128, 512], F32)
        tBr = Rot(nc, st, "wtB", 2, [128, 512], F32)
        k.att_rv = Rot(nc, st, "watt_rv", 2, [128, 512], F32)
        P.op("pool", lambda e: e.memset(VW[:], 1.0), writes=["VW"])
        P.dma("sp", esk[:], k.win_sink[li], writes=["esk"])
        P.op("act", lambda e: e.activation(out=esk[:], in_=esk[:], func=AF.Exp), reads=["esk"], writes=["esk"])
        P.dma("sp", Qp[:], k.zT[ZT_WQ * 128:(ZT_WQ + 8) * 128, :].rearrange("(k p) t -> p k t", p=128), reads=["zT"], writes=["Qp"])
        P.dma("sp", Kp[:], k.zT[ZT_WK * 128:(ZT_WK + 1) * 128, :], reads=["zT"], writes=["Kp"])
        P.dma("sp", rope[:], k.rope_win.rearrange("c p t -> p c t"), writes=["rope"])
        P.dma("pool", msk[:], k.wmask.rearrange("c p t -> p c t"), writes=["msk"])
        for c_ in range(2):
            P.dma("sp", VW[:, :, c_, 0:64], k.vtok[:, c_ * 64:(c_ + 1) * 64].rearrange("(t p) d -> p t d", p=128), reads=["vtok", "VW"], writes=["VW"])
        for j in range(9):
            qs_, qsk = Qsr.next()
            srow = (ZT_WQS + j) * 128 if j < 8 else ZT_WKS * 128
            P.dma("sp", qs_[:], k.zT[srow:srow + 128, LC:NT], reads=["zT"], writes=[qsk])
            for c in range(4):
                sl = slice(c * 512, (c + 1) * 512)
                sln = slice(LC + c * 512, LC + (c + 1) * 512)
                src = Qp[:, j, sln] if j < 8 else Kp[:, sln]
                dst = Qr[:, j, sl] if j < 8 else Kr[:, sl]
                tA, tAk = tAr.next()
                tB, tBk = tBr.next()
                P.op("dve", lambda e, sl=sl, src=src, tA=tA: e.tensor_tensor(tA[:], src, rope[:, 0, sl], ALU.mult), reads=["Qp", "Kp", "rope"], writes=[tAk])
                P.op("dve", lambda e, sl=sl, qs_=qs_, tB=tB: e.tensor_tensor(tB[:], qs_[:, sl], rope[:, 1, sl], ALU.mult), reads=[qsk, "rope"], writes=[tBk])
                P.op("dve", lambda e, dst=dst, tA=tA, tB=tB: e.tensor_tensor(dst, tA[:], tB[:], ALU.add), reads=[tAk, tBk], writes=["Qr", "Kr"])
        PTr = Rot(nc, st, "wPT", 4, [128, 512], BF16)
        ostr = Rot(nc, st, "wost", 3, [128, 512], BF16)
        for h in range(8):
            kk = h // 4
            for G in range(4):
                n0 = 4 * G
                q0 = LC + n0 * 128
                chunks = [("c", 0, 0, 512), ("c", 1, 0, 512)]
                for j in range(n0 - 1, n0 + 5):
                    if 0 <= j < 16:
                        a_ = max(j - 1, n0)
                        b_ = min(j + 1, n0 + 3)
                        chunks.append(("b", j, (a_ - n0) * 128, (b_ - a_ + 1) * 128))
                po, pok = psn(k, 0, 3)
                scr = {}

                def issue(ci, chunks=chunks, q0=q0, h=h):
                    typ, idx, c0, wq = chunks[ci]
                    pscr, psk = psn(k, 3, 8)
                    if typ == "c":
                        P.op("pe", lambda e: e.matmul(pscr[:, :wq], Kp[:, idx * 128:(idx + 1) * 128], Qp[:, h, q0:q0 + wq], start=True, stop=True), reads=["Kp", "Qp"], writes=[psk])
                    else:
                        P.op("pe", lambda e: e.matmul(pscr[:, :wq], Kr[:, idx * 128:(idx + 1) * 128], Qr[:, h, q0 - LC + c0:q0 - LC + c0 + wq], start=True, stop=True), reads=["Kr", "Qr"], writes=[psk])
                    scr[ci] = (pscr, psk)

                LA = 3
                for ci in range(min(LA, len(chunks))):
                    issue(ci)
                for ci, (typ, idx, c0, wq) in enumerate(chunks):
                    if ci + LA < len(chunks):
                        issue(ci + LA)
                    pscr, psk = scr.pop(ci)
                    pt, ptk = PTr.next()
                    P.op("act", lambda e, pscr=pscr, pt=pt, wq=wq: e.activation(out=pt[:, :wq], in_=pscr[:, :wq], func=AF.Exp, scale=SC), reads=[psk], writes=[ptk])
                    if typ == "b":
                        for sub in range(wq // 128):
                            d = idx - (n0 + c0 // 128 + sub)
                            if d != 0:
                                mi = 0 if d == -1 else 1
                                P.op("dve", lambda e, pt=pt, sub=sub, mi=mi: e.tensor_tensor(pt[:, sub * 128:(sub + 1) * 128], pt[:, sub * 128:(sub + 1) * 128], msk[:, mi, :], ALU.mult), reads=[ptk, "msk"], writes=[ptk])
                    tt = (2 + idx) if typ == "b" else idx
                    P.op("pe", lambda e, po=po, pt=pt, tt=tt, c0=c0, wq=wq, ci=ci, nch=len(chunks), kk=kk: e.matmul(po[:, c0:c0 + wq], VW[:, tt, kk, :], pt[:, :wq], start=(ci == 0), stop=(ci == nch - 1)), reads=["VW", ptk], writes=[pok])
                rv, rvk = k.att_rv.next()
                P.op("dve", lambda e, po=po, rv=rv, h=h: e.tensor_scalar(rv[0:64, :], po[64:128, :], esk[64:128, h:h + 1], None, ALU.add), reads=[pok, "esk"], writes=[rvk])
                P.op("dve", lambda e, rv=rv: e.reciprocal(rv[0:64, :], rv[0:64, :]), reads=[rvk], writes=[rvk])
                ot, otk = ostr.next()
                P.op("dve", lambda e, po=po, ot=ot, rv=rv: e.tensor_tensor(ot[0:64, :], po[0:64, :], rv[0:64, :], ALU.mult), reads=[pok, rvk], writes=[otk])
                P.dma("sp", k.brT[2][h * 64:(h + 1) * 64, q0:q0 + 512], ot[0:64, :], reads=[otk], writes=["winT"])
            chunks = [("c", 0, 0), ("c", 1, 0)]

            def score_fn(kc, pscr, psk, h=h):
                P.op("pe", lambda e: e.matmul(pscr[:, :LC], Kp[:, kc * 128:(kc + 1) * 128], Qp[:, h, 0:LC], start=True, stop=True), reads=["Kp", "Qp"], writes=[psk])

            softmax_pv(k, ostr, score_fn, 2, lambda kc, kk=kk: (VW[:, kc, kk, :], "VW"), LC, SC, k.brT[2][h * 64:(h + 1) * 64, 0:LC], "winT", PTr,
                       esk=esk[64:128, h:h + 1])
        stage_end(k)


def stage_merge(k, li):
    nc, P = k.nc, k.P
    with ExitStack() as st:
        sb = lambda name, shape, dt=F32: st.enter_context(nc.sbuf_tensor(U(name), shape, dt))
        wbr = sb("wbr", [128, 12, D], BF16)
        wout = sb("wout", [128, 8, D], BF16)
        lnp = sb("lnp", [128, 4, 8])
        k.lnp_t = lnp
        P.dma("sp", lnp[:], k.lnp[li], writes=["mod"])
        for j in range(12):
            P.dma("pool", wbr[:, j, :], k.w_branch[li][j * 128:(j + 1) * 128, :], writes=["wbr"])
        for j in range(8):
            P.dma("pool", wout[:, j, :], k.w_out[li][j * 128:(j + 1) * 128, :], writes=["wout"])
        tmp = ln_tmp(nc, st)
        obr = [sb("ob%d" % b, [128, 4, 512], BF16) for b in range(3)]
        gbr = [sb("gb%d" % b, [128, 8, 512], BF16) for b in range(3)]
        mT = sb("mT", [128, 8, 512], BF16)
        m1r = Rot(nc, st, "mgt1", 2, [128, 512], F32)
        m2r = Rot(nc, st, "mgt2", 3, [128, 512], F32)
        ytr = Rot(nc, st, "mgty", 2, [128, 512], F32)
        xb = sb("mxb", [128, 8, 512])
        rb = sb("mrb", [128, 8, 512])
        x1 = sb("mx1", [128, 8, 512])
        h2 = sb("mh2", [128, 8, 512], BF16)
        xTv = k.xT.rearrange("(k p) t -> p k t", p=128)
        h2v = k.h2T.rearrange("(k p) t -> p k t", p=128)
        for (t0, w, isc) in TBS:
            sl = slice(t0, t0 + w)
            for b in range(3):
                P.dma("sp", obr[b][:, :, :w], k.brT[b][:, sl].rearrange("(k p) t -> p k t", p=128), reads=["brT"], writes=["ob%d" % b])
                P.dma("sp", gbr[b][:, :, :w], k.zT[(ZT_G + 8 * b) * 128:(ZT_G + 8 + 8 * b) * 128, sl].rearrange("(k p) t -> p k t", p=128), reads=["zT"], writes=["gb%d" % b])
            P.dma("sp", xb[:, :, :w], xTv[:, :, sl], reads=["xT"], writes=["mxb"])
            for mo in range(8):
                mt1, m1k = m1r.next()
                for b in range(3):
                    pt, pk = psn(k)
                    for kk in range(4):
                        P.op("pe", lambda e, pt=pt, kk=kk, b=b, mo=mo, w=w: e.matmul(pt[:, :w], wbr[:, b * 4 + kk, mo * 128:(mo + 1) * 128], obr[b][:, kk, :w], start=(kk == 0), stop=(kk == 3)), reads=["wbr", "ob%d" % b], writes=[pk])
                    if b == 0:
                        P.op("dve", lambda e, pt=pt, mo=mo, w=w, mt1=mt1: e.tensor_tensor(mt1[:, :w], pt[:, :w], gbr[0][:, mo, :w], ALU.mult), reads=[pk, "gb0"], writes=[m1k])
                    elif b == 1:
                        mt2, m2k = m2r.next()
                        P.op("dve", lambda e, pt=pt, mo=mo, w=w, mt2=mt2: e.tensor_tensor(mt2[:, :w], pt[:, :w], gbr[1][:, mo, :w], ALU.mult), reads=[pk, "gb1"], writes=[m2k])
                        P.op("dve", lambda e, w=w, mt1=mt1, mt2=mt2: e.tensor_tensor(mt1[:, :w], mt1[:, :w], mt2[:, :w], ALU.add), reads=[m1k, m2k], writes=[m1k])
                    else:
                        mt2, m2k = m2r.next()
                        P.op("dve", lambda e, pt=pt, mo=mo, w=w, mt2=mt2: e.tensor_tensor(mt2[:, :w], pt[:, :w], gbr[2][:, mo, :w], ALU.mult), reads=[pk, "gb2"], writes=[m2k])
                        P.op("dve", lambda e, mo=mo, w=w, mt1=mt1, mt2=mt2: e.tensor_tensor(mT[:, mo, :w], mt1[:, :w], mt2[:, :w], ALU.add), reads=[m1k, m2k], writes=["mT"])
            for mo in range(8):
                pt, pk = psn(k)
                for kk in range(8):
                    P.op("pe", lambda e, pt=pt, kk=kk, mo=mo, w=w: e.matmul(pt[:, :w], wout[:, kk, mo * 128:(mo + 1) * 128], mT[:, kk, :w], start=(kk == 0), stop=(kk == 7)), reads=["wout", "mT"], writes=[pk])
                yt, ytk = ytr.next()
                P.op("act", lambda e, pt=pt, mo=mo, w=w, isc=isc, yt=yt: e.activation(out=yt[:, :w], in_=pt[:, :w], func=AF.Identity, scale=k.mod[:, li, 16 + mo, isc:isc + 1]), reads=[pk, "mod"], writes=[ytk])
                P.op("dve", lambda e, mo=mo, w=w, yt=yt: e.scalar_tensor_tensor(rb[:, mo, :w], xb[:, mo, :w], float(ALPHA), yt[:, :w], ALU.mult, ALU.add), reads=["mxb", ytk], writes=["mrb"])
            ln_stats(k, rb, "mrb", w, tmp)
            for kk in range(8):
                ln_apply(k, rb, "mrb", w, tmp, kk, x1[:, kk, :w], ["mx1"], lnp[:, 0, kk:kk + 1], lnp[:, 1, kk:kk + 1])
            P.dma("sp", xTv[:, :, sl], x1[:, :, :w], reads=["mx1"], writes=["xT"])
            ln_stats(k, x1, "mx1", w, tmp)
            for kk in range(8):
                ln_apply(k, x1, "mx1", w, tmp, kk, h2[:, kk, :w], ["mh2"], k.mod[:, li, 32 + kk, isc:isc + 1], k.mod[:, li, 24 + kk, isc:isc + 1])
            P.dma("sp", h2v[:, :, sl], h2[:, :, :w], reads=["mh2"], writes=["h2T"])
        stage_end(k)


TB9 = [(0, 256, 1)] + [(256 + i * 256, 256, 0) for i in range(8)]


def stage_moe(k, li):
    nc, P = k.nc, k.P
    with ExitStack() as st0:
        sb0 = lambda name, shape, dt=F32: st0.enter_context(nc.sbuf_tensor(U(name), shape, dt))
        lnp = sb0("elnp", [128, 4, 8])
        posm = sb0("eposm", [16, NT])
        sel = sb0("esel", [16, 16, 128])
        iop = sb0("eiop", [128, 3])
        P.dma("sp", lnp[:], k.lnp[li], writes=["mod"])
        P.dma("sp", sel[:], k.sel16, writes=["esel"])
        P.dma("sp", iop[:], k.iota_p3, writes=["eiop"])
        with ExitStack() as st1:
            sb1 = lambda name, shape, dt=F32: st1.enter_context(nc.sbuf_tensor(U(name), shape, dt))
            h2tok = sb1("eh2tok", [128, 18, D], BF16)
            posm_tok = sb1("eposmt", [128, 18, 16])
            gw_tok = sb1("egwt", [128, 18, 16], BF16)
            ios = sb1("eios", [128, 384])
            P.dma("sp", ios[:], k.iota_s, writes=["eios"])
            with ExitStack() as stA:
                sbA = lambda name, shape, dt=F32: stA.enter_context(nc.sbuf_tensor(U(name), shape, dt))
                h2 = sbA("eh2", [128, 8, NT], BF16)
                wr = sbA("ewr", [128, 8, 16], BF16)
                aff = sbA("eaff", [16, NT])
                wk_ = sbA("ewk", [16, NT])
                gw = sbA("egw", [16, NT])
                msk = sbA("emsk", [16, NT])
                m8 = sbA("em8", [16, 8])
                thr = sbA("ethr", [16, 2])
                P.dma("sp", h2[:], k.h2T.rearrange("(k p) t -> p k t", p=128), reads=["h2T"], writes=["eh2"])
                P.dma("pool", wr[:], k.w_router[li].rearrange("(k p) n -> p k n", p=128), writes=["ewr"])
                for (t0, w, isc) in TBS:
                    sl = slice(t0, t0 + w)
                    pt, pk = psn(k)
                    for kk in range(8):
                        P.op("pe", lambda e, pt=pt, kk=kk, sl=sl, w=w: e.matmul(pt[0:16, :w], wr[:, kk, :], h2[:, kk, sl], start=(kk == 0), stop=(kk == 7)), reads=["ewr", "eh2"], writes=[pk])
                    P.op("act", lambda e, pt=pt, sl=sl, w=w: e.activation(out=wk_[:, sl], in_=pt[0:16, :w], func=AF.Exp), reads=[pk], writes=["ewk"])
                    p2, k2 = psn(k)
                    P.op("pe", lambda e, p2=p2, sl=sl, w=w: e.matmul(p2[0:16, :w], k.onesf[0:16, 0:16], wk_[:, sl], start=True, stop=True), reads=["ewk", "onesf"], writes=[k2])
                    P.op("dve", lambda e, p2=p2, sl=sl, w=w: e.reciprocal(gw[:, sl], p2[0:16, :w]), reads=[k2], writes=["egw"])
                    P.op("dve", lambda e, sl=sl: e.tensor_tensor(aff[:, sl], wk_[:, sl], gw[:, sl], ALU.mult), reads=["ewk", "egw"], writes=["eaff"])
                P.op("dve", lambda e: e.tensor_copy(wk_[:], aff[:]), reads=["eaff"], writes=["ewk"])
                for si, (a_, b_, cap, off) in enumerate(((0, LC, 32, 256.0), (LC, NT, 256, 0.0))):
                    for r in range(cap // 8):
                        P.op("dve", lambda e, a_=a_, b_=b_: e.max(out=m8[:], in_=wk_[:, a_:b_]), reads=["ewk"], writes=["em8"])
                        if r < cap // 8 - 1:
                            P.op("dve", lambda e, a_=a_, b_=b_: e.match_replace(out=wk_[:, a_:b_], in_to_replace=m8[:], in_values=wk_[:, a_:b_], imm_value=-1.0), reads=["ewk", "em8"], writes=["ewk"])
                    P.op("dve", lambda e, si=si: e.tensor_copy(thr[:, si:si + 1], m8[:, 7:8]), reads=["em8"], writes=["ethr"])
                    P.op("dve", lambda e, a_=a_, b_=b_, si=si: e.tensor_scalar(msk[:, a_:b_], aff[:, a_:b_], thr[:, si:si + 1], None, ALU.is_ge), reads=["eaff", "ethr"], writes=["emsk"])
                    P.op("dve", lambda e, a_=a_, b_=b_: e.tensor_tensor(gw[:, a_:b_], aff[:, a_:b_], msk[:, a_:b_], ALU.mult), reads=["eaff", "emsk"], writes=["egw"])
                    P.op("dve", lambda e, a_=a_, b_=b_: e.tensor_tensor_scan(wk_[:, a_:b_], k.onesf[0:16, 0:1].to_broadcast([16, b_ - a_]), msk[:, a_:b_], 0.0, ALU.mult, ALU.add), reads=["emsk", "onesf", "ewk"], writes=["ewk"])
                    P.op("dve", lambda e, a_=a_, b_=b_, off=off: e.scalar_tensor_tensor(posm[:, a_:b_], wk_[:, a_:b_], float(off), msk[:, a_:b_], ALU.add, ALU.mult), reads=["ewk", "emsk"], writes=["eposm"])
                    P.op("dve", lambda e, a_=a_, b_=b_: e.tensor_scalar_add(posm[:, a_:b_], posm[:, a_:b_], -1.0), reads=["eposm"], writes=["eposm"])
                for (src, dst, skey, dkey) in ((posm, posm_tok, "eposm", "eposmt"), (gw, gw_tok, "egw", "egwt")):
                    pt, pk = psn(k)
                    for tt in range(18):
                        P.op("pe", lambda e, pt=pt, tt=tt, src=src: e.transpose(pt[:, tt * 16:(tt + 1) * 16], src[0:16, tt * 128:(tt + 1) * 128], k.identf[0:16, 0:16]), reads=[skey, "identf"], writes=[pk])
                    P.op("dve", lambda e, pt=pt, dst=dst: e.tensor_copy(dst[:], pt[:, 0:288].rearrange("p (t e) -> p t e", e=16)), reads=[pk], writes=[dkey])
                for tt in range(18):
                    pt, pk = psn(k)
                    ptb = pt[:].bitcast(BF16)
                    for kf in range(8):
                        P.op("pe", lambda e, ptb=ptb, kf=kf, tt=tt: e.transpose(ptb[:, kf * 128:(kf + 1) * 128], h2[:, kf, tt * 128:(tt + 1) * 128], k.identb[:]), reads=["eh2", "identb"], writes=[pk])
                    if tt % 2 == 0:
                        P.op("act", lambda e, ptb=ptb, tt=tt: e.copy(h2tok[:, tt, :], ptb[:, 0:1024]), reads=[pk], writes=["eh2tok"])
                    else:
                        P.op("dve", lambda e, ptb=ptb, tt=tt: e.tensor_copy(h2tok[:, tt, :], ptb[:, 0:1024]), reads=[pk], writes=["eh2tok"])
                P.barrier()
            mw = Rot(nc, st1, "emw", 32, [128, D], BF16)
            Sr = Rot(nc, st1, "eS", 2, [128, 18, 384], BF16)
            Xr = Rot(nc, st1, "eX", 2, [128, 8, 288], BF16)
            Ar = Rot(nc, st1, "eA", 2, [128, 8, 384], BF16)
            Yr = Rot(nc, st1, "eY", 2, [128, 3, D], BF16)
            sar = Rot(nc, st1, "esa", 2, [128, 288], F32)
            gsr = Rot(nc, st1, "egs", 2, [128, 3], F32)
            for j in range(2):
                At, Ak = Ar.next()
                P.op("pool", lambda e, At=At: e.memset(At[:], 0.0), writes=[Ak])
            ev = 0
            for ex in range(16):
                ws = {}
                for nm, src in (("g", k.w_gate), ("u", k.w_up), ("d", k.w_down)):
                    for kk in range(8):
                        t, tk = mw.next()
                        P.dma("pool", t[:], src[li, ex, kk * 128:(kk + 1) * 128, :], writes=[tk])
                        ws[(nm, kk)] = (t, tk)
                S, Sk = Sr.next()
                P.op("dve", lambda e, S=S, ex=ex: e.tensor_tensor(S[:], ios[:].unsqueeze(1).to_broadcast([128, 18, 384]), posm_tok[:, :, ex:ex + 1].to_broadcast([128, 18, 384]), ALU.is_equal), reads=["eios", "eposmt"], writes=[Sk])
                X, Xk = Xr.next()
                for ft in range(8):
                    pt, pk = psn(k, 0, 4)
                    for tt in range(2, 18):
                        P.op("pe", lambda e, pt=pt, tt=tt, ft=ft, S=S: e.matmul(pt[:, 0:256], h2tok[:, tt, ft * 128:(ft + 1) * 128], S[:, tt, 0:256], start=(tt == 2), stop=(tt == 17)), reads=["eh2tok", Sk], writes=[pk])
                    for tt in range(2):
                        P.op("pe", lambda e, pt=pt, tt=tt, ft=ft, S=S: e.matmul(pt[:, 256:288], h2tok[:, tt, ft * 128:(ft + 1) * 128], S[:, tt, 256:288], start=(tt == 0), stop=(tt == 1)), reads=["eh2tok", Sk], writes=[pk])
                    if ev % 2 == 0:
                        P.op("act", lambda e, pt=pt, ft=ft, X=X: e.copy(X[:, ft, :], pt[:, :288]), reads=[pk], writes=[Xk])
                    else:
                        P.op("dve", lambda e, pt=pt, ft=ft, X=X: e.tensor_copy(X[:, ft, :], pt[:, :288]), reads=[pk], writes=[Xk])
                    ev += 1
                pg, pgk = psn(k, 0, 4)
                for st_ in range(3):
                    tts = list(range(2, 18)) if st_ < 2 else [0, 1]
                    for tt in tts:
                        P.op("pe", lambda e, pg=pg, tt=tt, st_=st_, S=S, ex=ex, tts=tts: e.matmul(pg[:, st_:st_ + 1], S[:, tt, st_ * 128:(st_ + 1) * 128], gw_tok[:, tt, ex:ex + 1], start=(tt == tts[0]), stop=(tt == tts[-1])), reads=[Sk, "egwt"], writes=[pgk])
                gs, gsk = gsr.next()
                P.op("dve", lambda e, pg=pg, gs=gs: e.tensor_copy(gs[:], pg[:, 0:3]), reads=[pgk], writes=[gsk])
                At, Ak = Ar.next()
                for fo in range(8):
                    pa, pak = psn(k, 4, 6)
                    pu, puk = psn(k, 6, 8)
                    for kk in range(8):
                        wt, wtk = ws[("g", kk)]
                        P.op("pe", lambda e, pa=pa, wt=wt, kk=kk, fo=fo, X=X: e.matmul(pa[:, :288], wt[:, fo * 128:(fo + 1) * 128], X[:, kk, :], start=(kk == 0), stop=(kk == 7)), reads=[wtk, Xk], writes=[pak])
                    for kk in range(8):
                        wt, wtk = ws[("u", kk)]
                        P.op("pe", lambda e, pu=pu, wt=wt, kk=kk, fo=fo, X=X: e.matmul(pu[:, :288], wt[:, fo * 128:(fo + 1) * 128], X[:, kk, :], start=(kk == 0), stop=(kk == 7)), reads=[wtk, Xk], writes=[puk])
                    s_, sk_ = sar.next()
                    P.op("act", lambda e, pa=pa, s_=s_: e.activation(out=s_[:], in_=pa[:, :288], func=AF.Silu), reads=[pak], writes=[sk_])
                    P.op("dve", lambda e, pu=pu, s_=s_, At=At, fo=fo: e.tensor_tensor(At[:, fo, 0:288], pu[:, :288], s_[:], ALU.mult), reads=[puk, sk_], writes=[Ak])
                Y, Yk = Yr.next()
                for st_ in range(3):
                    for half in range(2):
                        py, pyk = psn(k, 0, 4)
                        for kk in range(8):
                            wt, wtk = ws[("d", kk)]
                            P.op("pe", lambda e, py=py, wt=wt, kk=kk, st_=st_, half=half, At=At: e.matmul(py[:, :512], At[:, kk, st_ * 128:(st_ + 1) * 128], wt[:, half * 512:(half + 1) * 512], start=(kk == 0), stop=(kk == 7)), reads=[wtk, Ak], writes=[pyk])
                        P.op("act", lambda e, py=py, st_=st_, half=half, Y=Y, gs=gs: e.activation(out=Y[:, st_, half * 512:(half + 1) * 512], in_=py[:, :512], func=AF.Identity, scale=gs[:, st_:st_ + 1]), reads=[pyk, gsk], writes=[Yk])
                P.dma("sp", k.yg[ex].rearrange("s p d -> p s d"), Y[:], reads=[Yk], writes=["yg"])
            P.barrier()
        with ExitStack() as st2:
            sb2 = lambda name, shape, dt=F32: st2.enter_context(nc.sbuf_tensor(U(name), shape, dt))
            Yall = sb2("eYall", [128, 48, D], BF16)
            for ex in range(16):
                P.dma("sp", Yall[:, ex * 3:(ex + 1) * 3, :], k.yg[ex].rearrange("s p d -> p s d"), reads=["yg"], writes=["eYall"])
            STr = Rot(nc, st2, "eST", 2, [128, 32, 256], BF16)
            tmp = ln_tmp(nc, st2, 256)
            xbr = Rot(nc, st2, "exb", 2, [128, 8, 256], F32)
            rbR = Rot(nc, st2, "erb", 2, [128, 8, 256], F32)
            xTv = k.xT.rearrange("(k p) t -> p k t", p=128)
            for (t0, w, isc) in TB9:
                sl = slice(t0, t0 + w)
                xb, xbk = xbr.next()
                rb, rbk = rbR.next()
                P.dma("sp", xb[:], xTv[:, :, sl], reads=["xT"], writes=[xbk])
                ST, STk = STr.next()
                sts = [2] if isc else [0, 1]
                n_ = len(sts)
                for ex in range(16):
                    pb, pbk = psn(k, 4, 8)
                    P.op("pe", lambda e, pb=pb, sl=sl, ex=ex: e.matmul(pb[:, :256], sel[:, ex, :], posm[:, sl], start=True, stop=True), reads=["esel", "eposm"], writes=[pbk])
                    P.op("dve", lambda e, pb=pb, ex=ex, ST=ST, sts=sts, n_=n_: e.tensor_tensor(ST[:, ex * n_:(ex + 1) * n_, :], pb[:, 0:256].unsqueeze(1).to_broadcast([128, n_, 256]), iop[:, sts[0]:sts[0] + n_].unsqueeze(2).to_broadcast([128, n_, 256]), ALU.is_equal), reads=[pbk, "eiop"], writes=[STk])
                js = [(ex * 3 + st_, ex * n_ + i_) for ex in range(16) for i_, st_ in enumerate(sts)]
                for mo in range(8):
                    pf, pfk = psn(k, 0, 4)
                    for (jy, jt) in js:
                        P.op("pe", lambda e, pf=pf, jy=jy, jt=jt, mo=mo, ST=ST, js=js: e.matmul(pf[:, :256], Yall[:, jy, mo * 128:(mo + 1) * 128], ST[:, jt, :], start=(jy == js[0][0]), stop=(jy == js[-1][0])), reads=["eYall", STk], writes=[pfk])
                    ft_, ftk = tmp["tr"].next()
                    P.op("act", lambda e, pf=pf, mo=mo, isc=isc, ft_=ft_: e.activation(out=ft_[:, :256], in_=pf[:, :256], func=AF.Identity, scale=k.mod[:, li, 40 + mo, isc:isc + 1]), reads=[pfk, "mod"], writes=[ftk])
                    P.op("dve", lambda e, mo=mo, xb=xb, ft_=ft_, rb=rb: e.scalar_tensor_tensor(rb[:, mo, :], xb[:, mo, :], float(ALPHA), ft_[:, :256], ALU.mult, ALU.add), reads=[xbk, ftk], writes=[rbk])
                ln_stats(k, rb, rbk, w, tmp)
                for kk in range(8):
                    ln_apply(k, rb, rbk, w, tmp, kk, xb[:, kk, :], [xbk], lnp[:, 2, kk:kk + 1], lnp[:, 3, kk:kk + 1])
                P.dma("sp", xTv[:, :, sl], xb[:], reads=[xbk], writes=["xT"])
        stage_end(k)


def stage_out(k):
    nc, P = k.nc, k.P
    with ExitStack() as st:
        xr = Rot(nc, st, "oxr", 2, [128, 8, 128], F32)
        xo = Rot(nc, st, "oxo", 2, [128, D], F32)
        xTv = k.xT.rearrange("(k p) t -> p k t", p=128)
        for tt in range(L // 128):
            xt, xk = xr.next()
            P.dma("sp", xt[:], xTv[:, :, LC + tt * 128:LC + (tt + 1) * 128], reads=["xT"], writes=[xk])
            ot, ok = xo.next()
            for half in range(2):
                pt, pk = psn(k)
                for kk in range(4):
                    kf = half * 4 + kk
                    P.op("pe", lambda e, pt=pt, kk=kk, kf=kf, xt=xt: e.transpose(pt[:, kk * 128:(kk + 1) * 128], xt[:, kf, :], k.identf[:]),
                         reads=[xk, "identf"], writes=[pk])
                if half == 0:
                    P.op("act", lambda e, pt=pt, ot=ot: e.copy(ot[:, 0:512], pt[:]), reads=[pk], writes=[ok + "h0"])
                else:
                    P.op("dve", lambda e, pt=pt, ot=ot: e.tensor_copy(ot[:, 512:1024], pt[:]), reads=[pk], writes=[ok + "h1"])
            P.dma("sp", k.out[tt * 128:(tt + 1) * 128, :], ot[:], reads=[ok + "h0", ok + "h1"], writes=["out"])
        stage_end(k)


Z_ORDER = None


def _zcols():
    u = np.arange(0, 512)
    qa = np.arange(512, 896)
    kva = np.arange(896, 1152)
    kr = np.arange(1152, 1184)
    wq = np.arange(1184, 1696)
    wk = np.arange(1696, 1824)
    wv = np.arange(1824, 1952)
    gates = np.arange(1952, 5024)
    Z = -np.ones(64, np.int64)

    def padq(cols8):
        out = []
        for h in range(8):
            kk = h // 4
            out.append(np.concatenate([cols8[h], Z]) if kk == 0 else np.concatenate([Z, cols8[h]]))
        return np.concatenate(out)
    wq8 = wq.reshape(8, 64)
    wq8s = wq.reshape(8, 2, 32)[:, ::-1, :].reshape(8, 64)
    wk_sw = wk.reshape(2, 2, 32)[:, ::-1, :].reshape(-1)
    kr_sw = kr.reshape(2, 16)[::-1].reshape(-1)
    cols = np.concatenate([u, qa, kva, padq(wq8), wk, wv, gates, padq(wq8s), wk_sw, kr, kr_sw, Z])
    assert cols.size == NZT * 128, cols.size
    return cols


def prep_shared(inp):
    f = lambda a: np.ascontiguousarray(np.asarray(a, np.float32))
    sh = {}
    sh["ident"] = np.eye(128, dtype=np.float32)
    sh["w_ada"] = f(inp["w_ada"])
    b = f(inp["b_ada"]).reshape(DEPTH, 48, 128).transpose(0, 2, 1)
    sh["bada2"] = f(np.repeat(b[:, :, :, None], 2, axis=3))
    cols = _zcols()
    wx = f(inp["w_in"])[:, :, np.maximum(cols, 0)]
    wx[:, :, cols < 0] = 0.0
    sh["w_inx"] = f(wx)
    G, PS, HG = 32, 64, 16
    bT = np.zeros((DEPTH, 2, 2, 16, 128, 128), np.float32)
    cL = np.zeros((DEPTH, 128, 2, 16, 2, 16), np.float32)
    lane = np.zeros((DEPTH, 128, 3, 32), np.float32)
    for ri, nm in enumerate(("s5_b_re", "s5_b_im")):
        bsrc = f(inp[nm])
        for lt in range(16):
            for half in range(2):
                g = 2 * lt + half
                gl = g % 8
                bT[:, :, ri, lt, gl * 16:(gl + 1) * 16, half * 64:(half + 1) * 64] = bsrc[:, :, g].transpose(0, 1, 3, 2)
    for ri, nm in enumerate(("s5_c_re", "s5_c_im")):
        csrc = f(inp[nm])
        for lt in range(16):
            for half in range(2):
                g = 2 * lt + half
                cL[:, half * 64:(half + 1) * 64, :, lt, ri, :] = csrc[:, :, g].transpose(0, 3, 1, 2)
    lre, lim, ldt = f(inp["s5_lam_re"]), f(inp["s5_lam_im"]), f(inp["s5_log_dt"])
    for d in range(2):
        for lt in range(16):
            for half in range(2):
                g = 2 * lt + half
                lane[:, half * 64:(half + 1) * 64, 0, d * 16 + lt] = lre[:, d, g, :]
                lane[:, half * 64:(half + 1) * 64, 1, d * 16 + lt] = lim[:, d, g, :]
                lane[:, half * 64:(half + 1) * 64, 2, d * 16 + lt] = ldt[:, d, g][:, None]
    sh["s5_bT"], sh["s5_cL"], sh["s5_lane"] = bT, cL, lane
    dg = np.zeros((DEPTH, 128, 2, 4), np.float32)
    dg[:, :, 0, :] = f(inp["s5_d"]).reshape(DEPTH, 4, 128).transpose(0, 2, 1)
    dg[:, :, 1, :] = f(inp["s5_b_glu"]).reshape(DEPTH, 4, 128).transpose(0, 2, 1)
    sh["s5_dg"] = dg
    sh["s5_wglu"] = f(inp["s5_w_glu"])
    mg = np.zeros((DEPTH, 128, 5), np.float32)
    mg[:, :, 0:3] = f(inp["mla_q_norm"]).reshape(DEPTH, 3, 128).transpose(0, 2, 1)
    mg[:, :, 3:5] = f(inp["mla_kv_norm"]).reshape(DEPTH, 2, 128).transpose(0, 2, 1)
    sh["mla_g"] = mg
    wuq = f(inp["mla_w_uq"]).reshape(DEPTH, 384, 8, 96)
    wm = np.zeros((DEPTH, 384, 8, 128), np.float32)
    wsw = np.zeros((DEPTH, 384, 8, 128), np.float32)
    wm[..., 0:96] = wuq
    wsw[..., 64:96] = wuq[..., 64:].reshape(DEPTH, 384, 8, 2, 16)[:, :, :, ::-1, :].reshape(DEPTH, 384, 8, 32)
    sh["w_uqx"] = f(np.concatenate([wm.reshape(DEPTH, 384, 1024), wsw.reshape(DEPTH, 384, 1024)], -1))
    wkv = f(inp["mla_w_ukv"]).reshape(DEPTH, 256, 8, 128)
    wkp = np.zeros((DEPTH, 256, 8, 128), np.float32)
    wkp[..., 0:64] = wkv[..., :64]
    sh["w_ukvk"] = f(wkp.reshape(DEPTH, 256, 1024))
    sh["w_ukvv"] = f(wkv[..., 64:].reshape(DEPTH, 256, 512))
    rows = L // 64
    row = np.repeat(np.arange(rows, dtype=np.float64), 64)
    col = np.tile(np.arange(64, dtype=np.float64), rows)

    def rope_tab(d, reps):
        nf = d // 4
        fr = 10000.0 ** (-np.arange(nf) / nf)
        ang = np.concatenate([row[:, None] * fr, col[:, None] * fr], -1)
        c = np.concatenate([np.cos(ang), np.cos(ang)], -1).T
        sn = np.concatenate([-np.sin(ang), np.sin(ang)], -1).T
        return np.stack([np.tile(c, (reps, 1)), np.tile(sn, (reps, 1))], 0).astype(np.float32)
    rm = np.zeros((2, 128, L), np.float32)
    rm[0] = 1.0
    rm[:, 64:96, :] = rope_tab(32, 1)
    sh["rope_mla"] = rm
    sh["rope_win"] = rope_tab(64, 2)
    jj = np.arange(128)[:, None]
    rr = np.arange(128)[None, :]
    sh["wmask"] = np.stack([(jj >= rr), (jj <= rr)], 0).astype(np.float32)
    sh["win_sink"] = f(np.broadcast_to(f(inp["win_sink"])[:, None, :], (DEPTH, 128, 8)))
    sh["w_branch"] = f(inp["w_branch"]).reshape(DEPTH, 1536, D)
    sh["w_out"] = f(inp["w_out"])
    lnp = np.stack([f(inp[n]).reshape(DEPTH, 8, 128).transpose(0, 2, 1) for n in ("ln1_g", "ln1_b", "ln2_g", "ln2_b")], 2)
    sh["lnp"] = f(lnp)
    sh["w_router"] = f(inp["w_router"])
    sh["w_gate"], sh["w_up"], sh["w_down"] = f(inp["w_gate"]), f(inp["w_up"]), f(inp["w_down"])
    sel = np.zeros((16, 16, 128), np.float32)
    for e_ in range(16):
        sel[e_, e_, :] = 1.0
    sh["sel16"] = sel
    sh["iota_s"] = f(np.broadcast_to(np.arange(384, dtype=np.float32), (128, 384)))
    sh["iota_p3"] = f(np.arange(128, dtype=np.float32)[:, None] + np.array([0.0, 128.0, 256.0], np.float32)[None, :])
    sh["tau"] = f(np.broadcast_to(np.arange(NT, dtype=np.float32), (128, NT)))
    return sh


def prep_core(inp, b):
    f = lambda a: np.ascontiguousarray(np.asarray(a, np.float32))
    m = {}
    m["xin"] = f(np.concatenate([inp["ctx"][b], inp["x"][b]], 0))
    cond = np.stack([np.asarray(inp["c"][b]), np.asarray(inp["c_ctx"])], -1)
    m["condT"] = f(cond.reshape(8, 128, 2).transpose(1, 0, 2))
    return m


def kernel(**inputs):
    nc = build()
    sh = prep_shared(inputs)
    in_maps = []
    for b in range(8):
        m = dict(sh)
        m.update(prep_core(inputs, b))
        in_maps.append(m)
    res = run_bass_kernel_spmd(nc, in_maps, core_ids=list(range(8)))
    return np.stack([np.asarray(r["out"], np.float32) for r in res.results], 0)
```

```python
import numpy as np
from contextlib import ExitStack
import concourse.bass as bass
import concourse.mybir as mybir
from concourse.bass_utils import run_bass_kernel_spmd

F32 = mybir.dt.float32
BF16 = mybir.dt.bfloat16
I32 = mybir.dt.int32
F32R = mybir.dt.float32r
AF = mybir.ActivationFunctionType
ALU = mybir.AluOpType

ENGS = ("pe", "dve", "act", "pool", "sp")
D = 1024
NT = 2304
LC = 256
L = 2048
DEPTH = 4
ALPHA = (2 * DEPTH) ** 0.25
EPS = 1e-6
NZT = 53
ZT_QA, ZT_KVA, ZT_WQ, ZT_WK, ZT_WV, ZT_G, ZT_WQS, ZT_WKS, ZT_KRX = 4, 7, 9, 17, 18, 19, 43, 51, 52
TBS = [(0, 256, 1), (256, 512, 0), (768, 512, 0), (1280, 512, 0), (1792, 512, 0)]


class Prog:
    def __init__(self, nc, es, n_dma_sems=24):
        self.nc = nc
        self.q = {e: [] for e in ENGS}
        self.sem = {}
        self.cnt = {}
        for e in ENGS:
            self.sem[e] = es.enter_context(nc.semaphore("s_" + e))
            self.cnt[e] = 0
        self.dma_sems = []
        for i in range(n_dma_sems):
            nm = "d%d" % i
            self.sem[nm] = es.enter_context(nc.semaphore("s_" + nm))
            self.cnt[nm] = 0
            self.dma_sems.append(nm)
        self.dma_rr = 0
        self.seen = {e: {} for e in ENGS}
        self.lastw = {}
        self.readers = {}
        self.nops = 0

    def _deps(self, eng, reads, writes):
        deps = {}

        def need(st, v):
            if st == eng and eng == "pe":
                return
            if deps.get(st, 0) < v:
                deps[st] = v

        for k in reads:
            lw = self.lastw.get(k)
            if lw is not None:
                need(*lw)
            if k.startswith("ps"):
                for r in self.readers.get(k, ()):
                    if r[0] != eng:
                        need(*r)
        for k in writes:
            lw = self.lastw.get(k)
            if lw is not None:
                need(*lw)
            for r in self.readers.get(k, ()):
                need(*r)
        return deps

    def _emit_waits(self, eng, deps):
        for st, v in deps.items():
            if self.seen[eng].get(st, 0) < v:
                self.seen[eng][st] = v
                sem = self.sem[st]
                self.q[eng].append(lambda e, sem=sem, v=v: e.wait_ge(sem, v))

    def _record(self, done, reads, writes):
        for k in reads:
            self.readers.setdefault(k, []).append(done)
        for k in writes:
            self.lastw[k] = done
            self.readers[k] = []

    def op(self, eng, fn, reads=(), writes=()):
        deps = self._deps(eng, reads, writes)
        self._emit_waits(eng, deps)
        self.cnt[eng] += 1
        sem = self.sem[eng]
        self.q[eng].append(lambda e, fn=fn, sem=sem: fn(e).then_inc(sem, 1))
        self._record((eng, self.cnt[eng]), reads, writes)
        self.nops += 1

    def dma(self, qeng, out, in_, reads=(), writes=(), **kw):
        st = self.dma_sems[self.dma_rr % len(self.dma_sems)]
        self.dma_rr += 1
        deps = self._deps(st, reads, writes)
        if self.cnt[st] > 0:
            deps[st] = self.cnt[st]
        self._emit_waits(qeng, deps)
        self.cnt[st] += 16
        sem = self.sem[st]
        self.q[qeng].append(
            lambda e, out=out, in_=in_, sem=sem, kw=kw: e.dma_start(out=out, in_=in_, **kw).then_inc(sem, 16))
        self._record((st, self.cnt[st]), reads, writes)
        self.nops += 1

    def barrier(self):
        for eng in ENGS:
            deps = {}
            for st in self.sem:
                if st != eng and self.cnt[st] > 0:
                    deps[st] = self.cnt[st]
            self._emit_waits(eng, deps)
        self.lastw = {}
        self.readers = {}

    def replay(self):
        nc = self.nc
        q = self.q
        with nc.Block() as block:
            @block.tensor
            def _(e):
                for f in q["pe"]:
                    f(e)

            @block.vector
            def _(e):
                for f in q["dve"]:
                    f(e)

            @block.scalar
            def _(e):
                for f in q["act"]:
                    f(e)

            @block.gpsimd
            def _(e):
                for f in q["pool"]:
                    f(e)

            @block.sync
            def _(e):
                for f in q["sp"]:
                    f(e)
        self.q = {e: [] for e in ENGS}


_UID = [0]


def U(name):
    _UID[0] += 1
    return "%s_u%d" % (name, _UID[0])


class Rot:
    def __init__(self, nc, es, name, n, shape, dtype, psum=False):
        self.name = name
        self.n = n
        self.i = 0
        if psum:
            self.t = [es.enter_context(nc.psum_tensor(U("%s%d" % (name, j)), shape, dtype)) for j in range(n)]
        else:
            self.t = [es.enter_context(nc.sbuf_tensor(U("%s%d" % (name, j)), shape, dtype)) for j in range(n)]

    def next(self):
        j = self.i % self.n
        self.i += 1
        return self.t[j], "%s%d" % (self.name, j)


class K:
    pass


def build(nlayers=DEPTH, dbg=()):
    nc = bass.Bass("TRN2", target_bir_lowering=False)
    k = K()
    k.nc = nc
    k.dbg = dbg
    din = lambda name, shape, dt=F32: nc.dram_tensor(name, list(shape), dt, kind="ExternalInput").ap()

    def dscr(name, shape, dt):
        kind = "ExternalOutput" if name in dbg else "Internal"
        return nc.dram_tensor(name, list(shape), dt, kind=kind).ap()

    k.xin = din("xin", [NT, D])
    k.condT = din("condT", [128, 8, 2])
    k.ident = din("ident", [128, 128])
    k.w_ada = din("w_ada", [DEPTH, D, 6 * D])
    k.bada2 = din("bada2", [DEPTH, 128, 48, 2])
    k.w_inx = din("w_inx", [DEPTH, D, NZT * 128])
    k.s5_bT = din("s5_bT", [DEPTH, 2, 2, 16, 128, 128])
    k.s5_cL = din("s5_cL", [DEPTH, 128, 2, 16, 2, 16])
    k.s5_lane = din("s5_lane", [DEPTH, 128, 3, 32])
    k.s5_dg = din("s5_dg", [DEPTH, 128, 2, 4])
    k.s5_wglu = din("s5_wglu", [DEPTH, 512, 512])
    k.tau = din("tau", [128, NT])
    k.mla_g = din("mla_g", [DEPTH, 128, 5])
    k.w_uqx = din("w_uqx", [DEPTH, 384, 2048])
    k.w_ukvk = din("w_ukvk", [DEPTH, 256, 1024])
    k.w_ukvv = din("w_ukvv", [DEPTH, 256, 512])
    k.rope_mla = din("rope_mla", [2, 128, L])
    k.rope_win = din("rope_win", [2, 128, L])
    k.wmask = din("wmask", [2, 128, 128])
    k.win_sink = din("win_sink", [DEPTH, 128, 8])
    k.w_branch = din("w_branch", [DEPTH, 1536, D])
    k.w_out = din("w_out", [DEPTH, D, D])
    k.lnp = din("lnp", [DEPTH, 128, 4, 8])
    k.w_router = din("w_router", [DEPTH, D, 16])
    k.w_gate = din("w_gate", [DEPTH, 16, D, D])
    k.w_up = din("w_up", [DEPTH, 16, D, D])
    k.w_down = din("w_down", [DEPTH, 16, D, D])
    k.sel16 = din("sel16", [16, 16, 128])
    k.iota_s = din("iota_s", [128, 384])
    k.iota_p3 = din("iota_p3", [128, 3])
    k.out = nc.dram_tensor("out", [L, D], F32, kind="ExternalOutput").ap()
    k.brT = [dscr(nm, [512, NT], BF16) for nm in ("s5T", "mlaT", "winT")]
    k.xT = dscr("xT", [D, NT], F32)
    k.zT = dscr("zT", [NZT * 128, NT], BF16)
    k.vtok = dscr("vtok", [NT, 128], BF16)
    k.h2T = dscr("h2T", [D, NT], BF16)
    k.yg = dscr("yg", [16, 3, 128, D], BF16)

    with ExitStack() as es:
        P = Prog(nc, es)
        k.P = P
        k.identf = es.enter_context(nc.sbuf_tensor(U("identf"), [128, 128], F32))
        k.identb = es.enter_context(nc.sbuf_tensor(U("identb"), [128, 128], BF16))
        k.onesm = es.enter_context(nc.sbuf_tensor(U("onesm"), [128, 128], F32))
        k.mod = es.enter_context(nc.sbuf_tensor(U("mod"), [128, DEPTH, 48, 2], F32))
        k.epsc = es.enter_context(nc.sbuf_tensor(U("epsc"), [128, 1], F32))
        k.ps = [es.enter_context(nc.psum_tensor("ps%d" % i, [128, 512], F32)) for i in range(8)]
        k.psi = 0

        stage_init(k)
        stage_ada(k, nlayers)
        k.onesmb = es.enter_context(nc.sbuf_tensor(U("onesmb"), [128, 128], BF16))
        P.op("dve", lambda e: e.memset(k.onesmb[:], 1.0 / D), writes=["onesmb"])
        k.onesb = es.enter_context(nc.sbuf_tensor(U("onesb"), [128, 512], BF16))
        k.onesf = es.enter_context(nc.sbuf_tensor(U("onesf"), [128, 128], F32))
        P.op("dve", lambda e: e.memset(k.onesb[:], 1.0), writes=["onesb"])
        P.op("dve", lambda e: e.memset(k.onesf[:], 1.0), writes=["onesf"])
        k.halfpi = es.enter_context(nc.sbuf_tensor(U("halfpi"), [128, 1], F32))
        P.op("dve", lambda e: e.memset(k.halfpi[:], float(np.pi / 2)), writes=["halfpi"])
        for li in range(nlayers):
            if "skip_win" not in dbg:
                stage_ln_win(k, li)
            if "mla_first" in dbg:
                stage_mla(k, li)
            if "skip_s5" not in dbg:
                stage_s5(k, li)
            if "skip_mla" not in dbg and "mla_first" not in dbg:
                stage_mla(k, li)
            if "skip_winb" not in dbg:
                stage_win(k, li)
            if "skip_mm" not in dbg:
                stage_merge(k, li)
                stage_moe(k, li)
        stage_out(k)
    return nc


def dump(k, name, ap, shape, dt, reads):
    if name not in k.dbg:
        return
    t = k.nc.dram_tensor(name, list(shape), dt, kind="ExternalOutput").ap()
    k.P.dma("sp", t, ap, reads=reads)


def psn(k, lo=0, hi=8):
    j = lo + (k.psi % (hi - lo))
    k.psi += 1
    return k.ps[j], "ps%d" % j


def stage_end(k):
    k.P.barrier()
    k.P.replay()


def stage_init(k):
    nc, P = k.nc, k.P
    P.dma("sp", k.identf[:], k.ident, writes=["identf"])
    P.dma("pool", k.identb[:], k.ident, writes=["identb"])
    P.op("dve", lambda e: e.memset(k.onesm[:], 1.0 / D), writes=["onesm"])
    P.op("dve", lambda e: e.memset(k.epsc[:], EPS), writes=["epsc"])
    with ExitStack() as st:
        xr = Rot(nc, st, "xr", 2, [128, D], F32)
        xo = Rot(nc, st, "xo", 2, [128, 8, 128], F32)
        xTv = k.xT.rearrange("(k p) t -> p k t", p=128)
        for tt in range(NT // 128):
            xt, xk = xr.next()
            P.dma("sp", xt[:], k.xin[tt * 128:(tt + 1) * 128, :], writes=[xk])
            ot, ok = xo.next()
            for half in range(2):
                pt, pk = psn(k)
                for kk in range(4):
                    kf = half * 4 + kk
                    P.op("pe", lambda e, pt=pt, kk=kk, kf=kf, xt=xt: e.transpose(pt[:, kk * 128:(kk + 1) * 128], xt[:, kf * 128:(kf + 1) * 128], k.identf[:]),
                         reads=[xk, "identf"], writes=[pk])
                eng = "act" if half == 0 else "dve"
                if eng == "act":
                    P.op("act", lambda e, pt=pt, ot=ot, half=half: e.copy(ot[:, half * 4:(half + 1) * 4, :], pt[:].rearrange("p (k t) -> p k t", k=4)),
                         reads=[pk], writes=[ok + "h%d" % half])
                else:
                    P.op("dve", lambda e, pt=pt, ot=ot, half=half: e.tensor_copy(ot[:, half * 4:(half + 1) * 4, :], pt[:].rearrange("p (k t) -> p k t", k=4)),
                         reads=[pk], writes=[ok + "h%d" % half])
            P.dma("sp", xTv[:, :, tt * 128:(tt + 1) * 128], ot[:], reads=[ok + "h0", ok + "h1"], writes=["xT"])
        stage_end(k)


def stage_ada(k, nlayers):
    nc, P = k.nc, k.P
    with ExitStack() as st:
        sc = st.enter_context(nc.sbuf_tensor(U("sc"), [128, 8, 2], F32))
        bt = st.enter_context(nc.sbuf_tensor(U("bt"), [128, DEPTH, 48, 2], F32))
        wa = Rot(nc, st, "wa", 2, [128, 8, 768], F32)
        P.dma("sp", sc[:], k.condT, writes=["sc"])
        P.dma("sp", bt[:], k.bada2.rearrange("l p m s -> p l m s"), writes=["bt"])
        P.op("act", lambda e: e.activation(out=sc[:], in_=sc[:], func=AF.Silu), reads=["sc"], writes=["sc"])
        for li in range(nlayers):
            wv = k.w_ada[li].rearrange("(k p) n -> p k n", p=128)
            for cb in range(8):
                wt, wk = wa.next()
                P.dma("sp", wt[:], wv[:, :, cb * 768:(cb + 1) * 768], writes=[wk])
                pt, pk = psn(k)
                for mt in range(6):
                    for kk in range(8):
                        P.op("pe", lambda e, pt=pt, wt=wt, mt=mt, kk=kk: e.matmul(pt[:, mt * 2:mt * 2 + 2], wt[:, kk, mt * 128:(mt + 1) * 128], sc[:, kk, :], start=(kk == 0), stop=(kk == 7)),
                             reads=[wk, "sc"], writes=[pk])
                P.op("dve", lambda e, pt=pt, li=li, cb=cb: e.tensor_tensor(k.mod[:, li, cb * 6:(cb + 1) * 6, :], pt[:, 0:12].rearrange("p (m s) -> p m s", s=2), bt[:, li, cb * 6:(cb + 1) * 6, :], ALU.add),
                     reads=[pk, "bt"], writes=["mod"])
            for j in (1, 4):
                P.op("dve", lambda e, li=li, j=j: e.tensor_scalar_add(k.mod[:, li, j * 8:(j + 1) * 8, :], k.mod[:, li, j * 8:(j + 1) * 8, :], 1.0),
                     reads=["mod"], writes=["mod"])
        stage_end(k)


def ln_stats(k, xb, xk, w, tmp, tag=""):
    nc, P = k.nc, k.P
    sq, mean, rstd, m2, xbf = tmp["sq"], tmp["mean"], tmp["rstd"], tmp["m2"], tmp["xbf"]
    P.op("act", lambda e: e.activation(out=sq[:, :, :w], in_=xb[:, :, :w], func=AF.Square), reads=[xk], writes=["sq" + tag])
    P.op("act", lambda e: e.copy(xbf[:, :, :w], xb[:, :, :w]), reads=[xk], writes=["xbf" + tag])
    p1, k1 = psn(k)
    p2, k2 = psn(k)
    for kk in range(8):
        P.op("pe", lambda e, kk=kk: e.matmul(p1[:, :w], k.onesmb[:], xbf[:, kk, :w], start=(kk == 0), stop=(kk == 7)), reads=["xbf" + tag, "onesmb"], writes=[k1])
    for kk in range(8):
        P.op("pe", lambda e, kk=kk: e.matmul(p2[:, :w], k.onesmb[:], sq[:, kk, :w], start=(kk == 0), stop=(kk == 7)), reads=["sq" + tag, "onesmb"], writes=[k2])
    P.op("act", lambda e: e.copy(mean[:, :w], p1[:, :w]), reads=[k1], writes=["mean" + tag])
    P.op("dve", lambda e: e.tensor_tensor(m2[:, :w], mean[:, :w], mean[:, :w], ALU.mult), reads=["mean" + tag], writes=["m2" + tag])
    P.op("dve", lambda e: e.tensor_tensor(m2[:, :w], p2[:, :w], m2[:, :w], ALU.subtract), reads=[k2, "m2" + tag], writes=["m2" + tag])
    P.op("act", lambda e: e.activation(out=m2[:, :w], in_=m2[:, :w], func=AF.Sqrt, bias=k.epsc[:], scale=1.0), reads=["m2" + tag, "epsc"], writes=["m2" + tag])
    P.op("dve", lambda e: e.reciprocal(rstd[:, :w], m2[:, :w]), reads=["m2" + tag], writes=["rstd" + tag])


def ln_tmp(nc, st, W=512):
    return {
        "sq": st.enter_context(nc.sbuf_tensor(U("ln_sq"), [128, 8, W], BF16)),
        "xbf": st.enter_context(nc.sbuf_tensor(U("ln_xbf"), [128, 8, W], BF16)),
        "mean": st.enter_context(nc.sbuf_tensor(U("ln_mean"), [128, W], F32)),
        "rstd": st.enter_context(nc.sbuf_tensor(U("ln_rstd"), [128, W], F32)),
        "m2": st.enter_context(nc.sbuf_tensor(U("ln_m2"), [128, W], F32)),
        "t": st.enter_context(nc.sbuf_tensor(U("ln_t"), [128, W], F32)),
        "tr": Rot(nc, st, U("ln_tr"), 3, [128, W], F32),
    }


def ln_apply(k, xb, xk, w, tmp, kk, out, okeys, scale_ap, bias_ap, extra_reads=(), tag=""):
    P = k.P
    t, tk = tmp["tr"].next()
    P.op("dve", lambda e: e.tensor_tensor(t[:, :w], xb[:, kk, :w], tmp["mean"][:, :w], ALU.subtract), reads=[xk, "mean" + tag], writes=[tk])
    P.op("dve", lambda e: e.tensor_tensor(t[:, :w], t[:, :w], tmp["rstd"][:, :w], ALU.mult), reads=[tk, "rstd" + tag], writes=[tk])
    P.op("act", lambda e: e.activation(out=out, in_=t[:, :w], func=AF.Identity, scale=scale_ap, bias=bias_ap), reads=[tk, "mod"] + list(extra_reads), writes=okeys)


def stage_ln_win(k, li):
    nc, P = k.nc, k.P
    with ExitStack() as st:
        hT = st.enter_context(nc.sbuf_tensor(U("hT"), [128, 8, NT], BF16))
        with ExitStack() as st2:
            tmp = ln_tmp(nc, st2)
            xbr = Rot(nc, st2, "xb", 2, [128, 8, 512], F32)
            xTv = k.xT.rearrange("(k p) t -> p k t", p=128)
            for (t0, w, isc) in TBS:
                xb, xk = xbr.next()
                P.dma("sp", xb[:, :, :w], xTv[:, :, t0:t0 + w], reads=["xT"], writes=[xk])
                ln_stats(k, xb, xk, w, tmp)
                for kk in range(8):
                    ln_apply(k, xb, xk, w, tmp, kk, hT[:, kk, t0:t0 + w], ["hT%d" % kk],
                             k.mod[:, li, 8 + kk, isc:isc + 1], k.mod[:, li, 0 + kk, isc:isc + 1])
            P.barrier()
        wr = Rot(nc, st, "wr", 3, [128, 8, 128], BF16)
        zs = Rot(nc, st, "zs", 3, [128, NT], BF16)
        vt = st.enter_context(nc.sbuf_tensor(U("vt"), [128, 18, 128], BF16))
        wv = k.w_inx[li].rearrange("(k p) n -> p k n", p=128)
        ev = 0
        for m in range(NZT):
            wt, wk = wr.next()
            P.dma("pool", wt[:], wv[:, :, m * 128:(m + 1) * 128], writes=[wk])
            zt, zk = zs.next()
            mrows = 64 if m == ZT_KRX else 128
            for (t0, w, isc) in TBS:
                pt, pk = psn(k)
                for kk in range(8):
                    P.op("pe", lambda e, pt=pt, wt=wt, kk=kk, t0=t0, w=w, mrows=mrows: e.matmul(pt[:mrows, :w], wt[:, kk, :mrows], hT[:, kk, t0:t0 + w], start=(kk == 0), stop=(kk == 7)),
                         reads=[wk, "hT%d" % kk], writes=[pk])
                gate = ZT_G <= m < ZT_G + 24
                if gate:
                    P.op("act", lambda e, pt=pt, zt=zt, t0=t0, w=w: e.activation(out=zt[:, t0:t0 + w], in_=pt[:, :w], func=AF.Sigmoid), reads=[pk], writes=[zk + "_%d" % t0])
                elif ev % 2 == 0:
                    P.op("act", lambda e, pt=pt, zt=zt, t0=t0, w=w, mrows=mrows: e.copy(zt[:mrows, t0:t0 + w], pt[:mrows, :w]), reads=[pk], writes=[zk + "_%d" % t0])
                else:
                    P.op("dve", lambda e, pt=pt, zt=zt, t0=t0, w=w, mrows=mrows: e.tensor_copy(zt[:mrows, t0:t0 + w], pt[:mrows, :w]), reads=[pk], writes=[zk + "_%d" % t0])
                ev += 1
            P.dma("sp", k.zT[m * 128:m * 128 + mrows, :], zt[:mrows, :], reads=[zk + "_%d" % t[0] for t in TBS], writes=["zT"])
            if m == ZT_WV:
                for tt in range(18):
                    pt, pk = psn(k)
                    for kk in range(8):
                        P.op("pe", lambda e, pt=pt, wt=wt, kk=kk, tt=tt: e.matmul(pt[:, :128], hT[:, kk, tt * 128:(tt + 1) * 128], wt[:, kk, :], start=(kk == 0), stop=(kk == 7)),
                             reads=[wk, "hT%d" % kk], writes=[pk])
                    P.op("dve", lambda e, pt=pt, tt=tt: e.tensor_copy(vt[:, tt, :], pt[:, :128]), reads=[pk], writes=["vt"])
                P.dma("sp", k.vtok.rearrange("(t p) c -> p t c", p=128), vt[:], reads=["vt"], writes=["vtok"])
        stage_end(k)


def stage_s5(k, li):
    S5E = "dve" if "s5pool" not in k.dbg else "pool"
    nc, P = k.nc, k.P
    TWO_PI = float(2 * np.pi)
    with ExitStack() as st:
        sb = lambda name, shape, dt=F32: st.enter_context(nc.sbuf_tensor(U(name), shape, dt))
        lane = sb("lane", [128, 3, 32])
        dg = sb("dg", [128, 2, 4])
        BW = sb("BW", [128, 64, 128], BF16)
        CW = sb("CW", [128, 96, 128], BF16)
        tau = sb("tau", [128, NT])
        names = ["dt", "rho", "thn", "fr", "sn", "cs", "ar", "ai", "rden", "qr", "qi", "nqr", "nqi", "tA", "tB"]
        lp = {n: sb("lp_" + n, [128, 32]) for n in names}
        lpi = sb("lp_it", [128, 32], I32)
        P.dma("sp", lane[:], k.s5_lane[li], writes=["lane"])
        P.dma("sp", dg[:], k.s5_dg[li], writes=["dg"])
        P.dma("sp", tau[:], k.tau, writes=["tau"])
        bsrc = k.s5_bT[li].rearrange("d r t p c -> p (d r t) c")
        for j in range(8):
            P.dma("pool", BW[:, j * 8:(j + 1) * 8, :], bsrc[:, j * 8:(j + 1) * 8, :], writes=["BW"])
        P.op("pool", lambda e: e.memset(CW[:], 0.0), writes=["CW"])
        lre, lim, ldt = lane[:, 0, :], lane[:, 1, :], lane[:, 2, :]
        R = ["lane", "lp"]
        W = ["lp"]
        V = lambda fn: P.op("dve", fn, reads=R, writes=W)
        A = lambda fn: P.op("act", fn, reads=R + ["halfpi"], writes=W)
        A(lambda e: e.activation(out=lp["dt"][:], in_=ldt, func=AF.Exp))
        V(lambda e: e.tensor_tensor(lp["tA"][:], lre, lp["dt"][:], ALU.mult))
        A(lambda e: e.activation(out=lp["rho"][:], in_=lp["tA"][:], func=AF.Exp))
        V(lambda e: e.tensor_tensor(lp["thn"][:], lim, lp["dt"][:], ALU.mult))
        V(lambda e: e.tensor_scalar(lp["thn"][:], lp["thn"][:], float(1.0 / TWO_PI), None, ALU.mult))
        V(lambda e: e.tensor_copy(lpi[:], lp["thn"][:]))
        V(lambda e: e.tensor_copy(lp["tB"][:], lpi[:]))
        V(lambda e: e.tensor_tensor(lp["fr"][:], lp["thn"][:], lp["tB"][:], ALU.subtract))
        A(lambda e: e.activation(out=lp["sn"][:], in_=lp["fr"][:], func=AF.Sin, scale=TWO_PI))
        A(lambda e: e.activation(out=lp["fr"][:], in_=lp["fr"][:], func=AF.Abs))
        A(lambda e: e.activation(out=lp["cs"][:], in_=lp["fr"][:], func=AF.Sin, scale=-TWO_PI, bias=k.halfpi[:]))
        V(lambda e: e.tensor_tensor(lp["ar"][:], lp["rho"][:], lp["cs"][:], ALU.mult))
        V(lambda e: e.tensor_tensor(lp["ai"][:], lp["rho"][:], lp["sn"][:], ALU.mult))
        V(lambda e: e.tensor_tensor(lp["tA"][:], lre, lre, ALU.mult))
        V(lambda e: e.tensor_tensor(lp["tB"][:], lim, lim, ALU.mult))
        V(lambda e: e.tensor_tensor(lp["tA"][:], lp["tA"][:], lp["tB"][:], ALU.add))
        V(lambda e: e.reciprocal(lp["rden"][:], lp["tA"][:]))
        V(lambda e: e.tensor_scalar_add(lp["ar"][:], lp["ar"][:], -1.0))
        V(lambda e: e.tensor_tensor(lp["tA"][:], lp["ar"][:], lre, ALU.mult))
        V(lambda e: e.tensor_tensor(lp["tB"][:], lp["ai"][:], lim, ALU.mult))
        V(lambda e: e.tensor_tensor(lp["tA"][:], lp["tA"][:], lp["tB"][:], ALU.add))
        V(lambda e: e.tensor_tensor(lp["qr"][:], lp["tA"][:], lp["rden"][:], ALU.mult))
        V(lambda e: e.tensor_tensor(lp["tA"][:], lp["ai"][:], lre, ALU.mult))
        V(lambda e: e.tensor_tensor(lp["tB"][:], lp["ar"][:], lim, ALU.mult))
        V(lambda e: e.tensor_tensor(lp["tA"][:], lp["tA"][:], lp["tB"][:], ALU.subtract))
        V(lambda e: e.tensor_tensor(lp["qi"][:], lp["tA"][:], lp["rden"][:], ALU.mult))
        V(lambda e: e.tensor_scalar(lp["nqr"][:], lp["qr"][:], -1.0, None, ALU.mult))
        V(lambda e: e.tensor_scalar(lp["nqi"][:], lp["qi"][:], -1.0, None, ALU.mult))
        for n_ in ("rho", "thn", "sn", "cs", "qr", "qi", "dt"):
            dump(k, "lp_" + n_, lp[n_][:], [128, 32], F32, ["lp"])
        stC = ExitStack()
        craw = stC.enter_context(nc.sbuf_tensor(U("craw"), [128, 2, 16, 2, 16], F32))
        ctmp = stC.enter_context(nc.sbuf_tensor(U("ctmp"), [128, 16], F32))
        P.dma("sp", craw[:], k.s5_cL[li], writes=["craw"])
        for d in range(2):
            for lt in range(16):
                col = d * 16 + lt
                cr, ci = craw[:, d, lt, 0, :], craw[:, d, lt, 1, :]
                for half in range(2):
                    g = 2 * lt + half
                    gl = g % 8
                    ps_ = slice(half * 64, half * 64 + 64)
                    for ri in range(3):
                        s1 = lp["qi"] if ri != 1 else lp["nqr"]
                        s2 = (lp["qr"], lp["nqi"], lp["nqr"])[ri]
                        op1 = (ALU.subtract, ALU.add, ALU.add)[ri]
                        P.op("dve", lambda e, ci=ci, s1=s1, col=col, ps_=ps_: e.tensor_scalar(ctmp[ps_, :], ci[ps_, :], s1[ps_, col:col + 1], None, ALU.mult), reads=["craw", "lp"], writes=["ctmp"])
                        P.op("dve", lambda e, cr=cr, s2=s2, col=col, ps_=ps_, ri=ri, gl=gl, op1=op1: e.scalar_tensor_tensor(CW[ps_, col * 3 + ri, gl * 16:(gl + 1) * 16], cr[ps_, :], s2[ps_, col:col + 1], ctmp[ps_, :], ALU.mult, op1), reads=["craw", "lp", "ctmp"], writes=["CW"])
        P.barrier()
        stC.close()
        gT = sb("s5g", [128, 4, NT], BF16)
        with ExitStack() as stU:
            sbu = lambda name, shape, dt=F32: stU.enter_context(nc.sbuf_tensor(U(name), shape, dt))
            utR = Rot(nc, stU, "s5ut", 2, [128, NT], BF16)
            it = sbu("s5it", [128, NT], I32)
            fr = sbu("s5fr", [128, NT])
            SnR = Rot(nc, stU, "s5S", 2, [128, NT], BF16)
            CsR = Rot(nc, stU, "s5C", 2, [128, NT], BF16)
            br = sbu("s5br", [128, NT], BF16)
            bi = sbu("s5bi", [128, NT], BF16)
            p1 = sbu("s5p1", [128, NT], BF16)
            p2 = sbu("s5p2", [128, NT], BF16)
            p3 = sbu("s5p3", [128, NT], BF16)
            wr = sbu("s5wr", [128, NT], BF16)
            wi = sbu("s5wi", [128, NT], BF16)
            zrR = Rot(nc, stU, "s5zr", 2, [128, NT], BF16)
            ziR = Rot(nc, stU, "s5zi", 2, [128, NT], BF16)
            qR = [Rot(nc, stU, "s5q%d" % j, 2, [128, NT], BF16) for j in range(4)]
            ysr = Rot(nc, stU, "s5ys", 1, [128, 512], F32)
            segs = [(0, LC), (LC, NT)]
            units = [(gt, d, l4) for gt in range(4) for d in range(2) for l4 in range(4)]
            uts = {}
            ctx_ = {}

            def alpha(u):
                gt, d, l4 = units[u]
                lt = gt * 4 + l4
                col = d * 16 + lt
                thn = lp["thn"][:, col:col + 1]
                if (d, l4) == (0, 0):
                    ut, utk = utR.next()
                    P.dma("sp", ut[:], k.zT[gt * 128:(gt + 1) * 128, :], reads=["zT"], writes=[utk])
                    uts[gt] = (ut, utk)
                Sn, Snk = SnR.next()
                Cs, Csk = CsR.next()
                ctx_[u] = dict(Sn=Sn, Snk=Snk, Cs=Cs, Csk=Csk, col=col, lt=lt)
                for (a_, b_) in segs:
                    src = tau[:, a_:b_] if d == 0 else (tau[:, b_ - 1::-1] if a_ == 0 else tau[:, b_ - 1:a_ - 1:-1])
                    P.op("dve", lambda e, src=src, a_=a_, b_=b_, thn=thn: e.tensor_scalar(it[:, a_:b_], src, thn, None, ALU.mult), reads=["tau", "lp"], writes=["it"])
                    P.op("dve", lambda e, src=src, a_=a_, b_=b_, thn=thn: e.scalar_tensor_tensor(fr[:, a_:b_], src, thn, it[:, a_:b_], ALU.mult, ALU.subtract), reads=["tau", "lp", "it"], writes=["fr"])
                P.op("act", lambda e, Sn=Sn: e.activation(out=Sn[:], in_=fr[:], func=AF.Sin, scale=TWO_PI), reads=["fr"], writes=[Snk])
                P.op("act", lambda e: e.activation(out=fr[:], in_=fr[:], func=AF.Abs), reads=["fr"], writes=["fr"])
                P.op("act", lambda e, Cs=Cs: e.activation(out=Cs[:], in_=fr[:], func=AF.Sin, scale=-TWO_PI, bias=k.halfpi[:]), reads=["fr", "halfpi"], writes=[Csk])

            def bu(u):
                gt, d, l4 = units[u]
                lt = ctx_[u]["lt"]
                ut, utk = uts[gt]
                for (t0, w, isc) in TBS:
                    pr, kr = psn(k, 5, 8)
                    pi_, ki = psn(k, 5, 8)
                    sl = slice(t0, t0 + w)
                    P.op("pe", lambda e, pr=pr, sl=sl, w=w, d=d, lt=lt, ut=ut: e.matmul(pr[:, :w], BW[:, (d * 2 + 0) * 16 + lt, :], ut[:, sl], start=True, stop=True), reads=["BW", utk], writes=[kr])
                    P.op("pe", lambda e, pi_=pi_, sl=sl, w=w, d=d, lt=lt, ut=ut: e.matmul(pi_[:, :w], BW[:, (d * 2 + 1) * 16 + lt, :], ut[:, sl], start=True, stop=True), reads=["BW", utk], writes=[ki])
                    P.op("act", lambda e, pr=pr, sl=sl, w=w: e.copy(br[:, sl], pr[:, :w]), reads=[kr], writes=["br"])
                    P.op("act", lambda e, pi_=pi_, sl=sl, w=w: e.copy(bi[:, sl], pi_[:, :w]), reads=[ki], writes=["bi"])

            def beta(u):
                c = ctx_[u]
                Sn, Snk, Cs, Csk = c["Sn"], c["Snk"], c["Cs"], c["Csk"]
                P.op("dve", lambda e: e.tensor_tensor(p1[:], Cs[:], br[:], ALU.mult), reads=[Csk, "br"], writes=["p1"])
                P.op("dve", lambda e: e.tensor_tensor(p2[:], Sn[:], bi[:], ALU.mult), reads=[Snk, "bi"], writes=["p2"])
                P.op("dve", lambda e: e.tensor_tensor(p3[:], Cs[:], bi[:], ALU.mult), reads=[Csk, "bi"], writes=["p3"])
                P.op("dve", lambda e: e.tensor_tensor(wr[:], p1[:], p2[:], ALU.add), reads=["p1", "p2"], writes=["wr"])
                P.op("dve", lambda e: e.tensor_tensor(p2[:], Sn[:], br[:], ALU.mult), reads=[Snk, "br", "wr"], writes=["p2"])
                P.op("dve", lambda e: e.tensor_tensor(wi[:], p3[:], p2[:], ALU.subtract), reads=["p3", "p2"], writes=["wi"])

            def gamma_delta(u):
                gt, d, l4 = units[u]
                c = ctx_.pop(u)
                Sn, Snk, Cs, Csk, col = c["Sn"], c["Snk"], c["Cs"], c["Csk"], c["col"]
                first = (d == 0 and l4 == 0)
                last = (d == 1 and l4 == 3)
                zr, zrk = zrR.next()
                zi, zik = ziR.next()
                qs = [r_.next() for r_ in qR]
                rho = lp["rho"][:, col:col + 1]
                for (src, dst, dk, sk_) in ((wr, zr, zrk, "wr"), (wi, zi, zik, "wi")):
                    if d == 0:
                        P.op("dve", lambda e, src=src, dst=dst: e.tensor_tensor_scan(dst[:, 0:LC], rho.to_broadcast([128, LC]), src[:, 0:LC], 0.0, ALU.mult, ALU.add), reads=[sk_, "lp"], writes=[dk])
                        P.op("dve", lambda e, src=src, dst=dst: e.tensor_tensor_scan(dst[:, LC:NT], rho.to_broadcast([128, L]), src[:, LC:NT], dst[:, LC - 1:LC], ALU.mult, ALU.add), reads=[sk_, "lp", dk], writes=[dk])
                    else:
                        P.op("dve", lambda e, src=src, dst=dst: e.tensor_tensor_scan(dst[:, LC - 1::-1], rho.to_broadcast([128, LC]), src[:, LC - 1::-1], 0.0, ALU.mult, ALU.add), reads=[sk_, "lp"], writes=[dk])
                        P.op("dve", lambda e, src=src, dst=dst: e.tensor_tensor_scan(dst[:, NT - 1:LC - 1:-1], rho.to_broadcast([128, L]), src[:, NT - 1:LC - 1:-1], dst[:, 0:1], ALU.mult, ALU.add), reads=[sk_, "lp", dk], writes=[dk])
                (q1, q1k), (q2, q2k), (q3, q3k), (q4, q4k) = qs
                P.op("dve", lambda e: e.tensor_tensor(q1[:], Cs[:], zr[:], ALU.mult), reads=[Csk, zrk], writes=[q1k])
                P.op("dve", lambda e: e.tensor_tensor(q2[:], Sn[:], zi[:], ALU.mult), reads=[Snk, zik], writes=[q2k])
                P.op("dve", lambda e: e.tensor_tensor(q3[:], Sn[:], zr[:], ALU.mult), reads=[Snk, zrk], writes=[q3k])
                P.op("dve", lambda e: e.tensor_tensor(q4[:], Cs[:], zi[:], ALU.mult), reads=[Csk, zik], writes=[q4k])
                for bi_, (t0, w, isc) in enumerate(TBS):
                    sl = slice(t0, t0 + w)
                    yk = "ps%d" % bi_
                    for j_, (qq, qk, wsl) in enumerate(((q1, q1k, 0), (q2, q2k, 2), (q3, q3k, 1), (q4, q4k, 1))):
                        P.op("pe", lambda e, bi_=bi_, sl=sl, w=w, qq=qq, wsl=wsl, j_=j_: e.matmul(k.ps[bi_][:, :w], CW[:, col * 3 + wsl, :], qq[:, sl], start=(first and j_ == 0), stop=(last and j_ == 3)), reads=["CW", qk], writes=[yk])
                if last:
                    ut, utk = uts[gt]
                    for bi_, (t0, w, isc) in enumerate(TBS):
                        sl = slice(t0, t0 + w)
                        ys, ysk = ysr.next()
                        P.op("dve", lambda e, bi_=bi_, sl=sl, w=w, ys=ys: e.scalar_tensor_tensor(ys[:, :w], ut[:, sl], dg[:, 0, gt:gt + 1], k.ps[bi_][:, :w], ALU.mult, ALU.add), reads=[utk, "dg", "ps%d" % bi_], writes=[ysk])
                        P.op("act", lambda e, sl=sl, w=w, ys=ys: e.activation(out=gT[:, gt, sl], in_=ys[:, :w], func=AF.Gelu_apprx_tanh), reads=[ysk], writes=["gT%d" % gt])

            alpha(0)
            bu(0)
            for u in range(len(units)):
                beta(u)
                if u + 1 < len(units):
                    alpha(u + 1)
                    bu(u + 1)
                gamma_delta(u)
            P.barrier()
        wglu = sb("wglu", [128, 4, 512], BF16)
        P.dma("pool", wglu[:], k.s5_wglu[li].rearrange("(k p) n -> p k n", p=128), writes=["wglu"])
        so = Rot(nc, st, "s5o", 2, [128, NT], BF16)
        sgr = Rot(nc, st, "s5sg", 2, [128, 512], F32)
        for mo in range(4):
            ot, ok = so.next()
            for (t0, w, isc) in TBS:
                sl = slice(t0, t0 + w)
                pt, pk = psn(k)
                for kk in range(4):
                    P.op("pe", lambda e, pt=pt, kk=kk, sl=sl, w=w, mo=mo: e.matmul(pt[:, :w], wglu[:, kk, mo * 128:(mo + 1) * 128], gT[:, kk, sl], start=(kk == 0), stop=(kk == 3)), reads=["wglu", "gT%d" % kk], writes=[pk])
                sg, sgk = sgr.next()
                P.op("act", lambda e, pt=pt, w=w, sg=sg, mo=mo: e.activation(out=sg[:, :w], in_=pt[:, :w], func=AF.Sigmoid, bias=dg[:, 1, mo:mo + 1], scale=1.0), reads=[pk, "dg"], writes=[sgk])
                P.op("dve", lambda e, sg=sg, sl=sl, w=w, ot=ot, mo=mo: e.tensor_tensor(ot[:, sl], gT[:, mo, sl], sg[:, :w], ALU.mult), reads=[sgk, "gT%d" % mo], writes=[ok + "_%d" % t0])
            P.dma("sp", k.brT[0][mo * 128:(mo + 1) * 128, :], ot[:], reads=[ok + "_%d" % t[0] for t in TBS], writes=["s5T"])
        stage_end(k)


def rms_norm_T(k, st, src, nk, gains, dst, tag):
    nc, P = k.nc, k.P
    sq = st.enter_context(nc.sbuf_tensor(U("rms_sq"), [128, nk, 512], BF16))
    rinv = st.enter_context(nc.sbuf_tensor(U("rms_ri"), [128, 512], F32))
    for (t0, w, isc) in TBS:
        sl = slice(t0, t0 + w)
        P.op("act", lambda e, sl=sl, w=w: e.activation(out=sq[:, :, :w], in_=src[:, :, sl], func=AF.Square), reads=[tag + "src"], writes=[tag + "sq"])
        pt, pk = psn(k)
        for kk in range(nk):
            P.op("pe", lambda e, pt=pt, kk=kk, w=w: e.matmul(pt[:, :w], k.onesb[:, 0:128], sq[:, kk, :w], start=(kk == 0), stop=(kk == nk - 1)), reads=[tag + "sq", "onesb"], writes=[pk])
        P.op("act", lambda e, pt=pt, w=w: e.activation(out=rinv[:, :w], in_=pt[:, :w], func=AF.Sqrt, scale=float(1.0 / (nk * 128)), bias=k.epsc[:]), reads=[pk, "epsc"], writes=[tag + "ri"])
        P.op("dve", lambda e, w=w: e.reciprocal(rinv[:, :w], rinv[:, :w]), reads=[tag + "ri"], writes=[tag + "ri"])
        for kk in range(nk):
            P.op("dve", lambda e, kk=kk, sl=sl, w=w: e.scalar_tensor_tensor(dst[:, kk, sl], src[:, kk, sl], gains[:, kk:kk + 1], rinv[:, :w], ALU.mult, ALU.mult), reads=[tag + "src", tag + "ri", "mg"], writes=[tag + "dst"])


def softmax_pv(k, ost_rot, score_fn, nkc, va_fn, nq, scale, out_dram, tagp, PTr, esk=None, post=None):
    nc, P = k.nc, k.P
    po, pok = psn(k, 0, 3)
    LA = 3
    scr = {}

    def issue_score(kc):
        pscr, psk = psn(k, 3, 8)
        score_fn(kc, pscr, psk)
        scr[kc] = (pscr, psk)

    for kc in range(min(LA, nkc)):
        issue_score(kc)
    for kc in range(nkc):
        if kc + LA < nkc:
            issue_score(kc + LA)
        pscr, psk = scr.pop(kc)
        pt, ptk = PTr.next()
        P.op("act", lambda e, pscr=pscr, pt=pt: e.activation(out=pt[:, :nq], in_=pscr[:, :nq], func=AF.Exp, scale=scale), reads=[psk], writes=[ptk])
        if post is not None:
            post(kc, pt, ptk)
        va, vak = va_fn(kc)
        P.op("pe", lambda e, po=po, va=va, pt=pt, kc=kc: e.matmul(po[:, :nq], va, pt[:, :nq], start=(kc == 0), stop=(kc == nkc - 1)), reads=[vak, ptk], writes=[pok])
    rv, rvk = k.att_rv.next()
    if esk is not None:
        P.op("dve", lambda e, po=po, rv=rv: e.tensor_scalar(rv[0:64, :nq], po[64:128, :nq], esk, None, ALU.add), reads=[pok, "esk"], writes=[rvk])
        P.op("dve", lambda e, rv=rv: e.reciprocal(rv[0:64, :nq], rv[0:64, :nq]), reads=[rvk], writes=[rvk])
    else:
        P.op("dve", lambda e, po=po, rv=rv: e.reciprocal(rv[0:64, :nq], po[64:128, :nq]), reads=[pok], writes=[rvk])
    ot, otk = ost_rot.next()
    P.op("dve", lambda e, po=po, ot=ot, rv=rv: e.tensor_tensor(ot[0:64, :nq], po[0:64, :nq], rv[0:64, :nq], ALU.mult), reads=[pok, rvk], writes=[otk])
    P.dma("sp", out_dram, ot[0:64, :nq], reads=[otk], writes=[tagp])


def stage_mla(k, li):
    nc, P = k.nc, k.P
    SC = float(96 ** -0.5)
    with ExitStack() as st:
        sb = lambda name, shape, dt=F32: st.enter_context(nc.sbuf_tensor(U(name), shape, dt))
        mg = sb("mg", [128, 5])
        P.dma("sp", mg[:], k.mla_g[li], writes=["mg"])
        VA = sb("VA", [128, 18, 8, 128], BF16)
        KRb = sb("KRb", [128, NT], BF16)
        qn = sb("qn", [128, 3, NT], BF16)
        kvn = sb("kvn", [128, 2, NT], BF16)
        rope = sb("ropem", [128, 2, L])
        wuq = sb("wuq", [128, 3, 2048], BF16)
        wkk = sb("wkk", [128, 2, 1024], BF16)
        k.att_rv = Rot(nc, st, "att_rv", 2, [128, 512], F32)
        P.op("pool", lambda e: e.memset(VA[:], 1.0), writes=["VA"])
        P.dma("sp", rope[:], k.rope_mla.rearrange("c p t -> p c t"), writes=["rope"])
        for j in range(3):
            for c_ in range(4):
                P.dma("pool", wuq[:, j, c_ * 512:(c_ + 1) * 512], k.w_uqx[li][j * 128:(j + 1) * 128, c_ * 512:(c_ + 1) * 512], writes=["wuq"])
        for j in range(2):
            for c_ in range(2):
                P.dma("pool", wkk[:, j, c_ * 512:(c_ + 1) * 512], k.w_ukvk[li][j * 128:(j + 1) * 128, c_ * 512:(c_ + 1) * 512], writes=["wkk"])
        with ExitStack() as st2:
            sb2 = lambda name, shape, dt=F32: st2.enter_context(nc.sbuf_tensor(U(name), shape, dt))
            qa = sb2("qa", [128, 3, NT], BF16)
            kva = sb2("kva", [128, 2, NT], BF16)
            KP = sb2("KP", [128, NT], BF16)
            KS = sb2("KS", [128, L], BF16)
            wvv = sb2("wvv", [128, 2, 512], BF16)
            tA = sb2("mtA", [128, 512])
            tB = sb2("mtB", [128, 512])
            P.dma("sp", qa[:], k.zT[ZT_QA * 128:(ZT_QA + 3) * 128, :].rearrange("(k p) t -> p k t", p=128), reads=["zT"], writes=["qsrc"])
            P.dma("sp", kva[:], k.zT[ZT_KVA * 128:(ZT_KVA + 2) * 128, :].rearrange("(k p) t -> p k t", p=128), reads=["zT"], writes=["ksrc"])
            P.op("pool", lambda e: e.memset(KP[:], 0.0), writes=["KP"])
            P.op("pool", lambda e: e.memset(KS[:], 0.0), writes=["KS"])
            P.dma("sp", KP[64:96, :], k.zT[ZT_KRX * 128:ZT_KRX * 128 + 32, :], reads=["zT"], writes=["KP"])
            P.dma("sp", KS[64:96, :], k.zT[ZT_KRX * 128 + 32:ZT_KRX * 128 + 64, LC:NT], reads=["zT"], writes=["KS"])
            P.dma("pool", wvv[:], k.w_ukvv[li].rearrange("(k p) n -> p k n", p=128), writes=["wvv"])
            rms_norm_T(k, st2, qa, 3, mg[:, 0:3], qn, "q")
            rms_norm_T(k, st2, kva, 2, mg[:, 3:5], kvn, "k")
            P.op("dve", lambda e: e.tensor_copy(KRb[:, 0:LC], KP[:, 0:LC]), reads=["KP"], writes=["KRb"])
            for c in range(4):
                sl = slice(c * 512, (c + 1) * 512)
                sln = slice(LC + c * 512, LC + (c + 1) * 512)
                P.op("dve", lambda e, sl=sl, sln=sln: e.tensor_tensor(tA[:, :], KP[:, sln], rope[:, 0, sl], ALU.mult), reads=["KP", "rope"], writes=["mtA"])
                P.op("dve", lambda e, sl=sl: e.tensor_tensor(tB[:, :], KS[:, sl], rope[:, 1, sl], ALU.mult), reads=["KS", "rope"], writes=["mtB"])
                P.op("dve", lambda e, sln=sln: e.tensor_tensor(KRb[:, sln], tA[:, :], tB[:, :], ALU.add), reads=["mtA", "mtB"], writes=["KRb"])
            dump(k, "d_KP", KP[:], [128, NT], BF16, ["KP"])
            dump(k, "d_KS", KS[:], [128, L], BF16, ["KS"])
            dump(k, "d_KRb", KRb[:], [128, NT], BF16, ["KRb"])
            for tt in range(18):
                pt, pk = psn(k)
                for kk in range(2):
                    P.op("pe", lambda e, pt=pt, kk=kk, tt=tt: e.matmul(pt[:, :512], kvn[:, kk, tt * 128:(tt + 1) * 128], wvv[:, kk, :], start=(kk == 0), stop=(kk == 1)), reads=["wvv", "kdst"], writes=[pk])
                P.op("dve", lambda e, pt=pt, tt=tt: e.tensor_copy(VA[:, tt, :, 0:64], pt[:, :512].rearrange("p (h d) -> p h d", h=8)), reads=[pk], writes=["VA"])
            P.barrier()
        if "mla_stop1" in k.dbg:
            stage_end(k)
            return
        PTr = Rot(nc, st, "mPT", 4, [128, 512], BF16)
        ostr = Rot(nc, st, "most", 3, [128, 512], BF16)
        t1r = Rot(nc, st, "mt1", 2, [128, 512], F32)
        t2r = Rot(nc, st, "mt2", 2, [128, 512], F32)
        for grp in range(2):
            with ExitStack() as st3:
                QP = st3.enter_context(nc.sbuf_tensor(U("QP"), [128, 4, NT], BF16))
                QR = st3.enter_context(nc.sbuf_tensor(U("QR"), [128, 4, L], BF16))
                KH = st3.enter_context(nc.sbuf_tensor(U("KH"), [128, 4, NT], BF16))
                for hh in range(4):
                    h = grp * 4 + hh
                    for (t0, w, isc) in TBS:
                        sl = slice(t0, t0 + w)
                        pm, pmk = psn(k, 3, 8)
                        for kk in range(3):
                            P.op("pe", lambda e, pm=pm, kk=kk, sl=sl, w=w, h=h: e.matmul(pm[:, :w], wuq[:, kk, h * 128:(h + 1) * 128], qn[:, kk, sl], start=(kk == 0), stop=(kk == 2)), reads=["wuq", "qdst"], writes=[pmk])
                        P.op("act", lambda e, pm=pm, sl=sl, w=w, hh=hh: e.copy(QP[:, hh, sl], pm[:, :w]), reads=[pmk], writes=["QP%d" % hh])
                        if not isc:
                            ls = slice(t0 - LC, t0 - LC + w)
                            psw, pswk = psn(k, 3, 8)
                            for kk in range(3):
                                P.op("pe", lambda e, psw=psw, kk=kk, sl=sl, w=w, h=h: e.matmul(psw[:, :w], wuq[:, kk, (8 + h) * 128:(9 + h) * 128], qn[:, kk, sl], start=(kk == 0), stop=(kk == 2)), reads=["wuq", "qdst"], writes=[pswk])
                            t1, t1k = t1r.next()
                            t2, t2k = t2r.next()
                            P.op("dve", lambda e, pm=pm, ls=ls, w=w, t1=t1: e.tensor_tensor(t1[:, :w], pm[:, :w], rope[:, 0, ls], ALU.mult), reads=[pmk, "rope"], writes=[t1k])
                            P.op("dve", lambda e, psw=psw, ls=ls, w=w, t2=t2: e.tensor_tensor(t2[:, :w], psw[:, :w], rope[:, 1, ls], ALU.mult), reads=[pswk, "rope"], writes=[t2k])
                            P.op("dve", lambda e, ls=ls, w=w, hh=hh, t1=t1, t2=t2: e.tensor_tensor(QR[:, hh, ls], t1[:, :w], t2[:, :w], ALU.add), reads=[t1k, t2k], writes=["QR%d" % hh])
                        pk_, pkk = psn(k, 3, 8)
                        for kk in range(2):
                            P.op("pe", lambda e, pk_=pk_, kk=kk, sl=sl, w=w, h=h: e.matmul(pk_[:, :w], wkk[:, kk, h * 128:(h + 1) * 128], kvn[:, kk, sl], start=(kk == 0), stop=(kk == 1)), reads=["wkk", "kdst"], writes=[pkk])
                        P.op("dve", lambda e, pk_=pk_, sl=sl, w=w, hh=hh: e.tensor_tensor(KH[:, hh, sl], pk_[:, :w], KRb[:, sl], ALU.add), reads=[pkk, "KRb"], writes=["KH%d" % hh])
                if grp == 0:
                    dump(k, "d_wkk", wkk[:], [128, 2, 1024], BF16, ["wkk"])
                    dump(k, "d_QP", QP[:, 0, :], [128, NT], BF16, ["QP0"])
                    dump(k, "d_QR", QR[:, 0, :], [128, L], BF16, ["QR0"])
                    dump(k, "d_KH", KH[:, 0, :], [128, NT], BF16, ["KH0"])
                    dump(k, "d_VA", VA[:, :, 0, :], [128, 18, 128], BF16, ["VA"])
                if "mla_stop2" in k.dbg:
                    P.barrier()
                    continue
                for hh in range(4):
                    h = grp * 4 + hh
                    for qb in range(5):
                        if qb < 4:
                            q0, nq, nkc = LC + qb * 512, 512, 18
                        else:
                            q0, nq, nkc = 0, LC, 2

                        def score_fn(kc, pscr, psk, q0=q0, nq=nq, hh=hh):
                            ks = slice(kc * 128, (kc + 1) * 128)
                            if kc >= 2:
                                P.op("pe", lambda e: e.matmul(pscr[:, :nq], KH[:, hh, ks], QR[:, hh, q0 - LC:q0 - LC + nq], start=True, stop=True), reads=["KH%d" % hh, "QR%d" % hh], writes=[psk])
                            else:
                                P.op("pe", lambda e: e.matmul(pscr[:, :nq], KH[:, hh, ks], QP[:, hh, q0:q0 + nq], start=True, stop=True), reads=["KH%d" % hh, "QP%d" % hh], writes=[psk])

                        softmax_pv(k, ostr, score_fn, nkc, lambda kc, h=h: (VA[:, kc, h, :], "VA"), nq, SC,
                                   k.brT[1][h * 64:(h + 1) * 64, q0:q0 + nq], "mlaT", PTr)
                P.barrier()
        stage_end(k)


def stage_win(k, li):
    nc, P = k.nc, k.P
    SC = float(64 ** -0.5)
    with ExitStack() as st:
        sb = lambda name, shape, dt=F32: st.enter_context(nc.sbuf_tensor(U(name), shape, dt))
        Qp = sb("wQp", [128, 8, NT], BF16)
        Qr = sb("wQr", [128, 8, L], BF16)
        Kp = sb("wKp", [128, NT], BF16)
        Kr = sb("wKr", [128, L], BF16)
        VW = sb("wVW", [128, 18, 2, 128], BF16)
        rope = sb("wrope", [128, 2, L])
        msk = sb("wmsk", [128, 2, 128], BF16)
        esk = sb("wesk", [128, 8])
        Qsr = Rot(nc, st, "wQs", 2, [128, L], BF16)
        tAr = Rot(nc, st, "wtA", 2, [128, 512], F32)
        tBr = Rot(nc, st, "wtB", 2, [128, 512], F32)
        k.att_rv = Rot(nc, st, "watt_rv", 2, [128, 512], F32)
        P.op("pool", lambda e: e.memset(VW[:], 1.0), writes=["VW"])
        P.dma("sp", esk[:], k.win_sink[li], writes=["esk"])
        P.op("act", lambda e: e.activation(out=esk[:], in_=esk[:], func=AF.Exp), reads=["esk"], writes=["esk"])
        P.dma("sp", Qp[:], k.zT[ZT_WQ * 128:(ZT_WQ + 8) * 128, :].rearrange("(k p) t -> p k t", p=128), reads=["zT"], writes=["Qp"])
        P.dma("sp", Kp[:], k.zT[ZT_WK * 128:(ZT_WK + 1) * 128, :], reads=["zT"], writes=["Kp"])
        P.dma("sp", rope[:], k.rope_win.rearrange("c p t -> p c t"), writes=["rope"])
        P.dma("pool", msk[:], k.wmask.rearrange("c p t -> p c t"), writes=["msk"])
        for c_ in range(2):
            P.dma("sp", VW[:, :, c_, 0:64], k.vtok[:, c_ * 64:(c_ + 1) * 64].rearrange("(t p) d -> p t d", p=128), reads=["vtok", "VW"], writes=["VW"])
        for j in range(9):
            qs_, qsk = Qsr.next()
            srow = (ZT_WQS + j) * 128 if j < 8 else ZT_WKS * 128
            P.dma("sp", qs_[:], k.zT[srow:srow + 128, LC:NT], reads=["zT"], writes=[qsk])
            for c in range(4):
                sl = slice(c * 512, (c + 1) * 512)
                sln = slice(LC + c * 512, LC + (c + 1) * 512)
                src = Qp[:, j, sln] if j < 8 else Kp[:, sln]
                dst = Qr[:, j, sl] if j < 8 else Kr[:, sl]
                tA, tAk = tAr.next()
                tB, tBk = tBr.next()
                P.op("dve", lambda e, sl=sl, src=src, tA=tA: e.tensor_tensor(tA[:], src, rope[:, 0, sl], ALU.mult), reads=["Qp", "Kp", "rope"], writes=[tAk])
                P.op("dve", lambda e, sl=sl, qs_=qs_, tB=tB: e.tensor_tensor(tB[:], qs_[:, sl], rope[:, 1, sl], ALU.mult), reads=[qsk, "rope"], writes=[tBk])
                P.op("dve", lambda e, dst=dst, tA=tA, tB=tB: e.tensor_tensor(dst, tA[:], tB[:], ALU.add), reads=[tAk, tBk], writes=["Qr", "Kr"])
        PTr = Rot(nc, st, "wPT", 4, [128, 512], BF16)
        ostr = Rot(nc, st, "wost", 3, [128, 512], BF16)
        for h in range(8):
            kk = h // 4
            for G in range(4):
                n0 = 4 * G
                q0 = LC + n0 * 128
                chunks = [("c", 0, 0, 512), ("c", 1, 0, 512)]
                for j in range(n0 - 1, n0 + 5):
                    if 0 <= j < 16:
                        a_ = max(j - 1, n0)
                        b_ = min(j + 1, n0 + 3)
                        chunks.append(("b", j, (a_ - n0) * 128, (b_ - a_ + 1) * 128))
                po, pok = psn(k, 0, 3)
                scr = {}

                def issue(ci, chunks=chunks, q0=q0, h=h):
                    typ, idx, c0, wq = chunks[ci]
                    pscr, psk = psn(k, 3, 8)
                    if typ == "c":
                        P.op("pe", lambda e: e.matmul(pscr[:, :wq], Kp[:, idx * 128:(idx + 1) * 128], Qp[:, h, q0:q0 + wq], start=True, stop=True), reads=["Kp", "Qp"], writes=[psk])
                    else:
                        P.op("pe", lambda e: e.matmul(pscr[:, :wq], Kr[:, idx * 128:(idx + 1) * 128], Qr[:, h, q0 - LC + c0:q0 - LC + c0 + wq], start=True, stop=True), reads=["Kr", "Qr"], writes=[psk])
                    scr[ci] = (pscr, psk)

                LA = 3
                for ci in range(min(LA, len(chunks))):
                    issue(ci)
                for ci, (typ, idx, c0, wq) in enumerate(chunks):
                    if ci + LA < len(chunks):
                        issue(ci + LA)
                    pscr, psk = scr.pop(ci)
                    pt, ptk = PTr.next()
                    P.op("act", lambda e, pscr=pscr, pt=pt, wq=wq: e.activation(out=pt[:, :wq], in_=pscr[:, :wq], func=AF.Exp, scale=SC), reads=[psk], writes=[ptk])
                    if typ == "b":
                        for sub in range(wq // 128):
                            d = idx - (n0 + c0 // 128 + sub)
                            if d != 0:
                                mi = 0 if d == -1 else 1
                                P.op("dve", lambda e, pt=pt, sub=sub, mi=mi: e.tensor_tensor(pt[:, sub * 128:(sub + 1) * 128], pt[:, sub * 128:(sub + 1) * 128], msk[:, mi, :], ALU.mult), reads=[ptk, "msk"], writes=[ptk])
                    tt = (2 + idx) if typ == "b" else idx
                    P.op("pe", lambda e, po=po, pt=pt, tt=tt, c0=c0, wq=wq, ci=ci, nch=len(chunks), kk=kk: e.matmul(po[:, c0:c0 + wq], VW[:, tt, kk, :], pt[:, :wq], start=(ci == 0), stop=(ci == nch - 1)), reads=["VW", ptk], writes=[pok])
                rv, rvk = k.att_rv.next()
                P.op("dve", lambda e, po=po, rv=rv, h=h: e.tensor_scalar(rv[0:64, :], po[64:128, :], esk[64:128, h:h + 1], None, ALU.add), reads=[pok, "esk"], writes=[rvk])
                P.op("dve", lambda e, rv=rv: e.reciprocal(rv[0:64, :], rv[0:64, :]), reads=[rvk], writes=[rvk])
                ot, otk = ostr.next()
                P.op("dve", lambda e, po=po, ot=ot, rv=rv: e.tensor_tensor(ot[0:64, :], po[0:64, :], rv[0:64, :], ALU.mult), reads=[pok, rvk], writes=[otk])
                P.dma("sp", k.brT[2][h * 64:(h + 1) * 64, q0:q0 + 512], ot[0:64, :], reads=[otk], writes=["winT"])
            chunks = [("c", 0, 0), ("c", 1, 0)]

            def score_fn(kc, pscr, psk, h=h):
                P.op("pe", lambda e: e.matmul(pscr[:, :LC], Kp[:, kc * 128:(kc + 1) * 128], Qp[:, h, 0:LC], start=True, stop=True), reads=["Kp", "Qp"], writes=[psk])

            softmax_pv(k, ostr, score_fn, 2, lambda kc, kk=kk: (VW[:, kc, kk, :], "VW"), LC, SC, k.brT[2][h * 64:(h + 1) * 64, 0:LC], "winT", PTr,
                       esk=esk[64:128, h:h + 1])
        stage_end(k)


def stage_merge(k, li):
    nc, P = k.nc, k.P
    with ExitStack() as st:
        sb = lambda name, shape, dt=F32: st.enter_context(nc.sbuf_tensor(U(name), shape, dt))
        wbr = sb("wbr", [128, 12, D], BF16)
        wout = sb("wout", [128, 8, D], BF16)
        lnp = sb("lnp", [128, 4, 8])
        k.lnp_t = lnp
        P.dma("sp", lnp[:], k.lnp[li], writes=["mod"])
        for j in range(12):
            P.dma("pool", wbr[:, j, :], k.w_branch[li][j * 128:(j + 1) * 128, :], writes=["wbr"])
        for j in range(8):
            P.dma("pool", wout[:, j, :], k.w_out[li][j * 128:(j + 1) * 128, :], writes=["wout"])
        tmp = ln_tmp(nc, st)
        obr = [sb("ob%d" % b, [128, 4, 512], BF16) for b in range(3)]
        gbr = [sb("gb%d" % b, [128, 8, 512], BF16) for b in range(3)]
        mT = sb("mT", [128, 8, 512], BF16)
        m1r = Rot(nc, st, "mgt1", 2, [128, 512], F32)
        m2r = Rot(nc, st, "mgt2", 3, [128, 512], F32)
        ytr = Rot(nc, st, "mgty", 2, [128, 512], F32)
        xb = sb("mxb", [128, 8, 512])
        rb = sb("mrb", [128, 8, 512])
        x1 = sb("mx1", [128, 8, 512])
        h2 = sb("mh2", [128, 8, 512], BF16)
        xTv = k.xT.rearrange("(k p) t -> p k t", p=128)
        h2v = k.h2T.rearrange("(k p) t -> p k t", p=128)
        def phaseA(t0, w, isc):
            sl = slice(t0, t0 + w)
            for b in range(3):
                P.dma("sp", obr[b][:, :, :w], k.brT[b][:, sl].rearrange("(k p) t -> p k t", p=128), reads=["brT"], writes=["ob%d" % b])
                P.dma("sp", gbr[b][:, :, :w], k.zT[(ZT_G + 8 * b) * 128:(ZT_G + 8 + 8 * b) * 128, sl].rearrange("(k p) t -> p k t", p=128), reads=["zT"], writes=["gb%d" % b])
            yield
            for mo in range(8):
                mt1, m1k = m1r.next()
                for b in range(3):
                    pt, pk = psn(k)
                    for kk in range(4):
                        P.op("pe", lambda e, pt=pt, kk=kk, b=b, mo=mo, w=w: e.matmul(pt[:, :w], wbr[:, b * 4 + kk, mo * 128:(mo + 1) * 128], obr[b][:, kk, :w], start=(kk == 0), stop=(kk == 3)), reads=["wbr", "ob%d" % b], writes=[pk])
                    if b == 0:
                        P.op("dve", lambda e, pt=pt, mo=mo, w=w, mt1=mt1: e.tensor_tensor(mt1[:, :w], pt[:, :w], gbr[0][:, mo, :w], ALU.mult), reads=[pk, "gb0"], writes=[m1k])
                    elif b == 1:
                        mt2, m2k = m2r.next()
                        P.op("dve", lambda e, pt=pt, mo=mo, w=w, mt2=mt2: e.tensor_tensor(mt2[:, :w], pt[:, :w], gbr[1][:, mo, :w], ALU.mult), reads=[pk, "gb1"], writes=[m2k])
                        P.op("dve", lambda e, w=w, mt1=mt1, mt2=mt2: e.tensor_tensor(mt1[:, :w], mt1[:, :w], mt2[:, :w], ALU.add), reads=[m1k, m2k], writes=[m1k])
                    else:
                        mt2, m2k = m2r.next()
                        P.op("dve", lambda e, pt=pt, mo=mo, w=w, mt2=mt2: e.tensor_tensor(mt2[:, :w], pt[:, :w], gbr[2][:, mo, :w], ALU.mult), reads=[pk, "gb2"], writes=[m2k])
                        P.op("dve", lambda e, mo=mo, w=w, mt1=mt1, mt2=mt2: e.tensor_tensor(mT[:, mo, :w], mt1[:, :w], mt2[:, :w], ALU.add), reads=[m1k, m2k], writes=["mT"])
                yield

        def phaseB(t0, w, isc):
            sl = slice(t0, t0 + w)
            P.dma("sp", xb[:, :, :w], xTv[:, :, sl], reads=["xT"], writes=["mxb"])
            for mo in range(8):
                pt, pk = psn(k)
                for kk in range(8):
                    P.op("pe", lambda e, pt=pt, kk=kk, mo=mo, w=w: e.matmul(pt[:, :w], wout[:, kk, mo * 128:(mo + 1) * 128], mT[:, kk, :w], start=(kk == 0), stop=(kk == 7)), reads=["wout", "mT"], writes=[pk])
                yt, ytk = ytr.next()
                P.op("act", lambda e, pt=pt, mo=mo, w=w, isc=isc, yt=yt: e.activation(out=yt[:, :w], in_=pt[:, :w], func=AF.Identity, scale=k.mod[:, li, 16 + mo, isc:isc + 1]), reads=[pk, "mod"], writes=[ytk])
                P.op("dve", lambda e, mo=mo, w=w, yt=yt: e.scalar_tensor_tensor(rb[:, mo, :w], xb[:, mo, :w], float(ALPHA), yt[:, :w], ALU.mult, ALU.add), reads=["mxb", ytk], writes=["mrb"])

        def phaseLN(t0, w, isc):
            sl = slice(t0, t0 + w)
            ln_stats(k, rb, "mrb", w, tmp)
            yield
            for kk in range(8):
                ln_apply(k, rb, "mrb", w, tmp, kk, x1[:, kk, :w], ["mx1"], lnp[:, 0, kk:kk + 1], lnp[:, 1, kk:kk + 1])
                yield
            P.dma("sp", xTv[:, :, sl], x1[:, :, :w], reads=["mx1"], writes=["xT"])
            ln_stats(k, x1, "mx1", w, tmp)
            yield
            for kk in range(8):
                ln_apply(k, x1, "mx1", w, tmp, kk, h2[:, kk, :w], ["mh2"], k.mod[:, li, 32 + kk, isc:isc + 1], k.mod[:, li, 24 + kk, isc:isc + 1])
                yield
            P.dma("sp", h2v[:, :, sl], h2[:, :, :w], reads=["mh2"], writes=["h2T"])

        for _ in phaseA(*TBS[0]):
            pass
        for i, tb in enumerate(TBS):
            phaseB(*tb)
            g1 = phaseLN(*tb)
            g2 = phaseA(*TBS[i + 1]) if i + 1 < len(TBS) else iter(())
            d1 = d2 = False
            while not (d1 and d2):
                if not d1:
                    try:
                        next(g1)
                    except StopIteration:
                        d1 = True
                if not d2:
                    try:
                        next(g2)
                    except StopIteration:
                        d2 = True
        stage_end(k)


TB9 = [(0, 256, 1)] + [(256 + i * 256, 256, 0) for i in range(8)]


def stage_moe(k, li):
    nc, P = k.nc, k.P
    with ExitStack() as st0:
        sb0 = lambda name, shape, dt=F32: st0.enter_context(nc.sbuf_tensor(U(name), shape, dt))
        lnp = sb0("elnp", [128, 4, 8])
        posm = sb0("eposm", [16, NT])
        sel = sb0("esel", [16, 16, 128])
        iop = sb0("eiop", [128, 3])
        P.dma("sp", lnp[:], k.lnp[li], writes=["mod"])
        P.dma("sp", sel[:], k.sel16, writes=["esel"])
        P.dma("sp", iop[:], k.iota_p3, writes=["eiop"])
        with ExitStack() as st1:
            sb1 = lambda name, shape, dt=F32: st1.enter_context(nc.sbuf_tensor(U(name), shape, dt))
            h2tok = sb1("eh2tok", [128, 18, D], BF16)
            posm_tok = sb1("eposmt", [128, 18, 16])
            gw_tok = sb1("egwt", [128, 18, 16], BF16)
            ios = sb1("eios", [128, 384])
            P.dma("sp", ios[:], k.iota_s, writes=["eios"])
            with ExitStack() as stA:
                sbA = lambda name, shape, dt=F32: stA.enter_context(nc.sbuf_tensor(U(name), shape, dt))
                h2 = sbA("eh2", [128, 8, NT], BF16)
                wr = sbA("ewr", [128, 8, 16], BF16)
                aff = sbA("eaff", [16, NT])
                wk_ = sbA("ewk", [16, NT])
                gw = sbA("egw", [16, NT])
                msk = sbA("emsk", [16, NT])
                m8 = sbA("em8", [16, 8])
                thr = sbA("ethr", [16, 2])
                P.dma("sp", h2[:], k.h2T.rearrange("(k p) t -> p k t", p=128), reads=["h2T"], writes=["eh2"])
                P.dma("pool", wr[:], k.w_router[li].rearrange("(k p) n -> p k n", p=128), writes=["ewr"])
                for (t0, w, isc) in TBS:
                    sl = slice(t0, t0 + w)
                    pt, pk = psn(k)
                    for kk in range(8):
                        P.op("pe", lambda e, pt=pt, kk=kk, sl=sl, w=w: e.matmul(pt[0:16, :w], wr[:, kk, :], h2[:, kk, sl], start=(kk == 0), stop=(kk == 7)), reads=["ewr", "eh2"], writes=[pk])
                    P.op("act", lambda e, pt=pt, sl=sl, w=w: e.activation(out=wk_[:, sl], in_=pt[0:16, :w], func=AF.Exp), reads=[pk], writes=["ewk"])
                    p2, k2 = psn(k)
                    P.op("pe", lambda e, p2=p2, sl=sl, w=w: e.matmul(p2[0:16, :w], k.onesf[0:16, 0:16], wk_[:, sl], start=True, stop=True), reads=["ewk", "onesf"], writes=[k2])
                    P.op("dve", lambda e, p2=p2, sl=sl, w=w: e.reciprocal(gw[:, sl], p2[0:16, :w]), reads=[k2], writes=["egw"])
                    P.op("dve", lambda e, sl=sl: e.tensor_tensor(aff[:, sl], wk_[:, sl], gw[:, sl], ALU.mult), reads=["ewk", "egw"], writes=["eaff"])
                P.op("dve", lambda e: e.tensor_copy(wk_[:], aff[:]), reads=["eaff"], writes=["ewk"])
                for si, (a_, b_, cap, off) in enumerate(((0, LC, 32, 256.0), (LC, NT, 256, 0.0))):
                    for r in range(cap // 8):
                        P.op("dve", lambda e, a_=a_, b_=b_: e.max(out=m8[:], in_=wk_[:, a_:b_]), reads=["ewk"], writes=["em8"])
                        if r < cap // 8 - 1:
                            P.op("dve", lambda e, a_=a_, b_=b_: e.match_replace(out=wk_[:, a_:b_], in_to_replace=m8[:], in_values=wk_[:, a_:b_], imm_value=-1.0), reads=["ewk", "em8"], writes=["ewk"])
                    P.op("dve", lambda e, si=si: e.tensor_copy(thr[:, si:si + 1], m8[:, 7:8]), reads=["em8"], writes=["ethr"])
                    P.op("dve", lambda e, a_=a_, b_=b_, si=si: e.tensor_scalar(msk[:, a_:b_], aff[:, a_:b_], thr[:, si:si + 1], None, ALU.is_ge), reads=["eaff", "ethr"], writes=["emsk"])
                    P.op("dve", lambda e, a_=a_, b_=b_: e.tensor_tensor(gw[:, a_:b_], aff[:, a_:b_], msk[:, a_:b_], ALU.mult), reads=["eaff", "emsk"], writes=["egw"])
                    P.op("dve", lambda e, a_=a_, b_=b_: e.tensor_tensor_scan(wk_[:, a_:b_], k.onesf[0:16, 0:1].to_broadcast([16, b_ - a_]), msk[:, a_:b_], 0.0, ALU.mult, ALU.add), reads=["emsk", "onesf", "ewk"], writes=["ewk"])
                    P.op("dve", lambda e, a_=a_, b_=b_, off=off: e.scalar_tensor_tensor(posm[:, a_:b_], wk_[:, a_:b_], float(off), msk[:, a_:b_], ALU.add, ALU.mult), reads=["ewk", "emsk"], writes=["eposm"])
                    P.op("dve", lambda e, a_=a_, b_=b_: e.tensor_scalar_add(posm[:, a_:b_], posm[:, a_:b_], -1.0), reads=["eposm"], writes=["eposm"])
                for (src, dst, skey, dkey) in ((posm, posm_tok, "eposm", "eposmt"), (gw, gw_tok, "egw", "egwt")):
                    pt, pk = psn(k)
                    for tt in range(18):
                        P.op("pe", lambda e, pt=pt, tt=tt, src=src: e.transpose(pt[:, tt * 16:(tt + 1) * 16], src[0:16, tt * 128:(tt + 1) * 128], k.identf[0:16, 0:16]), reads=[skey, "identf"], writes=[pk])
                    P.op("dve", lambda e, pt=pt, dst=dst: e.tensor_copy(dst[:], pt[:, 0:288].rearrange("p (t e) -> p t e", e=16)), reads=[pk], writes=[dkey])
                for tt in range(18):
                    pt, pk = psn(k)
                    ptb = pt[:].bitcast(BF16)
                    for kf in range(8):
                        P.op("pe", lambda e, ptb=ptb, kf=kf, tt=tt: e.transpose(ptb[:, kf * 128:(kf + 1) * 128], h2[:, kf, tt * 128:(tt + 1) * 128], k.identb[:]), reads=["eh2", "identb"], writes=[pk])
                    if tt % 2 == 0:
                        P.op("act", lambda e, ptb=ptb, tt=tt: e.copy(h2tok[:, tt, :], ptb[:, 0:1024]), reads=[pk], writes=["eh2tok"])
                    else:
                        P.op("dve", lambda e, ptb=ptb, tt=tt: e.tensor_copy(h2tok[:, tt, :], ptb[:, 0:1024]), reads=[pk], writes=["eh2tok"])
                P.barrier()
            mw = Rot(nc, st1, "emw", 32, [128, D], BF16)
            Sr = Rot(nc, st1, "eS", 2, [128, 18, 384], BF16)
            Xr = Rot(nc, st1, "eX", 2, [128, 8, 288], BF16)
            Ar = Rot(nc, st1, "eA", 2, [128, 8, 384], BF16)
            Yr = Rot(nc, st1, "eY", 2, [128, 3, D], BF16)
            sar = Rot(nc, st1, "esa", 2, [128, 288], F32)
            gsr = Rot(nc, st1, "egs", 2, [128, 3], F32)
            for j in range(2):
                At, Ak = Ar.next()
                P.op("pool", lambda e, At=At: e.memset(At[:], 0.0), writes=[Ak])
            ev = 0
            for ex in range(16):
                ws = {}
                for nm, src in (("g", k.w_gate), ("u", k.w_up), ("d", k.w_down)):
                    for kk in range(8):
                        t, tk = mw.next()
                        P.dma("pool", t[:], src[li, ex, kk * 128:(kk + 1) * 128, :], writes=[tk])
                        ws[(nm, kk)] = (t, tk)
                S, Sk = Sr.next()
                P.op("dve", lambda e, S=S, ex=ex: e.tensor_tensor(S[:], ios[:].unsqueeze(1).to_broadcast([128, 18, 384]), posm_tok[:, :, ex:ex + 1].to_broadcast([128, 18, 384]), ALU.is_equal), reads=["eios", "eposmt"], writes=[Sk])
                X, Xk = Xr.next()
                for ft in range(8):
                    pt, pk = psn(k, 0, 4)
                    for tt in range(2, 18):
                        P.op("pe", lambda e, pt=pt, tt=tt, ft=ft, S=S: e.matmul(pt[:, 0:256], h2tok[:, tt, ft * 128:(ft + 1) * 128], S[:, tt, 0:256], start=(tt == 2), stop=(tt == 17)), reads=["eh2tok", Sk], writes=[pk])
                    for tt in range(2):
                        P.op("pe", lambda e, pt=pt, tt=tt, ft=ft, S=S: e.matmul(pt[:, 256:288], h2tok[:, tt, ft * 128:(ft + 1) * 128], S[:, tt, 256:288], start=(tt == 0), stop=(tt == 1)), reads=["eh2tok", Sk], writes=[pk])
                    if ev % 2 == 0:
                        P.op("act", lambda e, pt=pt, ft=ft, X=X: e.copy(X[:, ft, :], pt[:, :288]), reads=[pk], writes=[Xk])
                    else:
                        P.op("dve", lambda e, pt=pt, ft=ft, X=X: e.tensor_copy(X[:, ft, :], pt[:, :288]), reads=[pk], writes=[Xk])
                    ev += 1
                pg, pgk = psn(k, 0, 4)
                for st_ in range(3):
                    tts = list(range(2, 18)) if st_ < 2 else [0, 1]
                    for tt in tts:
                        P.op("pe", lambda e, pg=pg, tt=tt, st_=st_, S=S, ex=ex, tts=tts: e.matmul(pg[:, st_:st_ + 1], S[:, tt, st_ * 128:(st_ + 1) * 128], gw_tok[:, tt, ex:ex + 1], start=(tt == tts[0]), stop=(tt == tts[-1])), reads=[Sk, "egwt"], writes=[pgk])
                gs, gsk = gsr.next()
                P.op("dve", lambda e, pg=pg, gs=gs: e.tensor_copy(gs[:], pg[:, 0:3]), reads=[pgk], writes=[gsk])
                At, Ak = Ar.next()
                for fo in range(8):
                    pa, pak = psn(k, 4, 6)
                    pu, puk = psn(k, 6, 8)
                    for kk in range(8):
                        wt, wtk = ws[("g", kk)]
                        P.op("pe", lambda e, pa=pa, wt=wt, kk=kk, fo=fo, X=X: e.matmul(pa[:, :288], wt[:, fo * 128:(fo + 1) * 128], X[:, kk, :], start=(kk == 0), stop=(kk == 7)), reads=[wtk, Xk], writes=[pak])
                    for kk in range(8):
                        wt, wtk = ws[("u", kk)]
                        P.op("pe", lambda e, pu=pu, wt=wt, kk=kk, fo=fo, X=X: e.matmul(pu[:, :288], wt[:, fo * 128:(fo + 1) * 128], X[:, kk, :], start=(kk == 0), stop=(kk == 7)), reads=[wtk, Xk], writes=[puk])
                    s_, sk_ = sar.next()
                    P.op("act", lambda e, pa=pa, s_=s_: e.activation(out=s_[:], in_=pa[:, :288], func=AF.Silu), reads=[pak], writes=[sk_])
                    P.op("dve", lambda e, pu=pu, s_=s_, At=At, fo=fo: e.tensor_tensor(At[:, fo, 0:288], pu[:, :288], s_[:], ALU.mult), reads=[puk, sk_], writes=[Ak])
                Y, Yk = Yr.next()
                for st_ in range(3):
                    for half in range(2):
                        py, pyk = psn(k, 0, 4)
                        for kk in range(8):
                            wt, wtk = ws[("d", kk)]
                            P.op("pe", lambda e, py=py, wt=wt, kk=kk, st_=st_, half=half, At=At: e.matmul(py[:, :512], At[:, kk, st_ * 128:(st_ + 1) * 128], wt[:, half * 512:(half + 1) * 512], start=(kk == 0), stop=(kk == 7)), reads=[wtk, Ak], writes=[pyk])
                        P.op("act", lambda e, py=py, st_=st_, half=half, Y=Y, gs=gs: e.activation(out=Y[:, st_, half * 512:(half + 1) * 512], in_=py[:, :512], func=AF.Identity, scale=gs[:, st_:st_ + 1]), reads=[pyk, gsk], writes=[Yk])
                P.dma("sp", k.yg[ex].rearrange("s p d -> p s d"), Y[:], reads=[Yk], writes=["yg"])
            P.barrier()
        with ExitStack() as st2:
            sb2 = lambda name, shape, dt=F32: st2.enter_context(nc.sbuf_tensor(U(name), shape, dt))
            Yall = sb2("eYall", [128, 48, D], BF16)
            for ex in range(16):
                P.dma("sp", Yall[:, ex * 3:(ex + 1) * 3, :], k.yg[ex].rearrange("s p d -> p s d"), reads=["yg"], writes=["eYall"])
            STr = Rot(nc, st2, "eST", 2, [128, 32, 256], BF16)
            tmp = ln_tmp(nc, st2, 256)
            xbr = Rot(nc, st2, "exb", 2, [128, 8, 256], F32)
            rbR = Rot(nc, st2, "erb", 2, [128, 8, 256], F32)
            xTv = k.xT.rearrange("(k p) t -> p k t", p=128)
            state = {}

            def scatter(i):
                t0, w, isc = TB9[i]
                sl = slice(t0, t0 + w)
                xb, xbk = xbr.next()
                rb, rbk = rbR.next()
                state[i] = (xb, xbk, rb, rbk)
                P.dma("sp", xb[:], xTv[:, :, sl], reads=["xT"], writes=[xbk])
                ST, STk = STr.next()
                sts = [2] if isc else [0, 1]
                n_ = len(sts)
                for ex in range(16):
                    pb, pbk = psn(k, 4, 8)
                    P.op("pe", lambda e, pb=pb, sl=sl, ex=ex: e.matmul(pb[:, :256], sel[:, ex, :], posm[:, sl], start=True, stop=True), reads=["esel", "eposm"], writes=[pbk])
                    P.op("dve", lambda e, pb=pb, ex=ex, ST=ST, sts=sts, n_=n_: e.tensor_tensor(ST[:, ex * n_:(ex + 1) * n_, :], pb[:, 0:256].unsqueeze(1).to_broadcast([128, n_, 256]), iop[:, sts[0]:sts[0] + n_].unsqueeze(2).to_broadcast([128, n_, 256]), ALU.is_equal), reads=[pbk, "eiop"], writes=[STk])
                    if ex % 4 == 3:
                        yield
                js = [(ex * 3 + st_, ex * n_ + i_) for ex in range(16) for i_, st_ in enumerate(sts)]
                for mo in range(8):
                    pf, pfk = psn(k, 0, 4)
                    for (jy, jt) in js:
                        P.op("pe", lambda e, pf=pf, jy=jy, jt=jt, mo=mo, ST=ST, js=js: e.matmul(pf[:, :256], Yall[:, jy, mo * 128:(mo + 1) * 128], ST[:, jt, :], start=(jy == js[0][0]), stop=(jy == js[-1][0])), reads=["eYall", STk], writes=[pfk])
                    ft_, ftk = tmp["tr"].next()
                    P.op("act", lambda e, pf=pf, mo=mo, isc=isc, ft_=ft_: e.activation(out=ft_[:, :256], in_=pf[:, :256], func=AF.Identity, scale=k.mod[:, li, 40 + mo, isc:isc + 1]), reads=[pfk, "mod"], writes=[ftk])
                    P.op("dve", lambda e, mo=mo, xb=xb, ft_=ft_, rb=rb: e.scalar_tensor_tensor(rb[:, mo, :], xb[:, mo, :], float(ALPHA), ft_[:, :256], ALU.mult, ALU.add), reads=[xbk, ftk], writes=[rbk])
                    yield

            def ln2(i):
                t0, w, isc = TB9[i]
                sl = slice(t0, t0 + w)
                xb, xbk, rb, rbk = state.pop(i)
                ln_stats(k, rb, rbk, w, tmp)
                yield
                for kk in range(8):
                    ln_apply(k, rb, rbk, w, tmp, kk, xb[:, kk, :], [xbk], lnp[:, 2, kk:kk + 1], lnp[:, 3, kk:kk + 1])
                    yield
                P.dma("sp", xTv[:, :, sl], xb[:], reads=[xbk], writes=["xT"])

            for _ in scatter(0):
                pass
            for i in range(len(TB9)):
                g1 = ln2(i)
                g2 = scatter(i + 1) if i + 1 < len(TB9) else iter(())
                d1 = d2 = False
                while not (d1 and d2):
                    if not d1:
                        try:
                            next(g1)
                        except StopIteration:
                            d1 = True
                    if not d2:
                        try:
                            next(g2)
                        except StopIteration:
                            d2 = True
        stage_end(k)


def stage_out(k):
    nc, P = k.nc, k.P
    with ExitStack() as st:
        xr = Rot(nc, st, "oxr", 2, [128, 8, 128], F32)
        xo = Rot(nc, st, "oxo", 2, [128, D], F32)
        xTv = k.xT.rearrange("(k p) t -> p k t", p=128)
        for tt in range(L // 128):
            xt, xk = xr.next()
            P.dma("sp", xt[:], xTv[:, :, LC + tt * 128:LC + (tt + 1) * 128], reads=["xT"], writes=[xk])
            ot, ok = xo.next()
            for half in range(2):
                pt, pk = psn(k)
                for kk in range(4):
                    kf = half * 4 + kk
                    P.op("pe", lambda e, pt=pt, kk=kk, kf=kf, xt=xt: e.transpose(pt[:, kk * 128:(kk + 1) * 128], xt[:, kf, :], k.identf[:]),
                         reads=[xk, "identf"], writes=[pk])
                if half == 0:
                    P.op("act", lambda e, pt=pt, ot=ot: e.copy(ot[:, 0:512], pt[:]), reads=[pk], writes=[ok + "h0"])
                else:
                    P.op("dve", lambda e, pt=pt, ot=ot: e.tensor_copy(ot[:, 512:1024], pt[:]), reads=[pk], writes=[ok + "h1"])
            P.dma("sp", k.out[tt * 128:(tt + 1) * 128, :], ot[:], reads=[ok + "h0", ok + "h1"], writes=["out"])
        stage_end(k)


Z_ORDER = None


def _zcols():
    u = np.arange(0, 512)
    qa = np.arange(512, 896)
    kva = np.arange(896, 1152)
    kr = np.arange(1152, 1184)
    wq = np.arange(1184, 1696)
    wk = np.arange(1696, 1824)
    wv = np.arange(1824, 1952)
    gates = np.arange(1952, 5024)
    Z = -np.ones(64, np.int64)

    def padq(cols8):
        out = []
        for h in range(8):
            kk = h // 4
            out.append(np.concatenate([cols8[h], Z]) if kk == 0 else np.concatenate([Z, cols8[h]]))
        return np.concatenate(out)
    wq8 = wq.reshape(8, 64)
    wq8s = wq.reshape(8, 2, 32)[:, ::-1, :].reshape(8, 64)
    wk_sw = wk.reshape(2, 2, 32)[:, ::-1, :].reshape(-1)
    kr_sw = kr.reshape(2, 16)[::-1].reshape(-1)
    cols = np.concatenate([u, qa, kva, padq(wq8), wk, wv, gates, padq(wq8s), wk_sw, kr, kr_sw, Z])
    assert cols.size == NZT * 128, cols.size
    return cols


def prep_shared(inp):
    f = lambda a: np.ascontiguousarray(np.asarray(a, np.float32))
    sh = {}
    sh["ident"] = np.eye(128, dtype=np.float32)
    sh["w_ada"] = f(inp["w_ada"])
    b = f(inp["b_ada"]).reshape(DEPTH, 48, 128).transpose(0, 2, 1)
    sh["bada2"] = f(np.repeat(b[:, :, :, None], 2, axis=3))
    cols = _zcols()
    wx = f(inp["w_in"])[:, :, np.maximum(cols, 0)]
    wx[:, :, cols < 0] = 0.0
    sh["w_inx"] = f(wx)
    G, PS, HG = 32, 64, 16
    bT = np.zeros((DEPTH, 2, 2, 16, 128, 128), np.float32)
    cL = np.zeros((DEPTH, 128, 2, 16, 2, 16), np.float32)
    lane = np.zeros((DEPTH, 128, 3, 32), np.float32)
    for ri, nm in enumerate(("s5_b_re", "s5_b_im")):
        bsrc = f(inp[nm])
        for lt in range(16):
            for half in range(2):
                g = 2 * lt + half
                gl = g % 8
                bT[:, :, ri, lt, gl * 16:(gl + 1) * 16, half * 64:(half + 1) * 64] = bsrc[:, :, g].transpose(0, 1, 3, 2)
    for ri, nm in enumerate(("s5_c_re", "s5_c_im")):
        csrc = f(inp[nm])
        for lt in range(16):
            for half in range(2):
                g = 2 * lt + half
                cL[:, half * 64:(half + 1) * 64, :, lt, ri, :] = csrc[:, :, g].transpose(0, 3, 1, 2)
    lre, lim, ldt = f(inp["s5_lam_re"]), f(inp["s5_lam_im"]), f(inp["s5_log_dt"])
    for d in range(2):
        for lt in range(16):
            for half in range(2):
                g = 2 * lt + half
                lane[:, half * 64:(half + 1) * 64, 0, d * 16 + lt] = lre[:, d, g, :]
                lane[:, half * 64:(half + 1) * 64, 1, d * 16 + lt] = lim[:, d, g, :]
                lane[:, half * 64:(half + 1) * 64, 2, d * 16 + lt] = ldt[:, d, g][:, None]
    sh["s5_bT"], sh["s5_cL"], sh["s5_lane"] = bT, cL, lane
    dg = np.zeros((DEPTH, 128, 2, 4), np.float32)
    dg[:, :, 0, :] = f(inp["s5_d"]).reshape(DEPTH, 4, 128).transpose(0, 2, 1)
    dg[:, :, 1, :] = f(inp["s5_b_glu"]).reshape(DEPTH, 4, 128).transpose(0, 2, 1)
    sh["s5_dg"] = dg
    sh["s5_wglu"] = f(inp["s5_w_glu"])
    mg = np.zeros((DEPTH, 128, 5), np.float32)
    mg[:, :, 0:3] = f(inp["mla_q_norm"]).reshape(DEPTH, 3, 128).transpose(0, 2, 1)
    mg[:, :, 3:5] = f(inp["mla_kv_norm"]).reshape(DEPTH, 2, 128).transpose(0, 2, 1)
    sh["mla_g"] = mg
    wuq = f(inp["mla_w_uq"]).reshape(DEPTH, 384, 8, 96)
    wm = np.zeros((DEPTH, 384, 8, 128), np.float32)
    wsw = np.zeros((DEPTH, 384, 8, 128), np.float32)
    wm[..., 0:96] = wuq
    wsw[..., 64:96] = wuq[..., 64:].reshape(DEPTH, 384, 8, 2, 16)[:, :, :, ::-1, :].reshape(DEPTH, 384, 8, 32)
    sh["w_uqx"] = f(np.concatenate([wm.reshape(DEPTH, 384, 1024), wsw.reshape(DEPTH, 384, 1024)], -1))
    wkv = f(inp["mla_w_ukv"]).reshape(DEPTH, 256, 8, 128)
    wkp = np.zeros((DEPTH, 256, 8, 128), np.float32)
    wkp[..., 0:64] = wkv[..., :64]
    sh["w_ukvk"] = f(wkp.reshape(DEPTH, 256, 1024))
    sh["w_ukvv"] = f(wkv[..., 64:].reshape(DEPTH, 256, 512))
    rows = L // 64
    row = np.repeat(np.arange(rows, dtype=np.float64), 64)
    col = np.tile(np.arange(64, dtype=np.float64), rows)

    def rope_tab(d, reps):
        nf = d // 4
        fr = 10000.0 ** (-np.arange(nf) / nf)
        ang = np.concatenate([row[:, None] * fr, col[:, None] * fr], -1)
        c = np.concatenate([np.cos(ang), np.cos(ang)], -1).T
        sn = np.concatenate([-np.sin(ang), np.sin(ang)], -1).T
        return np.stack([np.tile(c, (reps, 1)), np.tile(sn, (reps, 1))], 0).astype(np.float32)
    rm = np.zeros((2, 128, L), np.float32)
    rm[0] = 1.0
    rm[:, 64:96, :] = rope_tab(32, 1)
    sh["rope_mla"] = rm
    sh["rope_win"] = rope_tab(64, 2)
    jj = np.arange(128)[:, None]
    rr = np.arange(128)[None, :]
    sh["wmask"] = np.stack([(jj >= rr), (jj <= rr)], 0).astype(np.float32)
    sh["win_sink"] = f(np.broadcast_to(f(inp["win_sink"])[:, None, :], (DEPTH, 128, 8)))
    sh["w_branch"] = f(inp["w_branch"]).reshape(DEPTH, 1536, D)
    sh["w_out"] = f(inp["w_out"])
    lnp = np.stack([f(inp[n]).reshape(DEPTH, 8, 128).transpose(0, 2, 1) for n in ("ln1_g", "ln1_b", "ln2_g", "ln2_b")], 2)
    sh["lnp"] = f(lnp)
    sh["w_router"] = f(inp["w_router"])
    sh["w_gate"], sh["w_up"], sh["w_down"] = f(inp["w_gate"]), f(inp["w_up"]), f(inp["w_down"])
    sel = np.zeros((16, 16, 128), np.float32)
    for e_ in range(16):
        sel[e_, e_, :] = 1.0
    sh["sel16"] = sel
    sh["iota_s"] = f(np.broadcast_to(np.arange(384, dtype=np.float32), (128, 384)))
    sh["iota_p3"] = f(np.arange(128, dtype=np.float32)[:, None] + np.array([0.0, 128.0, 256.0], np.float32)[None, :])
    sh["tau"] = f(np.broadcast_to(np.arange(NT, dtype=np.float32), (128, NT)))
    return sh


def prep_core(inp, b):
    f = lambda a: np.ascontiguousarray(np.asarray(a, np.float32))
    m = {}
    m["xin"] = f(np.concatenate([inp["ctx"][b], inp["x"][b]], 0))
    cond = np.stack([np.asarray(inp["c"][b]), np.asarray(inp["c_ctx"])], -1)
    m["condT"] = f(cond.reshape(8, 128, 2).transpose(1, 0, 2))
    return m


def kernel(**inputs):
    nc = build()
    sh = prep_shared(inputs)
    in_maps = []
    for b in range(8):
        m = dict(sh)
        m.update(prep_core(inputs, b))
        in_maps.append(m)
    res = run_bass_kernel_spmd(nc, in_maps, core_ids=list(range(8)))
    return np.stack([np.asarray(r["out"], np.float32) for r in res.results], 0)
```
